# Optimizing a Trainium2 kernel written in Bass

```python
import math
import jax
import jax.numpy as jnp
from jax import lax
import numpy as np

D_MODEL = 1024
BATCH = 16
SEQ = 2048
DEPTH = 4

N_MIXERS = 3
D_FF = 2816
NORM_EPS = 1e-6
HALF_STEP = 0.5

ML_HEADS = 4
ML_DQK = D_MODEL // 8
ML_DV = D_MODEL // 4
ML_CHUNK = 64
ML_CONV = 4
ML_GATE_CAP = 15.0
ML_IN = 2 * ML_HEADS * ML_DQK + 2 * ML_HEADS * ML_DV + 2 * ML_HEADS

RW_HEAD = 64
RW_HEADS = D_MODEL // RW_HEAD
RW_LORA_W = 64
RW_LORA_A = 64
RW_LORA_G = 128
RW_LN_EPS = 64e-5

RT_HEADS = 4
RT_DK = D_MODEL // RT_HEADS
RT_DV = 2 * RT_DK
RT_CHUNK = 128
RT_ROPE_BASE = 10000.0
RT_IN = 2 * RT_HEADS * RT_DK + 2 * RT_HEADS * RT_DV

N_ML_LAYERS = (DEPTH + 2) // 3
N_RW_LAYERS = (DEPTH + 1) // 3
N_RT_LAYERS = DEPTH // 3

kernel_name = 'hybrid_mlstm_rwkv7_retention_macaron'

F32 = jnp.float32


def rms_norm(x, g, eps=NORM_EPS):
    xf = x.astype(F32)
    y = xf * lax.rsqrt(jnp.mean(xf * xf, axis=-1, keepdims=True) + eps)
    return (y * g.astype(F32)).astype(x.dtype)


def head_rms(y, eps=NORM_EPS):
    return y * lax.rsqrt(jnp.mean(y * y, axis=-1, keepdims=True) + eps)


def swiglu_ffn(x, w_gu, w_down):
    gate, up = jnp.split(x @ w_gu, 2, axis=-1)
    return (jax.nn.silu(gate) * up) @ w_down


def causal_depthwise_conv(x, w):
    K, C = w.shape
    return lax.conv_general_dilated(x, w[:, None, :].astype(x.dtype), window_strides=(1,),
                                    padding=[(K - 1, 0)], dimension_numbers=('NWC', 'WIO', 'NWC'),
                                    feature_group_count=C)


def to_chunks(a, L):
    B, T, H = a.shape[:3]
    a = a.reshape((B, T // L, L, H) + a.shape[3:])
    perm = (1, 0, 3, 2) + tuple(range(4, a.ndim))
    return jnp.transpose(a, perm)


def from_chunks(a):
    NC, B, H, L, d = a.shape
    return jnp.transpose(a, (1, 0, 3, 2, 4)).reshape(B, NC * L, H, d)


def mlstm_chunkwise(q, k, v, i_log, f_log):
    B, T, H, dk = q.shape
    dv = v.shape[-1]
    L = ML_CHUNK
    mask = jnp.tril(jnp.ones((L, L), dtype=bool))
    xs = tuple(to_chunks(a.astype(F32), L) for a in (q, k, v, i_log, f_log))

    def step(carry, xc):
        C, n, m = carry
        qc, kc, vc, ic, fc = xc
        b = jnp.cumsum(fc, axis=-1)
        log_d = jnp.where(mask, b[..., :, None] - b[..., None, :] + ic[..., None, :], -jnp.inf)
        log_inter = b + m[..., None]
        m_t = jnp.maximum(log_inter, jnp.max(log_d, axis=-1))
        s = jnp.einsum('bhtd,bhsd->bhts', qc, kc) * jnp.exp(log_d - m_t[..., None])
        inter = jnp.exp(log_inter - m_t)
        num = jnp.einsum('bhts,bhsv->bhtv', s, vc) + inter[..., None] * jnp.einsum('bhtd,bhdv->bhtv', qc, C)
        den = jnp.sum(s, axis=-1) + inter * jnp.einsum('bhtd,bhd->bht', qc, n)
        h = num / jnp.maximum(jnp.abs(den), jnp.exp(-m_t))[..., None]
        b_end = b[..., -1]
        log_w = b_end[..., None] - b + ic
        m_new = jnp.maximum(b_end + m, jnp.max(log_w, axis=-1))
        kw = kc * jnp.exp(log_w - m_new[..., None])[..., None]
        carry_decay = jnp.exp(b_end + m - m_new)
        C = carry_decay[..., None, None] * C + jnp.einsum('bhsd,bhsv->bhdv', kw, vc)
        n = carry_decay[..., None] * n + jnp.sum(kw, axis=-2)
        return (C, n, m_new), h

    init = (jnp.zeros((B, H, dk, dv), F32), jnp.zeros((B, H, dk), F32), jnp.zeros((B, H), F32))
    _, h = lax.scan(step, init, xs)
    return from_chunks(h)


def mlstm_mixer(x, w_in, b_if, conv_w, norm_g, w_out):
    B, T, _ = x.shape
    H, dk, dv = ML_HEADS, ML_DQK, ML_DV
    proj = x @ w_in
    qk, v, o, if_pre = jnp.split(proj, [2 * H * dk, 2 * H * dk + H * dv, 2 * H * dk + 2 * H * dv], axis=-1)
    qk = jax.nn.silu(causal_depthwise_conv(qk, conv_w))
    q, k = jnp.split(qk, 2, axis=-1)
    if_pre = if_pre.astype(F32) + b_if.astype(F32)
    if_pre = ML_GATE_CAP * jnp.tanh(if_pre / ML_GATE_CAP)
    i_log, f_pre = jnp.split(if_pre, 2, axis=-1)
    f_log = jax.nn.log_sigmoid(f_pre)
    q = q.reshape(B, T, H, dk).astype(F32) * (dk ** -0.5)
    h = mlstm_chunkwise(q, k.reshape(B, T, H, dk), v.reshape(B, T, H, dv), i_log, f_log)
    h = head_rms(h).reshape(B, T, H * dv) * norm_g.astype(F32)
    h = (h * jax.nn.sigmoid(o.astype(F32))).astype(x.dtype)
    return h @ w_out


def rwkv7_mixer(x, mu, w_rkv, w0, w1, w2, a0, a1, a2, g1, g2, k_k, k_a, r_k, ln_g, ln_b, w_out):
    B, T, D = x.shape
    H, N = RW_HEADS, RW_HEAD
    x_prev = jnp.pad(x, ((0, 0), (1, 0), (0, 0)))[:, :-1]
    xx = x_prev - x
    mix = lambda i: x + xx * mu[i]
    r = (mix(0) @ w_rkv[0]).astype(F32)
    k = (mix(1) @ w_rkv[1]).astype(F32)
    v = (mix(2) @ w_rkv[2]).astype(F32)
    w_log = -jax.nn.softplus(-(w0 + jnp.tanh(mix(3) @ w1) @ w2).astype(F32)) - 0.5
    decay = jnp.exp(-jnp.exp(w_log))
    a = jax.nn.sigmoid((a0 + (mix(4) @ a1) @ a2).astype(F32))
    g = (jax.nn.sigmoid(mix(5) @ g1) @ g2).astype(F32)
    heads = lambda t: t.reshape(B, T, H, N)
    kk = heads(k * k_k.astype(F32))
    kk = kk / jnp.maximum(jnp.sqrt(jnp.sum(kk * kk, axis=-1, keepdims=True)), 1e-12)
    k = k * (1.0 + (a - 1.0) * k_a.astype(F32))
    rh, kh, vh, ah = heads(r), heads(k), heads(v), heads(a)
    tm = lambda t: jnp.swapaxes(t, 0, 1)
    xs = (tm(rh), tm(heads(decay)), tm(kh), tm(vh), tm(-kk), tm(kk * ah))

    def step(S, xt):
        r_t, w_t, k_t, v_t, a_t, b_t = xt
        sa = jnp.einsum('bhij,bhj->bhi', S, a_t)
        S = S * w_t[:, :, None, :] + sa[..., :, None] * b_t[..., None, :] + v_t[..., :, None] * k_t[..., None, :]
        return S, jnp.einsum('bhij,bhj->bhi', S, r_t)

    _, ys = lax.scan(step, jnp.zeros((B, H, N, N), F32), xs)
    y = jnp.swapaxes(ys, 0, 1)
    y_mean = jnp.mean(y, axis=-1, keepdims=True)
    y_c = y - y_mean
    y = y_c * lax.rsqrt(jnp.mean(y_c * y_c, axis=-1, keepdims=True) + RW_LN_EPS)
    y = y.reshape(B, T, D) * ln_g.astype(F32) + ln_b.astype(F32)
    bonus = (jnp.sum(rh * kh * r_k.astype(F32), axis=-1, keepdims=True) * vh).reshape(B, T, D)
    return ((y + bonus) * g).astype(x.dtype) @ w_out


def retention_rotate(u, positions):
    d = u.shape[-1]
    inv_freq = 1.0 / (RT_ROPE_BASE ** jnp.linspace(0.0, 1.0, d // 2, dtype=F32))
    ang = positions.astype(F32)[:, :, None, None] * inv_freq
    cos, sin = jnp.cos(ang), jnp.sin(ang)
    u1, u2 = jnp.split(u, 2, axis=-1)
    return jnp.concatenate([u1 * cos - u2 * sin, u1 * sin + u2 * cos], axis=-1)


def retention_chunkwise(q, k, v):
    B, T, H, dk = q.shape
    dv = v.shape[-1]
    L = RT_CHUNK
    log_gamma = jnp.log(1.0 - 2.0 ** (-5.0 - jnp.arange(H, dtype=F32)))
    idx = jnp.arange(L, dtype=F32)
    diff = idx[:, None] - idx[None, :]
    d_mask = jnp.where(diff >= 0, jnp.exp(log_gamma[:, None, None] * jnp.maximum(diff, 0.0)), 0.0)
    q_decay = jnp.exp(log_gamma[:, None] * (idx[None, :] + 1.0))[None, :, :, None]
    k_decay = jnp.exp(log_gamma[:, None] * (L - 1.0 - idx[None, :]))[None, :, :, None]
    chunk_decay = jnp.exp(log_gamma * L)[None, :, None, None]
    xs = tuple(to_chunks(a, L) for a in (q, k, v))

    def step(R, xc):
        qc, kc, vc = xc
        inner = jnp.einsum('bhts,bhsv->bhtv', jnp.einsum('bhtd,bhsd->bhts', qc, kc) * d_mask, vc)
        cross = jnp.einsum('bhtd,bhdv->bhtv', qc, R) * q_decay
        R = chunk_decay * R + jnp.einsum('bhsd,bhsv->bhdv', kc * k_decay, vc)
        return R, inner + cross

    _, y = lax.scan(step, jnp.zeros((B, H, dk, dv), F32), xs)
    return from_chunks(y)


def retention_mixer(x, positions, w_in, w_out):
    B, T, _ = x.shape
    H, dk, dv = RT_HEADS, RT_DK, RT_DV
    q, k, v, g = jnp.split(x @ w_in, [H * dk, 2 * H * dk, 2 * H * dk + H * dv], axis=-1)
    q = retention_rotate(q.reshape(B, T, H, dk).astype(F32), positions)
    k = retention_rotate(k.reshape(B, T, H, dk).astype(F32), positions) * (dk ** -0.5)
    y = retention_chunkwise(q, k, v.reshape(B, T, H, dv).astype(F32))
    y = head_rms(y).reshape(B, T, H * dv)
    return (jax.nn.silu(g.astype(F32)) * y).astype(x.dtype) @ w_out


def _dense(k, shape, fan_in, scale=1.0):
    return jax.random.normal(k, shape, F32) * (scale * fan_in ** -0.5)


def setup_inputs(seed: int = 0) -> dict:
    key = jax.random.key(seed)
    ks = jax.random.split(key, 40)
    D = D_MODEL
    nm, nr, nt = N_ML_LAYERS, N_RW_LAYERS, N_RT_LAYERS
    nrm = lambda k, shape, s: jax.random.normal(k, shape, F32) * s
    x = jax.random.normal(ks[0], (BATCH, SEQ, D), F32)
    positions = (jnp.arange(SEQ, dtype=jnp.int32)[None, :]
                 + jax.random.randint(ks[1], (BATCH, 1), 0, SEQ, dtype=jnp.int32))
    norm_g = 1.0 + nrm(ks[2], (DEPTH, 6, D), 0.01)
    ffn_w_gu = _dense(ks[3], (DEPTH, 2, D, 2 * D_FF), D)
    ffn_w_down = _dense(ks[4], (DEPTH, 2, D_FF, D), D_FF)
    ml_w_in = _dense(ks[5], (nm, D, ML_IN), D)
    ml_b_if = jnp.concatenate([nrm(ks[6], (nm, ML_HEADS), 0.01),
                               jnp.linspace(3.0, 6.0, ML_HEADS, dtype=F32)[None, :] + nrm(ks[7], (nm, ML_HEADS), 0.01)],
                              axis=-1)
    ml_conv_w = _dense(ks[8], (nm, ML_CONV, 2 * ML_HEADS * ML_DQK), ML_CONV)
    ml_norm_g = 1.0 + nrm(ks[9], (nm, ML_HEADS * ML_DV), 0.01)
    ml_w_out = _dense(ks[10], (nm, ML_HEADS * ML_DV, D), ML_HEADS * ML_DV)
    rw_mu = jax.random.uniform(ks[11], (nr, 6, D), F32)
    rw_w_rkv = _dense(ks[12], (nr, 3, D, D), D)
    rw_w0 = jnp.linspace(-6.5, -1.5, D, dtype=F32)[None, :] + nrm(ks[13], (nr, D), 0.1)
    rw_w1 = _dense(ks[14], (nr, D, RW_LORA_W), D)
    rw_w2 = _dense(ks[15], (nr, RW_LORA_W, D), RW_LORA_W, 0.1)
    rw_a0 = nrm(ks[16], (nr, D), 0.01)
    rw_a1 = _dense(ks[17], (nr, D, RW_LORA_A), D)
    rw_a2 = _dense(ks[18], (nr, RW_LORA_A, D), RW_LORA_A, 0.1)
    rw_g1 = _dense(ks[19], (nr, D, RW_LORA_G), D)
    rw_g2 = _dense(ks[20], (nr, RW_LORA_G, D), RW_LORA_G)
    rw_k_k = 0.85 + nrm(ks[21], (nr, D), 0.01)
    rw_k_a = 1.0 + nrm(ks[22], (nr, D), 0.01)
    rw_r_k = nrm(ks[23], (nr, RW_HEADS, RW_HEAD), 0.1)
    rw_ln_g = 1.0 + nrm(ks[24], (nr, D), 0.01)
    rw_ln_b = nrm(ks[25], (nr, D), 0.01)
    rw_w_out = _dense(ks[26], (nr, D, D), D)
    rt_w_in = _dense(ks[27], (nt, D, RT_IN), D)
    rt_w_out = _dense(ks[28], (nt, RT_HEADS * RT_DV, D), RT_HEADS * RT_DV)
    return {'x': x, 'positions': positions, 'norm_g': norm_g, 'ffn_w_gu': ffn_w_gu, 'ffn_w_down': ffn_w_down,
            'ml_w_in': ml_w_in, 'ml_b_if': ml_b_if, 'ml_conv_w': ml_conv_w, 'ml_norm_g': ml_norm_g, 'ml_w_out': ml_w_out,
            'rw_mu': rw_mu, 'rw_w_rkv': rw_w_rkv, 'rw_w0': rw_w0, 'rw_w1': rw_w1, 'rw_w2': rw_w2,
            'rw_a0': rw_a0, 'rw_a1': rw_a1, 'rw_a2': rw_a2, 'rw_g1': rw_g1, 'rw_g2': rw_g2,
            'rw_k_k': rw_k_k, 'rw_k_a': rw_k_a, 'rw_r_k': rw_r_k, 'rw_ln_g': rw_ln_g, 'rw_ln_b': rw_ln_b,
            'rw_w_out': rw_w_out, 'rt_w_in': rt_w_in, 'rt_w_out': rt_w_out}


def reference(x, positions, norm_g, ffn_w_gu, ffn_w_down,
              ml_w_in, ml_b_if, ml_conv_w, ml_norm_g, ml_w_out,
              rw_mu, rw_w_rkv, rw_w0, rw_w1, rw_w2, rw_a0, rw_a1, rw_a2, rw_g1, rw_g2,
              rw_k_k, rw_k_a, rw_r_k, rw_ln_g, rw_ln_b, rw_w_out, rt_w_in, rt_w_out):
    for layer in range(DEPTH):
        g = norm_g[layer]
        h = swiglu_ffn(rms_norm(x, g[0]), ffn_w_gu[layer, 0], ffn_w_down[layer, 0])
        x = x + HALF_STEP * rms_norm(h, g[1])
        h = rms_norm(x, g[2])
        kind, j = layer % N_MIXERS, layer // N_MIXERS
        if kind == 0:
            h = mlstm_mixer(h, ml_w_in[j], ml_b_if[j], ml_conv_w[j], ml_norm_g[j], ml_w_out[j])
        elif kind == 1:
            h = rwkv7_mixer(h, rw_mu[j], rw_w_rkv[j], rw_w0[j], rw_w1[j], rw_w2[j], rw_a0[j], rw_a1[j], rw_a2[j],
                            rw_g1[j], rw_g2[j], rw_k_k[j], rw_k_a[j], rw_r_k[j], rw_ln_g[j], rw_ln_b[j], rw_w_out[j])
        else:
            h = retention_mixer(h, positions, rt_w_in[j], rt_w_out[j])
        x = x + rms_norm(h, g[3])
        h = swiglu_ffn(rms_norm(x, g[4]), ffn_w_gu[layer, 1], ffn_w_down[layer, 1])
        x = x + HALF_STEP * rms_norm(h, g[5])
    return x
```

```python
import numpy as np
import concourse.bass as bass
import concourse.mybir as mybir
from concourse.bass_utils import run_bass_kernel_spmd

F32 = mybir.dt.float32
BF16 = mybir.dt.bfloat16
I32 = mybir.dt.int32
ALU = mybir.AluOpType
AF = mybir.ActivationFunctionType
AX = mybir.AxisListType

NCORES = 8
D = 1024
DFF = 2816
NJ = DFF // 128
TPC = 4096
NT = TPC // 128
SEQ = 2048
EPS = 1e-6

ENGS = ["pe", "dve", "act", "pool", "sp"]
CHUNK = 8000
SAME_ENG_SYNC = True


class Buf:
    __slots__ = ("name", "lw", "rd", "excl")

    def __init__(self, name, excl=False):
        self.name = name
        self.lw = None
        self.rd = {}
        self.excl = excl


def _freeze(fn):
    import types
    if fn is None or fn.__closure__ is None:
        return fn
    cells = []
    for c in fn.__closure__:
        try:
            cells.append(types.CellType(c.cell_contents))
        except ValueError:
            cells.append(c)
    return types.FunctionType(fn.__code__, fn.__globals__, fn.__name__, fn.__defaults__, tuple(cells))


class Prog:
    def __init__(self, nc):
        self.nc = nc
        self.ops = {e: [] for e in ENGS}
        self.cnt = {e: 0 for e in ENGS}
        self.seen = {e: {} for e in ENGS}
        self.dcnt = {}

    def _deps(self, eng, reads, writes):
        need = {}

        def add(k, v):
            if need.get(k, 0) < v:
                need[k] = v

        for b in reads:
            if b.lw is not None:
                add(*b.lw)
            if b.excl:
                for k, v in b.rd.items():
                    if k != ("e", eng):
                        add(k, v)
        for b in writes:
            if b.lw is not None:
                add(*b.lw)
            for k, v in b.rd.items():
                add(k, v)
        s = self.seen[eng]
        waits = []
        for k, v in need.items():
            if k == ("e", eng) and (eng == "pe" or not SAME_ENG_SYNC):
                continue
            if s.get(k, 0) >= v:
                continue
            s[k] = v
            waits.append((k, v))
        return waits

    def _mark(self, tok, reads, writes):
        k, v = tok
        for b in reads:
            if b.rd.get(k, 0) < v:
                b.rd[k] = v
        for b in writes:
            b.lw = tok
            b.rd = {}

    def op(self, eng, fn, reads=(), writes=()):
        waits = self._deps(eng, reads, writes)
        self.cnt[eng] += 1
        tok = (("e", eng), self.cnt[eng])
        self.ops[eng].append((_freeze(fn), waits, tok))
        self._mark(tok, reads, writes)

    def dma(self, eng, fn, reads=(), writes=(), dkey=None):
        waits = self._deps(eng, reads, writes)
        n = self.dcnt.get(dkey, 0) + 16
        self.dcnt[dkey] = n
        tok = (("d", dkey), n)
        self.ops[eng].append((_freeze(fn), waits, tok))
        self._mark(tok, reads, writes)

    def barrier(self):
        for e in ENGS:
            s = self.seen[e]
            waits = []
            for o in ENGS:
                if o == e or self.cnt[o] == 0:
                    continue
                k = ("e", o)
                if s.get(k, 0) < self.cnt[o]:
                    s[k] = self.cnt[o]
                    waits.append((k, self.cnt[o]))
            for dk, n in self.dcnt.items():
                k = ("d", dk)
                if s.get(k, 0) < n:
                    s[k] = n
                    waits.append((k, n))
            if waits:
                self.ops[e].append((None, waits, None))

    def run(self, final_bufs=()):
        nc = self.nc
        from contextlib import ExitStack
        with ExitStack() as st:
            sems = {}
            for e in ENGS:
                nchunks = max(1, (self.cnt[e] + CHUNK - 1) // CHUNK)
                for c in range(nchunks):
                    sems[(("e", e), c)] = st.enter_context(nc.semaphore(f"s_{e}_{c}"))
            for dk in self.dcnt:
                sems[(("d", dk), 0)] = st.enter_context(nc.semaphore(f"d_{dk}"))
            block = st.enter_context(nc.Block())

            def semval(k, v):
                if k[0] == "e":
                    c = (v - 1) // CHUNK
                    return sems[(k, c)], v - c * CHUNK
                return sems[(k, 0)], v

            def emit(ename, eng):
                for fn, waits, tok in self.ops[ename]:
                    for (k, v) in waits:
                        s, vv = semval(k, v)
                        eng.wait_ge(s, vv)
                    if fn is None:
                        continue
                    ins = fn(eng)
                    k, v = tok
                    if k[0] == "e":
                        s, vv = semval(k, v)
                        ins.then_inc(s, 1)
                    else:
                        ins.then_inc(sems[(k, 0)], 16)
                if ename == "sp":
                    need = {}
                    for b in final_bufs:
                        toks = list(b.rd.items())
                        if b.lw is not None:
                            toks.append(b.lw)
                        for k, v in toks:
                            need[k] = max(need.get(k, 0), v)
                    for k, v in need.items():
                        s, vv = semval(k, v)
                        eng.wait_ge(s, vv)

            @block.tensor
            def _(e):
                emit("pe", e)

            @block.vector
            def _(e):
                emit("dve", e)

            @block.scalar
            def _(e):
                emit("act", e)

            @block.gpsimd
            def _(e):
                emit("pool", e)

            @block.sync
            def _(e):
                emit("sp", e)


class Arena:
    def __init__(self, nc, base, limit):
        self.nc, self.base, self.limit = nc, base, limit
        self.off = base
        self.gen = 0

    def reset(self):
        self.off = self.base
        self.gen += 1

    def alloc(self, name, shape, dtype):
        nbytes = int(np.prod(shape[1:])) * (4 if dtype in (F32, I32) else 2)
        nbytes = (nbytes + 63) // 64 * 64
        assert self.off + nbytes <= self.limit, (name, self.off, nbytes, self.limit)
        t = self.nc.alloc_sbuf_tensor_at(f"{name}_g{self.gen}", list(shape), dtype, offset=self.off)
        self.off += nbytes
        return t


class K:
    pass


def build_program(subs=tuple(range(12))):
    nc = bass.Bass("TRN2", target_bir_lowering=False)
    P = Prog(nc)
    k = K()
    k.nc, k.P = nc, P
    k.dram_names = []

    def dram(name, shape, dtype=F32, kind="ExternalInput"):
        k.dram_names.append(name)
        return nc.dram_tensor(name, list(shape), dtype, kind=kind).ap()

    k.dram = dram
    k.x_d = dram("x", [NT, 128, D])
    k.out_d = dram("out", [NT, 128, D], kind="ExternalOutput")
    k.ng_d = dram("ng", [24, D])
    k.ngT_d = dram("ngT", [128, 24 * 8])
    if any(s % 3 != 1 for s in subs):
        k.wgu_d = dram("wgu", [8 * NJ, 128, 2, 1024])
        k.wd_d = dram("wd", [8 * NJ, 128, 1024])

    RES_BYTES = NT * D * 4
    SB0 = 16640
    k.xres = nc.alloc_sbuf_tensor_at("xres", [128, NT, D], F32, offset=SB0 + 4096)
    k.Bx = [Buf(f"x{i}") for i in range(NT)]
    pers = Arena(nc, SB0, SB0 + 4096)
    k.big_arena = Arena(nc, SB0 + 4096 + 2 * D * 4, 229376)
    k.ident = pers.alloc("ident", [128, 128], BF16)
    k.ngT = pers.alloc("ngT", [128, 24 * 8], F32)
    k.ones = pers.alloc("ones", [128, 128], BF16)
    k.mhalf = pers.alloc("mhalf", [128, 1], F32)
    k.tri = pers.alloc("tri", [128, 128], BF16)
    k.trif = pers.alloc("trif", [128, 128], F32)
    k.onesf = pers.alloc("onesf", [128, 128], F32)
    k.sl64 = pers.alloc("sl64", [64, 64], BF16)
    k.m1 = pers.alloc("m1", [64, 128], BF16)
    k.Bconst = Buf("const")
    k.arena = Arena(nc, SB0 + RES_BYTES + 4096, 229376)
    k.ps = [nc.alloc_psum_tensor(f"ps{i}", [128, 512], F32) for i in range(8)]
    k.Bps = [Buf(f"ps{i}", excl=True) for i in range(8)]
    k.rot = 0

    P.op("pool", lambda e: e.memset(k.ones[:], 1.0), writes=[k.Bconst])
    P.op("pool", lambda e: e.memset(k.onesf[:], 1.0), writes=[k.Bconst])
    P.op("pool", lambda e: e.affine_select(out=k.ident[:], in_=k.ones[:], pattern=[[-1, 128]],
                                           compare_op=ALU.is_equal, fill=0.0, base=0, channel_multiplier=1),
         reads=[k.Bconst], writes=[k.Bconst])
    for tt in (k.tri, k.trif):
        P.op("pool", lambda e, tt=tt: e.affine_select(out=tt[:], in_=(k.ones if tt is k.tri else k.onesf)[:],
                                                      pattern=[[1, 128]], compare_op=ALU.is_ge, fill=0.0, base=0,
                                                      channel_multiplier=-1),
             reads=[k.Bconst], writes=[k.Bconst])
    P.op("pool", lambda e: e.memset(k.mhalf[:], -0.5), writes=[k.Bconst])
    P.op("pool", lambda e: e.affine_select(out=k.sl64[:], in_=k.ones[0:64, 0:64], pattern=[[-1, 64]],
                                           compare_op=ALU.is_gt, fill=0.0, base=0, channel_multiplier=1),
         reads=[k.Bconst], writes=[k.Bconst])
    P.op("pool", lambda e: e.affine_select(out=k.m1[:, 0:64], in_=k.ones[0:64, 0:64], pattern=[[1, 64]],
                                           compare_op=ALU.is_gt, fill=0.0, base=0, channel_multiplier=-1),
         reads=[k.Bconst], writes=[k.Bconst])
    P.op("pool", lambda e: e.affine_select(out=k.m1[:, 64:128], in_=k.ones[0:64, 0:64], pattern=[[1, 64]],
                                           compare_op=ALU.is_ge, fill=0.0, base=0, channel_multiplier=-1),
         reads=[k.Bconst], writes=[k.Bconst])
    P.dma("sp", lambda e: e.dma_start(out=k.ngT[:], in_=k.ngT_d[:, :]), writes=[k.Bconst], dkey="ngT")

    for i in range(0, NT, 2):
        P.dma("sp", lambda e, i=i: e.dma_start(out=k.xres[:, i:i + 2, :],
                                                in_=k.x_d[i:i + 2].rearrange("t p d -> p t d")),
              writes=[k.Bx[i], k.Bx[i + 1]], dkey=f"x{i // 2}")

    for sub in subs:
        layer, s3 = sub // 3, sub % 3
        if s3 == 0:
            ffn_phase(k, layer, 0)
        elif s3 == 2:
            ffn_phase(k, layer, 1)
        elif layer % 3 == 0:
            lin_mixer_phase(k, layer, 0, layer // 3)
        elif layer % 3 == 2:
            lin_mixer_phase(k, layer, 2, layer // 3)
        else:
            rwkv_phase(k, layer, layer // 3)

    for i in range(0, 16 if RW_DEBUG else NT, 2):
        P.dma("sp", lambda e, i=i: e.dma_start(out=k.out_d[i:i + 2].rearrange("t p d -> p t d"),
                                                in_=k.xres[:, i:i + 2, :]),
              reads=[k.Bx[i], k.Bx[i + 1]], dkey=f"x{i // 2}")
    P.run(final_bufs=k.Bx)
    return nc, k.dram_names


def rstd_from_ssq(k, ssq_ap, rstd_ap, Bs, Br, n_part=128):
    P = k.P
    P.op("pool", lambda e: e.tensor_scalar(out=rstd_ap, in0=ssq_ap, scalar1=1.0 / D, scalar2=EPS,
                                           op0=ALU.mult, op1=ALU.add), reads=[Bs], writes=[Br])
    P.op("pool", lambda e: e.tensor_tensor(out=rstd_ap, in0=rstd_ap, in1=k.mhalf[0:n_part, :], op=ALU.pow),
         reads=[Br, k.Bconst], writes=[Br])


def prenorm_tile(k, ti, gidx, xnT, BxnT, col, sc):
    P = k.P
    par = ti % 2
    junk, Bjunk = sc["junk"], sc["Bjunk"]
    xs, Bxs = sc["xs"][par], sc["Bxs"][par]
    ssq, Bssq = sc["ssq"][par], sc["Bssq"][par]
    rstd, Brstd = sc["rstd"][par], sc["Brstd"][par]
    psb = sc["ps_tr"][par]
    P.op("act", lambda e: e.activation(out=junk[:], in_=k.xres[:, ti, :], func=AF.Square, accum_out=ssq[:]),
         reads=[k.Bx[ti]], writes=[Bjunk, Bssq])
    rstd_from_ssq(k, ssq[:], rstd[:], Bssq, Brstd)
    P.op("act", lambda e: e.activation(out=xs[:], in_=k.xres[:, ti, :], func=AF.Copy, scale=rstd[:]),
         reads=[k.Bx[ti], Brstd], writes=[Bxs])
    pst = k.ps[psb][:].bitcast(BF16)
    for kk in range(8):
        P.op("pe", lambda e, kk=kk: e.transpose(out=pst[:, kk * 128:(kk + 1) * 128],
                                                in_=xs[:, kk * 128:(kk + 1) * 128], identity=k.ident[:]),
             reads=[Bxs, k.Bconst], writes=[k.Bps[psb]])
    gT = k.ngT[:, gidx * 8:(gidx + 1) * 8].unsqueeze(2).to_broadcast([128, 8, 128])
    P.op("dve", lambda e: e.tensor_tensor(out=xnT[:, :, col:col + 128],
                                          in0=pst.rearrange("p (a t) -> p a t", a=8), in1=gT, op=ALU.mult),
         reads=[k.Bps[psb], k.Bconst], writes=[BxnT])


def postnorm_residual(k, ti, halves, gpost, Bgpost, coef, sc):
    P = k.P
    par = ti % 2
    junk, Bjunk = sc["junk"], sc["Bjunk"]
    ss2, Bss2 = sc["ss2"][par], sc["Bss2"][par]
    rs2, Brs2 = sc["rs2"][par], sc["Brs2"][par]
    tmp, Btmp = sc["tmp"][par], sc["Btmp"][par]
    for h, pb in enumerate(halves):
        P.op("act", lambda e, h=h, pb=pb: e.activation(out=junk[:, 0:512], in_=k.ps[pb][:], func=AF.Square,
                                                       accum_out=ss2[:, h:h + 1]),
             reads=[k.Bps[pb]], writes=[Bjunk, Bss2])
    P.op("dve", lambda e: e.tensor_tensor(out=ss2[:, 2:3], in0=ss2[:, 0:1], in1=ss2[:, 1:2], op=ALU.add),
         reads=[Bss2], writes=[Bss2])
    rstd_from_ssq(k, ss2[:, 2:3], rs2[:], Bss2, Brs2)
    for h, pb in enumerate(halves):
        P.op("dve", lambda e, h=h, pb=pb: e.scalar_tensor_tensor(
            out=tmp[:, h * 512:(h + 1) * 512], in0=k.ps[pb][:], scalar=rs2[:], in1=gpost[:, h * 512:(h + 1) * 512],
            op0=ALU.mult, op1=ALU.mult), reads=[k.Bps[pb], Brs2, Bgpost], writes=[Btmp])
    P.op("dve", lambda e: e.scalar_tensor_tensor(out=k.xres[:, ti, :], in0=tmp[:], scalar=float(coef),
                                                 in1=k.xres[:, ti, :], op0=ALU.mult, op1=ALU.add),
         reads=[Btmp, k.Bx[ti]], writes=[k.Bx[ti]])


def common_scratch(k, A, ntmp=2):
    sc = {}
    sc["junk"] = A.alloc("junk", [128, 1024], BF16)
    sc["Bjunk"] = Buf("junk")
    for nm, shape, dtype in [("xs", [128, 1024], BF16), ("ssq", [128, 1], F32), ("rstd", [128, 1], F32),
                             ("ss2", [128, 4], F32), ("rs2", [128, 1], F32), ("tmp", [128, 1024], F32)]:
        n = ntmp if nm in ("tmp", "xs") else 2
        ts = [A.alloc(f"{nm}{i}", shape, dtype) for i in range(n)]
        bs = [Buf(f"{nm}{i}") for i in range(n)]
        sc[nm] = [ts[i % n] for i in range(2)]
        sc["B" + nm] = [bs[i % n] for i in range(2)]
    return sc


NWGU, NWD = 5, 4


def ffn_setup(k):
    if getattr(k, "ffn", None) is not None and k.ffn["gen"] == k.arena.gen:
        return k.ffn
    k.P.barrier()
    A = k.arena
    A.reset()
    f = {"gen": A.gen}
    f["sc"] = common_scratch(k, A, ntmp=1)
    f["sc"]["ps_tr"] = [4, 5]
    f["xnT"] = A.alloc("xnT", [128, 8, 512], BF16)
    f["BxnT"] = Buf("xnT")
    f["actT"] = A.alloc("actT", [128, NJ, 512], BF16)
    f["BactT"] = [Buf(f"actT{j}") for j in range(NJ)]
    f["wgu"] = A.alloc("wgu", [128, NWGU, 2, 1024], BF16)
    f["Bwgu"] = [Buf(f"wgu{i}") for i in range(NWGU)]
    f["wd"] = A.alloc("wd", [128, NWD, 1024], BF16)
    f["Bwd"] = [Buf(f"wd{i}") for i in range(NWD)]
    f["gpost"] = A.alloc("gpost", [128, 1024], F32)
    f["Bgpost"] = Buf("gpost")
    f["sg"] = [A.alloc(f"sg{i}", [128, 512], F32) for i in range(2)]
    f["Bsg"] = [Buf(f"sg{i}") for i in range(2)]
    f["wq"] = 0
    f["dq"] = 0
    k.ffn = f
    return f


def ffn_phase(k, layer, which):
    P = k.P
    f = ffn_setup(k)
    sc = f["sc"]
    fidx = layer * 2 + which
    g_pre = layer * 6 + (0 if which == 0 else 4)
    g_post = g_pre + 1
    xnT, BxnT, actT, BactT = f["xnT"], f["BxnT"], f["actT"], f["BactT"]
    P.dma("sp", lambda e: e.dma_start(out=f["gpost"][:], in_=k.ng_d[g_post, :].partition_broadcast(128)),
          writes=[f["Bgpost"]], dkey="gpost")
    NST = NT // 4

    def prenorm_st(st):
        for t in range(4):
            prenorm_tile(k, st * 4 + t, g_pre, xnT, BxnT, t * 128, sc)

    prenorm_st(0)
    for st in range(NST):
        for j in range(NJ):
            slot = f["wq"] % NWGU
            f["wq"] += 1
            P.dma("pool", lambda e, j=j, slot=slot: e.dma_start(out=f["wgu"][:, slot], in_=k.wgu_d[fidx * NJ + j]),
                  writes=[f["Bwgu"][slot]], dkey=f"wgu{slot}")
            pg, pu = (0, 1) if j % 2 == 0 else (2, 3)
            for half, pb in ((0, pg), (1, pu)):
                for kk in range(8):
                    P.op("pe", lambda e, pb=pb, slot=slot, half=half, kk=kk: e.matmul(
                        k.ps[pb][:], lhsT=f["wgu"][:, slot, half, kk * 128:(kk + 1) * 128], rhs=xnT[:, kk, :],
                        start=(kk == 0), stop=(kk == 7)),
                        reads=[f["Bwgu"][slot], BxnT], writes=[k.Bps[pb]])
            sg, Bsg = f["sg"][j % 2], f["Bsg"][j % 2]
            P.op("act", lambda e, pg=pg, sg=sg: e.activation(out=sg[:], in_=k.ps[pg][:], func=AF.Silu),
                 reads=[k.Bps[pg]], writes=[Bsg])
            P.op("dve", lambda e, pu=pu, sg=sg, j=j: e.tensor_tensor(out=actT[:, j, :], in0=sg[:], in1=k.ps[pu][:],
                                                                     op=ALU.mult),
                 reads=[Bsg, k.Bps[pu]], writes=[BactT[j]])
        if st + 1 < NST:
            prenorm_st(st + 1)
        for j in range(NJ):
            slot = f["dq"] % NWD
            f["dq"] += 1
            P.dma("pool", lambda e, j=j, slot=slot: e.dma_start(out=f["wd"][:, slot, :], in_=k.wd_d[fidx * NJ + j]),
                  writes=[f["Bwd"][slot]], dkey=f"wd{slot}")
            for tt in range(4):
                for half in range(2):
                    pb = tt * 2 + half
                    P.op("pe", lambda e, pb=pb, slot=slot, half=half, tt=tt, j=j: e.matmul(
                        k.ps[pb][:], lhsT=actT[:, j, tt * 128:(tt + 1) * 128],
                        rhs=f["wd"][:, slot, half * 512:(half + 1) * 512], start=(j == 0), stop=(j == NJ - 1)),
                        reads=[BactT[j], f["Bwd"][slot]], writes=[k.Bps[pb]])
        for tt in range(4):
            postnorm_residual(k, st * 4 + tt, [tt * 2, tt * 2 + 1], f["gpost"], f["Bgpost"], 0.5, sc)


import math
LN_S = {0: math.log(128 ** -0.5), 2: math.log(256 ** -0.5)}


def lin_setup(k, kind, layer):
    P = k.P
    P.barrier()
    A = k.arena
    A.reset()
    k.ffn = None
    m = {"kind": kind}
    H = 4
    NK = 1 if kind == 0 else 2
    DV = 256 if kind == 0 else 512
    NF = 8 if kind == 0 else 16
    HD = H * DV
    m.update(H=H, NK=NK, DV=DV, NF=NF, HD=HD, NKO=HD // 128, NTM=4 if kind == 0 else 8)
    m["sc"] = common_scratch(k, A, ntmp=1)
    m["sc"]["ps_tr"] = [6, 7]
    al = lambda nm, shape, dtype=BF16: (A.alloc(nm, shape, dtype), Buf(nm))
    m["xnT"], m["BxnT"] = al("xnT", [128, 8, 256])
    m["qk"], m["Bqk"] = al("qk", [128, NF, 256])
    m["vo"], _ = al("vo", [128, 2, HD])
    m["Bvo"] = [Buf("vo0"), Buf("vo1")]
    m["hbuf"], _ = al("hbuf", [128, 2, HD])
    m["Bhbuf"] = [Buf("hb0"), Buf("hb1")]
    m["hT"], m["BhT"] = al("hT", [128, HD // 128, 256])
    NR = 5 if kind == 0 else 2
    m["NR"] = NR
    m["ring"], _ = al("ring", [128, NR, 2048])
    m["Bring"] = [Buf(f"ring{i}") for i in range(NR)]
    m["rq"] = 0
    m["gpost"], m["Bgpost"] = al("gpost", [128, 1024], F32)
    m["Cbf"], m["BCbf"] = al("Cbf", [128, H * NK, DV])
    m["ktm"], m["Bktm"] = al("ktm", [128, H, NK * 128])
    m["PT"], m["BPT"] = al("PT", [128, H, 128])
    m["tmpC"], m["BtmpC"] = m["sc"]["tmp"][0], m["sc"]["Btmp"][0]
    m["e"], m["Be"] = al("e", [128, 2, 4], F32)
    m["base"], m["Bbase"] = al("base", [128, 2, 4], F32)
    m["cdec"], m["Bcdec"] = al("cdec", [128, 2, 4], F32)
    m["sm"], m["Bsm"] = al("sm", [128, 8, 4], F32)
    m["wd"] = k.dram(f"wm{layer}", [NF // 2 + 2 * m["NTM"] + m["NKO"] // 2, 128, 2048])
    if kind == 0:
        m["C"], m["BC"] = al("C", [128, H, DV], F32)
        m["nst"], m["Bnst"] = al("nst", [128, 4], F32)
        m["nbf"], m["Bnbf"] = al("nbf", [128, 4])
        m["pre"], m["Bpre"] = al("pre", [128, 8, 259], F32)
        m["acc"], m["Bacc"] = al("acc", [128, 256], F32)
        m["convT"], m["Bw"] = al("convT", [128, 32], F32)
        m["wif"], _ = al("wif", [128, 64])
        m["bifb"], _ = al("bifb", [128, 8], F32)
        m["mng"], _ = al("mng", [128, 1024], F32)
        m["gat"], m["Bgat"] = al("gat", [128, 2, 8], F32)
        m["gt2"], m["Bgt2"] = al("gt2", [128, 2, 8], F32)
        wif_d = k.dram(f"wif{layer}", [128, 64])
        bif_d = k.dram(f"bif{layer}", [1, 8])
        conv_d = k.dram(f"convT{layer}", [128, 32])
        mng_d = k.dram(f"mng{layer}", [1, 1024])
        Bw = m["Bw"]
        P.dma("pool", lambda e: e.dma_start(out=m["wif"][:], in_=wif_d[:, :]), writes=[Bw], dkey="mw0")
        P.dma("sp", lambda e: e.dma_start(out=m["bifb"][:], in_=bif_d[0, :].partition_broadcast(128)), writes=[Bw], dkey="mw1")
        P.dma("sp", lambda e: e.dma_start(out=m["convT"][:], in_=conv_d[:, :]), writes=[Bw], dkey="mw2")
        P.dma("sp", lambda e: e.dma_start(out=m["mng"][:], in_=mng_d[0, :].partition_broadcast(128)), writes=[Bw], dkey="mw3")
    else:
        if not hasattr(k, "pos_d"):
            k.pos_d = k.dram("pos", [1, 2 * SEQ], I32)
        m["posb"], m["Btrig"] = al("posb", [128, 256], F32)
        for nm in ("cosT", "sinT", "ta", "tb"):
            m[nm], _ = al(nm, [128, 256], F32)
        m["ang"] = m["posb"]
        m["ni"], _ = al("ni", [128, 256], I32)
        m["invf"], m["Binvf"] = al("invf", [128, 1], F32)
        m["ii"], _ = al("ii", [128, 1], I32)
        Bi = m["Binvf"]
        P.op("pool", lambda e: e.iota(m["ii"][:], pattern=[[0, 1]], base=0, channel_multiplier=1), writes=[Bi])
        P.op("dve", lambda e: e.tensor_copy(out=m["invf"][:], in_=m["ii"][:]), reads=[Bi], writes=[Bi])
        P.op("act", lambda e: e.activation(out=m["invf"][:], in_=m["invf"][:], func=AF.Exp,
                                           scale=-math.log(10000.0) / 127.0), reads=[Bi], writes=[Bi])
        Be = m["Be"]
        P.op("pool", lambda e: e.iota(m["ii"][:], pattern=[[0, 1]], base=1, channel_multiplier=1), writes=[Bi])
        P.op("dve", lambda e: e.tensor_copy(out=m["sm"][:, 0, 0:1], in_=m["ii"][:]), reads=[Bi], writes=[m["Bsm"]])
        for h in range(4):
            lg = math.log(1.0 - 2.0 ** (-5.0 - h))
            for tt in range(2):
                P.op("dve", lambda e, h=h, tt=tt, lg=lg: e.tensor_scalar(
                    out=m["e"][:, tt, h:h + 1], in0=m["sm"][:, 0, 0:1], scalar1=-lg, scalar2=LN_S[2],
                    op0=ALU.mult, op1=ALU.add), reads=[m["Bsm"]], writes=[Be])
                P.op("dve", lambda e, h=h, tt=tt, lg=lg: e.tensor_scalar(
                    out=m["base"][:, tt, h:h + 1], in0=m["sm"][:, 0, 0:1], scalar1=lg, scalar2=None,
                    op0=ALU.mult), reads=[m["Bsm"]], writes=[m["Bbase"]])
                P.op("pool", lambda e, h=h, tt=tt, lg=lg: e.memset(m["cdec"][:, tt, h:h + 1], math.exp(128.0 * lg)),
                     writes=[m["Bcdec"]])
        P.op("act", lambda e: e.activation(out=m["e"][:], in_=m["e"][:], func=AF.Exp), reads=[Be], writes=[Be])
        P.op("act", lambda e: e.activation(out=m["base"][:], in_=m["base"][:], func=AF.Exp),
             reads=[m["Bbase"]], writes=[m["Bbase"]])
    return m


def nbank(k):
    b = 4 + (k.rot % 4)
    k.rot += 1
    return b


def ring_load(k, m, unit):
    slot = m["rq"] % m["NR"]
    m["rq"] += 1
    k.P.dma("pool", lambda e: e.dma_start(out=m["ring"][:, slot, :], in_=m["wd"][unit]),
            writes=[m["Bring"][slot]], dkey=f"mr{slot}")
    return slot


def tm_proj(k, m, blocks, dst_off0):
    P = k.P
    for i, nb in enumerate(blocks):
        sa = ring_load(k, m, m["NF"] // 2 + 2 * nb)
        sb = ring_load(k, m, m["NF"] // 2 + 2 * nb + 1)
        for tt in range(2):
            b = nbank(k)
            for kk in range(8):
                sl = sa if kk < 4 else sb
                P.op("pe", lambda e, b=b, sl=sl, kk=kk, tt=tt: e.matmul(
                    k.ps[b][:], lhsT=m["xnT"][:, kk, tt * 128:(tt + 1) * 128],
                    rhs=m["ring"][:, sl, (kk % 4) * 512:(kk % 4 + 1) * 512], start=(kk == 0), stop=(kk == 7)),
                    reads=[m["BxnT"], m["Bring"][sl]], writes=[k.Bps[b]])
            off = dst_off0 + i * 512
            if (i + tt) % 2 == 0:
                P.op("act", lambda e, b=b, tt=tt, off=off: e.activation(out=m["vo"][:, tt, off:off + 512], in_=k.ps[b][:],
                                                                        func=AF.Copy),
                     reads=[k.Bps[b]], writes=[m["Bvo"][tt]])
            else:
                P.op("dve", lambda e, b=b, tt=tt, off=off: e.tensor_copy(out=m["vo"][:, tt, off:off + 512], in_=k.ps[b][:]),
                     reads=[k.Bps[b]], writes=[m["Bvo"][tt]])


def lin_mixer_phase(k, layer, kind, j):
    P = k.P
    m = lin_setup(k, kind, layer)
    sc = m["sc"]
    H, NK, DV, NF, HD, NKO, NTM = m["H"], m["NK"], m["DV"], m["NF"], m["HD"], m["NKO"], m["NTM"]
    g_pre, g_post = layer * 6 + 2, layer * 6 + 3
    xnT, qk, vo, hbuf, hT = m["xnT"], m["qk"], m["vo"], m["hbuf"], m["hT"]
    P.dma("sp", lambda e: e.dma_start(out=m["gpost"][:], in_=k.ng_d[g_post, :].partition_broadcast(128)),
          writes=[m["Bgpost"]], dkey="gpost")
    qidx = (lambda h, kc: h) if kind == 0 else (lambda h, kc: 2 * h + kc)
    kidx = (lambda h, kc: 4 + h) if kind == 0 else (lambda h, kc: 8 + 2 * h + kc)
    sm, Bsm = m["sm"], m["Bsm"]
    for st in range(NT // 2):
        first = (st % 8 == 0)
        for tt in range(2):
            prenorm_tile(k, st * 2 + tt, g_pre, xnT, m["BxnT"], tt * 128, sc)
        if first:
            P.op("pool", lambda e: e.memset(m["Cbf"][:], 0.0), writes=[m["BCbf"]])
            if kind == 0:
                P.op("pool", lambda e: e.memset(m["C"][:], 0.0), writes=[m["BC"]])
                P.op("pool", lambda e: e.memset(m["nst"][:], 0.0), writes=[m["Bnst"]])
                P.op("pool", lambda e: e.memset(m["nbf"][:], 0.0), writes=[m["Bnbf"]])
        if kind == 0:
            if first:
                P.op("pool", lambda e: e.memset(m["pre"][:, :, 0:3], 0.0), writes=[m["Bpre"]])
            else:
                P.op("pool", lambda e: e.tensor_copy(out=m["pre"][:, :, 0:3], in_=m["pre"][:, :, 256:259]),
                     reads=[m["Bpre"]], writes=[m["Bpre"]])
        else:
            Bt = m["Btrig"]
            P.dma("pool", lambda e, st=st: e.dma_start(out=m["posb"][:],
                                                        in_=k.pos_d[0, st * 256:(st + 1) * 256].partition_broadcast(128)),
                  writes=[Bt], dkey="posb")
            P.op("dve", lambda e: e.tensor_scalar(out=m["ang"][:], in0=m["posb"][:], scalar1=m["invf"][:], scalar2=None,
                                                  op0=ALU.mult), reads=[Bt, m["Binvf"]], writes=[Bt])
            for dst, shift in ((m["sinT"], 0.5), (m["cosT"], 0.75)):
                TWO_PI = 2.0 * math.pi
                P.op("dve", lambda e, shift=shift: e.tensor_scalar(out=m["ta"][:], in0=m["ang"][:], scalar1=1.0 / TWO_PI,
                                                                    scalar2=shift, op0=ALU.mult, op1=ALU.add),
                     reads=[Bt], writes=[Bt])
                P.op("dve", lambda e: e.tensor_copy(out=m["ni"][:], in_=m["ta"][:]), reads=[Bt], writes=[Bt])
                P.op("dve", lambda e: e.tensor_copy(out=m["tb"][:], in_=m["ni"][:]), reads=[Bt], writes=[Bt])
                P.op("dve", lambda e: e.tensor_tensor(out=m["ta"][:], in0=m["ta"][:], in1=m["tb"][:], op=ALU.subtract),
                     reads=[Bt], writes=[Bt])
                P.op("dve", lambda e: e.tensor_scalar(out=m["ta"][:], in0=m["ta"][:], scalar1=-0.5, scalar2=TWO_PI,
                                                      op0=ALU.add, op1=ALU.mult), reads=[Bt], writes=[Bt])
                P.op("dve", lambda e: e.tensor_scalar(out=m["tb"][:], in0=m["ta"][:], scalar1=math.pi, scalar2=-TWO_PI,
                                                      op0=ALU.is_gt, op1=ALU.mult), reads=[Bt], writes=[Bt])
                P.op("dve", lambda e: e.tensor_tensor(out=m["ta"][:], in0=m["ta"][:], in1=m["tb"][:], op=ALU.add),
                     reads=[Bt], writes=[Bt])
                P.op("dve", lambda e: e.tensor_scalar(out=m["tb"][:], in0=m["ta"][:], scalar1=-math.pi, scalar2=TWO_PI,
                                                      op0=ALU.is_lt, op1=ALU.mult), reads=[Bt], writes=[Bt])
                P.op("dve", lambda e: e.tensor_tensor(out=m["ta"][:], in0=m["ta"][:], in1=m["tb"][:], op=ALU.add),
                     reads=[Bt], writes=[Bt])
                P.op("dve", lambda e: e.tensor_scalar(out=m["ta"][:], in0=m["ta"][:], scalar1=-3.1415925, scalar2=3.1415925,
                                                      op0=ALU.max, op1=ALU.min), reads=[Bt], writes=[Bt])
                P.op("act", lambda e, dst=dst: e.activation(out=dst[:], in_=m["ta"][:], func=AF.Sin), reads=[Bt], writes=[Bt])
        for u in range(NF // 2):
            sl = ring_load(k, m, u)
            banks = []
            for c2 in range(2):
                b = nbank(k)
                banks.append(b)
                for kk in range(8):
                    P.op("pe", lambda e, b=b, sl=sl, c2=c2, kk=kk: e.matmul(
                        k.ps[b][:, 0:256], lhsT=m["ring"][:, sl, c2 * 1024 + kk * 128:c2 * 1024 + (kk + 1) * 128],
                        rhs=xnT[:, kk, :], start=(kk == 0), stop=(kk == 7)),
                        reads=[m["Bring"][sl], m["BxnT"]], writes=[k.Bps[b]])
                if kind == 0:
                    c = 2 * u + c2
                    P.op("act", lambda e, b=b, c=c: e.activation(out=m["pre"][:, c, 3:259], in_=k.ps[b][:, 0:256], func=AF.Copy),
                         reads=[k.Bps[b]], writes=[m["Bpre"]])
            if kind == 2:
                Bt = m["Btrig"]
                ba, bb = banks
                for o, (f1, f2, op) in enumerate(((m["cosT"], m["sinT"], ALU.subtract), (m["sinT"], m["cosT"], ALU.add))):
                    P.op("dve", lambda e, f1=f1: e.tensor_tensor(out=m["ta"][:], in0=k.ps[ba][:, 0:256], in1=f1[:], op=ALU.mult),
                         reads=[k.Bps[ba], Bt], writes=[Bt])
                    P.op("dve", lambda e, f2=f2: e.tensor_tensor(out=m["tb"][:], in0=k.ps[bb][:, 0:256], in1=f2[:], op=ALU.mult),
                         reads=[k.Bps[bb], Bt], writes=[Bt])
                    P.op("dve", lambda e, u=u, o=o, op=op: e.tensor_tensor(out=qk[:, 2 * u + o, :], in0=m["ta"][:], in1=m["tb"][:], op=op),
                         reads=[Bt], writes=[m["Bqk"]])
        if kind == 0:
            for c in range(8):
                cw = m["convT"]
                P.op("dve", lambda e, c=c: e.tensor_scalar(out=m["acc"][:], in0=m["pre"][:, c, 3:259],
                                                           scalar1=cw[:, c * 4 + 3:c * 4 + 4], scalar2=None, op0=ALU.mult),
                     reads=[m["Bpre"], m["Bw"]], writes=[m["Bacc"]])
                for tap in (2, 1, 0):
                    P.op("dve", lambda e, c=c, tap=tap: e.scalar_tensor_tensor(
                        out=m["acc"][:], in0=m["pre"][:, c, tap:tap + 256], scalar=cw[:, c * 4 + tap:c * 4 + tap + 1],
                        in1=m["acc"][:], op0=ALU.mult, op1=ALU.add), reads=[m["Bpre"], m["Bw"], m["Bacc"]], writes=[m["Bacc"]])
                P.op("act", lambda e, c=c: e.activation(out=qk[:, c, :], in_=m["acc"][:], func=AF.Silu),
                     reads=[m["Bacc"]], writes=[m["Bqk"]])
        tm_proj(k, m, list(range(NTM // 2)), 0)
        if kind == 0:
            gat, gt2, Bgat, Bgt2 = m["gat"], m["gt2"], m["Bgat"], m["Bgt2"]
            bg = nbank(k)
            for tt in range(2):
                for kk in range(8):
                    P.op("pe", lambda e, tt=tt, kk=kk: e.matmul(k.ps[bg][:, tt * 8:(tt + 1) * 8],
                                                                 lhsT=xnT[:, kk, tt * 128:(tt + 1) * 128],
                                                                 rhs=m["wif"][:, kk * 8:(kk + 1) * 8], start=(kk == 0), stop=(kk == 7)),
                         reads=[m["BxnT"], m["Bw"]], writes=[k.Bps[bg]])
                P.op("dve", lambda e, tt=tt: e.tensor_tensor(out=gat[:, tt, :], in0=k.ps[bg][:, tt * 8:(tt + 1) * 8],
                                                             in1=m["bifb"][:], op=ALU.add),
                     reads=[k.Bps[bg], m["Bw"]], writes=[Bgat])
            P.op("act", lambda e: e.activation(out=gat[:], in_=gat[:], func=AF.Tanh, scale=1.0 / 15.0), reads=[Bgat], writes=[Bgat])
            P.op("dve", lambda e: e.tensor_scalar(out=gat[:], in0=gat[:], scalar1=15.0, scalar2=None, op0=ALU.mult),
                 reads=[Bgat], writes=[Bgat])
            P.op("act", lambda e: e.activation(out=gt2[:, :, 4:8], in_=gat[:, :, 4:8], func=AF.Exp, scale=-1.0),
                 reads=[Bgat], writes=[Bgt2])
            P.op("dve", lambda e: e.tensor_scalar(out=gt2[:, :, 4:8], in0=gt2[:, :, 4:8], scalar1=1.0, scalar2=None, op0=ALU.add),
                 reads=[Bgt2], writes=[Bgt2])
            P.op("act", lambda e: e.activation(out=gt2[:, :, 4:8], in_=gt2[:, :, 4:8], func=AF.Ln), reads=[Bgt2], writes=[Bgt2])
            bc = nbank(k)
            for tt in range(2):
                P.op("pe", lambda e, tt=tt: e.matmul(k.ps[bc][:, tt * 4:(tt + 1) * 4], lhsT=k.trif[:], rhs=gt2[:, tt, 4:8],
                                                     start=True, stop=True), reads=[Bgt2, k.Bconst], writes=[k.Bps[bc]])
                P.op("pe", lambda e, tt=tt: e.matmul(k.ps[bc][:, 8 + tt * 4:8 + (tt + 1) * 4], lhsT=k.onesf[:], rhs=gt2[:, tt, 4:8],
                                                     start=True, stop=True), reads=[Bgt2, k.Bconst], writes=[k.Bps[bc]])
            cs = k.ps[bc][:, 0:8].rearrange("p (t h) -> p t h", t=2)
            tot = k.ps[bc][:, 8:16].rearrange("p (t h) -> p t h", t=2)
            P.op("dve", lambda e: e.scalar_tensor_tensor(out=m["e"][:], in0=cs, scalar=LN_S[0], in1=gat[:, :, 0:4],
                                                         op0=ALU.add, op1=ALU.add), reads=[k.Bps[bc], Bgat], writes=[m["Be"]])
            P.op("act", lambda e: e.activation(out=m["e"][:], in_=m["e"][:], func=AF.Exp), reads=[m["Be"]], writes=[m["Be"]])
            P.op("act", lambda e: e.activation(out=m["base"][:], in_=cs, func=AF.Exp), reads=[k.Bps[bc]], writes=[m["Bbase"]])
            P.op("act", lambda e: e.activation(out=m["cdec"][:], in_=tot, func=AF.Exp, scale=-1.0),
                 reads=[k.Bps[bc]], writes=[m["Bcdec"]])
        for tt in range(2):
            tok = slice(tt * 128, (tt + 1) * 128)
            bt = nbank(k)
            ptb = k.ps[bt][:].bitcast(BF16)
            for h in range(H):
                for kc in range(NK):
                    o = (h * NK + kc) * 128
                    P.op("pe", lambda e, h=h, kc=kc, o=o: e.transpose(out=ptb[:, o:o + 128], in_=qk[:, kidx(h, kc), tok],
                                                                       identity=k.ident[:]),
                         reads=[m["Bqk"], k.Bconst], writes=[k.Bps[bt]])
            for h in range(H):
                P.op("dve", lambda e, h=h: e.tensor_scalar(out=m["ktm"][:, h, :], in0=ptb[:, h * NK * 128:(h + 1) * NK * 128],
                                                           scalar1=m["e"][:, tt, h:h + 1], scalar2=None, op0=ALU.mult),
                     reads=[k.Bps[bt], m["Be"]], writes=[m["Bktm"]])
            bp = nbank(k)
            for h in range(H):
                for kc in range(NK):
                    P.op("pe", lambda e, h=h, kc=kc: e.matmul(k.ps[bp][:, h * 128:(h + 1) * 128], lhsT=qk[:, kidx(h, kc), tok],
                                                              rhs=qk[:, qidx(h, kc), tok], start=(kc == 0), stop=(kc == NK - 1)),
                         reads=[m["Bqk"]], writes=[k.Bps[bp]])
            for h in range(H):
                P.op("dve", lambda e, h=h: e.scalar_tensor_tensor(out=m["PT"][:, h, :], in0=k.ps[bp][:, h * 128:(h + 1) * 128],
                                                                  scalar=m["e"][:, tt, h:h + 1], in1=k.tri[:],
                                                                  op0=ALU.mult, op1=ALU.mult),
                     reads=[k.Bps[bp], m["Be"], k.Bconst], writes=[m["BPT"]])
            if kind == 0:
                bd = nbank(k)
                for h in range(H):
                    P.op("pe", lambda e, h=h: e.matmul(k.ps[bd][:, h:h + 1], lhsT=m["PT"][:, h, :], rhs=k.ones[:, 0:1],
                                                       start=True, stop=False), reads=[m["BPT"], k.Bconst], writes=[k.Bps[bd]])
                    P.op("pe", lambda e, h=h: e.matmul(k.ps[bd][:, h:h + 1], lhsT=qk[:, qidx(h, 0), tok], rhs=m["nbf"][:, h:h + 1],
                                                       start=False, stop=True), reads=[m["Bqk"], m["Bnbf"]], writes=[k.Bps[bd]])
                P.op("dve", lambda e: e.tensor_copy(out=sm[:, 0, :], in_=k.ps[bd][:, 0:4]), reads=[k.Bps[bd]], writes=[Bsm])
                P.op("dve", lambda e: e.tensor_scalar(out=sm[:, 6, :], in0=sm[:, 0, :], scalar1=-1.0, scalar2=None,
                                                      op0=ALU.mult), reads=[Bsm], writes=[Bsm])
                P.op("dve", lambda e: e.tensor_tensor(out=sm[:, 0, :], in0=sm[:, 0, :], in1=sm[:, 6, :], op=ALU.max),
                     reads=[Bsm], writes=[Bsm])
                P.op("dve", lambda e: e.tensor_tensor(out=sm[:, 0, :], in0=sm[:, 0, :], in1=m["base"][:, tt, :], op=ALU.max),
                     reads=[Bsm, m["Bbase"]], writes=[Bsm])
                P.op("dve", lambda e: e.reciprocal(out=sm[:, 1, :], in_=sm[:, 0, :]), reads=[Bsm], writes=[Bsm])
                basev = sm[:, 1, :]
            else:
                P.op("dve", lambda e: e.tensor_copy(out=sm[:, 1, :], in_=m["base"][:, tt, :]), reads=[m["Bbase"]], writes=[Bsm])
                basev = sm[:, 1, :]
            hpb = 512 // DV
            abank = {}
            for h in range(H):
                if h % hpb == 0:
                    ba = nbank(k)
                abank[h] = (ba, (h % hpb) * DV)
                ba, off = abank[h]
                P.op("pe", lambda e, h=h, ba=ba, off=off: e.matmul(k.ps[ba][:, off:off + DV], lhsT=m["PT"][:, h, :],
                                                                   rhs=vo[:, tt, h * DV:(h + 1) * DV], start=True, stop=False),
                     reads=[m["BPT"], m["Bvo"][tt]], writes=[k.Bps[ba]])
                for kc in range(NK):
                    P.op("pe", lambda e, h=h, kc=kc, ba=ba, off=off: e.matmul(
                        k.ps[ba][:, off:off + DV], lhsT=qk[:, qidx(h, kc), tok], rhs=m["Cbf"][:, h * NK + kc, :],
                        start=False, stop=(kc == NK - 1)), reads=[m["Bqk"], m["BCbf"]], writes=[k.Bps[ba]])
                P.op("act", lambda e, h=h, ba=ba, off=off: e.activation(out=sc["junk"][:, 0:DV], in_=k.ps[ba][:, off:off + DV],
                                                                        func=AF.Square, accum_out=sm[:, 2, h:h + 1]),
                     reads=[k.Bps[ba]], writes=[sc["Bjunk"], Bsm])
                if h % hpb == hpb - 1 or h == H - 1:
                    pass
            P.op("dve", lambda e: e.tensor_tensor(out=sm[:, 3, :], in0=basev, in1=basev, op=ALU.mult), reads=[Bsm], writes=[Bsm])
            P.op("dve", lambda e: e.tensor_tensor(out=sm[:, 3, :], in0=sm[:, 3, :], in1=sm[:, 2, :], op=ALU.mult), reads=[Bsm], writes=[Bsm])
            P.op("pool", lambda e: e.tensor_scalar(out=sm[:, 3, :], in0=sm[:, 3, :], scalar1=1.0 / DV, scalar2=EPS,
                                                   op0=ALU.mult, op1=ALU.add), reads=[Bsm], writes=[Bsm])
            P.op("pool", lambda e: e.tensor_tensor(out=sm[:, 3, :], in0=sm[:, 3, :], in1=k.mhalf[:, 0:1].to_broadcast([128, 4]),
                                                   op=ALU.pow), reads=[Bsm, k.Bconst], writes=[Bsm])
            P.op("dve", lambda e: e.tensor_tensor(out=sm[:, 4, :], in0=sm[:, 3, :], in1=basev, op=ALU.mult), reads=[Bsm], writes=[Bsm])
            for h in range(H):
                ba, off = abank[h]
                if kind == 0:
                    P.op("dve", lambda e, h=h, ba=ba, off=off: e.scalar_tensor_tensor(
                        out=hbuf[:, tt, h * DV:(h + 1) * DV], in0=k.ps[ba][:, off:off + DV], scalar=sm[:, 4, h:h + 1],
                        in1=m["mng"][:, h * DV:(h + 1) * DV], op0=ALU.mult, op1=ALU.mult),
                        reads=[k.Bps[ba], Bsm, m["Bw"]], writes=[m["Bhbuf"][tt]])
                else:
                    P.op("dve", lambda e, h=h, ba=ba, off=off: e.tensor_scalar(
                        out=hbuf[:, tt, h * DV:(h + 1) * DV], in0=k.ps[ba][:, off:off + DV], scalar1=sm[:, 4, h:h + 1],
                        scalar2=None, op0=ALU.mult), reads=[k.Bps[ba], Bsm], writes=[m["Bhbuf"][tt]])
            for h in range(H):
                cd = m["cdec"][:, tt, h:h + 1]
                for kc in range(NK):
                    bs = nbank(k)
                    P.op("pe", lambda e, h=h, kc=kc, bs=bs: e.matmul(k.ps[bs][:, 0:DV], lhsT=m["ktm"][:, h, kc * 128:(kc + 1) * 128],
                                                                     rhs=vo[:, tt, h * DV:(h + 1) * DV], start=True, stop=True),
                         reads=[m["Bktm"], m["Bvo"][tt]], writes=[k.Bps[bs]])
                    P.op("dve", lambda e, bs=bs, cd=cd: e.tensor_scalar(out=m["tmpC"][:, 0:DV], in0=k.ps[bs][:, 0:DV], scalar1=cd,
                                                                        scalar2=None, op0=ALU.mult),
                         reads=[k.Bps[bs], m["Bcdec"]], writes=[m["BtmpC"]])
                    if kind == 0:
                        P.op("dve", lambda e, h=h, cd=cd: e.scalar_tensor_tensor(out=m["C"][:, h, :], in0=m["C"][:, h, :], scalar=cd,
                                                                                 in1=m["tmpC"][:, 0:DV], op0=ALU.mult, op1=ALU.add),
                             reads=[m["BC"], m["Bcdec"], m["BtmpC"]], writes=[m["BC"]])
                        P.op("act", lambda e, h=h: e.activation(out=m["Cbf"][:, h, :], in_=m["C"][:, h, :], func=AF.Copy),
                             reads=[m["BC"]], writes=[m["BCbf"]])
                    else:
                        ci = h * NK + kc
                        P.op("dve", lambda e, ci=ci, cd=cd: e.scalar_tensor_tensor(out=m["Cbf"][:, ci, :], in0=m["Cbf"][:, ci, :], scalar=cd,
                                                                                   in1=m["tmpC"][:, 0:DV], op0=ALU.mult, op1=ALU.add),
                             reads=[m["BCbf"], m["Bcdec"], m["BtmpC"]], writes=[m["BCbf"]])
            if kind == 0:
                bn = nbank(k)
                for h in range(H):
                    P.op("pe", lambda e, h=h: e.matmul(k.ps[bn][:, h:h + 1], lhsT=m["ktm"][:, h, :], rhs=k.ones[:, 0:1],
                                                       start=True, stop=True), reads=[m["Bktm"], k.Bconst], writes=[k.Bps[bn]])
                P.op("dve", lambda e: e.tensor_tensor(out=sm[:, 5, :], in0=k.ps[bn][:, 0:4], in1=m["nst"][:], op=ALU.add),
                     reads=[k.Bps[bn], m["Bnst"]], writes=[Bsm])
                P.op("dve", lambda e: e.tensor_tensor(out=m["nst"][:], in0=sm[:, 5, :], in1=m["cdec"][:, tt, :], op=ALU.mult),
                     reads=[Bsm, m["Bcdec"]], writes=[m["Bnst"]])
                P.op("dve", lambda e: e.tensor_copy(out=m["nbf"][:], in_=m["nst"][:]), reads=[m["Bnst"]], writes=[m["Bnbf"]])
        tm_proj(k, m, list(range(NTM // 2, NTM)), 0)
        for tt in range(2):
            P.op("act", lambda e, tt=tt: e.activation(out=vo[:, tt, :], in_=vo[:, tt, :],
                                                      func=(AF.Sigmoid if kind == 0 else AF.Silu)),
                 reads=[m["Bvo"][tt]], writes=[m["Bvo"][tt]])
            P.op("dve", lambda e, tt=tt: e.tensor_tensor(out=hbuf[:, tt, :], in0=hbuf[:, tt, :], in1=vo[:, tt, :], op=ALU.mult),
                 reads=[m["Bhbuf"][tt], m["Bvo"][tt]], writes=[m["Bhbuf"][tt]])
            for g in range(NKO // 8):
                bt = nbank(k)
                ptb = k.ps[bt][:].bitcast(BF16)
                for i in range(8):
                    kk = g * 8 + i
                    P.op("pe", lambda e, tt=tt, kk=kk, i=i, ptb=ptb: e.transpose(out=ptb[:, i * 128:(i + 1) * 128],
                                                                                 in_=hbuf[:, tt, kk * 128:(kk + 1) * 128],
                                                                                 identity=k.ident[:]),
                         reads=[m["Bhbuf"][tt], k.Bconst], writes=[k.Bps[bt]])
                P.op("act", lambda e, tt=tt, g=g, ptb=ptb: e.activation(out=hT[:, g * 8:(g + 1) * 8, tt * 128:(tt + 1) * 128],
                                                                        in_=ptb.rearrange("p (a t) -> p a t", a=8), func=AF.Copy),
                     reads=[k.Bps[bt]], writes=[m["BhT"]])
        for u in range(NKO // 2):
            sl = ring_load(k, m, NF // 2 + 2 * NTM + u)
            for c2 in range(2):
                kk = 2 * u + c2
                for tt in range(2):
                    for half in range(2):
                        pb = tt * 2 + half
                        P.op("pe", lambda e, sl=sl, c2=c2, kk=kk, tt=tt, half=half, pb=pb: e.matmul(
                            k.ps[pb][:], lhsT=hT[:, kk, tt * 128:(tt + 1) * 128],
                            rhs=m["ring"][:, sl, c2 * 1024 + half * 512:c2 * 1024 + (half + 1) * 512],
                            start=(kk == 0), stop=(kk == NKO - 1)), reads=[m["BhT"], m["Bring"][sl]], writes=[k.Bps[pb]])
        for tt in range(2):
            postnorm_residual(k, st * 2 + tt, [tt * 2, tt * 2 + 1], m["gpost"], m["Bgpost"], 1.0, sc)


C0 = math.exp(-0.5)
RW_DEBUG = False
RW_STOP = 0


class _Stop(Exception):
    pass


def nb8(k):
    b = k.rot % 8
    k.rot += 1
    return b


def rwkv_setup(k, layer):
    P = k.P
    A = k.big_arena
    A.reset()
    k.ffn = None
    m = {}
    m["sc"] = common_scratch(k, A, ntmp=1)
    m["sc"]["ps_tr"] = [6, 7]
    al = lambda nm, shape, dtype=BF16: (A.alloc(nm, shape, dtype), Buf(nm))
    m["xnTh"], m["BxnT"] = al("xnTh", [128, 8, 129])
    m["xxT"], m["Bxx"] = al("xxT", [128, 8, 128])
    m["mixT"], _ = al("mixT", [128, 2, 8, 128])
    m["Bmix"] = [Buf(f"mix{i}") for i in range(2)]
    m["hT"], m["BhT"] = al("hT", [128, 8, 128])
    m["ring"], _ = al("ring", [128, 2, 2048])
    m["Bring"] = [Buf(f"ring{i}") for i in range(2)]
    m["NR"] = 2
    m["rq"] = 0
    m["gpost"], m["Bgpost"] = al("gpost", [128, 1024], F32)
    m["Bw"] = Buf("rw_w")
    for nm, shape, dtype in [("w1", [128, 8, 64], BF16), ("a1", [128, 8, 64], BF16), ("g1", [128, 8, 128], BF16),
                             ("w2", [64, 1024], BF16), ("a2", [64, 1024], BF16), ("g2", [128, 1024], BF16),
                             ("w0r", [64, 1024], F32), ("a0r", [64, 1024], F32), ("muT", [128, 48], F32),
                             ("kkb", [64, 1024], BF16), ("kab", [64, 1024], BF16), ("lgb", [64, 1024], BF16),
                             ("lbb", [64, 1024], BF16), ("rkb", [64, 1024], BF16)]:
        m[nm], _ = al(nm, shape, dtype)
    d = lambda nm, shape: k.dram(f"rw_{nm}", shape)
    m["wd"] = d("units", [16, 128, 2048])
    Bw = m["Bw"]
    for i, (nm, shape) in enumerate([("w1", [128, 8, 64]), ("a1", [128, 8, 64]), ("g1", [128, 8, 128]),
                                     ("w2", [64, 1024]), ("a2", [64, 1024]), ("g2", [128, 1024])]):
        src = d(nm, shape)
        P.dma("pool", lambda e, nm=nm, src=src: e.dma_start(out=m[nm][:], in_=src), writes=[Bw], dkey=f"rww{i}")
    for i, nm in enumerate(["w0r", "a0r"]):
        src = d(nm, [1, 1024])
        P.dma("sp", lambda e, nm=nm, src=src: e.dma_start(out=m[nm][:], in_=src[0, :].partition_broadcast(64)),
              writes=[Bw], dkey=f"rwr{i}")
    src = d("muT", [128, 48])
    P.dma("sp", lambda e, src=src: e.dma_start(out=m["muT"][:], in_=src), writes=[Bw], dkey="rwmu")
    for i, nm in enumerate(["kkb", "kab", "lgb", "lbb", "rkb"]):
        src = d(nm, [1, 1024])
        P.dma("pool", lambda e, nm=nm, src=src: e.dma_start(out=m[nm][:], in_=src[0, :].partition_broadcast(64)),
              writes=[Bw], dkey=f"rwb{i}")
    m["hidw"], m["Bhid"] = al("hidw", [64, 128])
    m["hida"], _ = al("hida", [64, 128])
    m["hidg"], _ = al("hidg", [128, 128])
    m["rkv"], _ = al("rkv", [64, 2, 3, 1024])
    m["Brkv"] = [[Buf(f"rkv{c}{i}") for i in range(3)] for c in range(2)]
    m["H"], m["BH"] = al("H", [64, 1024], F32)
    m["Hbf"], m["BHbf"] = al("Hbf", [64, 1024])

    def mk_cx(i):
        cx = dict(m)
        alc = lambda nm, shape, dtype=BF16: (A.alloc(f"{nm}c{i}", shape, dtype), Buf(f"{nm}c{i}"))
        for nm in ("F0", "F1", "F2", "PB", "TTf"):
            cx[nm], cx["B" + nm] = alc(nm, [64, 1024], F32)
        for nm in ("a", "gsb", "G", "Ginv", "Gprev", "kk", "bt", "at", "kt", "rt", "TT", "bT", "kT"):
            cx[nm], cx["B" + nm] = alc(nm, [64, 1024])
        for nm, src in (("akv", "G"), ("TAT", "Ginv"), ("Usb", "Gprev"), ("zb", "kk"), ("QA", "F0"), ("QB", "F1"), ("PA", "F2")):
            cx[nm], cx["B" + nm] = cx[src], cx["B" + src]
        for nm in ("M1", "M2", "arT"):
            cx[nm], cx["B" + nm] = alc(nm, [64, 16, 2, 64])
        cx["GL"], cx["BGL"] = alc("GL", [64, 16], F32)
        cx["sm"], cx["Bsm"] = alc("sm", [64, 8, 16], F32)
        return cx

    m["cx"] = [mk_cx(0), mk_cx(1)]
    m["eps24"], _ = al("eps24", [64, 1], F32)
    return m


def rwkv_seq(k, m, layer, seq):
    P = k.P
    sc = m["sc"]
    g_pre, g_post = layer * 6 + 2, layer * 6 + 3
    xnTh, xxT, mixT, hT = m["xnTh"], m["xxT"], m["mixT"], m["hT"]
    Bw = m["Bw"]
    H, BH = m["H"], m["BH"]
    cxs = m["cx"]
    hv = lambda t: t[:].rearrange("p (h j) -> p h j", h=16)
    bc = lambda ap: ap.unsqueeze(2).to_broadcast([64, 16, 64])
    P.op("pool", lambda e: e.memset(H[:], 0.0), writes=[BH])
    P.op("pool", lambda e: e.memset(m["Hbf"][:], 0.0), writes=[m["BHbf"]])

    def headmm(dst_evac, groups):
        for g in range(2):
            b = nb8(k)
            for hh in range(8):
                h = g * 8 + hh
                for gi, (lf, rf, rd) in enumerate(groups):
                    Lh, Rh, last = lf(h), rf(h), (gi == len(groups) - 1)
                    P.op("pe", lambda e, b=b, hh=hh, Lh=Lh, Rh=Rh, gi=gi, last=last: e.matmul(
                        k.ps[b][0:64, hh * 64:(hh + 1) * 64], lhsT=Lh, rhs=Rh, start=(gi == 0), stop=last),
                        reads=rd, writes=[k.Bps[b]])
            dst_evac(b, g)

    def evac_to(dst, Bdst, eng="act"):
        def f(b, g):
            if eng == "act":
                P.op("act", lambda e: e.activation(out=hv(dst)[:, g * 8:(g + 1) * 8],
                                                   in_=k.ps[b][0:64, :].rearrange("p (h x) -> p h x", h=8), func=AF.Copy),
                     reads=[k.Bps[b]], writes=[Bdst])
            else:
                P.op("dve", lambda e: e.tensor_copy(out=hv(dst)[:, g * 8:(g + 1) * 8],
                                                    in_=k.ps[b][0:64, :].rearrange("p (h x) -> p h x", h=8)),
                     reads=[k.Bps[b]], writes=[Bdst])
        return f

    for ti in range(16):
        xs_ = ti % 2
        gt = seq * 16 + ti
        P.dma("sp", lambda e: e.dma_start(out=k.xres[:, xs_, :], in_=k.scrX[gt]), reads=[k.Bscr[gt]], writes=[k.Bx[xs_]],
              dkey=f"x{xs_}")
        if ti == 0:
            P.op("pool", lambda e: e.memset(xnTh[:, :, 0:1], 0.0), writes=[m["BxnT"]])
        else:
            P.op("pool", lambda e: e.tensor_copy(out=xnTh[:, :, 0:1], in_=xnTh[:, :, 128:129]),
                 reads=[m["BxnT"]], writes=[m["BxnT"]])
        prenorm_tile(k, xs_, g_pre, xnTh, m["BxnT"], 1, sc)
        P.op("dve", lambda e: e.tensor_tensor(out=xxT[:], in0=xnTh[:, :, 0:128], in1=xnTh[:, :, 1:129], op=ALU.subtract),
             reads=[m["BxnT"]], writes=[m["Bxx"]])
        def make_mix(i):
            eng = "dve" if i % 2 == 0 else "pool"
            mu_b = m["muT"][:, i * 8:(i + 1) * 8].unsqueeze(2).to_broadcast([128, 8, 128])
            P.op(eng, lambda e, i=i, mu_b=mu_b: e.tensor_tensor(out=mixT[:, i % 2], in0=xxT[:], in1=mu_b, op=ALU.mult),
                 reads=[m["Bxx"], Bw], writes=[m["Bmix"][i % 2]])
            P.op(eng, lambda e, i=i: e.tensor_tensor(out=mixT[:, i % 2], in0=mixT[:, i % 2], in1=xnTh[:, :, 1:129], op=ALU.add),
                 reads=[m["Bmix"][i % 2], m["BxnT"]], writes=[m["Bmix"][i % 2]])
        for mi in range(3):
            make_mix(mi)
            for nb in range(2):
                sa = ring_load(k, m, mi * 4 + 2 * nb)
                sb = ring_load(k, m, mi * 4 + 2 * nb + 1)
                for c in range(2):
                    b = nb8(k)
                    for kk in range(8):
                        sl = sa if kk < 4 else sb
                        P.op("pe", lambda e, b=b, sl=sl, kk=kk, c=c, mi=mi: e.matmul(
                            k.ps[b][0:64, :], lhsT=mixT[:, mi % 2, kk, c * 64:(c + 1) * 64],
                            rhs=m["ring"][:, sl, (kk % 4) * 512:(kk % 4 + 1) * 512], start=(kk == 0), stop=(kk == 7)),
                            reads=[m["Bmix"][mi % 2], m["Bring"][sl]], writes=[k.Bps[b]])
                    P.op("act", lambda e, b=b, c=c, mi=mi, nb=nb: e.activation(
                        out=m["rkv"][:, c, mi, nb * 512:(nb + 1) * 512], in_=k.ps[b][0:64, :], func=AF.Copy),
                        reads=[k.Bps[b]], writes=[m["Brkv"][c][mi]])
        for (hid, w, mi, fn, np_) in ((m["hidw"], m["w1"], 3, AF.Tanh, 64), (m["hida"], m["a1"], 4, AF.Copy, 64),
                                      (m["hidg"], m["g1"], 5, AF.Sigmoid, 128)):
            make_mix(mi)
            b = nb8(k)
            for kk in range(8):
                P.op("pe", lambda e, b=b, w=w, mi=mi, kk=kk, np_=np_: e.matmul(
                    k.ps[b][0:np_, 0:128], lhsT=w[:, kk, :], rhs=mixT[:, mi % 2, kk, :], start=(kk == 0), stop=(kk == 7)),
                    reads=[Bw, m["Bmix"][mi % 2]], writes=[k.Bps[b]])
            P.op("act", lambda e, b=b, hid=hid, fn=fn, np_=np_: e.activation(out=hid[:], in_=k.ps[b][0:np_, 0:128], func=fn),
                 reads=[k.Bps[b]], writes=[m["Bhid"]])
        def chunk_indep(m, c):
            F0, F1, F2 = m["F0"], m["F1"], m["F2"]
            BF0, BF1, BF2 = m["BF0"], m["BF1"], m["BF2"]
            sm, Bsm = m["sm"], m["Bsm"]
            cs_ = slice(c * 64, (c + 1) * 64)
            r_, k_, v_ = m["rkv"][:, c, 0, :], m["rkv"][:, c, 1, :], m["rkv"][:, c, 2, :]
            Br, Bk, Bv = m["Brkv"][c]
            rv = m["rkv"][:, c, 0, :].rearrange("p (h j) -> p h j", h=16)
            vv = m["rkv"][:, c, 2, :].rearrange("p (h j) -> p h j", h=16)
            yield
            bw = [nb8(k), nb8(k)]
            for half in range(2):
                hs = slice(half * 512, (half + 1) * 512)
                P.op("pe", lambda e, half=half, hs=hs: e.matmul(k.ps[bw[half]][0:64, :], lhsT=m["hidw"][:, cs_], rhs=m["w2"][:, hs],
                                                                start=True, stop=True), reads=[m["Bhid"], Bw], writes=[k.Bps[bw[half]]])
                P.op("dve", lambda e, half=half, hs=hs: e.tensor_tensor(out=F0[:, hs], in0=k.ps[bw[half]][0:64, :], in1=m["w0r"][:, hs],
                                                                        op=ALU.add), reads=[k.Bps[bw[half]], Bw], writes=[BF0])
                P.op("act", lambda e, half=half, hs=hs: e.activation(out=F0[:, hs], in_=F0[:, hs], func=AF.Sigmoid),
                     reads=[BF0], writes=[BF0])
            ba = [nb8(k), nb8(k)]
            for half in range(2):
                hs = slice(half * 512, (half + 1) * 512)
                P.op("pe", lambda e, half=half, hs=hs: e.matmul(k.ps[ba[half]][0:64, :], lhsT=m["hida"][:, cs_], rhs=m["a2"][:, hs],
                                                                start=True, stop=True), reads=[m["Bhid"], Bw], writes=[k.Bps[ba[half]]])
                P.op("dve", lambda e, half=half, hs=hs: e.tensor_tensor(out=F1[:, hs], in0=k.ps[ba[half]][0:64, :], in1=m["a0r"][:, hs],
                                                                        op=ALU.add), reads=[k.Bps[ba[half]], Bw], writes=[BF1])
                P.op("act", lambda e, half=half, hs=hs: e.activation(out=m["a"][:, hs], in_=F1[:, hs], func=AF.Sigmoid),
                     reads=[BF1], writes=[m["Ba"]])
            for half in range(2):
                hs = slice(half * 512, (half + 1) * 512)
                b = nb8(k)
                P.op("pe", lambda e, b=b, hs=hs: e.matmul(k.ps[b][0:64, :], lhsT=m["hidg"][:, cs_], rhs=m["g2"][:, hs],
                                                          start=True, stop=True), reads=[m["Bhid"], Bw], writes=[k.Bps[b]])
                P.op("act", lambda e, b=b, hs=hs: e.activation(out=m["gsb"][:, hs], in_=k.ps[b][0:64, :], func=AF.Copy),
                     reads=[k.Bps[b]], writes=[m["Bgsb"]])
            yield
            bcs = [nb8(k), nb8(k)]
            for half in range(2):
                hs = slice(half * 512, (half + 1) * 512)
                b = bcs[half]
                P.op("pe", lambda e, b=b, hs=hs: e.matmul(k.ps[b][0:64, :], lhsT=k.trif[0:64, 0:64], rhs=F0[:, hs],
                                                          start=True, stop=True), reads=[BF0, k.Bconst], writes=[k.Bps[b]])
                P.op("act", lambda e, b=b, hs=hs: e.activation(out=m["G"][:, hs], in_=k.ps[b][0:64, :], func=AF.Exp, scale=-C0),
                     reads=[k.Bps[b]], writes=[m["BG"]])
                P.op("act", lambda e, b=b, hs=hs: e.activation(out=m["Ginv"][:, hs], in_=k.ps[b][0:64, :], func=AF.Exp, scale=C0),
                     reads=[k.Bps[b]], writes=[m["BGinv"]])
                P.op("dve", lambda e, b=b, hs=hs: e.tensor_tensor(out=F1[:, hs], in0=k.ps[b][0:64, :], in1=F0[:, hs], op=ALU.subtract),
                     reads=[k.Bps[b], BF0], writes=[BF1])
                P.op("act", lambda e, hs=hs: e.activation(out=m["Gprev"][:, hs], in_=F1[:, hs], func=AF.Exp, scale=-C0),
                     reads=[BF1], writes=[m["BGprev"]])
            bgl = nb8(k)
            for h in range(16):
                P.op("pe", lambda e, h=h: e.matmul(k.ps[bgl][0:64, 2 * h:2 * h + 2], lhsT=F0[:, h * 64:(h + 1) * 64], rhs=k.onesf[0:64, 0:2],
                                                   start=True, stop=True), reads=[BF0, k.Bconst], writes=[k.Bps[bgl]])
            P.op("act", lambda e: e.activation(out=m["GL"][:], in_=k.ps[bgl][0:64, 0:32].rearrange("p (h two) -> p h two", two=2)[:, :, 0],
                                               func=AF.Exp, scale=-C0),
                 reads=[k.Bps[bgl]], writes=[m["BGL"]])
            yield
            P.op("dve", lambda e: e.tensor_tensor(out=F1[:], in0=k_, in1=m["kkb"][:], op=ALU.mult), reads=[Bk, Bw], writes=[BF1])
            P.op("pool", lambda e: e.tensor_tensor(out=F2[:], in0=F1[:], in1=F1[:], op=ALU.mult), reads=[BF1], writes=[BF2])
            P.op("dve", lambda e: e.tensor_reduce(out=sm[:, 0, :], in_=hv(F2), axis=AX.X, op=ALU.add), reads=[BF2], writes=[Bsm])
            P.op("pool", lambda e: e.tensor_scalar(out=sm[:, 0, :], in0=sm[:, 0, :], scalar1=1e-24, scalar2=None, op0=ALU.max),
                 reads=[Bsm], writes=[Bsm])
            P.op("pool", lambda e: e.tensor_tensor(out=sm[:, 0, :], in0=sm[:, 0, :], in1=k.mhalf[0:64, 0:1].to_broadcast([64, 16]),
                                                   op=ALU.pow), reads=[Bsm, k.Bconst], writes=[Bsm])
            P.op("dve", lambda e: e.tensor_tensor(out=hv(m["kk"]), in0=hv(F1), in1=bc(sm[:, 0, :]), op=ALU.mult),
                 reads=[BF1, Bsm], writes=[m["Bkk"]])
            yield
            P.op("dve", lambda e: e.scalar_tensor_tensor(out=F1[:], in0=m["a"][:], scalar=-1.0, in1=m["kab"][:], op0=ALU.add, op1=ALU.mult),
                 reads=[m["Ba"], Bw], writes=[BF1])
            P.op("dve", lambda e: e.scalar_tensor_tensor(out=F2[:], in0=F1[:], scalar=1.0, in1=k_, op0=ALU.add, op1=ALU.mult),
                 reads=[BF1, Bk], writes=[BF2])
            yield
            P.op("pool", lambda e: e.tensor_tensor(out=F1[:], in0=F2[:], in1=m["rkb"][:], op=ALU.mult), reads=[BF2, Bw], writes=[BF1])
            P.op("dve", lambda e: e.tensor_tensor(out=F1[:], in0=F1[:], in1=r_, op=ALU.mult), reads=[BF1, Br], writes=[BF1])
            P.op("dve", lambda e: e.tensor_reduce(out=sm[:, 1, :], in_=hv(F1), axis=AX.X, op=ALU.add), reads=[BF1], writes=[Bsm])
            yield
            P.op("dve", lambda e: e.tensor_tensor(out=m["kt"][:], in0=F2[:], in1=m["Ginv"][:], op=ALU.mult),
                 reads=[BF2, m["BGinv"]], writes=[m["Bkt"]])
            P.op("pool", lambda e: e.tensor_tensor(out=m["rt"][:], in0=r_, in1=m["G"][:], op=ALU.mult),
                 reads=[Br, m["BG"]], writes=[m["Brt"]])
            P.op("pool", lambda e: e.tensor_tensor(out=F1[:], in0=m["kk"][:], in1=m["a"][:], op=ALU.mult),
                 reads=[m["Bkk"], m["Ba"]], writes=[BF1])
            P.op("dve", lambda e: e.tensor_tensor(out=m["bt"][:], in0=F1[:], in1=m["Ginv"][:], op=ALU.mult),
                 reads=[BF1, m["BGinv"]], writes=[m["Bbt"]])
            P.op("dve", lambda e: e.scalar_tensor_tensor(out=m["at"][:], in0=m["kk"][:], scalar=-1.0, in1=m["Gprev"][:],
                                                         op0=ALU.mult, op1=ALU.mult), reads=[m["Bkk"], m["BGprev"]], writes=[m["Bat"]])
            yield
            for src, Bsrc, dst, Bdst, two in ((m["at"], m["Bat"], m["arT"], m["BarT"], 0), (m["rt"], m["Brt"], m["arT"], m["BarT"], 1),
                                              (m["bt"], m["Bbt"], m["bT"], m["BbT"], None), (m["kt"], m["Bkt"], m["kT"], m["BkT"], None)):
                b = nb8(k)
                ptb = k.ps[b][0:64, :].bitcast(BF16)
                for h in range(16):
                    P.op("pe", lambda e, h=h, src=src, ptb=ptb: e.transpose(out=ptb[:, h * 64:(h + 1) * 64], in_=src[:, h * 64:(h + 1) * 64],
                                                                             identity=k.ident[0:64, 0:64]),
                         reads=[Bsrc, k.Bconst], writes=[k.Bps[b]])
                pv = ptb.rearrange("p (h t) -> p h t", h=16)
                if two is None:
                    P.op("act", lambda e, pv=pv, dst=dst: e.activation(out=hv(dst), in_=pv, func=AF.Copy), reads=[k.Bps[b]], writes=[Bdst])
                else:
                    P.op("act", lambda e, pv=pv, dst=dst, two=two: e.activation(out=dst[:, :, two, :], in_=pv, func=AF.Copy),
                         reads=[k.Bps[b]], writes=[Bdst])
            yield
            for (lt, Blt, M, BM) in ((m["bT"], m["BbT"], m["M1"], m["BM1"]), (m["kT"], m["BkT"], m["M2"], m["BM2"])):
                for g in range(4):
                    b = nb8(k)
                    for hh in range(4):
                        h = g * 4 + hh
                        P.op("pe", lambda e, b=b, h=h, hh=hh, lt=lt: e.matmul(
                            k.ps[b][0:64, hh * 128:(hh + 1) * 128], lhsT=lt[:, h * 64:(h + 1) * 64],
                            rhs=m["arT"][:, h].rearrange("p a t -> p (a t)"), start=True, stop=True),
                            reads=[Blt, m["BarT"]], writes=[k.Bps[b]])
                    P.op("dve", lambda e, b=b, g=g, M=M: e.tensor_tensor(
                        out=M[:, g * 4:(g + 1) * 4].rearrange("p h a t -> p h (a t)"),
                        in0=k.ps[b][0:64, :].rearrange("p (h x) -> p h x", h=4),
                        in1=k.m1[:].unsqueeze(1).to_broadcast([64, 4, 128]), op=ALU.mult),
                        reads=[k.Bps[b], k.Bconst], writes=[BM])
                    if M is m["M1"]:
                        P.op("dve", lambda e, b=b, g=g: e.tensor_tensor(
                            out=hv(m["QA"])[:, g * 4:(g + 1) * 4],
                            in0=k.ps[b][0:64, :].rearrange("p (h x) -> p h x", h=4)[:, :, 0:64],
                            in1=k.m1[:, 0:64].unsqueeze(1).to_broadcast([64, 4, 64]), op=ALU.mult),
                            reads=[k.Bps[b], k.Bconst], writes=[m["BQA"]])
            for g in range(2):
                b = nb8(k)
                for hh in range(8):
                    h = g * 8 + hh
                    P.op("pe", lambda e, b=b, h=h, hh=hh: e.matmul(k.ps[b][0:64, hh * 64:(hh + 1) * 64], lhsT=m["arT"][:, h, 0, :],
                                                                   rhs=m["bT"][:, h * 64:(h + 1) * 64], start=True, stop=True),
                         reads=[m["BarT"], m["BbT"]], writes=[k.Bps[b]])
                P.op("dve", lambda e, b=b, g=g: e.tensor_tensor(
                    out=hv(m["PA"])[:, g * 8:(g + 1) * 8], in0=k.ps[b][0:64, :].rearrange("p (h x) -> p h x", h=8),
                    in1=k.sl64[:].unsqueeze(1).to_broadcast([64, 8, 64]), op=ALU.mult),
                    reads=[k.Bps[b], k.Bconst], writes=[m["BPA"]])
            yield
            TTf = hv(m["TTf"])
            P.op("dve", lambda e: e.tensor_tensor(out=TTf, in0=hv(m["QA"]),
                                                  in1=k.ident[0:64, 0:64].unsqueeze(1).to_broadcast([64, 16, 64]), op=ALU.add),
                 reads=[m["BQA"], k.Bconst], writes=[m["BTTf"]])
            yield
            Qc, BQc = hv(m["QA"]), m["BQA"]
            Pc, BPc = hv(m["PA"]), m["BPA"]
            for lv in range(5):
                Qn, BQn = (hv(m["QB"]), m["BQB"]) if lv % 2 == 0 else (hv(m["QA"]), m["BQA"])
                Pn, BPn = (hv(m["PB"]), m["BPB"]) if lv % 2 == 0 else (hv(m["PA"]), m["BPA"])
                jobs = [(Qc, BQc, Pc, BPc, Pn, BPn)]
                if lv < 4:
                    jobs.append((Pc, BPc, Qc, BQc, Qn, BQn))
                for (L, BL, R, BR, O, BO) in jobs:
                    for g in range(2):
                        b = nb8(k)
                        for hh in range(8):
                            h = g * 8 + hh
                            P.op("pe", lambda e, b=b, h=h, hh=hh, L=L, R=R: e.matmul(k.ps[b][0:64, hh * 64:(hh + 1) * 64], lhsT=L[:, h, :],
                                                                                   rhs=R[:, h, :], start=True, stop=True),
                                 reads=[BL, BR], writes=[k.Bps[b]])
                        P.op("act", lambda e, b=b, g=g, O=O: e.activation(out=O[:, g * 8:(g + 1) * 8],
                                                                          in_=k.ps[b][0:64, :].rearrange("p (h x) -> p h x", h=8),
                                                                          func=AF.Copy), reads=[k.Bps[b]], writes=[BO])
                for g in range(2):
                    b = nb8(k)
                    for hh in range(8):
                        h = g * 8 + hh
                        P.op("pe", lambda e, b=b, h=h, hh=hh, Pn=Pn: e.matmul(k.ps[b][0:64, hh * 64:(hh + 1) * 64], lhsT=Pn[:, h, :],
                                                                             rhs=TTf[:, h, :], start=True, stop=True),
                             reads=[BPn, m["BTTf"]], writes=[k.Bps[b]])
                    P.op("dve", lambda e, b=b, g=g: e.tensor_tensor(out=TTf[:, g * 8:(g + 1) * 8],
                                                                    in0=k.ps[b][0:64, :].rearrange("p (h x) -> p h x", h=8),
                                                                    in1=TTf[:, g * 8:(g + 1) * 8], op=ALU.add),
                         reads=[k.Bps[b], m["BTTf"]], writes=[m["BTTf"]])
                Qc, BQc, Pc, BPc = Qn, BQn, Pn, BPn
                yield
            P.op("act", lambda e: e.activation(out=m["TT"][:], in_=m["TTf"][:], func=AF.Copy), reads=[m["BTTf"]], writes=[m["BTT"]])
            TT = hv(m["TT"])

            vh = lambda h: m["rkv"][:, c, 2, h * 64:(h + 1) * 64]
            yield
            headmm(evac_to(m["akv"], m["Bakv"]), [(lambda h: m["M2"][:, h, 0, :], vh, [m["BM2"], Bv])])
            headmm(evac_to(m["TAT"], m["BTAT"], "dve"),
                   [(lambda h: m["at"][:, h * 64:(h + 1) * 64], lambda h: TT[:, h, :], [m["Bat"], m["BTT"]])])
            yield

        def chunk_dep(m, c):
            F0, F1, F2 = m["F0"], m["F1"], m["F2"]
            BF0, BF1, BF2 = m["BF0"], m["BF1"], m["BF2"]
            sm, Bsm = m["sm"], m["Bsm"]
            cs_ = slice(c * 64, (c + 1) * 64)
            Br, Bk, Bv = m["Brkv"][c]
            vv = m["rkv"][:, c, 2, :].rearrange("p (h j) -> p h j", h=16)
            TT = hv(m["TT"])
            vh = lambda h: m["rkv"][:, c, 2, h * 64:(h + 1) * 64]
            headmm(evac_to(m["Usb"], m["BUsb"]),
                   [(lambda h: TT[:, h, :], lambda h: hv(m["akv"])[:, h, :], [m["BTT"], m["Bakv"]]),
                    (lambda h: hv(m["TAT"])[:, h, :], lambda h: hv(m["Hbf"])[:, h, :], [m["BTAT"], m["BHbf"]])])
            headmm(evac_to(F1, BF1),
                   [(lambda h: m["arT"][:, h, 1, :], lambda h: hv(m["Hbf"])[:, h, :], [m["BarT"], m["BHbf"]]),
                    (lambda h: m["M1"][:, h, 1, :], lambda h: hv(m["Usb"])[:, h, :], [m["BM1"], m["BUsb"]]),
                    (lambda h: m["M2"][:, h, 1, :], vh, [m["BM2"], Bv])])

            def evac_H(b, g):
                gs = slice(g * 8, (g + 1) * 8)
                P.op("dve", lambda e: e.tensor_tensor(out=hv(H)[:, gs], in0=k.ps[b][0:64, :].rearrange("p (h x) -> p h x", h=8),
                                                      in1=hv(H)[:, gs], op=ALU.add), reads=[k.Bps[b], BH], writes=[BH])
                P.op("dve", lambda e: e.tensor_tensor(out=hv(H)[:, gs], in0=hv(H)[:, gs],
                                                      in1=m["GL"][:, gs].unsqueeze(2).to_broadcast([64, 8, 64]), op=ALU.mult),
                     reads=[BH, m["BGL"]], writes=[BH])
            headmm(evac_H,
                   [(lambda h: m["bt"][:, h * 64:(h + 1) * 64], lambda h: hv(m["Usb"])[:, h, :], [m["Bbt"], m["BUsb"]]),
                    (lambda h: m["kt"][:, h * 64:(h + 1) * 64], vh, [m["Bkt"], Bv])])
            P.op("act", lambda e: e.activation(out=m["Hbf"][:], in_=H[:], func=AF.Copy), reads=[BH], writes=[m["BHbf"]])
            P.op("dve", lambda e: e.tensor_reduce(out=sm[:, 2, :], in_=hv(F1), axis=AX.X, op=ALU.add), reads=[BF1], writes=[Bsm])
            P.op("pool", lambda e: e.tensor_tensor(out=F2[:], in0=F1[:], in1=F1[:], op=ALU.mult), reads=[BF1], writes=[BF2])
            P.op("dve", lambda e: e.tensor_reduce(out=sm[:, 3, :], in_=hv(F2), axis=AX.X, op=ALU.add), reads=[BF2], writes=[Bsm])
            P.op("dve", lambda e: e.tensor_scalar(out=sm[:, 2, :], in0=sm[:, 2, :], scalar1=1.0 / 64.0, scalar2=None, op0=ALU.mult),
                 reads=[Bsm], writes=[Bsm])
            P.op("dve", lambda e: e.tensor_tensor(out=sm[:, 4, :], in0=sm[:, 2, :], in1=sm[:, 2, :], op=ALU.mult), reads=[Bsm], writes=[Bsm])
            P.op("dve", lambda e: e.scalar_tensor_tensor(out=sm[:, 3, :], in0=sm[:, 3, :], scalar=1.0 / 64.0, in1=sm[:, 4, :],
                                                         op0=ALU.mult, op1=ALU.subtract), reads=[Bsm], writes=[Bsm])
            P.op("pool", lambda e: e.tensor_scalar(out=sm[:, 3, :], in0=sm[:, 3, :], scalar1=64e-5, scalar2=None, op0=ALU.add),
                 reads=[Bsm], writes=[Bsm])
            P.op("pool", lambda e: e.tensor_tensor(out=sm[:, 3, :], in0=sm[:, 3, :], in1=k.mhalf[0:64, 0:1].to_broadcast([64, 16]),
                                                   op=ALU.pow), reads=[Bsm, k.Bconst], writes=[Bsm])
            P.op("dve", lambda e: e.tensor_tensor(out=hv(F1), in0=hv(F1), in1=bc(sm[:, 2, :]), op=ALU.subtract), reads=[BF1, Bsm], writes=[BF1])
            P.op("dve", lambda e: e.tensor_tensor(out=hv(F1), in0=hv(F1), in1=bc(sm[:, 3, :]), op=ALU.mult), reads=[BF1, Bsm], writes=[BF1])
            P.op("pool", lambda e: e.tensor_tensor(out=F1[:], in0=F1[:], in1=m["lgb"][:], op=ALU.mult), reads=[BF1, Bw], writes=[BF1])
            P.op("pool", lambda e: e.tensor_tensor(out=F1[:], in0=F1[:], in1=m["lbb"][:], op=ALU.add), reads=[BF1, Bw], writes=[BF1])
            P.op("dve", lambda e: e.tensor_tensor(out=hv(F2), in0=vv, in1=bc(sm[:, 1, :]), op=ALU.mult), reads=[Bv, Bsm], writes=[BF2])
            P.op("dve", lambda e: e.tensor_tensor(out=F1[:], in0=F1[:], in1=F2[:], op=ALU.add), reads=[BF1, BF2], writes=[BF1])
            P.op("dve", lambda e: e.tensor_tensor(out=m["zb"][:], in0=F1[:], in1=m["gsb"][:], op=ALU.mult),
                 reads=[BF1, m["Bgsb"]], writes=[m["Bzb"]])
            b = nb8(k)
            ptb = k.ps[b][:].bitcast(BF16)
            for kk in range(8):
                P.op("pe", lambda e, kk=kk, ptb=ptb: e.transpose(out=ptb[:, kk * 64:(kk + 1) * 64], in_=m["zb"][:, kk * 128:(kk + 1) * 128],
                                                                 identity=k.ident[0:64, 0:64]),
                     reads=[m["Bzb"], k.Bconst], writes=[k.Bps[b]])
            P.op("act", lambda e, ptb=ptb: e.activation(out=hT[:, :, cs_], in_=ptb[:, 0:512].rearrange("p (a t) -> p a t", a=8), func=AF.Copy),
                 reads=[k.Bps[b]], writes=[m["BhT"]])

        gens = [chunk_indep(cxs[0], 0), chunk_indep(cxs[1], 1)]
        while gens:
            for g_ in list(gens):
                try:
                    next(g_)
                except StopIteration:
                    gens.remove(g_)
        chunk_dep(cxs[0], 0)
        chunk_dep(cxs[1], 1)
        bo = [nb8(k), nb8(k)]
        for u in range(4):
            sl = ring_load(k, m, 12 + u)
            for c2 in range(2):
                kk = 2 * u + c2
                for half in range(2):
                    P.op("pe", lambda e, sl=sl, c2=c2, kk=kk, half=half: e.matmul(
                        k.ps[bo[half]][:], lhsT=hT[:, kk, :], rhs=m["ring"][:, sl, c2 * 1024 + half * 512:c2 * 1024 + (half + 1) * 512],
                        start=(kk == 0), stop=(kk == 7)), reads=[m["BhT"], m["Bring"][sl]], writes=[k.Bps[bo[half]]])
        postnorm_residual(k, xs_, bo, m["gpost"], m["Bgpost"], 1.0, sc)
        P.dma("sp", lambda e: e.dma_start(out=k.scrX[gt], in_=k.xres[:, xs_, :]), reads=[k.Bx[xs_]], writes=[k.Bscr[gt]],
              dkey=f"x{xs_}")


def rwkv_phase(k, layer, j):
    P = k.P
    if not hasattr(k, "scrX"):
        k.scrX = k.nc.dram_tensor("scrX", [NT, 128, D], F32, kind="Internal").ap()
        k.Bscr = [Buf(f"scr{i}") for i in range(NT)]
    P.barrier()
    for i in range(0, NT, 2):
        P.dma("sp", lambda e: e.dma_start(out=k.scrX[i:i + 2].rearrange("t p d -> p t d"), in_=k.xres[:, i:i + 2, :]),
              reads=[k.Bx[i], k.Bx[i + 1]], writes=[k.Bscr[i], k.Bscr[i + 1]], dkey=f"x{i // 2}")
    P.barrier()
    m = rwkv_setup(k, layer)
    P.dma("sp", lambda e: e.dma_start(out=m["gpost"][:], in_=k.ng_d[layer * 6 + 3, :].partition_broadcast(128)),
          writes=[m["Bgpost"]], dkey="gpost")
    rwkv_seq(k, m, layer, 0)
    rwkv_seq(k, m, layer, 1)
    P.barrier()
    for i in range(0, NT, 2):
        P.dma("sp", lambda e: e.dma_start(out=k.xres[:, i:i + 2, :], in_=k.scrX[i:i + 2].rearrange("t p d -> p t d")),
              reads=[k.Bscr[i], k.Bscr[i + 1]], writes=[k.Bx[i], k.Bx[i + 1]], dkey=f"x{i // 2}")
    P.barrier()


def _units_lin(w_in, n_f, tm0, n_tm, w_out):
    wf = w_in[:, :n_f * 128].reshape(8, 128, n_f, 128).transpose(2, 1, 0, 3)
    wf = wf.reshape(n_f // 2, 2, 128, 1024).transpose(0, 2, 1, 3).reshape(n_f // 2, 128, 2048)
    wt = w_in[:, tm0:tm0 + n_tm * 512].reshape(2, 4, 128, n_tm, 512).transpose(3, 0, 2, 1, 4)
    wt = wt.reshape(n_tm * 2, 128, 2048)
    nko = w_out.shape[0] // 128
    wo = w_out.reshape(nko // 2, 2, 128, 1024).transpose(0, 2, 1, 3).reshape(nko // 2, 128, 2048)
    return np.ascontiguousarray(np.concatenate([wf, wt, wo], axis=0), dtype=np.float32)


def _prep_inputs(inputs, x_override=None):
    xin = inputs["x"] if x_override is None else x_override
    x = np.ascontiguousarray(xin, dtype=np.float32).reshape(NCORES, NT, 128, D)
    wgu = inputs["ffn_w_gu"].reshape(8, 8, 128, 2, NJ, 128)
    wgu = np.ascontiguousarray(wgu.transpose(0, 4, 2, 3, 1, 5)).reshape(8 * NJ, 128, 2, 1024)
    wd = np.ascontiguousarray(inputs["ffn_w_down"]).reshape(8 * NJ, 128, 1024)
    ng = np.ascontiguousarray(inputs["norm_g"]).reshape(24, D)
    ngT = np.ascontiguousarray(ng.reshape(24, 8, 128).transpose(2, 0, 1)).reshape(128, 24 * 8)
    shared = {"wgu": wgu, "wd": wd, "ng": ng, "ngT": ngT}
    for j, layer in ((0, 0), (1, 3)):
        w_in = inputs["ml_w_in"][j]
        shared[f"wm{layer}"] = _units_lin(w_in, 8, 1024, 4, inputs["ml_w_out"][j])
        shared[f"wif{layer}"] = np.ascontiguousarray(w_in[:, 3072:3080].reshape(8, 128, 8).transpose(1, 0, 2)).reshape(128, 64)
        shared[f"bif{layer}"] = np.ascontiguousarray(inputs["ml_b_if"][j]).reshape(1, 8)
        shared[f"convT{layer}"] = np.ascontiguousarray(
            inputs["ml_conv_w"][j].reshape(4, 8, 128).transpose(2, 1, 0)).reshape(128, 32)
        shared[f"mng{layer}"] = np.ascontiguousarray(inputs["ml_norm_g"][j]).reshape(1, 1024)
    shared["wm2"] = _units_lin(inputs["rt_w_in"][0], 16, 2048, 8, inputs["rt_w_out"][0])
    wtm = lambda w: np.ascontiguousarray(w.reshape(2, 4, 128, 2, 512).transpose(3, 0, 2, 1, 4)).reshape(4, 128, 2048)
    wo = inputs["rw_w_out"][0].reshape(4, 2, 128, 1024).transpose(0, 2, 1, 3).reshape(4, 128, 2048)
    shared["rw_units"] = np.ascontiguousarray(np.concatenate(
        [wtm(inputs["rw_w_rkv"][0, 0]), wtm(inputs["rw_w_rkv"][0, 1]), wtm(inputs["rw_w_rkv"][0, 2]), wo], axis=0), dtype=np.float32)
    for nm in ("w1", "a1", "g1"):
        w = inputs["rw_" + nm][0]
        shared["rw_" + nm] = np.ascontiguousarray(w.reshape(8, 128, w.shape[1]).transpose(1, 0, 2))
    for nm in ("w2", "a2", "g2"):
        shared["rw_" + nm] = np.ascontiguousarray(inputs["rw_" + nm][0])
    shared["rw_w0r"] = np.ascontiguousarray(inputs["rw_w0"][0]).reshape(1, 1024)
    shared["rw_a0r"] = np.ascontiguousarray(inputs["rw_a0"][0]).reshape(1, 1024)
    shared["rw_muT"] = np.ascontiguousarray(inputs["rw_mu"][0].reshape(6, 8, 128).transpose(2, 0, 1)).reshape(128, 48)
    for nm, src in (("kkb", "rw_k_k"), ("kab", "rw_k_a"), ("lgb", "rw_ln_g"), ("lbb", "rw_ln_b"), ("rkb", "rw_r_k")):
        shared["rw_" + nm] = np.ascontiguousarray(inputs[src][0]).reshape(1, 1024)
    pos = np.ascontiguousarray(inputs["positions"]).astype(np.int32).reshape(NCORES, 1, 2 * SEQ)
    return [dict(shared, x=x[c], pos=pos[c]) for c in range(NCORES)]


def run_partial(inputs, subs=tuple(range(12)), trace=False, cores=NCORES, x_override=None):
    nc, names = build_program(tuple(subs))
    in_maps = _prep_inputs(inputs, x_override)
    in_maps = [{n: m[n] for n in names if n in m} for m in in_maps[:cores]]
    res = run_bass_kernel_spmd(nc, in_maps, core_ids=list(range(cores)), trace=trace)
    out = np.stack([np.asarray(r["out"]) for r in res.results], axis=0)
    return out.reshape(2 * cores, SEQ, D).astype(np.float32), res


def kernel(**inputs):
    out, _ = run_partial(inputs)
    return out
```

```python
import numpy as np
import concourse.bass as bass
import concourse.mybir as mybir
from concourse.bass_utils import run_bass_kernel_spmd

F32 = mybir.dt.float32
BF16 = mybir.dt.bfloat16
I32 = mybir.dt.int32
ALU = mybir.AluOpType
AF = mybir.ActivationFunctionType
AX = mybir.AxisListType

NCORES = 8
D = 1024
DFF = 2816
NJ = DFF // 128
TPC = 4096
NT = TPC // 128
SEQ = 2048
EPS = 1e-6

ENGS = ["pe", "dve", "act", "pool", "sp"]
CHUNK = 8000
SAME_ENG_SYNC = True


class Buf:
    __slots__ = ("name", "lw", "rd", "excl")

    def __init__(self, name, excl=False):
        self.name = name
        self.lw = None
        self.rd = {}
        self.excl = excl


def _freeze(fn):
    import types
    if fn is None or fn.__closure__ is None:
        return fn
    cells = []
    for c in fn.__closure__:
        try:
            cells.append(types.CellType(c.cell_contents))
        except ValueError:
            cells.append(c)
    return types.FunctionType(fn.__code__, fn.__globals__, fn.__name__, fn.__defaults__, tuple(cells))


class Prog:
    def __init__(self, nc):
        self.nc = nc
        self.ops = {e: [] for e in ENGS}
        self.cnt = {e: 0 for e in ENGS}
        self.seen = {e: {} for e in ENGS}
        self.dcnt = {}

    def _deps(self, eng, reads, writes):
        need = {}

        def add(k, v):
            if need.get(k, 0) < v:
                need[k] = v

        for b in reads:
            if b.lw is not None:
                add(*b.lw)
            if b.excl:
                for k, v in b.rd.items():
                    if k != ("e", eng):
                        add(k, v)
        for b in writes:
            if b.lw is not None:
                add(*b.lw)
            for k, v in b.rd.items():
                add(k, v)
        s = self.seen[eng]
        waits = []
        for k, v in need.items():
            if k == ("e", eng) and (eng == "pe" or not SAME_ENG_SYNC):
                continue
            if s.get(k, 0) >= v:
                continue
            s[k] = v
            waits.append((k, v))
        return waits

    def _mark(self, tok, reads, writes):
        k, v = tok
        for b in reads:
            if b.rd.get(k, 0) < v:
                b.rd[k] = v
        for b in writes:
            b.lw = tok
            b.rd = {}

    def op(self, eng, fn, reads=(), writes=()):
        waits = self._deps(eng, reads, writes)
        self.cnt[eng] += 1
        tok = (("e", eng), self.cnt[eng])
        self.ops[eng].append((_freeze(fn), waits, tok))
        self._mark(tok, reads, writes)

    def dma(self, eng, fn, reads=(), writes=(), dkey=None):
        waits = self._deps(eng, reads, writes)
        n = self.dcnt.get(dkey, 0) + 16
        self.dcnt[dkey] = n
        tok = (("d", dkey), n)
        self.ops[eng].append((_freeze(fn), waits, tok))
        self._mark(tok, reads, writes)

    def barrier(self):
        for e in ENGS:
            s = self.seen[e]
            waits = []
            for o in ENGS:
                if o == e or self.cnt[o] == 0:
                    continue
                k = ("e", o)
                if s.get(k, 0) < self.cnt[o]:
                    s[k] = self.cnt[o]
                    waits.append((k, self.cnt[o]))
            for dk, n in self.dcnt.items():
                k = ("d", dk)
                if s.get(k, 0) < n:
                    s[k] = n
                    waits.append((k, n))
            if waits:
                self.ops[e].append((None, waits, None))

    def run(self, final_bufs=()):
        nc = self.nc
        from contextlib import ExitStack
        with ExitStack() as st:
            sems = {}
            for e in ENGS:
                nchunks = max(1, (self.cnt[e] + CHUNK - 1) // CHUNK)
                for c in range(nchunks):
                    sems[(("e", e), c)] = st.enter_context(nc.semaphore(f"s_{e}_{c}"))
            for dk in self.dcnt:
                sems[(("d", dk), 0)] = st.enter_context(nc.semaphore(f"d_{dk}"))
            block = st.enter_context(nc.Block())

            def semval(k, v):
                if k[0] == "e":
                    c = (v - 1) // CHUNK
                    return sems[(k, c)], v - c * CHUNK
                return sems[(k, 0)], v

            def emit(ename, eng):
                for fn, waits, tok in self.ops[ename]:
                    for (k, v) in waits:
                        s, vv = semval(k, v)
                        eng.wait_ge(s, vv)
                    if fn is None:
                        continue
                    ins = fn(eng)
                    k, v = tok
                    if k[0] == "e":
                        s, vv = semval(k, v)
                        ins.then_inc(s, 1)
                    else:
                        ins.then_inc(sems[(k, 0)], 16)
                if ename == "sp":
                    need = {}
                    for b in final_bufs:
                        toks = list(b.rd.items())
                        if b.lw is not None:
                            toks.append(b.lw)
                        for k, v in toks:
                            need[k] = max(need.get(k, 0), v)
                    for k, v in need.items():
                        s, vv = semval(k, v)
                        eng.wait_ge(s, vv)

            @block.tensor
            def _(e):
                emit("pe", e)

            @block.vector
            def _(e):
                emit("dve", e)

            @block.scalar
            def _(e):
                emit("act", e)

            @block.gpsimd
            def _(e):
                emit("pool", e)

            @block.sync
            def _(e):
                emit("sp", e)


class Arena:
    def __init__(self, nc, base, limit):
        self.nc, self.base, self.limit = nc, base, limit
        self.off = base
        self.gen = 0

    def reset(self):
        self.off = self.base
        self.gen += 1

    def alloc(self, name, shape, dtype):
        nbytes = int(np.prod(shape[1:])) * (4 if dtype in (F32, I32) else 2)
        nbytes = (nbytes + 63) // 64 * 64
        assert self.off + nbytes <= self.limit, (name, self.off, nbytes, self.limit)
        t = self.nc.alloc_sbuf_tensor_at(f"{name}_g{self.gen}", list(shape), dtype, offset=self.off)
        self.off += nbytes
        return t


class K:
    pass


def build_program(subs=tuple(range(12))):
    nc = bass.Bass("TRN2", target_bir_lowering=False)
    P = Prog(nc)
    k = K()
    k.nc, k.P = nc, P
    k.dram_names = []

    def dram(name, shape, dtype=F32, kind="ExternalInput"):
        k.dram_names.append(name)
        return nc.dram_tensor(name, list(shape), dtype, kind=kind).ap()

    k.dram = dram
    k.x_d = dram("x", [NT, 128, D])
    k.out_d = dram("out", [NT, 128, D], kind="ExternalOutput")
    k.ng_d = dram("ng", [24, D])
    k.ngT_d = dram("ngT", [128, 24 * 8])
    if any(s % 3 != 1 for s in subs):
        k.wgu_d = dram("wgu", [8 * NJ, 128, 2, 1024])
        k.wd_d = dram("wd", [8 * NJ, 128, 1024])

    RES_BYTES = NT * D * 4
    SB0 = 16640
    k.xres = nc.alloc_sbuf_tensor_at("xres", [128, NT, D], F32, offset=SB0 + 4096)
    k.Bx = [Buf(f"x{i}") for i in range(NT)]
    pers = Arena(nc, SB0, SB0 + 4096)
    k.big_arena = Arena(nc, SB0 + 4096 + 2 * D * 4, 229376)
    k.ident = pers.alloc("ident", [128, 128], BF16)
    k.ngT = pers.alloc("ngT", [128, 24 * 8], F32)
    k.ones = pers.alloc("ones", [128, 128], BF16)
    k.mhalf = pers.alloc("mhalf", [128, 1], F32)
    k.tri = pers.alloc("tri", [128, 128], BF16)
    k.trif = pers.alloc("trif", [128, 128], F32)
    k.onesf = pers.alloc("onesf", [128, 128], F32)
    k.sl64 = pers.alloc("sl64", [64, 64], BF16)
    k.m1 = pers.alloc("m1", [64, 128], BF16)
    k.Bconst = Buf("const")
    k.arena = Arena(nc, SB0 + RES_BYTES + 4096, 229376)
    k.ps = [nc.alloc_psum_tensor(f"ps{i}", [128, 512], F32) for i in range(8)]
    k.Bps = [Buf(f"ps{i}", excl=True) for i in range(8)]
    k.rot = 0

    P.op("pool", lambda e: e.memset(k.ones[:], 1.0), writes=[k.Bconst])
    P.op("pool", lambda e: e.memset(k.onesf[:], 1.0), writes=[k.Bconst])
    P.op("pool", lambda e: e.affine_select(out=k.ident[:], in_=k.ones[:], pattern=[[-1, 128]],
                                           compare_op=ALU.is_equal, fill=0.0, base=0, channel_multiplier=1),
         reads=[k.Bconst], writes=[k.Bconst])
    for tt in (k.tri, k.trif):
        P.op("pool", lambda e, tt=tt: e.affine_select(out=tt[:], in_=(k.ones if tt is k.tri else k.onesf)[:],
                                                      pattern=[[1, 128]], compare_op=ALU.is_ge, fill=0.0, base=0,
                                                      channel_multiplier=-1),
             reads=[k.Bconst], writes=[k.Bconst])
    P.op("pool", lambda e: e.memset(k.mhalf[:], -0.5), writes=[k.Bconst])
    P.op("pool", lambda e: e.affine_select(out=k.sl64[:], in_=k.ones[0:64, 0:64], pattern=[[-1, 64]],
                                           compare_op=ALU.is_gt, fill=0.0, base=0, channel_multiplier=1),
         reads=[k.Bconst], writes=[k.Bconst])
    P.op("pool", lambda e: e.affine_select(out=k.m1[:, 0:64], in_=k.ones[0:64, 0:64], pattern=[[1, 64]],
                                           compare_op=ALU.is_gt, fill=0.0, base=0, channel_multiplier=-1),
         reads=[k.Bconst], writes=[k.Bconst])
    P.op("pool", lambda e: e.affine_select(out=k.m1[:, 64:128], in_=k.ones[0:64, 0:64], pattern=[[1, 64]],
                                           compare_op=ALU.is_ge, fill=0.0, base=0, channel_multiplier=-1),
         reads=[k.Bconst], writes=[k.Bconst])
    P.dma("sp", lambda e: e.dma_start(out=k.ngT[:], in_=k.ngT_d[:, :]), writes=[k.Bconst], dkey="ngT")

    for i in range(0, NT, 2):
        P.dma("sp", lambda e, i=i: e.dma_start(out=k.xres[:, i:i + 2, :],
                                                in_=k.x_d[i:i + 2].rearrange("t p d -> p t d")),
              writes=[k.Bx[i], k.Bx[i + 1]], dkey=f"x{i // 2}")

    for sub in subs:
        layer, s3 = sub // 3, sub % 3
        if s3 == 0:
            ffn_phase(k, layer, 0)
        elif s3 == 2:
            ffn_phase(k, layer, 1)
        elif layer % 3 == 0:
            lin_mixer_phase(k, layer, 0, layer // 3)
        elif layer % 3 == 2:
            lin_mixer_phase(k, layer, 2, layer // 3)
        else:
            rwkv_phase(k, layer, layer // 3)

    for i in range(0, 16 if RW_DEBUG else NT, 2):
        P.dma("sp", lambda e, i=i: e.dma_start(out=k.out_d[i:i + 2].rearrange("t p d -> p t d"),
                                                in_=k.xres[:, i:i + 2, :]),
              reads=[k.Bx[i], k.Bx[i + 1]], dkey=f"x{i // 2}")
    P.run(final_bufs=k.Bx)
    return nc, k.dram_names


def rstd_from_ssq(k, ssq_ap, rstd_ap, Bs, Br, n_part=128):
    P = k.P
    P.op("pool", lambda e: e.tensor_scalar(out=rstd_ap, in0=ssq_ap, scalar1=1.0 / D, scalar2=EPS,
                                           op0=ALU.mult, op1=ALU.add), reads=[Bs], writes=[Br])
    P.op("pool", lambda e: e.tensor_tensor(out=rstd_ap, in0=rstd_ap, in1=k.mhalf[0:n_part, :], op=ALU.pow),
         reads=[Br, k.Bconst], writes=[Br])


def prenorm_tile(k, ti, gidx, xnT, BxnT, col, sc):
    P = k.P
    par = ti % 2
    junk, Bjunk = sc["junk"], sc["Bjunk"]
    xs, Bxs = sc["xs"][par], sc["Bxs"][par]
    ssq, Bssq = sc["ssq"][par], sc["Bssq"][par]
    rstd, Brstd = sc["rstd"][par], sc["Brstd"][par]
    psb = sc["ps_tr"][par]
    P.op("act", lambda e: e.activation(out=junk[:], in_=k.xres[:, ti, :], func=AF.Square, accum_out=ssq[:]),
         reads=[k.Bx[ti]], writes=[Bjunk, Bssq])
    rstd_from_ssq(k, ssq[:], rstd[:], Bssq, Brstd)
    P.op("act", lambda e: e.activation(out=xs[:], in_=k.xres[:, ti, :], func=AF.Copy, scale=rstd[:]),
         reads=[k.Bx[ti], Brstd], writes=[Bxs])
    pst = k.ps[psb][:].bitcast(BF16)
    for kk in range(8):
        P.op("pe", lambda e, kk=kk: e.transpose(out=pst[:, kk * 128:(kk + 1) * 128],
                                                in_=xs[:, kk * 128:(kk + 1) * 128], identity=k.ident[:]),
             reads=[Bxs, k.Bconst], writes=[k.Bps[psb]])
    gT = k.ngT[:, gidx * 8:(gidx + 1) * 8].unsqueeze(2).to_broadcast([128, 8, 128])
    P.op("dve", lambda e: e.tensor_tensor(out=xnT[:, :, col:col + 128],
                                          in0=pst.rearrange("p (a t) -> p a t", a=8), in1=gT, op=ALU.mult),
         reads=[k.Bps[psb], k.Bconst], writes=[BxnT])


def postnorm_residual(k, ti, halves, gpost, Bgpost, coef, sc):
    P = k.P
    par = ti % 2
    junk, Bjunk = sc["junk"], sc["Bjunk"]
    ss2, Bss2 = sc["ss2"][par], sc["Bss2"][par]
    rs2, Brs2 = sc["rs2"][par], sc["Brs2"][par]
    tmp, Btmp = sc["tmp"][par], sc["Btmp"][par]
    for h, pb in enumerate(halves):
        P.op("act", lambda e, h=h, pb=pb: e.activation(out=junk[:, 0:512], in_=k.ps[pb][:], func=AF.Square,
                                                       accum_out=ss2[:, h:h + 1]),
             reads=[k.Bps[pb]], writes=[Bjunk, Bss2])
    P.op("dve", lambda e: e.tensor_tensor(out=ss2[:, 2:3], in0=ss2[:, 0:1], in1=ss2[:, 1:2], op=ALU.add),
         reads=[Bss2], writes=[Bss2])
    rstd_from_ssq(k, ss2[:, 2:3], rs2[:], Bss2, Brs2)
    for h, pb in enumerate(halves):
        P.op("dve", lambda e, h=h, pb=pb: e.scalar_tensor_tensor(
            out=tmp[:, h * 512:(h + 1) * 512], in0=k.ps[pb][:], scalar=rs2[:], in1=gpost[:, h * 512:(h + 1) * 512],
            op0=ALU.mult, op1=ALU.mult), reads=[k.Bps[pb], Brs2, Bgpost], writes=[Btmp])
    P.op("dve", lambda e: e.scalar_tensor_tensor(out=k.xres[:, ti, :], in0=tmp[:], scalar=float(coef),
                                                 in1=k.xres[:, ti, :], op0=ALU.mult, op1=ALU.add),
         reads=[Btmp, k.Bx[ti]], writes=[k.Bx[ti]])


def common_scratch(k, A, ntmp=2):
    sc = {}
    sc["junk"] = A.alloc("junk", [128, 1024], BF16)
    sc["Bjunk"] = Buf("junk")
    for nm, shape, dtype in [("xs", [128, 1024], BF16), ("ssq", [128, 1], F32), ("rstd", [128, 1], F32),
                             ("ss2", [128, 4], F32), ("rs2", [128, 1], F32), ("tmp", [128, 1024], F32)]:
        n = ntmp if nm in ("tmp", "xs") else 2
        ts = [A.alloc(f"{nm}{i}", shape, dtype) for i in range(n)]
        bs = [Buf(f"{nm}{i}") for i in range(n)]
        sc[nm] = [ts[i % n] for i in range(2)]
        sc["B" + nm] = [bs[i % n] for i in range(2)]
    return sc


NWGU, NWD = 5, 4
CASTW = True


def ffn_setup(k):
    if getattr(k, "ffn", None) is not None and k.ffn["gen"] == k.arena.gen:
        return k.ffn
    k.P.barrier()
    A = k.arena
    A.reset()
    f = {"gen": A.gen}
    f["sc"] = common_scratch(k, A, ntmp=1)
    f["sc"]["ps_tr"] = [4, 5]
    f["xnT"] = A.alloc("xnT", [128, 8, 512], BF16)
    f["BxnT"] = Buf("xnT")
    f["actT"] = A.alloc("actT", [128, NJ, 512], BF16)
    f["BactT"] = [Buf(f"actT{j}") for j in range(NJ)]
    f["wgu"] = A.alloc("wgu", [128, NWGU, 2, 1024], BF16)
    f["Bwgu"] = [Buf(f"wgu{i}") for i in range(NWGU)]
    f["wd"] = A.alloc("wd", [128, NWD, 1024], BF16)
    f["Bwd"] = [Buf(f"wd{i}") for i in range(NWD)]
    f["gpost"] = A.alloc("gpost", [128, 1024], F32)
    f["Bgpost"] = Buf("gpost")
    f["sg"] = [A.alloc(f"sg{i}", [128, 512], F32) for i in range(2)]
    f["Bsg"] = [Buf(f"sg{i}") for i in range(2)]
    f["wq"] = 0
    f["dq"] = 0
    k.ffn = f
    return f


def ffn_phase(k, layer, which):
    P = k.P
    f = ffn_setup(k)
    sc = f["sc"]
    fidx = layer * 2 + which
    g_pre = layer * 6 + (0 if which == 0 else 4)
    g_post = g_pre + 1
    xnT, BxnT, actT, BactT = f["xnT"], f["BxnT"], f["actT"], f["BactT"]
    P.dma("sp", lambda e: e.dma_start(out=f["gpost"][:], in_=k.ng_d[g_post, :].partition_broadcast(128)),
          writes=[f["Bgpost"]], dkey="gpost")
    NST = NT // 4
    if CASTW and not hasattr(k, "wgu_b"):
        k.wgu_b = k.nc.dram_tensor("wgu_b", [8 * NJ, 128, 2, 1024], BF16, kind="Internal").ap()
        k.wd_b = k.nc.dram_tensor("wd_b", [8 * NJ, 128, 1024], BF16, kind="Internal").ap()
        k.Bcast = [Buf(f"cast{i}") for i in range(8)]
        k.cast_done = set()
    use_bf = CASTW and fidx in k.cast_done
    nxt = fidx + 1
    casts = []
    if CASTW and nxt < 8 and nxt not in k.cast_done:
        for j in range(NJ):
            casts.append((k.wgu_b[nxt * NJ + j], k.wgu_d[nxt * NJ + j]))
            casts.append((k.wd_b[nxt * NJ + j], k.wd_d[nxt * NJ + j]))
        k.cast_done.add(nxt)

    def emit_casts(n):
        for _ in range(n):
            if casts:
                dst, src = casts.pop(0)
                P.dma("pool", lambda e, dst=dst, src=src: e.dma_start(out=dst, in_=src), writes=[k.Bcast[nxt]], dkey=f"cw{nxt}")

    def prenorm_st(st):
        for t in range(4):
            prenorm_tile(k, st * 4 + t, g_pre, xnT, BxnT, t * 128, sc)

    prenorm_st(0)
    for st in range(NST):
        for j in range(NJ):
            slot = f["wq"] % NWGU
            f["wq"] += 1
            if use_bf:
                P.dma("sp", lambda e, j=j, slot=slot: e.dma_start(out=f["wgu"][:, slot], in_=k.wgu_b[fidx * NJ + j]),
                      reads=[k.Bcast[fidx]], writes=[f["Bwgu"][slot]], dkey=f"wgub{slot}")
            else:
                P.dma("pool", lambda e, j=j, slot=slot: e.dma_start(out=f["wgu"][:, slot], in_=k.wgu_d[fidx * NJ + j]),
                      writes=[f["Bwgu"][slot]], dkey=f"wgu{slot}")
            if j % 4 == 0:
                emit_casts(1)
            pg, pu = (0, 1) if j % 2 == 0 else (2, 3)
            for half, pb in ((0, pg), (1, pu)):
                for kk in range(8):
                    P.op("pe", lambda e, pb=pb, slot=slot, half=half, kk=kk: e.matmul(
                        k.ps[pb][:], lhsT=f["wgu"][:, slot, half, kk * 128:(kk + 1) * 128], rhs=xnT[:, kk, :],
                        start=(kk == 0), stop=(kk == 7)),
                        reads=[f["Bwgu"][slot], BxnT], writes=[k.Bps[pb]])
            sg, Bsg = f["sg"][j % 2], f["Bsg"][j % 2]
            P.op("act", lambda e, pg=pg, sg=sg: e.activation(out=sg[:], in_=k.ps[pg][:], func=AF.Silu),
                 reads=[k.Bps[pg]], writes=[Bsg])
            P.op("dve", lambda e, pu=pu, sg=sg, j=j: e.tensor_tensor(out=actT[:, j, :], in0=sg[:], in1=k.ps[pu][:],
                                                                     op=ALU.mult),
                 reads=[Bsg, k.Bps[pu]], writes=[BactT[j]])
        if st + 1 < NST:
            prenorm_st(st + 1)
        for j in range(NJ):
            slot = f["dq"] % NWD
            f["dq"] += 1
            if use_bf:
                P.dma("sp", lambda e, j=j, slot=slot: e.dma_start(out=f["wd"][:, slot, :], in_=k.wd_b[fidx * NJ + j]),
                      reads=[k.Bcast[fidx]], writes=[f["Bwd"][slot]], dkey=f"wdb{slot}")
            else:
                P.dma("pool", lambda e, j=j, slot=slot: e.dma_start(out=f["wd"][:, slot, :], in_=k.wd_d[fidx * NJ + j]),
                      writes=[f["Bwd"][slot]], dkey=f"wd{slot}")
            if st < 4 and j % 8 == 0:
                emit_casts(1)
            for tt in range(4):
                for half in range(2):
                    pb = tt * 2 + half
                    P.op("pe", lambda e, pb=pb, slot=slot, half=half, tt=tt, j=j: e.matmul(
                        k.ps[pb][:], lhsT=actT[:, j, tt * 128:(tt + 1) * 128],
                        rhs=f["wd"][:, slot, half * 512:(half + 1) * 512], start=(j == 0), stop=(j == NJ - 1)),
                        reads=[BactT[j], f["Bwd"][slot]], writes=[k.Bps[pb]])
        for tt in range(4):
            postnorm_residual(k, st * 4 + tt, [tt * 2, tt * 2 + 1], f["gpost"], f["Bgpost"], 0.5, sc)


import math
LN_S = {0: math.log(128 ** -0.5), 2: math.log(256 ** -0.5)}


def lin_setup(k, kind, layer):
    P = k.P
    P.barrier()
    A = k.arena
    A.reset()
    k.ffn = None
    m = {"kind": kind}
    H = 4
    NK = 1 if kind == 0 else 2
    DV = 256 if kind == 0 else 512
    NF = 8 if kind == 0 else 16
    HD = H * DV
    m.update(H=H, NK=NK, DV=DV, NF=NF, HD=HD, NKO=HD // 128, NTM=4 if kind == 0 else 8)
    m["sc"] = common_scratch(k, A, ntmp=1)
    m["sc"]["ps_tr"] = [6, 7]
    al = lambda nm, shape, dtype=BF16: (A.alloc(nm, shape, dtype), Buf(nm))
    m["xnT"], m["BxnT"] = al("xnT", [128, 8, 256])
    m["qk"], m["Bqk"] = al("qk", [128, NF, 256])
    m["vo"], _ = al("vo", [128, 2, HD])
    m["Bvo"] = [Buf("vo0"), Buf("vo1")]
    m["hbuf"], _ = al("hbuf", [128, 2, HD])
    m["Bhbuf"] = [Buf("hb0"), Buf("hb1")]
    m["hT"], m["BhT"] = al("hT", [128, HD // 128, 256])
    NR = 5 if kind == 0 else 2
    m["NR"] = NR
    m["ring"], _ = al("ring", [128, NR, 2048])
    m["Bring"] = [Buf(f"ring{i}") for i in range(NR)]
    m["rq"] = 0
    m["gpost"], m["Bgpost"] = al("gpost", [128, 1024], F32)
    m["Cbf"], m["BCbf"] = al("Cbf", [128, H * NK, DV])
    m["ktm"], m["Bktm"] = al("ktm", [128, H, NK * 128])
    m["PT"], m["BPT"] = al("PT", [128, H, 128])
    m["tmpC"], m["BtmpC"] = m["sc"]["tmp"][0], m["sc"]["Btmp"][0]
    m["e"], m["Be"] = al("e", [128, 2, 4], F32)
    m["base"], m["Bbase"] = al("base", [128, 2, 4], F32)
    m["cdec"], m["Bcdec"] = al("cdec", [128, 2, 4], F32)
    m["sm"], m["Bsm"] = al("sm", [128, 8, 4], F32)
    m["wd"] = k.dram(f"wm{layer}", [NF // 2 + 2 * m["NTM"] + m["NKO"] // 2, 128, 2048])
    if kind == 0:
        m["C"], m["BC"] = al("C", [128, H, DV], F32)
        m["nst"], m["Bnst"] = al("nst", [128, 4], F32)
        m["nbf"], m["Bnbf"] = al("nbf", [128, 4])
        m["pre"], m["Bpre"] = al("pre", [128, 8, 259], F32)
        m["acc"], m["Bacc"] = al("acc", [128, 256], F32)
        m["convT"], m["Bw"] = al("convT", [128, 32], F32)
        m["wif"], _ = al("wif", [128, 64])
        m["bifb"], _ = al("bifb", [128, 8], F32)
        m["mng"], _ = al("mng", [128, 1024], F32)
        m["gat"], m["Bgat"] = al("gat", [128, 2, 8], F32)
        m["gt2"], m["Bgt2"] = al("gt2", [128, 2, 8], F32)
        wif_d = k.dram(f"wif{layer}", [128, 64])
        bif_d = k.dram(f"bif{layer}", [1, 8])
        conv_d = k.dram(f"convT{layer}", [128, 32])
        mng_d = k.dram(f"mng{layer}", [1, 1024])
        Bw = m["Bw"]
        P.dma("pool", lambda e: e.dma_start(out=m["wif"][:], in_=wif_d[:, :]), writes=[Bw], dkey="mw0")
        P.dma("sp", lambda e: e.dma_start(out=m["bifb"][:], in_=bif_d[0, :].partition_broadcast(128)), writes=[Bw], dkey="mw1")
        P.dma("sp", lambda e: e.dma_start(out=m["convT"][:], in_=conv_d[:, :]), writes=[Bw], dkey="mw2")
        P.dma("sp", lambda e: e.dma_start(out=m["mng"][:], in_=mng_d[0, :].partition_broadcast(128)), writes=[Bw], dkey="mw3")
    else:
        if not hasattr(k, "pos_d"):
            k.pos_d = k.dram("pos", [1, 2 * SEQ], I32)
        m["posb"], m["Btrig"] = al("posb", [128, 256], F32)
        for nm in ("cosT", "sinT", "ta", "tb"):
            m[nm], _ = al(nm, [128, 256], F32)
        m["ang"] = m["posb"]
        m["ni"], _ = al("ni", [128, 256], I32)
        m["invf"], m["Binvf"] = al("invf", [128, 1], F32)
        m["ii"], _ = al("ii", [128, 1], I32)
        Bi = m["Binvf"]
        P.op("pool", lambda e: e.iota(m["ii"][:], pattern=[[0, 1]], base=0, channel_multiplier=1), writes=[Bi])
        P.op("dve", lambda e: e.tensor_copy(out=m["invf"][:], in_=m["ii"][:]), reads=[Bi], writes=[Bi])
        P.op("act", lambda e: e.activation(out=m["invf"][:], in_=m["invf"][:], func=AF.Exp,
                                           scale=-math.log(10000.0) / 127.0), reads=[Bi], writes=[Bi])
        Be = m["Be"]
        P.op("pool", lambda e: e.iota(m["ii"][:], pattern=[[0, 1]], base=1, channel_multiplier=1), writes=[Bi])
        P.op("dve", lambda e: e.tensor_copy(out=m["sm"][:, 0, 0:1], in_=m["ii"][:]), reads=[Bi], writes=[m["Bsm"]])
        for h in range(4):
            lg = math.log(1.0 - 2.0 ** (-5.0 - h))
            for tt in range(2):
                P.op("dve", lambda e, h=h, tt=tt, lg=lg: e.tensor_scalar(
                    out=m["e"][:, tt, h:h + 1], in0=m["sm"][:, 0, 0:1], scalar1=-lg, scalar2=LN_S[2],
                    op0=ALU.mult, op1=ALU.add), reads=[m["Bsm"]], writes=[Be])
                P.op("dve", lambda e, h=h, tt=tt, lg=lg: e.tensor_scalar(
                    out=m["base"][:, tt, h:h + 1], in0=m["sm"][:, 0, 0:1], scalar1=lg, scalar2=None,
                    op0=ALU.mult), reads=[m["Bsm"]], writes=[m["Bbase"]])
                P.op("pool", lambda e, h=h, tt=tt, lg=lg: e.memset(m["cdec"][:, tt, h:h + 1], math.exp(128.0 * lg)),
                     writes=[m["Bcdec"]])
        P.op("act", lambda e: e.activation(out=m["e"][:], in_=m["e"][:], func=AF.Exp), reads=[Be], writes=[Be])
        P.op("act", lambda e: e.activation(out=m["base"][:], in_=m["base"][:], func=AF.Exp),
             reads=[m["Bbase"]], writes=[m["Bbase"]])
    return m


def nbank(k):
    b = 4 + (k.rot % 4)
    k.rot += 1
    return b


def ring_load(k, m, unit):
    slot = m["rq"] % m["NR"]
    m["rq"] += 1
    k.P.dma("pool", lambda e: e.dma_start(out=m["ring"][:, slot, :], in_=m["wd"][unit]),
            writes=[m["Bring"][slot]], dkey=f"mr{slot}")
    return slot


def tm_proj(k, m, blocks, dst_off0):
    P = k.P
    for i, nb in enumerate(blocks):
        sa = ring_load(k, m, m["NF"] // 2 + 2 * nb)
        sb = ring_load(k, m, m["NF"] // 2 + 2 * nb + 1)
        for tt in range(2):
            b = nbank(k)
            for kk in range(8):
                sl = sa if kk < 4 else sb
                P.op("pe", lambda e, b=b, sl=sl, kk=kk, tt=tt: e.matmul(
                    k.ps[b][:], lhsT=m["xnT"][:, kk, tt * 128:(tt + 1) * 128],
                    rhs=m["ring"][:, sl, (kk % 4) * 512:(kk % 4 + 1) * 512], start=(kk == 0), stop=(kk == 7)),
                    reads=[m["BxnT"], m["Bring"][sl]], writes=[k.Bps[b]])
            off = dst_off0 + i * 512
            if (i + tt) % 2 == 0:
                P.op("act", lambda e, b=b, tt=tt, off=off: e.activation(out=m["vo"][:, tt, off:off + 512], in_=k.ps[b][:],
                                                                        func=AF.Copy),
                     reads=[k.Bps[b]], writes=[m["Bvo"][tt]])
            else:
                P.op("dve", lambda e, b=b, tt=tt, off=off: e.tensor_copy(out=m["vo"][:, tt, off:off + 512], in_=k.ps[b][:]),
                     reads=[k.Bps[b]], writes=[m["Bvo"][tt]])


def lin_mixer_phase(k, layer, kind, j):
    P = k.P
    m = lin_setup(k, kind, layer)
    sc = m["sc"]
    H, NK, DV, NF, HD, NKO, NTM = m["H"], m["NK"], m["DV"], m["NF"], m["HD"], m["NKO"], m["NTM"]
    g_pre, g_post = layer * 6 + 2, layer * 6 + 3
    xnT, qk, vo, hbuf, hT = m["xnT"], m["qk"], m["vo"], m["hbuf"], m["hT"]
    P.dma("sp", lambda e: e.dma_start(out=m["gpost"][:], in_=k.ng_d[g_post, :].partition_broadcast(128)),
          writes=[m["Bgpost"]], dkey="gpost")
    qidx = (lambda h, kc: h) if kind == 0 else (lambda h, kc: 2 * h + kc)
    kidx = (lambda h, kc: 4 + h) if kind == 0 else (lambda h, kc: 8 + 2 * h + kc)
    sm, Bsm = m["sm"], m["Bsm"]
    for st in range(NT // 2):
        first = (st % 8 == 0)
        for tt in range(2):
            prenorm_tile(k, st * 2 + tt, g_pre, xnT, m["BxnT"], tt * 128, sc)
        if first:
            P.op("pool", lambda e: e.memset(m["Cbf"][:], 0.0), writes=[m["BCbf"]])
            if kind == 0:
                P.op("pool", lambda e: e.memset(m["C"][:], 0.0), writes=[m["BC"]])
                P.op("pool", lambda e: e.memset(m["nst"][:], 0.0), writes=[m["Bnst"]])
                P.op("pool", lambda e: e.memset(m["nbf"][:], 0.0), writes=[m["Bnbf"]])
        if kind == 0:
            if first:
                P.op("pool", lambda e: e.memset(m["pre"][:, :, 0:3], 0.0), writes=[m["Bpre"]])
            else:
                P.op("pool", lambda e: e.tensor_copy(out=m["pre"][:, :, 0:3], in_=m["pre"][:, :, 256:259]),
                     reads=[m["Bpre"]], writes=[m["Bpre"]])
        else:
            Bt = m["Btrig"]
            P.dma("pool", lambda e, st=st: e.dma_start(out=m["posb"][:],
                                                        in_=k.pos_d[0, st * 256:(st + 1) * 256].partition_broadcast(128)),
                  writes=[Bt], dkey="posb")
            P.op("dve", lambda e: e.tensor_scalar(out=m["ang"][:], in0=m["posb"][:], scalar1=m["invf"][:], scalar2=None,
                                                  op0=ALU.mult), reads=[Bt, m["Binvf"]], writes=[Bt])
            for dst, shift in ((m["sinT"], 0.5), (m["cosT"], 0.75)):
                TWO_PI = 2.0 * math.pi
                P.op("dve", lambda e, shift=shift: e.tensor_scalar(out=m["ta"][:], in0=m["ang"][:], scalar1=1.0 / TWO_PI,
                                                                    scalar2=shift, op0=ALU.mult, op1=ALU.add),
                     reads=[Bt], writes=[Bt])
                P.op("dve", lambda e: e.tensor_copy(out=m["ni"][:], in_=m["ta"][:]), reads=[Bt], writes=[Bt])
                P.op("dve", lambda e: e.tensor_copy(out=m["tb"][:], in_=m["ni"][:]), reads=[Bt], writes=[Bt])
                P.op("dve", lambda e: e.tensor_tensor(out=m["ta"][:], in0=m["ta"][:], in1=m["tb"][:], op=ALU.subtract),
                     reads=[Bt], writes=[Bt])
                P.op("dve", lambda e: e.tensor_scalar(out=m["ta"][:], in0=m["ta"][:], scalar1=-0.5, scalar2=TWO_PI,
                                                      op0=ALU.add, op1=ALU.mult), reads=[Bt], writes=[Bt])
                P.op("dve", lambda e: e.tensor_scalar(out=m["tb"][:], in0=m["ta"][:], scalar1=math.pi, scalar2=-TWO_PI,
                                                      op0=ALU.is_gt, op1=ALU.mult), reads=[Bt], writes=[Bt])
                P.op("dve", lambda e: e.tensor_tensor(out=m["ta"][:], in0=m["ta"][:], in1=m["tb"][:], op=ALU.add),
                     reads=[Bt], writes=[Bt])
                P.op("dve", lambda e: e.tensor_scalar(out=m["tb"][:], in0=m["ta"][:], scalar1=-math.pi, scalar2=TWO_PI,
                                                      op0=ALU.is_lt, op1=ALU.mult), reads=[Bt], writes=[Bt])
                P.op("dve", lambda e: e.tensor_tensor(out=m["ta"][:], in0=m["ta"][:], in1=m["tb"][:], op=ALU.add),
                     reads=[Bt], writes=[Bt])
                P.op("dve", lambda e: e.tensor_scalar(out=m["ta"][:], in0=m["ta"][:], scalar1=-3.1415925, scalar2=3.1415925,
                                                      op0=ALU.max, op1=ALU.min), reads=[Bt], writes=[Bt])
                P.op("act", lambda e, dst=dst: e.activation(out=dst[:], in_=m["ta"][:], func=AF.Sin), reads=[Bt], writes=[Bt])
        for u in range(NF // 2):
            sl = ring_load(k, m, u)
            banks = []
            for c2 in range(2):
                b = nbank(k)
                banks.append(b)
                for kk in range(8):
                    P.op("pe", lambda e, b=b, sl=sl, c2=c2, kk=kk: e.matmul(
                        k.ps[b][:, 0:256], lhsT=m["ring"][:, sl, c2 * 1024 + kk * 128:c2 * 1024 + (kk + 1) * 128],
                        rhs=xnT[:, kk, :], start=(kk == 0), stop=(kk == 7)),
                        reads=[m["Bring"][sl], m["BxnT"]], writes=[k.Bps[b]])
                if kind == 0:
                    c = 2 * u + c2
                    P.op("act", lambda e, b=b, c=c: e.activation(out=m["pre"][:, c, 3:259], in_=k.ps[b][:, 0:256], func=AF.Copy),
                         reads=[k.Bps[b]], writes=[m["Bpre"]])
            if kind == 2:
                Bt = m["Btrig"]
                ba, bb = banks
                for o, (f1, f2, op) in enumerate(((m["cosT"], m["sinT"], ALU.subtract), (m["sinT"], m["cosT"], ALU.add))):
                    P.op("dve", lambda e, f1=f1: e.tensor_tensor(out=m["ta"][:], in0=k.ps[ba][:, 0:256], in1=f1[:], op=ALU.mult),
                         reads=[k.Bps[ba], Bt], writes=[Bt])
                    P.op("dve", lambda e, f2=f2: e.tensor_tensor(out=m["tb"][:], in0=k.ps[bb][:, 0:256], in1=f2[:], op=ALU.mult),
                         reads=[k.Bps[bb], Bt], writes=[Bt])
                    P.op("dve", lambda e, u=u, o=o, op=op: e.tensor_tensor(out=qk[:, 2 * u + o, :], in0=m["ta"][:], in1=m["tb"][:], op=op),
                         reads=[Bt], writes=[m["Bqk"]])
        if kind == 0:
            for c in range(8):
                cw = m["convT"]
                P.op("dve", lambda e, c=c: e.tensor_scalar(out=m["acc"][:], in0=m["pre"][:, c, 3:259],
                                                           scalar1=cw[:, c * 4 + 3:c * 4 + 4], scalar2=None, op0=ALU.mult),
                     reads=[m["Bpre"], m["Bw"]], writes=[m["Bacc"]])
                for tap in (2, 1, 0):
                    P.op("dve", lambda e, c=c, tap=tap: e.scalar_tensor_tensor(
                        out=m["acc"][:], in0=m["pre"][:, c, tap:tap + 256], scalar=cw[:, c * 4 + tap:c * 4 + tap + 1],
                        in1=m["acc"][:], op0=ALU.mult, op1=ALU.add), reads=[m["Bpre"], m["Bw"], m["Bacc"]], writes=[m["Bacc"]])
                P.op("act", lambda e, c=c: e.activation(out=qk[:, c, :], in_=m["acc"][:], func=AF.Silu),
                     reads=[m["Bacc"]], writes=[m["Bqk"]])
        tm_proj(k, m, list(range(NTM // 2)), 0)
        if kind == 0:
            gat, gt2, Bgat, Bgt2 = m["gat"], m["gt2"], m["Bgat"], m["Bgt2"]
            bg = nbank(k)
            for tt in range(2):
                for kk in range(8):
                    P.op("pe", lambda e, tt=tt, kk=kk: e.matmul(k.ps[bg][:, tt * 8:(tt + 1) * 8],
                                                                 lhsT=xnT[:, kk, tt * 128:(tt + 1) * 128],
                                                                 rhs=m["wif"][:, kk * 8:(kk + 1) * 8], start=(kk == 0), stop=(kk == 7)),
                         reads=[m["BxnT"], m["Bw"]], writes=[k.Bps[bg]])
                P.op("dve", lambda e, tt=tt: e.tensor_tensor(out=gat[:, tt, :], in0=k.ps[bg][:, tt * 8:(tt + 1) * 8],
                                                             in1=m["bifb"][:], op=ALU.add),
                     reads=[k.Bps[bg], m["Bw"]], writes=[Bgat])
            P.op("act", lambda e: e.activation(out=gat[:], in_=gat[:], func=AF.Tanh, scale=1.0 / 15.0), reads=[Bgat], writes=[Bgat])
            P.op("dve", lambda e: e.tensor_scalar(out=gat[:], in0=gat[:], scalar1=15.0, scalar2=None, op0=ALU.mult),
                 reads=[Bgat], writes=[Bgat])
            P.op("act", lambda e: e.activation(out=gt2[:, :, 4:8], in_=gat[:, :, 4:8], func=AF.Exp, scale=-1.0),
                 reads=[Bgat], writes=[Bgt2])
            P.op("dve", lambda e: e.tensor_scalar(out=gt2[:, :, 4:8], in0=gt2[:, :, 4:8], scalar1=1.0, scalar2=None, op0=ALU.add),
                 reads=[Bgt2], writes=[Bgt2])
            P.op("act", lambda e: e.activation(out=gt2[:, :, 4:8], in_=gt2[:, :, 4:8], func=AF.Ln), reads=[Bgt2], writes=[Bgt2])
            bc = nbank(k)
            for tt in range(2):
                P.op("pe", lambda e, tt=tt: e.matmul(k.ps[bc][:, tt * 4:(tt + 1) * 4], lhsT=k.trif[:], rhs=gt2[:, tt, 4:8],
                                                     start=True, stop=True), reads=[Bgt2, k.Bconst], writes=[k.Bps[bc]])
                P.op("pe", lambda e, tt=tt: e.matmul(k.ps[bc][:, 8 + tt * 4:8 + (tt + 1) * 4], lhsT=k.onesf[:], rhs=gt2[:, tt, 4:8],
                                                     start=True, stop=True), reads=[Bgt2, k.Bconst], writes=[k.Bps[bc]])
            cs = k.ps[bc][:, 0:8].rearrange("p (t h) -> p t h", t=2)
            tot = k.ps[bc][:, 8:16].rearrange("p (t h) -> p t h", t=2)
            P.op("dve", lambda e: e.scalar_tensor_tensor(out=m["e"][:], in0=cs, scalar=LN_S[0], in1=gat[:, :, 0:4],
                                                         op0=ALU.add, op1=ALU.add), reads=[k.Bps[bc], Bgat], writes=[m["Be"]])
            P.op("act", lambda e: e.activation(out=m["e"][:], in_=m["e"][:], func=AF.Exp), reads=[m["Be"]], writes=[m["Be"]])
            P.op("act", lambda e: e.activation(out=m["base"][:], in_=cs, func=AF.Exp), reads=[k.Bps[bc]], writes=[m["Bbase"]])
            P.op("act", lambda e: e.activation(out=m["cdec"][:], in_=tot, func=AF.Exp, scale=-1.0),
                 reads=[k.Bps[bc]], writes=[m["Bcdec"]])
        for tt in range(2):
            tok = slice(tt * 128, (tt + 1) * 128)
            bt = nbank(k)
            ptb = k.ps[bt][:].bitcast(BF16)
            for h in range(H):
                for kc in range(NK):
                    o = (h * NK + kc) * 128
                    P.op("pe", lambda e, h=h, kc=kc, o=o: e.transpose(out=ptb[:, o:o + 128], in_=qk[:, kidx(h, kc), tok],
                                                                       identity=k.ident[:]),
                         reads=[m["Bqk"], k.Bconst], writes=[k.Bps[bt]])
            for h in range(H):
                P.op("dve", lambda e, h=h: e.tensor_scalar(out=m["ktm"][:, h, :], in0=ptb[:, h * NK * 128:(h + 1) * NK * 128],
                                                           scalar1=m["e"][:, tt, h:h + 1], scalar2=None, op0=ALU.mult),
                     reads=[k.Bps[bt], m["Be"]], writes=[m["Bktm"]])
            bp = nbank(k)
            for h in range(H):
                for kc in range(NK):
                    P.op("pe", lambda e, h=h, kc=kc: e.matmul(k.ps[bp][:, h * 128:(h + 1) * 128], lhsT=qk[:, kidx(h, kc), tok],
                                                              rhs=qk[:, qidx(h, kc), tok], start=(kc == 0), stop=(kc == NK - 1)),
                         reads=[m["Bqk"]], writes=[k.Bps[bp]])
            for h in range(H):
                P.op("dve", lambda e, h=h: e.scalar_tensor_tensor(out=m["PT"][:, h, :], in0=k.ps[bp][:, h * 128:(h + 1) * 128],
                                                                  scalar=m["e"][:, tt, h:h + 1], in1=k.tri[:],
                                                                  op0=ALU.mult, op1=ALU.mult),
                     reads=[k.Bps[bp], m["Be"], k.Bconst], writes=[m["BPT"]])
            if kind == 0:
                bd = nbank(k)
                for h in range(H):
                    P.op("pe", lambda e, h=h: e.matmul(k.ps[bd][:, h:h + 1], lhsT=m["PT"][:, h, :], rhs=k.ones[:, 0:1],
                                                       start=True, stop=False), reads=[m["BPT"], k.Bconst], writes=[k.Bps[bd]])
                    P.op("pe", lambda e, h=h: e.matmul(k.ps[bd][:, h:h + 1], lhsT=qk[:, qidx(h, 0), tok], rhs=m["nbf"][:, h:h + 1],
                                                       start=False, stop=True), reads=[m["Bqk"], m["Bnbf"]], writes=[k.Bps[bd]])
                P.op("dve", lambda e: e.tensor_copy(out=sm[:, 0, :], in_=k.ps[bd][:, 0:4]), reads=[k.Bps[bd]], writes=[Bsm])
                P.op("dve", lambda e: e.tensor_scalar(out=sm[:, 6, :], in0=sm[:, 0, :], scalar1=-1.0, scalar2=None,
                                                      op0=ALU.mult), reads=[Bsm], writes=[Bsm])
                P.op("dve", lambda e: e.tensor_tensor(out=sm[:, 0, :], in0=sm[:, 0, :], in1=sm[:, 6, :], op=ALU.max),
                     reads=[Bsm], writes=[Bsm])
                P.op("dve", lambda e: e.tensor_tensor(out=sm[:, 0, :], in0=sm[:, 0, :], in1=m["base"][:, tt, :], op=ALU.max),
                     reads=[Bsm, m["Bbase"]], writes=[Bsm])
                P.op("dve", lambda e: e.reciprocal(out=sm[:, 1, :], in_=sm[:, 0, :]), reads=[Bsm], writes=[Bsm])
                basev = sm[:, 1, :]
            else:
                P.op("dve", lambda e: e.tensor_copy(out=sm[:, 1, :], in_=m["base"][:, tt, :]), reads=[m["Bbase"]], writes=[Bsm])
                basev = sm[:, 1, :]
            hpb = 512 // DV
            abank = {}
            for h in range(H):
                if h % hpb == 0:
                    ba = nbank(k)
                abank[h] = (ba, (h % hpb) * DV)
                ba, off = abank[h]
                P.op("pe", lambda e, h=h, ba=ba, off=off: e.matmul(k.ps[ba][:, off:off + DV], lhsT=m["PT"][:, h, :],
                                                                   rhs=vo[:, tt, h * DV:(h + 1) * DV], start=True, stop=False),
                     reads=[m["BPT"], m["Bvo"][tt]], writes=[k.Bps[ba]])
                for kc in range(NK):
                    P.op("pe", lambda e, h=h, kc=kc, ba=ba, off=off: e.matmul(
                        k.ps[ba][:, off:off + DV], lhsT=qk[:, qidx(h, kc), tok], rhs=m["Cbf"][:, h * NK + kc, :],
                        start=False, stop=(kc == NK - 1)), reads=[m["Bqk"], m["BCbf"]], writes=[k.Bps[ba]])
                P.op("act", lambda e, h=h, ba=ba, off=off: e.activation(out=sc["junk"][:, 0:DV], in_=k.ps[ba][:, off:off + DV],
                                                                        func=AF.Square, accum_out=sm[:, 2, h:h + 1]),
                     reads=[k.Bps[ba]], writes=[sc["Bjunk"], Bsm])
                if h % hpb == hpb - 1 or h == H - 1:
                    pass
            P.op("dve", lambda e: e.tensor_tensor(out=sm[:, 3, :], in0=basev, in1=basev, op=ALU.mult), reads=[Bsm], writes=[Bsm])
            P.op("dve", lambda e: e.tensor_tensor(out=sm[:, 3, :], in0=sm[:, 3, :], in1=sm[:, 2, :], op=ALU.mult), reads=[Bsm], writes=[Bsm])
            P.op("pool", lambda e: e.tensor_scalar(out=sm[:, 3, :], in0=sm[:, 3, :], scalar1=1.0 / DV, scalar2=EPS,
                                                   op0=ALU.mult, op1=ALU.add), reads=[Bsm], writes=[Bsm])
            P.op("pool", lambda e: e.tensor_tensor(out=sm[:, 3, :], in0=sm[:, 3, :], in1=k.mhalf[:, 0:1].to_broadcast([128, 4]),
                                                   op=ALU.pow), reads=[Bsm, k.Bconst], writes=[Bsm])
            P.op("dve", lambda e: e.tensor_tensor(out=sm[:, 4, :], in0=sm[:, 3, :], in1=basev, op=ALU.mult), reads=[Bsm], writes=[Bsm])
            for h in range(H):
                ba, off = abank[h]
                if kind == 0:
                    P.op("dve", lambda e, h=h, ba=ba, off=off: e.scalar_tensor_tensor(
                        out=hbuf[:, tt, h * DV:(h + 1) * DV], in0=k.ps[ba][:, off:off + DV], scalar=sm[:, 4, h:h + 1],
                        in1=m["mng"][:, h * DV:(h + 1) * DV], op0=ALU.mult, op1=ALU.mult),
                        reads=[k.Bps[ba], Bsm, m["Bw"]], writes=[m["Bhbuf"][tt]])
                else:
                    P.op("dve", lambda e, h=h, ba=ba, off=off: e.tensor_scalar(
                        out=hbuf[:, tt, h * DV:(h + 1) * DV], in0=k.ps[ba][:, off:off + DV], scalar1=sm[:, 4, h:h + 1],
                        scalar2=None, op0=ALU.mult), reads=[k.Bps[ba], Bsm], writes=[m["Bhbuf"][tt]])
            for h in range(H):
                cd = m["cdec"][:, tt, h:h + 1]
                for kc in range(NK):
                    bs = nbank(k)
                    P.op("pe", lambda e, h=h, kc=kc, bs=bs: e.matmul(k.ps[bs][:, 0:DV], lhsT=m["ktm"][:, h, kc * 128:(kc + 1) * 128],
                                                                     rhs=vo[:, tt, h * DV:(h + 1) * DV], start=True, stop=True),
                         reads=[m["Bktm"], m["Bvo"][tt]], writes=[k.Bps[bs]])
                    P.op("dve", lambda e, bs=bs, cd=cd: e.tensor_scalar(out=m["tmpC"][:, 0:DV], in0=k.ps[bs][:, 0:DV], scalar1=cd,
                                                                        scalar2=None, op0=ALU.mult),
                         reads=[k.Bps[bs], m["Bcdec"]], writes=[m["BtmpC"]])
                    if kind == 0:
                        P.op("dve", lambda e, h=h, cd=cd: e.scalar_tensor_tensor(out=m["C"][:, h, :], in0=m["C"][:, h, :], scalar=cd,
                                                                                 in1=m["tmpC"][:, 0:DV], op0=ALU.mult, op1=ALU.add),
                             reads=[m["BC"], m["Bcdec"], m["BtmpC"]], writes=[m["BC"]])
                        P.op("act", lambda e, h=h: e.activation(out=m["Cbf"][:, h, :], in_=m["C"][:, h, :], func=AF.Copy),
                             reads=[m["BC"]], writes=[m["BCbf"]])
                    else:
                        ci = h * NK + kc
                        P.op("dve", lambda e, ci=ci, cd=cd: e.scalar_tensor_tensor(out=m["Cbf"][:, ci, :], in0=m["Cbf"][:, ci, :], scalar=cd,
                                                                                   in1=m["tmpC"][:, 0:DV], op0=ALU.mult, op1=ALU.add),
                             reads=[m["BCbf"], m["Bcdec"], m["BtmpC"]], writes=[m["BCbf"]])
            if kind == 0:
                bn = nbank(k)
                for h in range(H):
                    P.op("pe", lambda e, h=h: e.matmul(k.ps[bn][:, h:h + 1], lhsT=m["ktm"][:, h, :], rhs=k.ones[:, 0:1],
                                                       start=True, stop=True), reads=[m["Bktm"], k.Bconst], writes=[k.Bps[bn]])
                P.op("dve", lambda e: e.tensor_tensor(out=sm[:, 5, :], in0=k.ps[bn][:, 0:4], in1=m["nst"][:], op=ALU.add),
                     reads=[k.Bps[bn], m["Bnst"]], writes=[Bsm])
                P.op("dve", lambda e: e.tensor_tensor(out=m["nst"][:], in0=sm[:, 5, :], in1=m["cdec"][:, tt, :], op=ALU.mult),
                     reads=[Bsm, m["Bcdec"]], writes=[m["Bnst"]])
                P.op("dve", lambda e: e.tensor_copy(out=m["nbf"][:], in_=m["nst"][:]), reads=[m["Bnst"]], writes=[m["Bnbf"]])
        tm_proj(k, m, list(range(NTM // 2, NTM)), 0)
        for tt in range(2):
            P.op("act", lambda e, tt=tt: e.activation(out=vo[:, tt, :], in_=vo[:, tt, :],
                                                      func=(AF.Sigmoid if kind == 0 else AF.Silu)),
                 reads=[m["Bvo"][tt]], writes=[m["Bvo"][tt]])
            P.op("dve", lambda e, tt=tt: e.tensor_tensor(out=hbuf[:, tt, :], in0=hbuf[:, tt, :], in1=vo[:, tt, :], op=ALU.mult),
                 reads=[m["Bhbuf"][tt], m["Bvo"][tt]], writes=[m["Bhbuf"][tt]])
            for g in range(NKO // 8):
                bt = nbank(k)
                ptb = k.ps[bt][:].bitcast(BF16)
                for i in range(8):
                    kk = g * 8 + i
                    P.op("pe", lambda e, tt=tt, kk=kk, i=i, ptb=ptb: e.transpose(out=ptb[:, i * 128:(i + 1) * 128],
                                                                                 in_=hbuf[:, tt, kk * 128:(kk + 1) * 128],
                                                                                 identity=k.ident[:]),
                         reads=[m["Bhbuf"][tt], k.Bconst], writes=[k.Bps[bt]])
                P.op("act", lambda e, tt=tt, g=g, ptb=ptb: e.activation(out=hT[:, g * 8:(g + 1) * 8, tt * 128:(tt + 1) * 128],
                                                                        in_=ptb.rearrange("p (a t) -> p a t", a=8), func=AF.Copy),
                     reads=[k.Bps[bt]], writes=[m["BhT"]])
        for u in range(NKO // 2):
            sl = ring_load(k, m, NF // 2 + 2 * NTM + u)
            for c2 in range(2):
                kk = 2 * u + c2
                for tt in range(2):
                    for half in range(2):
                        pb = tt * 2 + half
                        P.op("pe", lambda e, sl=sl, c2=c2, kk=kk, tt=tt, half=half, pb=pb: e.matmul(
                            k.ps[pb][:], lhsT=hT[:, kk, tt * 128:(tt + 1) * 128],
                            rhs=m["ring"][:, sl, c2 * 1024 + half * 512:c2 * 1024 + (half + 1) * 512],
                            start=(kk == 0), stop=(kk == NKO - 1)), reads=[m["BhT"], m["Bring"][sl]], writes=[k.Bps[pb]])
        for tt in range(2):
            postnorm_residual(k, st * 2 + tt, [tt * 2, tt * 2 + 1], m["gpost"], m["Bgpost"], 1.0, sc)


C0 = math.exp(-0.5)
RW_DEBUG = False
RW_STOP = 0


class _Stop(Exception):
    pass


def nb8(k):
    b = k.rot % 8
    k.rot += 1
    return b


def rwkv_setup(k, layer):
    P = k.P
    A = k.big_arena
    A.reset()
    k.ffn = None
    m = {}
    m["sc"] = common_scratch(k, A, ntmp=1)
    m["sc"]["ps_tr"] = [6, 7]
    al = lambda nm, shape, dtype=BF16: (A.alloc(nm, shape, dtype), Buf(nm))
    m["xnTh"], m["BxnT"] = al("xnTh", [128, 8, 129])
    m["xxT"], m["Bxx"] = al("xxT", [128, 8, 128])
    m["mixT"], _ = al("mixT", [128, 2, 8, 128])
    m["Bmix"] = [Buf(f"mix{i}") for i in range(2)]
    m["hT"], m["BhT"] = al("hT", [128, 8, 128])
    m["ring"], _ = al("ring", [128, 2, 2048])
    m["Bring"] = [Buf(f"ring{i}") for i in range(2)]
    m["NR"] = 2
    m["rq"] = 0
    m["gpost"], m["Bgpost"] = al("gpost", [128, 1024], F32)
    m["Bw"] = Buf("rw_w")
    for nm, shape, dtype in [("w1", [128, 8, 64], BF16), ("a1", [128, 8, 64], BF16), ("g1", [128, 8, 128], BF16),
                             ("w2", [64, 1024], BF16), ("a2", [64, 1024], BF16), ("g2", [128, 1024], BF16),
                             ("w0r", [64, 1024], F32), ("a0r", [64, 1024], F32), ("muT", [128, 48], F32),
                             ("kkb", [64, 1024], BF16), ("kab", [64, 1024], BF16), ("lgb", [64, 1024], BF16),
                             ("lbb", [64, 1024], BF16), ("rkb", [64, 1024], BF16)]:
        m[nm], _ = al(nm, shape, dtype)
    d = lambda nm, shape: k.dram(f"rw_{nm}", shape)
    m["wd"] = d("units", [16, 128, 2048])
    Bw = m["Bw"]
    for i, (nm, shape) in enumerate([("w1", [128, 8, 64]), ("a1", [128, 8, 64]), ("g1", [128, 8, 128]),
                                     ("w2", [64, 1024]), ("a2", [64, 1024]), ("g2", [128, 1024])]):
        src = d(nm, shape)
        P.dma("pool", lambda e, nm=nm, src=src: e.dma_start(out=m[nm][:], in_=src), writes=[Bw], dkey=f"rww{i}")
    for i, nm in enumerate(["w0r", "a0r"]):
        src = d(nm, [1, 1024])
        P.dma("sp", lambda e, nm=nm, src=src: e.dma_start(out=m[nm][:], in_=src[0, :].partition_broadcast(64)),
              writes=[Bw], dkey=f"rwr{i}")
    src = d("muT", [128, 48])
    P.dma("sp", lambda e, src=src: e.dma_start(out=m["muT"][:], in_=src), writes=[Bw], dkey="rwmu")
    for i, nm in enumerate(["kkb", "kab", "lgb", "lbb", "rkb"]):
        src = d(nm, [1, 1024])
        P.dma("pool", lambda e, nm=nm, src=src: e.dma_start(out=m[nm][:], in_=src[0, :].partition_broadcast(64)),
              writes=[Bw], dkey=f"rwb{i}")
    m["hidw"], m["Bhid"] = al("hidw", [64, 128])
    m["hida"], _ = al("hida", [64, 128])
    m["hidg"], _ = al("hidg", [128, 128])
    m["rkv"], _ = al("rkv", [64, 2, 3, 1024])
    m["Brkv"] = [[Buf(f"rkv{c}{i}") for i in range(3)] for c in range(2)]
    m["H"], m["BH"] = al("H", [64, 1024], F32)
    m["Hbf"], m["BHbf"] = al("Hbf", [64, 1024])

    def mk_cx(i):
        cx = dict(m)
        alc = lambda nm, shape, dtype=BF16: (A.alloc(f"{nm}c{i}", shape, dtype), Buf(f"{nm}c{i}"))
        for nm in ("F0", "F1", "F2", "PB", "TTf"):
            cx[nm], cx["B" + nm] = alc(nm, [64, 1024], F32)
        for nm in ("a", "gsb", "G", "Ginv", "Gprev", "kk", "bt", "at", "kt", "rt", "TT", "bT", "kT"):
            cx[nm], cx["B" + nm] = alc(nm, [64, 1024])
        for nm, src in (("akv", "G"), ("TAT", "Ginv"), ("Usb", "Gprev"), ("zb", "kk"), ("QA", "F0"), ("QB", "F1"), ("PA", "F2")):
            cx[nm], cx["B" + nm] = cx[src], cx["B" + src]
        for nm in ("M1", "M2", "arT"):
            cx[nm], cx["B" + nm] = alc(nm, [64, 16, 2, 64])
        cx["GL"], cx["BGL"] = alc("GL", [64, 16], F32)
        cx["sm"], cx["Bsm"] = alc("sm", [64, 8, 16], F32)
        return cx

    m["cx"] = [mk_cx(0), mk_cx(1)]
    m["eps24"], _ = al("eps24", [64, 1], F32)
    return m


def rwkv_seq(k, m, layer, seq):
    P = k.P
    sc = m["sc"]
    g_pre, g_post = layer * 6 + 2, layer * 6 + 3
    xnTh, xxT, mixT, hT = m["xnTh"], m["xxT"], m["mixT"], m["hT"]
    Bw = m["Bw"]
    H, BH = m["H"], m["BH"]
    cxs = m["cx"]
    hv = lambda t: t[:].rearrange("p (h j) -> p h j", h=16)
    bc = lambda ap: ap.unsqueeze(2).to_broadcast([64, 16, 64])
    P.op("pool", lambda e: e.memset(H[:], 0.0), writes=[BH])
    P.op("pool", lambda e: e.memset(m["Hbf"][:], 0.0), writes=[m["BHbf"]])

    def headmm(dst_evac, groups):
        for g in range(2):
            b = nb8(k)
            for hh in range(8):
                h = g * 8 + hh
                for gi, (lf, rf, rd) in enumerate(groups):
                    Lh, Rh, last = lf(h), rf(h), (gi == len(groups) - 1)
                    P.op("pe", lambda e, b=b, hh=hh, Lh=Lh, Rh=Rh, gi=gi, last=last: e.matmul(
                        k.ps[b][0:64, hh * 64:(hh + 1) * 64], lhsT=Lh, rhs=Rh, start=(gi == 0), stop=last),
                        reads=rd, writes=[k.Bps[b]])
            dst_evac(b, g)

    def evac_to(dst, Bdst, eng="act"):
        def f(b, g):
            if eng == "act":
                P.op("act", lambda e: e.activation(out=hv(dst)[:, g * 8:(g + 1) * 8],
                                                   in_=k.ps[b][0:64, :].rearrange("p (h x) -> p h x", h=8), func=AF.Copy),
                     reads=[k.Bps[b]], writes=[Bdst])
            else:
                P.op("dve", lambda e: e.tensor_copy(out=hv(dst)[:, g * 8:(g + 1) * 8],
                                                    in_=k.ps[b][0:64, :].rearrange("p (h x) -> p h x", h=8)),
                     reads=[k.Bps[b]], writes=[Bdst])
        return f

    for ti in range(16):
        xs_ = ti % 2
        gt = seq * 16 + ti
        P.dma("sp", lambda e: e.dma_start(out=k.xres[:, xs_, :], in_=k.scrX[gt]), reads=[k.Bscr[gt]], writes=[k.Bx[xs_]],
              dkey=f"x{xs_}")
        if ti == 0:
            P.op("pool", lambda e: e.memset(xnTh[:, :, 0:1], 0.0), writes=[m["BxnT"]])
        else:
            P.op("pool", lambda e: e.tensor_copy(out=xnTh[:, :, 0:1], in_=xnTh[:, :, 128:129]),
                 reads=[m["BxnT"]], writes=[m["BxnT"]])
        prenorm_tile(k, xs_, g_pre, xnTh, m["BxnT"], 1, sc)
        P.op("dve", lambda e: e.tensor_tensor(out=xxT[:], in0=xnTh[:, :, 0:128], in1=xnTh[:, :, 1:129], op=ALU.subtract),
             reads=[m["BxnT"]], writes=[m["Bxx"]])
        def make_mix(i):
            eng = "dve" if i % 2 == 0 else "pool"
            mu_b = m["muT"][:, i * 8:(i + 1) * 8].unsqueeze(2).to_broadcast([128, 8, 128])
            P.op(eng, lambda e, i=i, mu_b=mu_b: e.tensor_tensor(out=mixT[:, i % 2], in0=xxT[:], in1=mu_b, op=ALU.mult),
                 reads=[m["Bxx"], Bw], writes=[m["Bmix"][i % 2]])
            P.op(eng, lambda e, i=i: e.tensor_tensor(out=mixT[:, i % 2], in0=mixT[:, i % 2], in1=xnTh[:, :, 1:129], op=ALU.add),
                 reads=[m["Bmix"][i % 2], m["BxnT"]], writes=[m["Bmix"][i % 2]])
        for mi in range(3):
            make_mix(mi)
            for nb in range(2):
                sa = ring_load(k, m, mi * 4 + 2 * nb)
                sb = ring_load(k, m, mi * 4 + 2 * nb + 1)
                for c in range(2):
                    b = nb8(k)
                    for kk in range(8):
                        sl = sa if kk < 4 else sb
                        P.op("pe", lambda e, b=b, sl=sl, kk=kk, c=c, mi=mi: e.matmul(
                            k.ps[b][0:64, :], lhsT=mixT[:, mi % 2, kk, c * 64:(c + 1) * 64],
                            rhs=m["ring"][:, sl, (kk % 4) * 512:(kk % 4 + 1) * 512], start=(kk == 0), stop=(kk == 7)),
                            reads=[m["Bmix"][mi % 2], m["Bring"][sl]], writes=[k.Bps[b]])
                    P.op("act", lambda e, b=b, c=c, mi=mi, nb=nb: e.activation(
                        out=m["rkv"][:, c, mi, nb * 512:(nb + 1) * 512], in_=k.ps[b][0:64, :], func=AF.Copy),
                        reads=[k.Bps[b]], writes=[m["Brkv"][c][mi]])
        for (hid, w, mi, fn, np_) in ((m["hidw"], m["w1"], 3, AF.Tanh, 64), (m["hida"], m["a1"], 4, AF.Copy, 64),
                                      (m["hidg"], m["g1"], 5, AF.Sigmoid, 128)):
            make_mix(mi)
            b = nb8(k)
            for kk in range(8):
                P.op("pe", lambda e, b=b, w=w, mi=mi, kk=kk, np_=np_: e.matmul(
                    k.ps[b][0:np_, 0:128], lhsT=w[:, kk, :], rhs=mixT[:, mi % 2, kk, :], start=(kk == 0), stop=(kk == 7)),
                    reads=[Bw, m["Bmix"][mi % 2]], writes=[k.Bps[b]])
            P.op("act", lambda e, b=b, hid=hid, fn=fn, np_=np_: e.activation(out=hid[:], in_=k.ps[b][0:np_, 0:128], func=fn),
                 reads=[k.Bps[b]], writes=[m["Bhid"]])
        def chunk_indep(m, c):
            F0, F1, F2 = m["F0"], m["F1"], m["F2"]
            BF0, BF1, BF2 = m["BF0"], m["BF1"], m["BF2"]
            sm, Bsm = m["sm"], m["Bsm"]
            cs_ = slice(c * 64, (c + 1) * 64)
            r_, k_, v_ = m["rkv"][:, c, 0, :], m["rkv"][:, c, 1, :], m["rkv"][:, c, 2, :]
            Br, Bk, Bv = m["Brkv"][c]
            rv = m["rkv"][:, c, 0, :].rearrange("p (h j) -> p h j", h=16)
            vv = m["rkv"][:, c, 2, :].rearrange("p (h j) -> p h j", h=16)
            yield
            bw = [nb8(k), nb8(k)]
            for half in range(2):
                hs = slice(half * 512, (half + 1) * 512)
                P.op("pe", lambda e, half=half, hs=hs: e.matmul(k.ps[bw[half]][0:64, :], lhsT=m["hidw"][:, cs_], rhs=m["w2"][:, hs],
                                                                start=True, stop=True), reads=[m["Bhid"], Bw], writes=[k.Bps[bw[half]]])
                P.op("dve", lambda e, half=half, hs=hs: e.tensor_tensor(out=F0[:, hs], in0=k.ps[bw[half]][0:64, :], in1=m["w0r"][:, hs],
                                                                        op=ALU.add), reads=[k.Bps[bw[half]], Bw], writes=[BF0])
                P.op("act", lambda e, half=half, hs=hs: e.activation(out=F0[:, hs], in_=F0[:, hs], func=AF.Sigmoid),
                     reads=[BF0], writes=[BF0])
            ba = [nb8(k), nb8(k)]
            for half in range(2):
                hs = slice(half * 512, (half + 1) * 512)
                P.op("pe", lambda e, half=half, hs=hs: e.matmul(k.ps[ba[half]][0:64, :], lhsT=m["hida"][:, cs_], rhs=m["a2"][:, hs],
                                                                start=True, stop=True), reads=[m["Bhid"], Bw], writes=[k.Bps[ba[half]]])
                P.op("dve", lambda e, half=half, hs=hs: e.tensor_tensor(out=F1[:, hs], in0=k.ps[ba[half]][0:64, :], in1=m["a0r"][:, hs],
                                                                        op=ALU.add), reads=[k.Bps[ba[half]], Bw], writes=[BF1])
                P.op("act", lambda e, half=half, hs=hs: e.activation(out=m["a"][:, hs], in_=F1[:, hs], func=AF.Sigmoid),
                     reads=[BF1], writes=[m["Ba"]])
            for half in range(2):
                hs = slice(half * 512, (half + 1) * 512)
                b = nb8(k)
                P.op("pe", lambda e, b=b, hs=hs: e.matmul(k.ps[b][0:64, :], lhsT=m["hidg"][:, cs_], rhs=m["g2"][:, hs],
                                                          start=True, stop=True), reads=[m["Bhid"], Bw], writes=[k.Bps[b]])
                P.op("act", lambda e, b=b, hs=hs: e.activation(out=m["gsb"][:, hs], in_=k.ps[b][0:64, :], func=AF.Copy),
                     reads=[k.Bps[b]], writes=[m["Bgsb"]])
            yield
            bcs = [nb8(k), nb8(k)]
            for half in range(2):
                hs = slice(half * 512, (half + 1) * 512)
                b = bcs[half]
                P.op("pe", lambda e, b=b, hs=hs: e.matmul(k.ps[b][0:64, :], lhsT=k.trif[0:64, 0:64], rhs=F0[:, hs],
                                                          start=True, stop=True), reads=[BF0, k.Bconst], writes=[k.Bps[b]])
                P.op("act", lambda e, b=b, hs=hs: e.activation(out=m["G"][:, hs], in_=k.ps[b][0:64, :], func=AF.Exp, scale=-C0),
                     reads=[k.Bps[b]], writes=[m["BG"]])
                P.op("act", lambda e, b=b, hs=hs: e.activation(out=m["Ginv"][:, hs], in_=k.ps[b][0:64, :], func=AF.Exp, scale=C0),
                     reads=[k.Bps[b]], writes=[m["BGinv"]])
                P.op("dve", lambda e, b=b, hs=hs: e.tensor_tensor(out=F1[:, hs], in0=k.ps[b][0:64, :], in1=F0[:, hs], op=ALU.subtract),
                     reads=[k.Bps[b], BF0], writes=[BF1])
                P.op("act", lambda e, hs=hs: e.activation(out=m["Gprev"][:, hs], in_=F1[:, hs], func=AF.Exp, scale=-C0),
                     reads=[BF1], writes=[m["BGprev"]])
            bgl = nb8(k)
            for h in range(16):
                P.op("pe", lambda e, h=h: e.matmul(k.ps[bgl][0:64, 2 * h:2 * h + 2], lhsT=F0[:, h * 64:(h + 1) * 64], rhs=k.onesf[0:64, 0:2],
                                                   start=True, stop=True), reads=[BF0, k.Bconst], writes=[k.Bps[bgl]])
            P.op("act", lambda e: e.activation(out=m["GL"][:], in_=k.ps[bgl][0:64, 0:32].rearrange("p (h two) -> p h two", two=2)[:, :, 0],
                                               func=AF.Exp, scale=-C0),
                 reads=[k.Bps[bgl]], writes=[m["BGL"]])
            yield
            P.op("dve", lambda e: e.tensor_tensor(out=F1[:], in0=k_, in1=m["kkb"][:], op=ALU.mult), reads=[Bk, Bw], writes=[BF1])
            P.op("pool", lambda e: e.tensor_tensor(out=F2[:], in0=F1[:], in1=F1[:], op=ALU.mult), reads=[BF1], writes=[BF2])
            P.op("dve", lambda e: e.tensor_reduce(out=sm[:, 0, :], in_=hv(F2), axis=AX.X, op=ALU.add), reads=[BF2], writes=[Bsm])
            P.op("pool", lambda e: e.tensor_scalar(out=sm[:, 0, :], in0=sm[:, 0, :], scalar1=1e-24, scalar2=None, op0=ALU.max),
                 reads=[Bsm], writes=[Bsm])
            P.op("pool", lambda e: e.tensor_tensor(out=sm[:, 0, :], in0=sm[:, 0, :], in1=k.mhalf[0:64, 0:1].to_broadcast([64, 16]),
                                                   op=ALU.pow), reads=[Bsm, k.Bconst], writes=[Bsm])
            P.op("dve", lambda e: e.tensor_tensor(out=hv(m["kk"]), in0=hv(F1), in1=bc(sm[:, 0, :]), op=ALU.mult),
                 reads=[BF1, Bsm], writes=[m["Bkk"]])
            yield
            P.op("dve", lambda e: e.scalar_tensor_tensor(out=F1[:], in0=m["a"][:], scalar=-1.0, in1=m["kab"][:], op0=ALU.add, op1=ALU.mult),
                 reads=[m["Ba"], Bw], writes=[BF1])
            P.op("dve", lambda e: e.scalar_tensor_tensor(out=F2[:], in0=F1[:], scalar=1.0, in1=k_, op0=ALU.add, op1=ALU.mult),
                 reads=[BF1, Bk], writes=[BF2])
            yield
            P.op("pool", lambda e: e.tensor_tensor(out=F1[:], in0=F2[:], in1=m["rkb"][:], op=ALU.mult), reads=[BF2, Bw], writes=[BF1])
            P.op("dve", lambda e: e.tensor_tensor(out=F1[:], in0=F1[:], in1=r_, op=ALU.mult), reads=[BF1, Br], writes=[BF1])
            P.op("dve", lambda e: e.tensor_reduce(out=sm[:, 1, :], in_=hv(F1), axis=AX.X, op=ALU.add), reads=[BF1], writes=[Bsm])
            yield
            P.op("dve", lambda e: e.tensor_tensor(out=m["kt"][:], in0=F2[:], in1=m["Ginv"][:], op=ALU.mult),
                 reads=[BF2, m["BGinv"]], writes=[m["Bkt"]])
            P.op("pool", lambda e: e.tensor_tensor(out=m["rt"][:], in0=r_, in1=m["G"][:], op=ALU.mult),
                 reads=[Br, m["BG"]], writes=[m["Brt"]])
            P.op("pool", lambda e: e.tensor_tensor(out=F1[:], in0=m["kk"][:], in1=m["a"][:], op=ALU.mult),
                 reads=[m["Bkk"], m["Ba"]], writes=[BF1])
            P.op("dve", lambda e: e.tensor_tensor(out=m["bt"][:], in0=F1[:], in1=m["Ginv"][:], op=ALU.mult),
                 reads=[BF1, m["BGinv"]], writes=[m["Bbt"]])
            P.op("dve", lambda e: e.scalar_tensor_tensor(out=m["at"][:], in0=m["kk"][:], scalar=-1.0, in1=m["Gprev"][:],
                                                         op0=ALU.mult, op1=ALU.mult), reads=[m["Bkk"], m["BGprev"]], writes=[m["Bat"]])
            yield
            for src, Bsrc, dst, Bdst, two in ((m["at"], m["Bat"], m["arT"], m["BarT"], 0), (m["rt"], m["Brt"], m["arT"], m["BarT"], 1),
                                              (m["bt"], m["Bbt"], m["bT"], m["BbT"], None), (m["kt"], m["Bkt"], m["kT"], m["BkT"], None)):
                b = nb8(k)
                ptb = k.ps[b][0:64, :].bitcast(BF16)
                for h in range(16):
                    P.op("pe", lambda e, h=h, src=src, ptb=ptb: e.transpose(out=ptb[:, h * 64:(h + 1) * 64], in_=src[:, h * 64:(h + 1) * 64],
                                                                             identity=k.ident[0:64, 0:64]),
                         reads=[Bsrc, k.Bconst], writes=[k.Bps[b]])
                pv = ptb.rearrange("p (h t) -> p h t", h=16)
                if two is None:
                    P.op("act", lambda e, pv=pv, dst=dst: e.activation(out=hv(dst), in_=pv, func=AF.Copy), reads=[k.Bps[b]], writes=[Bdst])
                else:
                    P.op("act", lambda e, pv=pv, dst=dst, two=two: e.activation(out=dst[:, :, two, :], in_=pv, func=AF.Copy),
                         reads=[k.Bps[b]], writes=[Bdst])
            yield
            for (lt, Blt, M, BM) in ((m["bT"], m["BbT"], m["M1"], m["BM1"]), (m["kT"], m["BkT"], m["M2"], m["BM2"])):
                for g in range(4):
                    b = nb8(k)
                    for hh in range(4):
                        h = g * 4 + hh
                        P.op("pe", lambda e, b=b, h=h, hh=hh, lt=lt: e.matmul(
                            k.ps[b][0:64, hh * 128:(hh + 1) * 128], lhsT=lt[:, h * 64:(h + 1) * 64],
                            rhs=m["arT"][:, h].rearrange("p a t -> p (a t)"), start=True, stop=True),
                            reads=[Blt, m["BarT"]], writes=[k.Bps[b]])
                    P.op("dve", lambda e, b=b, g=g, M=M: e.tensor_tensor(
                        out=M[:, g * 4:(g + 1) * 4].rearrange("p h a t -> p h (a t)"),
                        in0=k.ps[b][0:64, :].rearrange("p (h x) -> p h x", h=4),
                        in1=k.m1[:].unsqueeze(1).to_broadcast([64, 4, 128]), op=ALU.mult),
                        reads=[k.Bps[b], k.Bconst], writes=[BM])
                    if M is m["M1"]:
                        P.op("dve", lambda e, b=b, g=g: e.tensor_tensor(
                            out=hv(m["QA"])[:, g * 4:(g + 1) * 4],
                            in0=k.ps[b][0:64, :].rearrange("p (h x) -> p h x", h=4)[:, :, 0:64],
                            in1=k.m1[:, 0:64].unsqueeze(1).to_broadcast([64, 4, 64]), op=ALU.mult),
                            reads=[k.Bps[b], k.Bconst], writes=[m["BQA"]])
            for g in range(2):
                b = nb8(k)
                for hh in range(8):
                    h = g * 8 + hh
                    P.op("pe", lambda e, b=b, h=h, hh=hh: e.matmul(k.ps[b][0:64, hh * 64:(hh + 1) * 64], lhsT=m["arT"][:, h, 0, :],
                                                                   rhs=m["bT"][:, h * 64:(h + 1) * 64], start=True, stop=True),
                         reads=[m["BarT"], m["BbT"]], writes=[k.Bps[b]])
                P.op("dve", lambda e, b=b, g=g: e.tensor_tensor(
                    out=hv(m["PA"])[:, g * 8:(g + 1) * 8], in0=k.ps[b][0:64, :].rearrange("p (h x) -> p h x", h=8),
                    in1=k.sl64[:].unsqueeze(1).to_broadcast([64, 8, 64]), op=ALU.mult),
                    reads=[k.Bps[b], k.Bconst], writes=[m["BPA"]])
            yield
            TTf = hv(m["TTf"])
            P.op("dve", lambda e: e.tensor_tensor(out=TTf, in0=hv(m["QA"]),
                                                  in1=k.ident[0:64, 0:64].unsqueeze(1).to_broadcast([64, 16, 64]), op=ALU.add),
                 reads=[m["BQA"], k.Bconst], writes=[m["BTTf"]])
            yield
            Qc, BQc = hv(m["QA"]), m["BQA"]
            Pc, BPc = hv(m["PA"]), m["BPA"]
            for lv in range(5):
                Qn, BQn = (hv(m["QB"]), m["BQB"]) if lv % 2 == 0 else (hv(m["QA"]), m["BQA"])
                Pn, BPn = (hv(m["PB"]), m["BPB"]) if lv % 2 == 0 else (hv(m["PA"]), m["BPA"])
                jobs = [(Qc, BQc, Pc, BPc, Pn, BPn)]
                if lv < 4:
                    jobs.append((Pc, BPc, Qc, BQc, Qn, BQn))
                for (L, BL, R, BR, O, BO) in jobs:
                    for g in range(2):
                        b = nb8(k)
                        for hh in range(8):
                            h = g * 8 + hh
                            P.op("pe", lambda e, b=b, h=h, hh=hh, L=L, R=R: e.matmul(k.ps[b][0:64, hh * 64:(hh + 1) * 64], lhsT=L[:, h, :],
                                                                                   rhs=R[:, h, :], start=True, stop=True),
                                 reads=[BL, BR], writes=[k.Bps[b]])
                        P.op("act", lambda e, b=b, g=g, O=O: e.activation(out=O[:, g * 8:(g + 1) * 8],
                                                                          in_=k.ps[b][0:64, :].rearrange("p (h x) -> p h x", h=8),
                                                                          func=AF.Copy), reads=[k.Bps[b]], writes=[BO])
                for g in range(2):
                    b = nb8(k)
                    for hh in range(8):
                        h = g * 8 + hh
                        P.op("pe", lambda e, b=b, h=h, hh=hh, Pn=Pn: e.matmul(k.ps[b][0:64, hh * 64:(hh + 1) * 64], lhsT=Pn[:, h, :],
                                                                             rhs=TTf[:, h, :], start=True, stop=True),
                             reads=[BPn, m["BTTf"]], writes=[k.Bps[b]])
                    P.op("dve", lambda e, b=b, g=g: e.tensor_tensor(out=TTf[:, g * 8:(g + 1) * 8],
                                                                    in0=k.ps[b][0:64, :].rearrange("p (h x) -> p h x", h=8),
                                                                    in1=TTf[:, g * 8:(g + 1) * 8], op=ALU.add),
                         reads=[k.Bps[b], m["BTTf"]], writes=[m["BTTf"]])
                Qc, BQc, Pc, BPc = Qn, BQn, Pn, BPn
                yield
            P.op("act", lambda e: e.activation(out=m["TT"][:], in_=m["TTf"][:], func=AF.Copy), reads=[m["BTTf"]], writes=[m["BTT"]])
            TT = hv(m["TT"])

            vh = lambda h: m["rkv"][:, c, 2, h * 64:(h + 1) * 64]
            yield
            headmm(evac_to(m["akv"], m["Bakv"]), [(lambda h: m["M2"][:, h, 0, :], vh, [m["BM2"], Bv])])
            headmm(evac_to(m["TAT"], m["BTAT"], "dve"),
                   [(lambda h: m["at"][:, h * 64:(h + 1) * 64], lambda h: TT[:, h, :], [m["Bat"], m["BTT"]])])
            yield

        def chunk_dep(m, c):
            F0, F1, F2 = m["F0"], m["F1"], m["F2"]
            BF0, BF1, BF2 = m["BF0"], m["BF1"], m["BF2"]
            sm, Bsm = m["sm"], m["Bsm"]
            cs_ = slice(c * 64, (c + 1) * 64)
            Br, Bk, Bv = m["Brkv"][c]
            vv = m["rkv"][:, c, 2, :].rearrange("p (h j) -> p h j", h=16)
            TT = hv(m["TT"])
            vh = lambda h: m["rkv"][:, c, 2, h * 64:(h + 1) * 64]
            headmm(evac_to(m["Usb"], m["BUsb"]),
                   [(lambda h: TT[:, h, :], lambda h: hv(m["akv"])[:, h, :], [m["BTT"], m["Bakv"]]),
                    (lambda h: hv(m["TAT"])[:, h, :], lambda h: hv(m["Hbf"])[:, h, :], [m["BTAT"], m["BHbf"]])])
            headmm(evac_to(F1, BF1),
                   [(lambda h: m["arT"][:, h, 1, :], lambda h: hv(m["Hbf"])[:, h, :], [m["BarT"], m["BHbf"]]),
                    (lambda h: m["M1"][:, h, 1, :], lambda h: hv(m["Usb"])[:, h, :], [m["BM1"], m["BUsb"]]),
                    (lambda h: m["M2"][:, h, 1, :], vh, [m["BM2"], Bv])])

            def evac_H(b, g):
                gs = slice(g * 8, (g + 1) * 8)
                P.op("dve", lambda e: e.tensor_tensor(out=hv(H)[:, gs], in0=k.ps[b][0:64, :].rearrange("p (h x) -> p h x", h=8),
                                                      in1=hv(H)[:, gs], op=ALU.add), reads=[k.Bps[b], BH], writes=[BH])
                P.op("dve", lambda e: e.tensor_tensor(out=hv(H)[:, gs], in0=hv(H)[:, gs],
                                                      in1=m["GL"][:, gs].unsqueeze(2).to_broadcast([64, 8, 64]), op=ALU.mult),
                     reads=[BH, m["BGL"]], writes=[BH])
            headmm(evac_H,
                   [(lambda h: m["bt"][:, h * 64:(h + 1) * 64], lambda h: hv(m["Usb"])[:, h, :], [m["Bbt"], m["BUsb"]]),
                    (lambda h: m["kt"][:, h * 64:(h + 1) * 64], vh, [m["Bkt"], Bv])])
            P.op("act", lambda e: e.activation(out=m["Hbf"][:], in_=H[:], func=AF.Copy), reads=[BH], writes=[m["BHbf"]])
            P.op("dve", lambda e: e.tensor_reduce(out=sm[:, 2, :], in_=hv(F1), axis=AX.X, op=ALU.add), reads=[BF1], writes=[Bsm])
            P.op("pool", lambda e: e.tensor_tensor(out=F2[:], in0=F1[:], in1=F1[:], op=ALU.mult), reads=[BF1], writes=[BF2])
            P.op("dve", lambda e: e.tensor_reduce(out=sm[:, 3, :], in_=hv(F2), axis=AX.X, op=ALU.add), reads=[BF2], writes=[Bsm])
            P.op("dve", lambda e: e.tensor_scalar(out=sm[:, 2, :], in0=sm[:, 2, :], scalar1=1.0 / 64.0, scalar2=None, op0=ALU.mult),
                 reads=[Bsm], writes=[Bsm])
            P.op("dve", lambda e: e.tensor_tensor(out=sm[:, 4, :], in0=sm[:, 2, :], in1=sm[:, 2, :], op=ALU.mult), reads=[Bsm], writes=[Bsm])
            P.op("dve", lambda e: e.scalar_tensor_tensor(out=sm[:, 3, :], in0=sm[:, 3, :], scalar=1.0 / 64.0, in1=sm[:, 4, :],
                                                         op0=ALU.mult, op1=ALU.subtract), reads=[Bsm], writes=[Bsm])
            P.op("pool", lambda e: e.tensor_scalar(out=sm[:, 3, :], in0=sm[:, 3, :], scalar1=64e-5, scalar2=None, op0=ALU.add),
                 reads=[Bsm], writes=[Bsm])
            P.op("pool", lambda e: e.tensor_tensor(out=sm[:, 3, :], in0=sm[:, 3, :], in1=k.mhalf[0:64, 0:1].to_broadcast([64, 16]),
                                                   op=ALU.pow), reads=[Bsm, k.Bconst], writes=[Bsm])
            P.op("dve", lambda e: e.tensor_tensor(out=hv(F1), in0=hv(F1), in1=bc(sm[:, 2, :]), op=ALU.subtract), reads=[BF1, Bsm], writes=[BF1])
            P.op("dve", lambda e: e.tensor_tensor(out=hv(F1), in0=hv(F1), in1=bc(sm[:, 3, :]), op=ALU.mult), reads=[BF1, Bsm], writes=[BF1])
            P.op("pool", lambda e: e.tensor_tensor(out=F1[:], in0=F1[:], in1=m["lgb"][:], op=ALU.mult), reads=[BF1, Bw], writes=[BF1])
            P.op("pool", lambda e: e.tensor_tensor(out=F1[:], in0=F1[:], in1=m["lbb"][:], op=ALU.add), reads=[BF1, Bw], writes=[BF1])
            P.op("dve", lambda e: e.tensor_tensor(out=hv(F2), in0=vv, in1=bc(sm[:, 1, :]), op=ALU.mult), reads=[Bv, Bsm], writes=[BF2])
            P.op("dve", lambda e: e.tensor_tensor(out=F1[:], in0=F1[:], in1=F2[:], op=ALU.add), reads=[BF1, BF2], writes=[BF1])
            P.op("dve", lambda e: e.tensor_tensor(out=m["zb"][:], in0=F1[:], in1=m["gsb"][:], op=ALU.mult),
                 reads=[BF1, m["Bgsb"]], writes=[m["Bzb"]])
            b = nb8(k)
            ptb = k.ps[b][:].bitcast(BF16)
            for kk in range(8):
                P.op("pe", lambda e, kk=kk, ptb=ptb: e.transpose(out=ptb[:, kk * 64:(kk + 1) * 64], in_=m["zb"][:, kk * 128:(kk + 1) * 128],
                                                                 identity=k.ident[0:64, 0:64]),
                     reads=[m["Bzb"], k.Bconst], writes=[k.Bps[b]])
            P.op("act", lambda e, ptb=ptb: e.activation(out=hT[:, :, cs_], in_=ptb[:, 0:512].rearrange("p (a t) -> p a t", a=8), func=AF.Copy),
                 reads=[k.Bps[b]], writes=[m["BhT"]])

        gens = [chunk_indep(cxs[0], 0), chunk_indep(cxs[1], 1)]
        while gens:
            for g_ in list(gens):
                try:
                    next(g_)
                except StopIteration:
                    gens.remove(g_)
        chunk_dep(cxs[0], 0)
        chunk_dep(cxs[1], 1)
        bo = [nb8(k), nb8(k)]
        for u in range(4):
            sl = ring_load(k, m, 12 + u)
            for c2 in range(2):
                kk = 2 * u + c2
                for half in range(2):
                    P.op("pe", lambda e, sl=sl, c2=c2, kk=kk, half=half: e.matmul(
                        k.ps[bo[half]][:], lhsT=hT[:, kk, :], rhs=m["ring"][:, sl, c2 * 1024 + half * 512:c2 * 1024 + (half + 1) * 512],
                        start=(kk == 0), stop=(kk == 7)), reads=[m["BhT"], m["Bring"][sl]], writes=[k.Bps[bo[half]]])
        postnorm_residual(k, xs_, bo, m["gpost"], m["Bgpost"], 1.0, sc)
        P.dma("sp", lambda e: e.dma_start(out=k.scrX[gt], in_=k.xres[:, xs_, :]), reads=[k.Bx[xs_]], writes=[k.Bscr[gt]],
              dkey=f"x{xs_}")


def rwkv_phase(k, layer, j):
    P = k.P
    if not hasattr(k, "scrX"):
        k.scrX = k.nc.dram_tensor("scrX", [NT, 128, D], F32, kind="Internal").ap()
        k.Bscr = [Buf(f"scr{i}") for i in range(NT)]
    P.barrier()
    for i in range(0, NT, 2):
        P.dma("sp", lambda e: e.dma_start(out=k.scrX[i:i + 2].rearrange("t p d -> p t d"), in_=k.xres[:, i:i + 2, :]),
              reads=[k.Bx[i], k.Bx[i + 1]], writes=[k.Bscr[i], k.Bscr[i + 1]], dkey=f"x{i // 2}")
    P.barrier()
    m = rwkv_setup(k, layer)
    P.dma("sp", lambda e: e.dma_start(out=m["gpost"][:], in_=k.ng_d[layer * 6 + 3, :].partition_broadcast(128)),
          writes=[m["Bgpost"]], dkey="gpost")
    rwkv_seq(k, m, layer, 0)
    rwkv_seq(k, m, layer, 1)
    P.barrier()
    for i in range(0, NT, 2):
        P.dma("sp", lambda e: e.dma_start(out=k.xres[:, i:i + 2, :], in_=k.scrX[i:i + 2].rearrange("t p d -> p t d")),
              reads=[k.Bscr[i], k.Bscr[i + 1]], writes=[k.Bx[i], k.Bx[i + 1]], dkey=f"x{i // 2}")
    P.barrier()


def _units_lin(w_in, n_f, tm0, n_tm, w_out):
    wf = w_in[:, :n_f * 128].reshape(8, 128, n_f, 128).transpose(2, 1, 0, 3)
    wf = wf.reshape(n_f // 2, 2, 128, 1024).transpose(0, 2, 1, 3).reshape(n_f // 2, 128, 2048)
    wt = w_in[:, tm0:tm0 + n_tm * 512].reshape(2, 4, 128, n_tm, 512).transpose(3, 0, 2, 1, 4)
    wt = wt.reshape(n_tm * 2, 128, 2048)
    nko = w_out.shape[0] // 128
    wo = w_out.reshape(nko // 2, 2, 128, 1024).transpose(0, 2, 1, 3).reshape(nko // 2, 128, 2048)
    return np.ascontiguousarray(np.concatenate([wf, wt, wo], axis=0), dtype=np.float32)


def _prep_inputs(inputs, x_override=None):
    xin = inputs["x"] if x_override is None else x_override
    x = np.ascontiguousarray(xin, dtype=np.float32).reshape(NCORES, NT, 128, D)
    wgu = inputs["ffn_w_gu"].reshape(8, 8, 128, 2, NJ, 128)
    wgu = np.ascontiguousarray(wgu.transpose(0, 4, 2, 3, 1, 5)).reshape(8 * NJ, 128, 2, 1024)
    wd = np.ascontiguousarray(inputs["ffn_w_down"]).reshape(8 * NJ, 128, 1024)
    ng = np.ascontiguousarray(inputs["norm_g"]).reshape(24, D)
    ngT = np.ascontiguousarray(ng.reshape(24, 8, 128).transpose(2, 0, 1)).reshape(128, 24 * 8)
    shared = {"wgu": wgu, "wd": wd, "ng": ng, "ngT": ngT}
    for j, layer in ((0, 0), (1, 3)):
        w_in = inputs["ml_w_in"][j]
        shared[f"wm{layer}"] = _units_lin(w_in, 8, 1024, 4, inputs["ml_w_out"][j])
        shared[f"wif{layer}"] = np.ascontiguousarray(w_in[:, 3072:3080].reshape(8, 128, 8).transpose(1, 0, 2)).reshape(128, 64)
        shared[f"bif{layer}"] = np.ascontiguousarray(inputs["ml_b_if"][j]).reshape(1, 8)
        shared[f"convT{layer}"] = np.ascontiguousarray(
            inputs["ml_conv_w"][j].reshape(4, 8, 128).transpose(2, 1, 0)).reshape(128, 32)
        shared[f"mng{layer}"] = np.ascontiguousarray(inputs["ml_norm_g"][j]).reshape(1, 1024)
    shared["wm2"] = _units_lin(inputs["rt_w_in"][0], 16, 2048, 8, inputs["rt_w_out"][0])
    wtm = lambda w: np.ascontiguousarray(w.reshape(2, 4, 128, 2, 512).transpose(3, 0, 2, 1, 4)).reshape(4, 128, 2048)
    wo = inputs["rw_w_out"][0].reshape(4, 2, 128, 1024).transpose(0, 2, 1, 3).reshape(4, 128, 2048)
    shared["rw_units"] = np.ascontiguousarray(np.concatenate(
        [wtm(inputs["rw_w_rkv"][0, 0]), wtm(inputs["rw_w_rkv"][0, 1]), wtm(inputs["rw_w_rkv"][0, 2]), wo], axis=0), dtype=np.float32)
    for nm in ("w1", "a1", "g1"):
        w = inputs["rw_" + nm][0]
        shared["rw_" + nm] = np.ascontiguousarray(w.reshape(8, 128, w.shape[1]).transpose(1, 0, 2))
    for nm in ("w2", "a2", "g2"):
        shared["rw_" + nm] = np.ascontiguousarray(inputs["rw_" + nm][0])
    shared["rw_w0r"] = np.ascontiguousarray(inputs["rw_w0"][0]).reshape(1, 1024)
    shared["rw_a0r"] = np.ascontiguousarray(inputs["rw_a0"][0]).reshape(1, 1024)
    shared["rw_muT"] = np.ascontiguousarray(inputs["rw_mu"][0].reshape(6, 8, 128).transpose(2, 0, 1)).reshape(128, 48)
    for nm, src in (("kkb", "rw_k_k"), ("kab", "rw_k_a"), ("lgb", "rw_ln_g"), ("lbb", "rw_ln_b"), ("rkb", "rw_r_k")):
        shared["rw_" + nm] = np.ascontiguousarray(inputs[src][0]).reshape(1, 1024)
    pos = np.ascontiguousarray(inputs["positions"]).astype(np.int32).reshape(NCORES, 1, 2 * SEQ)
    return [dict(shared, x=x[c], pos=pos[c]) for c in range(NCORES)]


def run_partial(inputs, subs=tuple(range(12)), trace=False, cores=NCORES, x_override=None):
    nc, names = build_program(tuple(subs))
    in_maps = _prep_inputs(inputs, x_override)
    in_maps = [{n: m[n] for n in names if n in m} for m in in_maps[:cores]]
    res = run_bass_kernel_spmd(nc, in_maps, core_ids=list(range(cores)), trace=trace)
    out = np.stack([np.asarray(r["out"]) for r in res.results], axis=0)
    return out.reshape(2 * cores, SEQ, D).astype(np.float32), res


def kernel(**inputs):
    out, _ = run_partial(inputs)
    return out
```

```python
import numpy as np
import concourse.bass as bass
import concourse.mybir as mybir
from concourse.bass_utils import run_bass_kernel_spmd

F32 = mybir.dt.float32
BF16 = mybir.dt.bfloat16
I32 = mybir.dt.int32
ALU = mybir.AluOpType
AF = mybir.ActivationFunctionType
AX = mybir.AxisListType

NCORES = 8
D = 1024
DFF = 2816
NJ = DFF // 128
TPC = 4096
NT = TPC // 128
SEQ = 2048
EPS = 1e-6

ENGS = ["pe", "dve", "act", "pool", "sp"]
CHUNK = 8000
SAME_ENG_SYNC = True


class Buf:
    __slots__ = ("name", "lw", "rd", "excl")

    def __init__(self, name, excl=False):
        self.name = name
        self.lw = None
        self.rd = {}
        self.excl = excl


def _freeze(fn):
    import types
    if fn is None or fn.__closure__ is None:
        return fn
    cells = []
    for c in fn.__closure__:
        try:
            cells.append(types.CellType(c.cell_contents))
        except ValueError:
            cells.append(c)
    return types.FunctionType(fn.__code__, fn.__globals__, fn.__name__, fn.__defaults__, tuple(cells))


class Prog:
    def __init__(self, nc):
        self.nc = nc
        self.ops = {e: [] for e in ENGS}
        self.cnt = {e: 0 for e in ENGS}
        self.seen = {e: {} for e in ENGS}
        self.dcnt = {}

    def _deps(self, eng, reads, writes):
        need = {}

        def add(k, v):
            if need.get(k, 0) < v:
                need[k] = v

        for b in reads:
            if b.lw is not None:
                add(*b.lw)
            if b.excl:
                for k, v in b.rd.items():
                    if k != ("e", eng):
                        add(k, v)
        for b in writes:
            if b.lw is not None:
                add(*b.lw)
            for k, v in b.rd.items():
                add(k, v)
        s = self.seen[eng]
        waits = []
        for k, v in need.items():
            if k == ("e", eng) and (eng == "pe" or not SAME_ENG_SYNC):
                continue
            if s.get(k, 0) >= v:
                continue
            s[k] = v
            waits.append((k, v))
        return waits

    def _mark(self, tok, reads, writes):
        k, v = tok
        for b in reads:
            if b.rd.get(k, 0) < v:
                b.rd[k] = v
        for b in writes:
            b.lw = tok
            b.rd = {}

    def op(self, eng, fn, reads=(), writes=()):
        waits = self._deps(eng, reads, writes)
        self.cnt[eng] += 1
        tok = (("e", eng), self.cnt[eng])
        self.ops[eng].append((_freeze(fn), waits, tok))
        self._mark(tok, reads, writes)

    def dma(self, eng, fn, reads=(), writes=(), dkey=None):
        waits = self._deps(eng, reads, writes)
        n = self.dcnt.get(dkey, 0) + 16
        self.dcnt[dkey] = n
        tok = (("d", dkey), n)
        self.ops[eng].append((_freeze(fn), waits, tok))
        self._mark(tok, reads, writes)

    def barrier(self):
        for e in ENGS:
            s = self.seen[e]
            waits = []
            for o in ENGS:
                if o == e or self.cnt[o] == 0:
                    continue
                k = ("e", o)
                if s.get(k, 0) < self.cnt[o]:
                    s[k] = self.cnt[o]
                    waits.append((k, self.cnt[o]))
            for dk, n in self.dcnt.items():
                k = ("d", dk)
                if s.get(k, 0) < n:
                    s[k] = n
                    waits.append((k, n))
            if waits:
                self.ops[e].append((None, waits, None))

    def run(self, final_bufs=()):
        nc = self.nc
        from contextlib import ExitStack
        with ExitStack() as st:
            sems = {}
            for e in ENGS:
                nchunks = max(1, (self.cnt[e] + CHUNK - 1) // CHUNK)
                for c in range(nchunks):
                    sems[(("e", e), c)] = st.enter_context(nc.semaphore(f"s_{e}_{c}"))
            for dk in self.dcnt:
                sems[(("d", dk), 0)] = st.enter_context(nc.semaphore(f"d_{dk}"))
            block = st.enter_context(nc.Block())

            def semval(k, v):
                if k[0] == "e":
                    c = (v - 1) // CHUNK
                    return sems[(k, c)], v - c * CHUNK
                return sems[(k, 0)], v

            def emit(ename, eng):
                for fn, waits, tok in self.ops[ename]:
                    for (k, v) in waits:
                        s, vv = semval(k, v)
                        eng.wait_ge(s, vv)
                    if fn is None:
                        continue
                    ins = fn(eng)
                    k, v = tok
                    if k[0] == "e":
                        s, vv = semval(k, v)
                        ins.then_inc(s, 1)
                    else:
                        ins.then_inc(sems[(k, 0)], 16)
                if ename == "sp":
                    need = {}
                    for b in final_bufs:
                        toks = list(b.rd.items())
                        if b.lw is not None:
                            toks.append(b.lw)
                        for k, v in toks:
                            need[k] = max(need.get(k, 0), v)
                    for k, v in need.items():
                        s, vv = semval(k, v)
                        eng.wait_ge(s, vv)

            @block.tensor
            def _(e):
                emit("pe", e)

            @block.vector
            def _(e):
                emit("dve", e)

            @block.scalar
            def _(e):
                emit("act", e)

            @block.gpsimd
            def _(e):
                emit("pool", e)

            @block.sync
            def _(e):
                emit("sp", e)


class Arena:
    def __init__(self, nc, base, limit):
        self.nc, self.base, self.limit = nc, base, limit
        self.off = base
        self.gen = 0

    def reset(self):
        self.off = self.base
        self.gen += 1

    def alloc(self, name, shape, dtype):
        nbytes = int(np.prod(shape[1:])) * (4 if dtype in (F32, I32) else 2)
        nbytes = (nbytes + 63) // 64 * 64
        assert self.off + nbytes <= self.limit, (name, self.off, nbytes, self.limit)
        t = self.nc.alloc_sbuf_tensor_at(f"{name}_g{self.gen}", list(shape), dtype, offset=self.off)
        self.off += nbytes
        return t


class K:
    pass


def build_program(subs=tuple(range(12))):
    nc = bass.Bass("TRN2", target_bir_lowering=False)
    P = Prog(nc)
    k = K()
    k.nc, k.P = nc, P
    k.dram_names = []

    def dram(name, shape, dtype=F32, kind="ExternalInput"):
        k.dram_names.append(name)
        return nc.dram_tensor(name, list(shape), dtype, kind=kind).ap()

    k.dram = dram
    k.x_d = dram("x", [NT, 128, D])
    k.out_d = dram("out", [NT, 128, D], kind="ExternalOutput")
    k.ng_d = dram("ng", [24, D])
    k.ngT_d = dram("ngT", [128, 24 * 8])
    if any(s % 3 != 1 for s in subs):
        k.wgu_d = dram("wgu", [8 * NJ, 128, 2, 1024])
        k.wd_d = dram("wd", [8 * NJ, 128, 1024])

    RES_BYTES = NT * D * 4
    SB0 = 16640
    k.xres = nc.alloc_sbuf_tensor_at("xres", [128, NT, D], F32, offset=SB0 + 4096)
    k.Bx = [Buf(f"x{i}") for i in range(NT)]
    pers = Arena(nc, SB0, SB0 + 4096)
    k.big_arena = Arena(nc, SB0 + 4096 + 2 * D * 4, 229376)
    k.ident = pers.alloc("ident", [128, 128], BF16)
    k.ngT = pers.alloc("ngT", [128, 24 * 8], F32)
    k.ones = pers.alloc("ones", [128, 128], BF16)
    k.mhalf = pers.alloc("mhalf", [128, 1], F32)
    k.tri = pers.alloc("tri", [128, 128], BF16)
    k.trif = pers.alloc("trif", [128, 128], F32)
    k.onesf = pers.alloc("onesf", [128, 128], F32)
    k.sl64 = pers.alloc("sl64", [64, 64], BF16)
    k.m1 = pers.alloc("m1", [64, 128], BF16)
    k.Bconst = Buf("const")
    k.arena = Arena(nc, SB0 + RES_BYTES + 4096, 229376)
    k.ps = [nc.alloc_psum_tensor(f"ps{i}", [128, 512], F32) for i in range(8)]
    k.Bps = [Buf(f"ps{i}", excl=True) for i in range(8)]
    k.rot = 0

    P.op("pool", lambda e: e.memset(k.ones[:], 1.0), writes=[k.Bconst])
    P.op("pool", lambda e: e.memset(k.onesf[:], 1.0), writes=[k.Bconst])
    P.op("pool", lambda e: e.affine_select(out=k.ident[:], in_=k.ones[:], pattern=[[-1, 128]],
                                           compare_op=ALU.is_equal, fill=0.0, base=0, channel_multiplier=1),
         reads=[k.Bconst], writes=[k.Bconst])
    for tt in (k.tri, k.trif):
        P.op("pool", lambda e, tt=tt: e.affine_select(out=tt[:], in_=(k.ones if tt is k.tri else k.onesf)[:],
                                                      pattern=[[1, 128]], compare_op=ALU.is_ge, fill=0.0, base=0,
                                                      channel_multiplier=-1),
             reads=[k.Bconst], writes=[k.Bconst])
    P.op("pool", lambda e: e.memset(k.mhalf[:], -0.5), writes=[k.Bconst])
    P.op("pool", lambda e: e.affine_select(out=k.sl64[:], in_=k.ones[0:64, 0:64], pattern=[[-1, 64]],
                                           compare_op=ALU.is_gt, fill=0.0, base=0, channel_multiplier=1),
         reads=[k.Bconst], writes=[k.Bconst])
    P.op("pool", lambda e: e.affine_select(out=k.m1[:, 0:64], in_=k.ones[0:64, 0:64], pattern=[[1, 64]],
                                           compare_op=ALU.is_gt, fill=0.0, base=0, channel_multiplier=-1),
         reads=[k.Bconst], writes=[k.Bconst])
    P.op("pool", lambda e: e.affine_select(out=k.m1[:, 64:128], in_=k.ones[0:64, 0:64], pattern=[[1, 64]],
                                           compare_op=ALU.is_ge, fill=0.0, base=0, channel_multiplier=-1),
         reads=[k.Bconst], writes=[k.Bconst])
    P.dma("sp", lambda e: e.dma_start(out=k.ngT[:], in_=k.ngT_d[:, :]), writes=[k.Bconst], dkey="ngT")

    for i in range(0, NT, 2):
        P.dma("sp", lambda e, i=i: e.dma_start(out=k.xres[:, i:i + 2, :],
                                                in_=k.x_d[i:i + 2].rearrange("t p d -> p t d")),
              writes=[k.Bx[i], k.Bx[i + 1]], dkey=f"x{i // 2}")

    for sub in subs:
        layer, s3 = sub // 3, sub % 3
        if s3 == 0:
            ffn_phase(k, layer, 0)
        elif s3 == 2:
            ffn_phase(k, layer, 1)
        elif layer % 3 == 0:
            lin_mixer_phase(k, layer, 0, layer // 3)
        elif layer % 3 == 2:
            lin_mixer_phase(k, layer, 2, layer // 3)
        else:
            rwkv_phase(k, layer, layer // 3)

    for i in range(0, 16 if RW_DEBUG else NT, 2):
        P.dma("sp", lambda e, i=i: e.dma_start(out=k.out_d[i:i + 2].rearrange("t p d -> p t d"),
                                                in_=k.xres[:, i:i + 2, :]),
              reads=[k.Bx[i], k.Bx[i + 1]], dkey=f"x{i // 2}")
    P.run(final_bufs=k.Bx)
    return nc, k.dram_names


def rstd_from_ssq(k, ssq_ap, rstd_ap, Bs, Br, n_part=128):
    P = k.P
    P.op("pool", lambda e: e.tensor_scalar(out=rstd_ap, in0=ssq_ap, scalar1=1.0 / D, scalar2=EPS,
                                           op0=ALU.mult, op1=ALU.add), reads=[Bs], writes=[Br])
    P.op("pool", lambda e: e.tensor_tensor(out=rstd_ap, in0=rstd_ap, in1=k.mhalf[0:n_part, :], op=ALU.pow),
         reads=[Br, k.Bconst], writes=[Br])


def prenorm_tile(k, ti, gidx, xnT, BxnT, col, sc):
    P = k.P
    par = ti % 2
    junk, Bjunk = sc["junk"], sc["Bjunk"]
    xs, Bxs = sc["xs"][par], sc["Bxs"][par]
    ssq, Bssq = sc["ssq"][par], sc["Bssq"][par]
    rstd, Brstd = sc["rstd"][par], sc["Brstd"][par]
    psb = sc["ps_tr"][par]
    P.op("act", lambda e: e.activation(out=junk[:], in_=k.xres[:, ti, :], func=AF.Square, accum_out=ssq[:]),
         reads=[k.Bx[ti]], writes=[Bjunk, Bssq])
    rstd_from_ssq(k, ssq[:], rstd[:], Bssq, Brstd)
    P.op("act", lambda e: e.activation(out=xs[:], in_=k.xres[:, ti, :], func=AF.Copy, scale=rstd[:]),
         reads=[k.Bx[ti], Brstd], writes=[Bxs])
    pst = k.ps[psb][:].bitcast(BF16)
    for kk in range(8):
        P.op("pe", lambda e, kk=kk: e.transpose(out=pst[:, kk * 128:(kk + 1) * 128],
                                                in_=xs[:, kk * 128:(kk + 1) * 128], identity=k.ident[:]),
             reads=[Bxs, k.Bconst], writes=[k.Bps[psb]])
    gT = k.ngT[:, gidx * 8:(gidx + 1) * 8].unsqueeze(2).to_broadcast([128, 8, 128])
    P.op("dve", lambda e: e.tensor_tensor(out=xnT[:, :, col:col + 128],
                                          in0=pst.rearrange("p (a t) -> p a t", a=8), in1=gT, op=ALU.mult),
         reads=[k.Bps[psb], k.Bconst], writes=[BxnT])


def postnorm_residual(k, ti, halves, gpost, Bgpost, coef, sc):
    P = k.P
    par = ti % 2
    junk, Bjunk = sc["junk"], sc["Bjunk"]
    ss2, Bss2 = sc["ss2"][par], sc["Bss2"][par]
    rs2, Brs2 = sc["rs2"][par], sc["Brs2"][par]
    tmp, Btmp = sc["tmp"][par], sc["Btmp"][par]
    for h, pb in enumerate(halves):
        P.op("act", lambda e, h=h, pb=pb: e.activation(out=junk[:, 0:512], in_=k.ps[pb][:], func=AF.Square,
                                                       accum_out=ss2[:, h:h + 1]),
             reads=[k.Bps[pb]], writes=[Bjunk, Bss2])
    P.op("dve", lambda e: e.tensor_tensor(out=ss2[:, 2:3], in0=ss2[:, 0:1], in1=ss2[:, 1:2], op=ALU.add),
         reads=[Bss2], writes=[Bss2])
    rstd_from_ssq(k, ss2[:, 2:3], rs2[:], Bss2, Brs2)
    for h, pb in enumerate(halves):
        P.op("dve", lambda e, h=h, pb=pb: e.scalar_tensor_tensor(
            out=tmp[:, h * 512:(h + 1) * 512], in0=k.ps[pb][:], scalar=rs2[:], in1=gpost[:, h * 512:(h + 1) * 512],
            op0=ALU.mult, op1=ALU.mult), reads=[k.Bps[pb], Brs2, Bgpost], writes=[Btmp])
    P.op("dve", lambda e: e.scalar_tensor_tensor(out=k.xres[:, ti, :], in0=tmp[:], scalar=float(coef),
                                                 in1=k.xres[:, ti, :], op0=ALU.mult, op1=ALU.add),
         reads=[Btmp, k.Bx[ti]], writes=[k.Bx[ti]])


def common_scratch(k, A, ntmp=2):
    sc = {}
    sc["junk"] = A.alloc("junk", [128, 1024], BF16)
    sc["Bjunk"] = Buf("junk")
    for nm, shape, dtype in [("xs", [128, 1024], BF16), ("ssq", [128, 1], F32), ("rstd", [128, 1], F32),
                             ("ss2", [128, 4], F32), ("rs2", [128, 1], F32), ("tmp", [128, 1024], F32)]:
        n = ntmp if nm in ("tmp", "xs") else 2
        ts = [A.alloc(f"{nm}{i}", shape, dtype) for i in range(n)]
        bs = [Buf(f"{nm}{i}") for i in range(n)]
        sc[nm] = [ts[i % n] for i in range(2)]
        sc["B" + nm] = [bs[i % n] for i in range(2)]
    return sc


NWGU, NWD = 5, 4
CASTW = True


def ffn_setup(k):
    if getattr(k, "ffn", None) is not None and k.ffn["gen"] == k.arena.gen:
        return k.ffn
    k.P.barrier()
    A = k.arena
    A.reset()
    f = {"gen": A.gen}
    f["sc"] = common_scratch(k, A, ntmp=1)
    f["sc"]["ps_tr"] = [4, 5]
    f["xnT"] = A.alloc("xnT", [128, 8, 512], BF16)
    f["BxnT"] = Buf("xnT")
    f["actT"] = A.alloc("actT", [128, NJ, 512], BF16)
    f["BactT"] = [Buf(f"actT{j}") for j in range(NJ)]
    f["wgu"] = A.alloc("wgu", [128, NWGU, 2, 1024], BF16)
    f["Bwgu"] = [Buf(f"wgu{i}") for i in range(NWGU)]
    f["wd"] = A.alloc("wd", [128, NWD, 1024], BF16)
    f["Bwd"] = [Buf(f"wd{i}") for i in range(NWD)]
    f["gpost"] = A.alloc("gpost", [128, 1024], F32)
    f["Bgpost"] = Buf("gpost")
    f["sg"] = [A.alloc(f"sg{i}", [128, 512], F32) for i in range(2)]
    f["Bsg"] = [Buf(f"sg{i}") for i in range(2)]
    f["wq"] = 0
    f["dq"] = 0
    k.ffn = f
    return f


def ffn_phase(k, layer, which):
    P = k.P
    f = ffn_setup(k)
    sc = f["sc"]
    fidx = layer * 2 + which
    g_pre = layer * 6 + (0 if which == 0 else 4)
    g_post = g_pre + 1
    xnT, BxnT, actT, BactT = f["xnT"], f["BxnT"], f["actT"], f["BactT"]
    P.dma("sp", lambda e: e.dma_start(out=f["gpost"][:], in_=k.ng_d[g_post, :].partition_broadcast(128)),
          writes=[f["Bgpost"]], dkey="gpost")
    NST = NT // 4
    if CASTW and not hasattr(k, "wgu_b"):
        k.wgu_b = k.nc.dram_tensor("wgu_b", [8 * NJ, 128, 2, 1024], BF16, kind="Internal").ap()
        k.wd_b = k.nc.dram_tensor("wd_b", [8 * NJ, 128, 1024], BF16, kind="Internal").ap()
        k.Bcast = [Buf(f"cast{i}") for i in range(8)]
        k.cast_done = set()
    use_bf = CASTW and fidx in k.cast_done
    nxt = fidx + 1
    casts = []
    if CASTW and which == 0 and layer % 3 != 1:
        mw = mixer_units(k, layer)
        if not mw["cast"]:
            for u in range(mw["n"]):
                casts.append((mw["dst"][u], mw["src"][u], mw["B"], f"cwm{layer}"))
            mw["cast"] = True
    if CASTW and nxt < 8 and nxt not in k.cast_done:
        for j in range(NJ):
            casts.append((k.wgu_b[nxt * NJ + j], k.wgu_d[nxt * NJ + j], k.Bcast[nxt], f"cw{nxt}"))
            casts.append((k.wd_b[nxt * NJ + j], k.wd_d[nxt * NJ + j], k.Bcast[nxt], f"cw{nxt}"))
        k.cast_done.add(nxt)

    def emit_casts(n):
        for _ in range(n):
            if casts:
                dst, src, Bc, key = casts.pop(0)
                P.dma("pool", lambda e, dst=dst, src=src: e.dma_start(out=dst, in_=src), writes=[Bc], dkey=key)

    def prenorm_st(st):
        for t in range(4):
            prenorm_tile(k, st * 4 + t, g_pre, xnT, BxnT, t * 128, sc)

    prenorm_st(0)
    for st in range(NST):
        for j in range(NJ):
            slot = f["wq"] % NWGU
            f["wq"] += 1
            if use_bf:
                P.dma("sp", lambda e, j=j, slot=slot: e.dma_start(out=f["wgu"][:, slot], in_=k.wgu_b[fidx * NJ + j]),
                      reads=[k.Bcast[fidx]], writes=[f["Bwgu"][slot]], dkey=f"wgub{slot}")
            else:
                P.dma("pool", lambda e, j=j, slot=slot: e.dma_start(out=f["wgu"][:, slot], in_=k.wgu_d[fidx * NJ + j]),
                      writes=[f["Bwgu"][slot]], dkey=f"wgu{slot}")
            if j % 3 == 0:
                emit_casts(1)
            pg, pu = (0, 1) if j % 2 == 0 else (2, 3)
            for half, pb in ((0, pg), (1, pu)):
                for kk in range(8):
                    P.op("pe", lambda e, pb=pb, slot=slot, half=half, kk=kk: e.matmul(
                        k.ps[pb][:], lhsT=f["wgu"][:, slot, half, kk * 128:(kk + 1) * 128], rhs=xnT[:, kk, :],
                        start=(kk == 0), stop=(kk == 7)),
                        reads=[f["Bwgu"][slot], BxnT], writes=[k.Bps[pb]])
            sg, Bsg = f["sg"][j % 2], f["Bsg"][j % 2]
            P.op("act", lambda e, pg=pg, sg=sg: e.activation(out=sg[:], in_=k.ps[pg][:], func=AF.Silu),
                 reads=[k.Bps[pg]], writes=[Bsg])
            P.op("dve", lambda e, pu=pu, sg=sg, j=j: e.tensor_tensor(out=actT[:, j, :], in0=sg[:], in1=k.ps[pu][:],
                                                                     op=ALU.mult),
                 reads=[Bsg, k.Bps[pu]], writes=[BactT[j]])
        if st + 1 < NST:
            prenorm_st(st + 1)
        for j in range(NJ):
            slot = f["dq"] % NWD
            f["dq"] += 1
            if use_bf:
                P.dma("sp", lambda e, j=j, slot=slot: e.dma_start(out=f["wd"][:, slot, :], in_=k.wd_b[fidx * NJ + j]),
                      reads=[k.Bcast[fidx]], writes=[f["Bwd"][slot]], dkey=f"wdb{slot}")
            else:
                P.dma("pool", lambda e, j=j, slot=slot: e.dma_start(out=f["wd"][:, slot, :], in_=k.wd_d[fidx * NJ + j]),
                      writes=[f["Bwd"][slot]], dkey=f"wd{slot}")
            if j % 8 == 0:
                emit_casts(1)
            for tt in range(4):
                for half in range(2):
                    pb = tt * 2 + half
                    P.op("pe", lambda e, pb=pb, slot=slot, half=half, tt=tt, j=j: e.matmul(
                        k.ps[pb][:], lhsT=actT[:, j, tt * 128:(tt + 1) * 128],
                        rhs=f["wd"][:, slot, half * 512:(half + 1) * 512], start=(j == 0), stop=(j == NJ - 1)),
                        reads=[BactT[j], f["Bwd"][slot]], writes=[k.Bps[pb]])
        for tt in range(4):
            postnorm_residual(k, st * 4 + tt, [tt * 2, tt * 2 + 1], f["gpost"], f["Bgpost"], 0.5, sc)


import math
LN_S = {0: math.log(128 ** -0.5), 2: math.log(256 ** -0.5)}


def mixer_units(k, layer):
    if not hasattr(k, "mixw"):
        k.mixw = {}
    if layer not in k.mixw:
        kind = 0 if layer % 3 == 0 else 2
        n = (8 if kind == 0 else 16) // 2 + 2 * (4 if kind == 0 else 8) + ((4 * (256 if kind == 0 else 512)) // 128) // 2
        src = k.dram(f"wm{layer}", [n, 128, 2048])
        dst = k.nc.dram_tensor(f"wm{layer}_b", [n, 128, 2048], BF16, kind="Internal").ap() if CASTW else None
        k.mixw[layer] = {"src": src, "dst": dst, "n": n, "B": Buf(f"mcast{layer}"), "cast": False}
    return k.mixw[layer]


def lin_setup(k, kind, layer):
    P = k.P
    P.barrier()
    A = k.arena
    A.reset()
    k.ffn = None
    m = {"kind": kind}
    H = 4
    NK = 1 if kind == 0 else 2
    DV = 256 if kind == 0 else 512
    NF = 8 if kind == 0 else 16
    HD = H * DV
    m.update(H=H, NK=NK, DV=DV, NF=NF, HD=HD, NKO=HD // 128, NTM=4 if kind == 0 else 8)
    m["sc"] = common_scratch(k, A, ntmp=1)
    m["sc"]["ps_tr"] = [6, 7]
    al = lambda nm, shape, dtype=BF16: (A.alloc(nm, shape, dtype), Buf(nm))
    m["xnT"], m["BxnT"] = al("xnT", [128, 8, 256])
    m["qk"], m["Bqk"] = al("qk", [128, NF, 256])
    m["vo"], _ = al("vo", [128, 2, HD])
    m["Bvo"] = [Buf("vo0"), Buf("vo1")]
    m["hbuf"], _ = al("hbuf", [128, 2, HD])
    m["Bhbuf"] = [Buf("hb0"), Buf("hb1")]
    m["hT"], m["BhT"] = al("hT", [128, HD // 128, 256])
    NR = 5 if kind == 0 else 2
    m["NR"] = NR
    m["ring"], _ = al("ring", [128, NR, 2048])
    m["Bring"] = [Buf(f"ring{i}") for i in range(NR)]
    m["rq"] = 0
    m["gpost"], m["Bgpost"] = al("gpost", [128, 1024], F32)
    m["Cbf"], m["BCbf"] = al("Cbf", [128, H * NK, DV])
    m["ktm"], m["Bktm"] = al("ktm", [128, H, NK * 128])
    m["PT"], m["BPT"] = al("PT", [128, H, 128])
    m["tmpC"], m["BtmpC"] = m["sc"]["tmp"][0], m["sc"]["Btmp"][0]
    m["e"], m["Be"] = al("e", [128, 2, 4], F32)
    m["base"], m["Bbase"] = al("base", [128, 2, 4], F32)
    m["cdec"], m["Bcdec"] = al("cdec", [128, 2, 4], F32)
    m["sm"], m["Bsm"] = al("sm", [128, 8, 4], F32)
    m["mw"] = mixer_units(k, layer)
    m["wd"] = m["mw"]["src"]
    if kind == 0:
        m["C"], m["BC"] = al("C", [128, H, DV], F32)
        m["nst"], m["Bnst"] = al("nst", [128, 4], F32)
        m["nbf"], m["Bnbf"] = al("nbf", [128, 4])
        m["pre"], m["Bpre"] = al("pre", [128, 8, 259], F32)
        m["acc"], m["Bacc"] = al("acc", [128, 256], F32)
        m["convT"], m["Bw"] = al("convT", [128, 32], F32)
        m["wif"], _ = al("wif", [128, 64])
        m["bifb"], _ = al("bifb", [128, 8], F32)
        m["mng"], _ = al("mng", [128, 1024], F32)
        m["gat"], m["Bgat"] = al("gat", [128, 2, 8], F32)
        m["gt2"], m["Bgt2"] = al("gt2", [128, 2, 8], F32)
        wif_d = k.dram(f"wif{layer}", [128, 64])
        bif_d = k.dram(f"bif{layer}", [1, 8])
        conv_d = k.dram(f"convT{layer}", [128, 32])
        mng_d = k.dram(f"mng{layer}", [1, 1024])
        Bw = m["Bw"]
        P.dma("pool", lambda e: e.dma_start(out=m["wif"][:], in_=wif_d[:, :]), writes=[Bw], dkey="mw0")
        P.dma("sp", lambda e: e.dma_start(out=m["bifb"][:], in_=bif_d[0, :].partition_broadcast(128)), writes=[Bw], dkey="mw1")
        P.dma("sp", lambda e: e.dma_start(out=m["convT"][:], in_=conv_d[:, :]), writes=[Bw], dkey="mw2")
        P.dma("sp", lambda e: e.dma_start(out=m["mng"][:], in_=mng_d[0, :].partition_broadcast(128)), writes=[Bw], dkey="mw3")
    else:
        if not hasattr(k, "pos_d"):
            k.pos_d = k.dram("pos", [1, 2 * SEQ], I32)
        m["posb"], m["Btrig"] = al("posb", [128, 256], F32)
        for nm in ("cosT", "sinT", "ta", "tb"):
            m[nm], _ = al(nm, [128, 256], F32)
        m["ang"] = m["posb"]
        m["ni"], _ = al("ni", [128, 256], I32)
        m["invf"], m["Binvf"] = al("invf", [128, 1], F32)
        m["ii"], _ = al("ii", [128, 1], I32)
        Bi = m["Binvf"]
        P.op("pool", lambda e: e.iota(m["ii"][:], pattern=[[0, 1]], base=0, channel_multiplier=1), writes=[Bi])
        P.op("dve", lambda e: e.tensor_copy(out=m["invf"][:], in_=m["ii"][:]), reads=[Bi], writes=[Bi])
        P.op("act", lambda e: e.activation(out=m["invf"][:], in_=m["invf"][:], func=AF.Exp,
                                           scale=-math.log(10000.0) / 127.0), reads=[Bi], writes=[Bi])
        Be = m["Be"]
        P.op("pool", lambda e: e.iota(m["ii"][:], pattern=[[0, 1]], base=1, channel_multiplier=1), writes=[Bi])
        P.op("dve", lambda e: e.tensor_copy(out=m["sm"][:, 0, 0:1], in_=m["ii"][:]), reads=[Bi], writes=[m["Bsm"]])
        for h in range(4):
            lg = math.log(1.0 - 2.0 ** (-5.0 - h))
            for tt in range(2):
                P.op("dve", lambda e, h=h, tt=tt, lg=lg: e.tensor_scalar(
                    out=m["e"][:, tt, h:h + 1], in0=m["sm"][:, 0, 0:1], scalar1=-lg, scalar2=LN_S[2],
                    op0=ALU.mult, op1=ALU.add), reads=[m["Bsm"]], writes=[Be])
                P.op("dve", lambda e, h=h, tt=tt, lg=lg: e.tensor_scalar(
                    out=m["base"][:, tt, h:h + 1], in0=m["sm"][:, 0, 0:1], scalar1=lg, scalar2=None,
                    op0=ALU.mult), reads=[m["Bsm"]], writes=[m["Bbase"]])
                P.op("pool", lambda e, h=h, tt=tt, lg=lg: e.memset(m["cdec"][:, tt, h:h + 1], math.exp(128.0 * lg)),
                     writes=[m["Bcdec"]])
        P.op("act", lambda e: e.activation(out=m["e"][:], in_=m["e"][:], func=AF.Exp), reads=[Be], writes=[Be])
        P.op("act", lambda e: e.activation(out=m["base"][:], in_=m["base"][:], func=AF.Exp),
             reads=[m["Bbase"]], writes=[m["Bbase"]])
    return m


def nbank(k):
    b = 4 + (k.rot % 4)
    k.rot += 1
    return b


def ring_load(k, m, unit):
    slot = m["rq"] % m["NR"]
    m["rq"] += 1
    mw = m.get("mw")
    if mw is not None and mw["cast"]:
        k.P.dma("sp", lambda e: e.dma_start(out=m["ring"][:, slot, :], in_=mw["dst"][unit]),
                reads=[mw["B"]], writes=[m["Bring"][slot]], dkey=f"mrb{slot}")
    else:
        k.P.dma("pool", lambda e: e.dma_start(out=m["ring"][:, slot, :], in_=m["wd"][unit]),
                writes=[m["Bring"][slot]], dkey=f"mr{slot}")
    return slot


def tm_proj(k, m, blocks, dst_off0):
    P = k.P
    for i, nb in enumerate(blocks):
        sa = ring_load(k, m, m["NF"] // 2 + 2 * nb)
        sb = ring_load(k, m, m["NF"] // 2 + 2 * nb + 1)
        for tt in range(2):
            b = nbank(k)
            for kk in range(8):
                sl = sa if kk < 4 else sb
                P.op("pe", lambda e, b=b, sl=sl, kk=kk, tt=tt: e.matmul(
                    k.ps[b][:], lhsT=m["xnT"][:, kk, tt * 128:(tt + 1) * 128],
                    rhs=m["ring"][:, sl, (kk % 4) * 512:(kk % 4 + 1) * 512], start=(kk == 0), stop=(kk == 7)),
                    reads=[m["BxnT"], m["Bring"][sl]], writes=[k.Bps[b]])
            off = dst_off0 + i * 512
            if (i + tt) % 2 == 0:
                P.op("act", lambda e, b=b, tt=tt, off=off: e.activation(out=m["vo"][:, tt, off:off + 512], in_=k.ps[b][:],
                                                                        func=AF.Copy),
                     reads=[k.Bps[b]], writes=[m["Bvo"][tt]])
            else:
                P.op("dve", lambda e, b=b, tt=tt, off=off: e.tensor_copy(out=m["vo"][:, tt, off:off + 512], in_=k.ps[b][:]),
                     reads=[k.Bps[b]], writes=[m["Bvo"][tt]])


def lin_mixer_phase(k, layer, kind, j):
    P = k.P
    m = lin_setup(k, kind, layer)
    sc = m["sc"]
    H, NK, DV, NF, HD, NKO, NTM = m["H"], m["NK"], m["DV"], m["NF"], m["HD"], m["NKO"], m["NTM"]
    g_pre, g_post = layer * 6 + 2, layer * 6 + 3
    xnT, qk, vo, hbuf, hT = m["xnT"], m["qk"], m["vo"], m["hbuf"], m["hT"]
    P.dma("sp", lambda e: e.dma_start(out=m["gpost"][:], in_=k.ng_d[g_post, :].partition_broadcast(128)),
          writes=[m["Bgpost"]], dkey="gpost")
    qidx = (lambda h, kc: h) if kind == 0 else (lambda h, kc: 2 * h + kc)
    kidx = (lambda h, kc: 4 + h) if kind == 0 else (lambda h, kc: 8 + 2 * h + kc)
    sm, Bsm = m["sm"], m["Bsm"]
    for st in range(NT // 2):
        first = (st % 8 == 0)
        for tt in range(2):
            prenorm_tile(k, st * 2 + tt, g_pre, xnT, m["BxnT"], tt * 128, sc)
        if first:
            P.op("pool", lambda e: e.memset(m["Cbf"][:], 0.0), writes=[m["BCbf"]])
            if kind == 0:
                P.op("pool", lambda e: e.memset(m["C"][:], 0.0), writes=[m["BC"]])
                P.op("pool", lambda e: e.memset(m["nst"][:], 0.0), writes=[m["Bnst"]])
                P.op("pool", lambda e: e.memset(m["nbf"][:], 0.0), writes=[m["Bnbf"]])
        if kind == 0:
            if first:
                P.op("pool", lambda e: e.memset(m["pre"][:, :, 0:3], 0.0), writes=[m["Bpre"]])
            else:
                P.op("pool", lambda e: e.tensor_copy(out=m["pre"][:, :, 0:3], in_=m["pre"][:, :, 256:259]),
                     reads=[m["Bpre"]], writes=[m["Bpre"]])
        else:
            Bt = m["Btrig"]
            P.dma("pool", lambda e, st=st: e.dma_start(out=m["posb"][:],
                                                        in_=k.pos_d[0, st * 256:(st + 1) * 256].partition_broadcast(128)),
                  writes=[Bt], dkey="posb")
            P.op("dve", lambda e: e.tensor_scalar(out=m["ang"][:], in0=m["posb"][:], scalar1=m["invf"][:], scalar2=None,
                                                  op0=ALU.mult), reads=[Bt, m["Binvf"]], writes=[Bt])
            for dst, shift in ((m["sinT"], 0.5), (m["cosT"], 0.75)):
                TWO_PI = 2.0 * math.pi
                P.op("dve", lambda e, shift=shift: e.tensor_scalar(out=m["ta"][:], in0=m["ang"][:], scalar1=1.0 / TWO_PI,
                                                                    scalar2=shift, op0=ALU.mult, op1=ALU.add),
                     reads=[Bt], writes=[Bt])
                P.op("dve", lambda e: e.tensor_copy(out=m["ni"][:], in_=m["ta"][:]), reads=[Bt], writes=[Bt])
                P.op("dve", lambda e: e.tensor_copy(out=m["tb"][:], in_=m["ni"][:]), reads=[Bt], writes=[Bt])
                P.op("dve", lambda e: e.tensor_tensor(out=m["ta"][:], in0=m["ta"][:], in1=m["tb"][:], op=ALU.subtract),
                     reads=[Bt], writes=[Bt])
                P.op("dve", lambda e: e.tensor_scalar(out=m["ta"][:], in0=m["ta"][:], scalar1=-0.5, scalar2=TWO_PI,
                                                      op0=ALU.add, op1=ALU.mult), reads=[Bt], writes=[Bt])
                P.op("dve", lambda e: e.tensor_scalar(out=m["tb"][:], in0=m["ta"][:], scalar1=math.pi, scalar2=-TWO_PI,
                                                      op0=ALU.is_gt, op1=ALU.mult), reads=[Bt], writes=[Bt])
                P.op("dve", lambda e: e.tensor_tensor(out=m["ta"][:], in0=m["ta"][:], in1=m["tb"][:], op=ALU.add),
                     reads=[Bt], writes=[Bt])
                P.op("dve", lambda e: e.tensor_scalar(out=m["tb"][:], in0=m["ta"][:], scalar1=-math.pi, scalar2=TWO_PI,
                                                      op0=ALU.is_lt, op1=ALU.mult), reads=[Bt], writes=[Bt])
                P.op("dve", lambda e: e.tensor_tensor(out=m["ta"][:], in0=m["ta"][:], in1=m["tb"][:], op=ALU.add),
                     reads=[Bt], writes=[Bt])
                P.op("dve", lambda e: e.tensor_scalar(out=m["ta"][:], in0=m["ta"][:], scalar1=-3.1415925, scalar2=3.1415925,
                                                      op0=ALU.max, op1=ALU.min), reads=[Bt], writes=[Bt])
                P.op("act", lambda e, dst=dst: e.activation(out=dst[:], in_=m["ta"][:], func=AF.Sin), reads=[Bt], writes=[Bt])
        for u in range(NF // 2):
            sl = ring_load(k, m, u)
            banks = []
            for c2 in range(2):
                b = nbank(k)
                banks.append(b)
                for kk in range(8):
                    P.op("pe", lambda e, b=b, sl=sl, c2=c2, kk=kk: e.matmul(
                        k.ps[b][:, 0:256], lhsT=m["ring"][:, sl, c2 * 1024 + kk * 128:c2 * 1024 + (kk + 1) * 128],
                        rhs=xnT[:, kk, :], start=(kk == 0), stop=(kk == 7)),
                        reads=[m["Bring"][sl], m["BxnT"]], writes=[k.Bps[b]])
                if kind == 0:
                    c = 2 * u + c2
                    P.op("act", lambda e, b=b, c=c: e.activation(out=m["pre"][:, c, 3:259], in_=k.ps[b][:, 0:256], func=AF.Copy),
                         reads=[k.Bps[b]], writes=[m["Bpre"]])
            if kind == 2:
                Bt = m["Btrig"]
                ba, bb = banks
                for o, (f1, f2, op) in enumerate(((m["cosT"], m["sinT"], ALU.subtract), (m["sinT"], m["cosT"], ALU.add))):
                    P.op("dve", lambda e, f1=f1: e.tensor_tensor(out=m["ta"][:], in0=k.ps[ba][:, 0:256], in1=f1[:], op=ALU.mult),
                         reads=[k.Bps[ba], Bt], writes=[Bt])
                    P.op("dve", lambda e, f2=f2: e.tensor_tensor(out=m["tb"][:], in0=k.ps[bb][:, 0:256], in1=f2[:], op=ALU.mult),
                         reads=[k.Bps[bb], Bt], writes=[Bt])
                    P.op("dve", lambda e, u=u, o=o, op=op: e.tensor_tensor(out=qk[:, 2 * u + o, :], in0=m["ta"][:], in1=m["tb"][:], op=op),
                         reads=[Bt], writes=[m["Bqk"]])
        if kind == 0:
            for c in range(8):
                cw = m["convT"]
                P.op("dve", lambda e, c=c: e.tensor_scalar(out=m["acc"][:], in0=m["pre"][:, c, 3:259],
                                                           scalar1=cw[:, c * 4 + 3:c * 4 + 4], scalar2=None, op0=ALU.mult),
                     reads=[m["Bpre"], m["Bw"]], writes=[m["Bacc"]])
                for tap in (2, 1, 0):
                    P.op("dve", lambda e, c=c, tap=tap: e.scalar_tensor_tensor(
                        out=m["acc"][:], in0=m["pre"][:, c, tap:tap + 256], scalar=cw[:, c * 4 + tap:c * 4 + tap + 1],
                        in1=m["acc"][:], op0=ALU.mult, op1=ALU.add), reads=[m["Bpre"], m["Bw"], m["Bacc"]], writes=[m["Bacc"]])
                P.op("act", lambda e, c=c: e.activation(out=qk[:, c, :], in_=m["acc"][:], func=AF.Silu),
                     reads=[m["Bacc"]], writes=[m["Bqk"]])
        tm_proj(k, m, list(range(NTM // 2)), 0)
        if kind == 0:
            gat, gt2, Bgat, Bgt2 = m["gat"], m["gt2"], m["Bgat"], m["Bgt2"]
            bg = nbank(k)
            for tt in range(2):
                for kk in range(8):
                    P.op("pe", lambda e, tt=tt, kk=kk: e.matmul(k.ps[bg][:, tt * 8:(tt + 1) * 8],
                                                                 lhsT=xnT[:, kk, tt * 128:(tt + 1) * 128],
                                                                 rhs=m["wif"][:, kk * 8:(kk + 1) * 8], start=(kk == 0), stop=(kk == 7)),
                         reads=[m["BxnT"], m["Bw"]], writes=[k.Bps[bg]])
                P.op("dve", lambda e, tt=tt: e.tensor_tensor(out=gat[:, tt, :], in0=k.ps[bg][:, tt * 8:(tt + 1) * 8],
                                                             in1=m["bifb"][:], op=ALU.add),
                     reads=[k.Bps[bg], m["Bw"]], writes=[Bgat])
            P.op("act", lambda e: e.activation(out=gat[:], in_=gat[:], func=AF.Tanh, scale=1.0 / 15.0), reads=[Bgat], writes=[Bgat])
            P.op("dve", lambda e: e.tensor_scalar(out=gat[:], in0=gat[:], scalar1=15.0, scalar2=None, op0=ALU.mult),
                 reads=[Bgat], writes=[Bgat])
            P.op("act", lambda e: e.activation(out=gt2[:, :, 4:8], in_=gat[:, :, 4:8], func=AF.Exp, scale=-1.0),
                 reads=[Bgat], writes=[Bgt2])
            P.op("dve", lambda e: e.tensor_scalar(out=gt2[:, :, 4:8], in0=gt2[:, :, 4:8], scalar1=1.0, scalar2=None, op0=ALU.add),
                 reads=[Bgt2], writes=[Bgt2])
            P.op("act", lambda e: e.activation(out=gt2[:, :, 4:8], in_=gt2[:, :, 4:8], func=AF.Ln), reads=[Bgt2], writes=[Bgt2])
            bc = nbank(k)
            for tt in range(2):
                P.op("pe", lambda e, tt=tt: e.matmul(k.ps[bc][:, tt * 4:(tt + 1) * 4], lhsT=k.trif[:], rhs=gt2[:, tt, 4:8],
                                                     start=True, stop=True), reads=[Bgt2, k.Bconst], writes=[k.Bps[bc]])
                P.op("pe", lambda e, tt=tt: e.matmul(k.ps[bc][:, 8 + tt * 4:8 + (tt + 1) * 4], lhsT=k.onesf[:], rhs=gt2[:, tt, 4:8],
                                                     start=True, stop=True), reads=[Bgt2, k.Bconst], writes=[k.Bps[bc]])
            cs = k.ps[bc][:, 0:8].rearrange("p (t h) -> p t h", t=2)
            tot = k.ps[bc][:, 8:16].rearrange("p (t h) -> p t h", t=2)
            P.op("dve", lambda e: e.scalar_tensor_tensor(out=m["e"][:], in0=cs, scalar=LN_S[0], in1=gat[:, :, 0:4],
                                                         op0=ALU.add, op1=ALU.add), reads=[k.Bps[bc], Bgat], writes=[m["Be"]])
            P.op("act", lambda e: e.activation(out=m["e"][:], in_=m["e"][:], func=AF.Exp), reads=[m["Be"]], writes=[m["Be"]])
            P.op("act", lambda e: e.activation(out=m["base"][:], in_=cs, func=AF.Exp), reads=[k.Bps[bc]], writes=[m["Bbase"]])
            P.op("act", lambda e: e.activation(out=m["cdec"][:], in_=tot, func=AF.Exp, scale=-1.0),
                 reads=[k.Bps[bc]], writes=[m["Bcdec"]])
        for tt in range(2):
            tok = slice(tt * 128, (tt + 1) * 128)
            bt = nbank(k)
            ptb = k.ps[bt][:].bitcast(BF16)
            for h in range(H):
                for kc in range(NK):
                    o = (h * NK + kc) * 128
                    P.op("pe", lambda e, h=h, kc=kc, o=o: e.transpose(out=ptb[:, o:o + 128], in_=qk[:, kidx(h, kc), tok],
                                                                       identity=k.ident[:]),
                         reads=[m["Bqk"], k.Bconst], writes=[k.Bps[bt]])
            for h in range(H):
                P.op("dve", lambda e, h=h: e.tensor_scalar(out=m["ktm"][:, h, :], in0=ptb[:, h * NK * 128:(h + 1) * NK * 128],
                                                           scalar1=m["e"][:, tt, h:h + 1], scalar2=None, op0=ALU.mult),
                     reads=[k.Bps[bt], m["Be"]], writes=[m["Bktm"]])
            bp = nbank(k)
            for h in range(H):
                for kc in range(NK):
                    P.op("pe", lambda e, h=h, kc=kc: e.matmul(k.ps[bp][:, h * 128:(h + 1) * 128], lhsT=qk[:, kidx(h, kc), tok],
                                                              rhs=qk[:, qidx(h, kc), tok], start=(kc == 0), stop=(kc == NK - 1)),
                         reads=[m["Bqk"]], writes=[k.Bps[bp]])
            for h in range(H):
                P.op("dve", lambda e, h=h: e.scalar_tensor_tensor(out=m["PT"][:, h, :], in0=k.ps[bp][:, h * 128:(h + 1) * 128],
                                                                  scalar=m["e"][:, tt, h:h + 1], in1=k.tri[:],
                                                                  op0=ALU.mult, op1=ALU.mult),
                     reads=[k.Bps[bp], m["Be"], k.Bconst], writes=[m["BPT"]])
            if kind == 0:
                bd = nbank(k)
                for h in range(H):
                    P.op("pe", lambda e, h=h: e.matmul(k.ps[bd][:, h:h + 1], lhsT=m["PT"][:, h, :], rhs=k.ones[:, 0:1],
                                                       start=True, stop=False), reads=[m["BPT"], k.Bconst], writes=[k.Bps[bd]])
                    P.op("pe", lambda e, h=h: e.matmul(k.ps[bd][:, h:h + 1], lhsT=qk[:, qidx(h, 0), tok], rhs=m["nbf"][:, h:h + 1],
                                                       start=False, stop=True), reads=[m["Bqk"], m["Bnbf"]], writes=[k.Bps[bd]])
                P.op("dve", lambda e: e.tensor_copy(out=sm[:, 0, :], in_=k.ps[bd][:, 0:4]), reads=[k.Bps[bd]], writes=[Bsm])
                P.op("dve", lambda e: e.tensor_scalar(out=sm[:, 6, :], in0=sm[:, 0, :], scalar1=-1.0, scalar2=None,
                                                      op0=ALU.mult), reads=[Bsm], writes=[Bsm])
                P.op("dve", lambda e: e.tensor_tensor(out=sm[:, 0, :], in0=sm[:, 0, :], in1=sm[:, 6, :], op=ALU.max),
                     reads=[Bsm], writes=[Bsm])
                P.op("dve", lambda e: e.tensor_tensor(out=sm[:, 0, :], in0=sm[:, 0, :], in1=m["base"][:, tt, :], op=ALU.max),
                     reads=[Bsm, m["Bbase"]], writes=[Bsm])
                P.op("dve", lambda e: e.reciprocal(out=sm[:, 1, :], in_=sm[:, 0, :]), reads=[Bsm], writes=[Bsm])
                basev = sm[:, 1, :]
            else:
                P.op("dve", lambda e: e.tensor_copy(out=sm[:, 1, :], in_=m["base"][:, tt, :]), reads=[m["Bbase"]], writes=[Bsm])
                basev = sm[:, 1, :]
            hpb = 512 // DV
            abank = {}
            for h in range(H):
                if h % hpb == 0:
                    ba = nbank(k)
                abank[h] = (ba, (h % hpb) * DV)
                ba, off = abank[h]
                P.op("pe", lambda e, h=h, ba=ba, off=off: e.matmul(k.ps[ba][:, off:off + DV], lhsT=m["PT"][:, h, :],
                                                                   rhs=vo[:, tt, h * DV:(h + 1) * DV], start=True, stop=False),
                     reads=[m["BPT"], m["Bvo"][tt]], writes=[k.Bps[ba]])
                for kc in range(NK):
                    P.op("pe", lambda e, h=h, kc=kc, ba=ba, off=off: e.matmul(
                        k.ps[ba][:, off:off + DV], lhsT=qk[:, qidx(h, kc), tok], rhs=m["Cbf"][:, h * NK + kc, :],
                        start=False, stop=(kc == NK - 1)), reads=[m["Bqk"], m["BCbf"]], writes=[k.Bps[ba]])
                P.op("act", lambda e, h=h, ba=ba, off=off: e.activation(out=sc["junk"][:, 0:DV], in_=k.ps[ba][:, off:off + DV],
                                                                        func=AF.Square, accum_out=sm[:, 2, h:h + 1]),
                     reads=[k.Bps[ba]], writes=[sc["Bjunk"], Bsm])
                if h % hpb == hpb - 1 or h == H - 1:
                    pass
            P.op("dve", lambda e: e.tensor_tensor(out=sm[:, 3, :], in0=basev, in1=basev, op=ALU.mult), reads=[Bsm], writes=[Bsm])
            P.op("dve", lambda e: e.tensor_tensor(out=sm[:, 3, :], in0=sm[:, 3, :], in1=sm[:, 2, :], op=ALU.mult), reads=[Bsm], writes=[Bsm])
            P.op("pool", lambda e: e.tensor_scalar(out=sm[:, 3, :], in0=sm[:, 3, :], scalar1=1.0 / DV, scalar2=EPS,
                                                   op0=ALU.mult, op1=ALU.add), reads=[Bsm], writes=[Bsm])
            P.op("pool", lambda e: e.tensor_tensor(out=sm[:, 3, :], in0=sm[:, 3, :], in1=k.mhalf[:, 0:1].to_broadcast([128, 4]),
                                                   op=ALU.pow), reads=[Bsm, k.Bconst], writes=[Bsm])
            P.op("dve", lambda e: e.tensor_tensor(out=sm[:, 4, :], in0=sm[:, 3, :], in1=basev, op=ALU.mult), reads=[Bsm], writes=[Bsm])
            for h in range(H):
                ba, off = abank[h]
                if kind == 0:
                    P.op("dve", lambda e, h=h, ba=ba, off=off: e.scalar_tensor_tensor(
                        out=hbuf[:, tt, h * DV:(h + 1) * DV], in0=k.ps[ba][:, off:off + DV], scalar=sm[:, 4, h:h + 1],
                        in1=m["mng"][:, h * DV:(h + 1) * DV], op0=ALU.mult, op1=ALU.mult),
                        reads=[k.Bps[ba], Bsm, m["Bw"]], writes=[m["Bhbuf"][tt]])
                else:
                    P.op("dve", lambda e, h=h, ba=ba, off=off: e.tensor_scalar(
                        out=hbuf[:, tt, h * DV:(h + 1) * DV], in0=k.ps[ba][:, off:off + DV], scalar1=sm[:, 4, h:h + 1],
                        scalar2=None, op0=ALU.mult), reads=[k.Bps[ba], Bsm], writes=[m["Bhbuf"][tt]])
            for h in range(H):
                cd = m["cdec"][:, tt, h:h + 1]
                for kc in range(NK):
                    bs = nbank(k)
                    P.op("pe", lambda e, h=h, kc=kc, bs=bs: e.matmul(k.ps[bs][:, 0:DV], lhsT=m["ktm"][:, h, kc * 128:(kc + 1) * 128],
                                                                     rhs=vo[:, tt, h * DV:(h + 1) * DV], start=True, stop=True),
                         reads=[m["Bktm"], m["Bvo"][tt]], writes=[k.Bps[bs]])
                    P.op("dve", lambda e, bs=bs, cd=cd: e.tensor_scalar(out=m["tmpC"][:, 0:DV], in0=k.ps[bs][:, 0:DV], scalar1=cd,
                                                                        scalar2=None, op0=ALU.mult),
                         reads=[k.Bps[bs], m["Bcdec"]], writes=[m["BtmpC"]])
                    if kind == 0:
                        P.op("dve", lambda e, h=h, cd=cd: e.scalar_tensor_tensor(out=m["C"][:, h, :], in0=m["C"][:, h, :], scalar=cd,
                                                                                 in1=m["tmpC"][:, 0:DV], op0=ALU.mult, op1=ALU.add),
                             reads=[m["BC"], m["Bcdec"], m["BtmpC"]], writes=[m["BC"]])
                        P.op("act", lambda e, h=h: e.activation(out=m["Cbf"][:, h, :], in_=m["C"][:, h, :], func=AF.Copy),
                             reads=[m["BC"]], writes=[m["BCbf"]])
                    else:
                        ci = h * NK + kc
                        P.op("dve", lambda e, ci=ci, cd=cd: e.scalar_tensor_tensor(out=m["Cbf"][:, ci, :], in0=m["Cbf"][:, ci, :], scalar=cd,
                                                                                   in1=m["tmpC"][:, 0:DV], op0=ALU.mult, op1=ALU.add),
                             reads=[m["BCbf"], m["Bcdec"], m["BtmpC"]], writes=[m["BCbf"]])
            if kind == 0:
                bn = nbank(k)
                for h in range(H):
                    P.op("pe", lambda e, h=h: e.matmul(k.ps[bn][:, h:h + 1], lhsT=m["ktm"][:, h, :], rhs=k.ones[:, 0:1],
                                                       start=True, stop=True), reads=[m["Bktm"], k.Bconst], writes=[k.Bps[bn]])
                P.op("dve", lambda e: e.tensor_tensor(out=sm[:, 5, :], in0=k.ps[bn][:, 0:4], in1=m["nst"][:], op=ALU.add),
                     reads=[k.Bps[bn], m["Bnst"]], writes=[Bsm])
                P.op("dve", lambda e: e.tensor_tensor(out=m["nst"][:], in0=sm[:, 5, :], in1=m["cdec"][:, tt, :], op=ALU.mult),
                     reads=[Bsm, m["Bcdec"]], writes=[m["Bnst"]])
                P.op("dve", lambda e: e.tensor_copy(out=m["nbf"][:], in_=m["nst"][:]), reads=[m["Bnst"]], writes=[m["Bnbf"]])
        tm_proj(k, m, list(range(NTM // 2, NTM)), 0)
        for tt in range(2):
            P.op("act", lambda e, tt=tt: e.activation(out=vo[:, tt, :], in_=vo[:, tt, :],
                                                      func=(AF.Sigmoid if kind == 0 else AF.Silu)),
                 reads=[m["Bvo"][tt]], writes=[m["Bvo"][tt]])
            P.op("dve", lambda e, tt=tt: e.tensor_tensor(out=hbuf[:, tt, :], in0=hbuf[:, tt, :], in1=vo[:, tt, :], op=ALU.mult),
                 reads=[m["Bhbuf"][tt], m["Bvo"][tt]], writes=[m["Bhbuf"][tt]])
            for g in range(NKO // 8):
                bt = nbank(k)
                ptb = k.ps[bt][:].bitcast(BF16)
                for i in range(8):
                    kk = g * 8 + i
                    P.op("pe", lambda e, tt=tt, kk=kk, i=i, ptb=ptb: e.transpose(out=ptb[:, i * 128:(i + 1) * 128],
                                                                                 in_=hbuf[:, tt, kk * 128:(kk + 1) * 128],
                                                                                 identity=k.ident[:]),
                         reads=[m["Bhbuf"][tt], k.Bconst], writes=[k.Bps[bt]])
                P.op("act", lambda e, tt=tt, g=g, ptb=ptb: e.activation(out=hT[:, g * 8:(g + 1) * 8, tt * 128:(tt + 1) * 128],
                                                                        in_=ptb.rearrange("p (a t) -> p a t", a=8), func=AF.Copy),
                     reads=[k.Bps[bt]], writes=[m["BhT"]])
        for u in range(NKO // 2):
            sl = ring_load(k, m, NF // 2 + 2 * NTM + u)
            for c2 in range(2):
                kk = 2 * u + c2
                for tt in range(2):
                    for half in range(2):
                        pb = tt * 2 + half
                        P.op("pe", lambda e, sl=sl, c2=c2, kk=kk, tt=tt, half=half, pb=pb: e.matmul(
                            k.ps[pb][:], lhsT=hT[:, kk, tt * 128:(tt + 1) * 128],
                            rhs=m["ring"][:, sl, c2 * 1024 + half * 512:c2 * 1024 + (half + 1) * 512],
                            start=(kk == 0), stop=(kk == NKO - 1)), reads=[m["BhT"], m["Bring"][sl]], writes=[k.Bps[pb]])
        for tt in range(2):
            postnorm_residual(k, st * 2 + tt, [tt * 2, tt * 2 + 1], m["gpost"], m["Bgpost"], 1.0, sc)


C0 = math.exp(-0.5)
RW_DEBUG = False
RW_STOP = 0


class _Stop(Exception):
    pass


def nb8(k):
    b = k.rot % 8
    k.rot += 1
    return b


def rwkv_setup(k, layer):
    P = k.P
    A = k.big_arena
    A.reset()
    k.ffn = None
    m = {}
    m["sc"] = common_scratch(k, A, ntmp=1)
    m["sc"]["ps_tr"] = [6, 7]
    al = lambda nm, shape, dtype=BF16: (A.alloc(nm, shape, dtype), Buf(nm))
    m["xnTh"], m["BxnT"] = al("xnTh", [128, 8, 129])
    m["xxT"], m["Bxx"] = al("xxT", [128, 8, 128])
    m["mixT"], _ = al("mixT", [128, 2, 8, 128])
    m["Bmix"] = [Buf(f"mix{i}") for i in range(2)]
    m["hT"], m["BhT"] = al("hT", [128, 8, 128])
    m["ring"], _ = al("ring", [128, 2, 2048])
    m["Bring"] = [Buf(f"ring{i}") for i in range(2)]
    m["NR"] = 2
    m["rq"] = 0
    m["gpost"], m["Bgpost"] = al("gpost", [128, 1024], F32)
    m["Bw"] = Buf("rw_w")
    for nm, shape, dtype in [("w1", [128, 8, 64], BF16), ("a1", [128, 8, 64], BF16), ("g1", [128, 8, 128], BF16),
                             ("w2", [64, 1024], BF16), ("a2", [64, 1024], BF16), ("g2", [128, 1024], BF16),
                             ("w0r", [64, 1024], F32), ("a0r", [64, 1024], F32), ("muT", [128, 48], F32),
                             ("kkb", [64, 1024], BF16), ("kab", [64, 1024], BF16), ("lgb", [64, 1024], BF16),
                             ("lbb", [64, 1024], BF16), ("rkb", [64, 1024], BF16)]:
        m[nm], _ = al(nm, shape, dtype)
    d = lambda nm, shape: k.dram(f"rw_{nm}", shape)
    m["wd"] = d("units", [16, 128, 2048])
    Bw = m["Bw"]
    for i, (nm, shape) in enumerate([("w1", [128, 8, 64]), ("a1", [128, 8, 64]), ("g1", [128, 8, 128]),
                                     ("w2", [64, 1024]), ("a2", [64, 1024]), ("g2", [128, 1024])]):
        src = d(nm, shape)
        P.dma("pool", lambda e, nm=nm, src=src: e.dma_start(out=m[nm][:], in_=src), writes=[Bw], dkey=f"rww{i}")
    for i, nm in enumerate(["w0r", "a0r"]):
        src = d(nm, [1, 1024])
        P.dma("sp", lambda e, nm=nm, src=src: e.dma_start(out=m[nm][:], in_=src[0, :].partition_broadcast(64)),
              writes=[Bw], dkey=f"rwr{i}")
    src = d("muT", [128, 48])
    P.dma("sp", lambda e, src=src: e.dma_start(out=m["muT"][:], in_=src), writes=[Bw], dkey="rwmu")
    for i, nm in enumerate(["kkb", "kab", "lgb", "lbb", "rkb"]):
        src = d(nm, [1, 1024])
        P.dma("pool", lambda e, nm=nm, src=src: e.dma_start(out=m[nm][:], in_=src[0, :].partition_broadcast(64)),
              writes=[Bw], dkey=f"rwb{i}")
    m["hidw"], m["Bhid"] = al("hidw", [64, 128])
    m["hida"], _ = al("hida", [64, 128])
    m["hidg"], _ = al("hidg", [128, 128])
    m["rkv"], _ = al("rkv", [64, 2, 3, 1024])
    m["Brkv"] = [[Buf(f"rkv{c}{i}") for i in range(3)] for c in range(2)]
    m["H"], m["BH"] = al("H", [64, 1024], F32)
    m["Hbf"], m["BHbf"] = al("Hbf", [64, 1024])

    def mk_cx(i):
        cx = dict(m)
        alc = lambda nm, shape, dtype=BF16: (A.alloc(f"{nm}c{i}", shape, dtype), Buf(f"{nm}c{i}"))
        for nm in ("F0", "F1", "F2", "PB", "TTf"):
            cx[nm], cx["B" + nm] = alc(nm, [64, 1024], F32)
        for nm in ("a", "gsb", "G", "Ginv", "Gprev", "kk", "bt", "at", "kt", "rt", "TT", "bT", "kT"):
            cx[nm], cx["B" + nm] = alc(nm, [64, 1024])
        for nm, src in (("akv", "G"), ("TAT", "Ginv"), ("Usb", "Gprev"), ("zb", "kk"), ("QA", "F0"), ("QB", "F1"), ("PA", "F2")):
            cx[nm], cx["B" + nm] = cx[src], cx["B" + src]
        for nm in ("M1", "M2", "arT"):
            cx[nm], cx["B" + nm] = alc(nm, [64, 16, 2, 64])
        cx["GL"], cx["BGL"] = alc("GL", [64, 16], F32)
        cx["sm"], cx["Bsm"] = alc("sm", [64, 8, 16], F32)
        return cx

    m["cx"] = [mk_cx(0), mk_cx(1)]
    m["eps24"], _ = al("eps24", [64, 1], F32)
    return m


def rwkv_seq(k, m, layer, seq):
    P = k.P
    sc = m["sc"]
    g_pre, g_post = layer * 6 + 2, layer * 6 + 3
    xnTh, xxT, mixT, hT = m["xnTh"], m["xxT"], m["mixT"], m["hT"]
    Bw = m["Bw"]
    H, BH = m["H"], m["BH"]
    cxs = m["cx"]
    hv = lambda t: t[:].rearrange("p (h j) -> p h j", h=16)
    bc = lambda ap: ap.unsqueeze(2).to_broadcast([64, 16, 64])
    P.op("pool", lambda e: e.memset(H[:], 0.0), writes=[BH])
    P.op("pool", lambda e: e.memset(m["Hbf"][:], 0.0), writes=[m["BHbf"]])

    def headmm(dst_evac, groups):
        for g in range(2):
            b = nb8(k)
            for hh in range(8):
                h = g * 8 + hh
                for gi, (lf, rf, rd) in enumerate(groups):
                    Lh, Rh, last = lf(h), rf(h), (gi == len(groups) - 1)
                    P.op("pe", lambda e, b=b, hh=hh, Lh=Lh, Rh=Rh, gi=gi, last=last: e.matmul(
                        k.ps[b][0:64, hh * 64:(hh + 1) * 64], lhsT=Lh, rhs=Rh, start=(gi == 0), stop=last),
                        reads=rd, writes=[k.Bps[b]])
            dst_evac(b, g)

    def evac_to(dst, Bdst, eng="act"):
        def f(b, g):
            if eng == "act":
                P.op("act", lambda e: e.activation(out=hv(dst)[:, g * 8:(g + 1) * 8],
                                                   in_=k.ps[b][0:64, :].rearrange("p (h x) -> p h x", h=8), func=AF.Copy),
                     reads=[k.Bps[b]], writes=[Bdst])
            else:
                P.op("dve", lambda e: e.tensor_copy(out=hv(dst)[:, g * 8:(g + 1) * 8],
                                                    in_=k.ps[b][0:64, :].rearrange("p (h x) -> p h x", h=8)),
                     reads=[k.Bps[b]], writes=[Bdst])
        return f

    for ti in range(16):
        xs_ = ti % 2
        gt = seq * 16 + ti
        P.dma("sp", lambda e: e.dma_start(out=k.xres[:, xs_, :], in_=k.scrX[gt]), reads=[k.Bscr[gt]], writes=[k.Bx[xs_]],
              dkey=f"x{xs_}")
        if ti == 0:
            P.op("pool", lambda e: e.memset(xnTh[:, :, 0:1], 0.0), writes=[m["BxnT"]])
        else:
            P.op("pool", lambda e: e.tensor_copy(out=xnTh[:, :, 0:1], in_=xnTh[:, :, 128:129]),
                 reads=[m["BxnT"]], writes=[m["BxnT"]])
        prenorm_tile(k, xs_, g_pre, xnTh, m["BxnT"], 1, sc)
        P.op("dve", lambda e: e.tensor_tensor(out=xxT[:], in0=xnTh[:, :, 0:128], in1=xnTh[:, :, 1:129], op=ALU.subtract),
             reads=[m["BxnT"]], writes=[m["Bxx"]])
        def make_mix(i):
            eng = "dve" if i % 2 == 0 else "pool"
            mu_b = m["muT"][:, i * 8:(i + 1) * 8].unsqueeze(2).to_broadcast([128, 8, 128])
            P.op(eng, lambda e, i=i, mu_b=mu_b: e.tensor_tensor(out=mixT[:, i % 2], in0=xxT[:], in1=mu_b, op=ALU.mult),
                 reads=[m["Bxx"], Bw], writes=[m["Bmix"][i % 2]])
            P.op(eng, lambda e, i=i: e.tensor_tensor(out=mixT[:, i % 2], in0=mixT[:, i % 2], in1=xnTh[:, :, 1:129], op=ALU.add),
                 reads=[m["Bmix"][i % 2], m["BxnT"]], writes=[m["Bmix"][i % 2]])
        for mi in range(3):
            make_mix(mi)
            for nb in range(2):
                sa = ring_load(k, m, mi * 4 + 2 * nb)
                sb = ring_load(k, m, mi * 4 + 2 * nb + 1)
                for c in range(2):
                    b = nb8(k)
                    for kk in range(8):
                        sl = sa if kk < 4 else sb
                        P.op("pe", lambda e, b=b, sl=sl, kk=kk, c=c, mi=mi: e.matmul(
                            k.ps[b][0:64, :], lhsT=mixT[:, mi % 2, kk, c * 64:(c + 1) * 64],
                            rhs=m["ring"][:, sl, (kk % 4) * 512:(kk % 4 + 1) * 512], start=(kk == 0), stop=(kk == 7)),
                            reads=[m["Bmix"][mi % 2], m["Bring"][sl]], writes=[k.Bps[b]])
                    P.op("act", lambda e, b=b, c=c, mi=mi, nb=nb: e.activation(
                        out=m["rkv"][:, c, mi, nb * 512:(nb + 1) * 512], in_=k.ps[b][0:64, :], func=AF.Copy),
                        reads=[k.Bps[b]], writes=[m["Brkv"][c][mi]])
        for (hid, w, mi, fn, np_) in ((m["hidw"], m["w1"], 3, AF.Tanh, 64), (m["hida"], m["a1"], 4, AF.Copy, 64),
                                      (m["hidg"], m["g1"], 5, AF.Sigmoid, 128)):
            make_mix(mi)
            b = nb8(k)
            for kk in range(8):
                P.op("pe", lambda e, b=b, w=w, mi=mi, kk=kk, np_=np_: e.matmul(
                    k.ps[b][0:np_, 0:128], lhsT=w[:, kk, :], rhs=mixT[:, mi % 2, kk, :], start=(kk == 0), stop=(kk == 7)),
                    reads=[Bw, m["Bmix"][mi % 2]], writes=[k.Bps[b]])
            P.op("act", lambda e, b=b, hid=hid, fn=fn, np_=np_: e.activation(out=hid[:], in_=k.ps[b][0:np_, 0:128], func=fn),
                 reads=[k.Bps[b]], writes=[m["Bhid"]])
        def chunk_indep(m, c):
            F0, F1, F2 = m["F0"], m["F1"], m["F2"]
            BF0, BF1, BF2 = m["BF0"], m["BF1"], m["BF2"]
            sm, Bsm = m["sm"], m["Bsm"]
            cs_ = slice(c * 64, (c + 1) * 64)
            r_, k_, v_ = m["rkv"][:, c, 0, :], m["rkv"][:, c, 1, :], m["rkv"][:, c, 2, :]
            Br, Bk, Bv = m["Brkv"][c]
            rv = m["rkv"][:, c, 0, :].rearrange("p (h j) -> p h j", h=16)
            vv = m["rkv"][:, c, 2, :].rearrange("p (h j) -> p h j", h=16)
            yield
            bw = [nb8(k), nb8(k)]
            for half in range(2):
                hs = slice(half * 512, (half + 1) * 512)
                P.op("pe", lambda e, half=half, hs=hs: e.matmul(k.ps[bw[half]][0:64, :], lhsT=m["hidw"][:, cs_], rhs=m["w2"][:, hs],
                                                                start=True, stop=True), reads=[m["Bhid"], Bw], writes=[k.Bps[bw[half]]])
                P.op("dve", lambda e, half=half, hs=hs: e.tensor_tensor(out=F0[:, hs], in0=k.ps[bw[half]][0:64, :], in1=m["w0r"][:, hs],
                                                                        op=ALU.add), reads=[k.Bps[bw[half]], Bw], writes=[BF0])
                P.op("act", lambda e, half=half, hs=hs: e.activation(out=F0[:, hs], in_=F0[:, hs], func=AF.Sigmoid),
                     reads=[BF0], writes=[BF0])
            ba = [nb8(k), nb8(k)]
            for half in range(2):
                hs = slice(half * 512, (half + 1) * 512)
                P.op("pe", lambda e, half=half, hs=hs: e.matmul(k.ps[ba[half]][0:64, :], lhsT=m["hida"][:, cs_], rhs=m["a2"][:, hs],
                                                                start=True, stop=True), reads=[m["Bhid"], Bw], writes=[k.Bps[ba[half]]])
                P.op("dve", lambda e, half=half, hs=hs: e.tensor_tensor(out=F1[:, hs], in0=k.ps[ba[half]][0:64, :], in1=m["a0r"][:, hs],
                                                                        op=ALU.add), reads=[k.Bps[ba[half]], Bw], writes=[BF1])
                P.op("act", lambda e, half=half, hs=hs: e.activation(out=m["a"][:, hs], in_=F1[:, hs], func=AF.Sigmoid),
                     reads=[BF1], writes=[m["Ba"]])
            for half in range(2):
                hs = slice(half * 512, (half + 1) * 512)
                b = nb8(k)
                P.op("pe", lambda e, b=b, hs=hs: e.matmul(k.ps[b][0:64, :], lhsT=m["hidg"][:, cs_], rhs=m["g2"][:, hs],
                                                          start=True, stop=True), reads=[m["Bhid"], Bw], writes=[k.Bps[b]])
                P.op("act", lambda e, b=b, hs=hs: e.activation(out=m["gsb"][:, hs], in_=k.ps[b][0:64, :], func=AF.Copy),
                     reads=[k.Bps[b]], writes=[m["Bgsb"]])
            yield
            bcs = [nb8(k), nb8(k)]
            for half in range(2):
                hs = slice(half * 512, (half + 1) * 512)
                b = bcs[half]
                P.op("pe", lambda e, b=b, hs=hs: e.matmul(k.ps[b][0:64, :], lhsT=k.trif[0:64, 0:64], rhs=F0[:, hs],
                                                          start=True, stop=True), reads=[BF0, k.Bconst], writes=[k.Bps[b]])
                P.op("act", lambda e, b=b, hs=hs: e.activation(out=m["G"][:, hs], in_=k.ps[b][0:64, :], func=AF.Exp, scale=-C0),
                     reads=[k.Bps[b]], writes=[m["BG"]])
                P.op("act", lambda e, b=b, hs=hs: e.activation(out=m["Ginv"][:, hs], in_=k.ps[b][0:64, :], func=AF.Exp, scale=C0),
                     reads=[k.Bps[b]], writes=[m["BGinv"]])
                P.op("dve", lambda e, b=b, hs=hs: e.tensor_tensor(out=F1[:, hs], in0=k.ps[b][0:64, :], in1=F0[:, hs], op=ALU.subtract),
                     reads=[k.Bps[b], BF0], writes=[BF1])
                P.op("act", lambda e, hs=hs: e.activation(out=m["Gprev"][:, hs], in_=F1[:, hs], func=AF.Exp, scale=-C0),
                     reads=[BF1], writes=[m["BGprev"]])
            bgl = nb8(k)
            for h in range(16):
                P.op("pe", lambda e, h=h: e.matmul(k.ps[bgl][0:64, 2 * h:2 * h + 2], lhsT=F0[:, h * 64:(h + 1) * 64], rhs=k.onesf[0:64, 0:2],
                                                   start=True, stop=True), reads=[BF0, k.Bconst], writes=[k.Bps[bgl]])
            P.op("act", lambda e: e.activation(out=m["GL"][:], in_=k.ps[bgl][0:64, 0:32].rearrange("p (h two) -> p h two", two=2)[:, :, 0],
                                               func=AF.Exp, scale=-C0),
                 reads=[k.Bps[bgl]], writes=[m["BGL"]])
            yield
            P.op("dve", lambda e: e.tensor_tensor(out=F1[:], in0=k_, in1=m["kkb"][:], op=ALU.mult), reads=[Bk, Bw], writes=[BF1])
            P.op("pool", lambda e: e.tensor_tensor(out=F2[:], in0=F1[:], in1=F1[:], op=ALU.mult), reads=[BF1], writes=[BF2])
            P.op("dve", lambda e: e.tensor_reduce(out=sm[:, 0, :], in_=hv(F2), axis=AX.X, op=ALU.add), reads=[BF2], writes=[Bsm])
            P.op("pool", lambda e: e.tensor_scalar(out=sm[:, 0, :], in0=sm[:, 0, :], scalar1=1e-24, scalar2=None, op0=ALU.max),
                 reads=[Bsm], writes=[Bsm])
            P.op("pool", lambda e: e.tensor_tensor(out=sm[:, 0, :], in0=sm[:, 0, :], in1=k.mhalf[0:64, 0:1].to_broadcast([64, 16]),
                                                   op=ALU.pow), reads=[Bsm, k.Bconst], writes=[Bsm])
            P.op("dve", lambda e: e.tensor_tensor(out=hv(m["kk"]), in0=hv(F1), in1=bc(sm[:, 0, :]), op=ALU.mult),
                 reads=[BF1, Bsm], writes=[m["Bkk"]])
            yield
            P.op("dve", lambda e: e.scalar_tensor_tensor(out=F1[:], in0=m["a"][:], scalar=-1.0, in1=m["kab"][:], op0=ALU.add, op1=ALU.mult),
                 reads=[m["Ba"], Bw], writes=[BF1])
            P.op("dve", lambda e: e.scalar_tensor_tensor(out=F2[:], in0=F1[:], scalar=1.0, in1=k_, op0=ALU.add, op1=ALU.mult),
                 reads=[BF1, Bk], writes=[BF2])
            yield
            P.op("pool", lambda e: e.tensor_tensor(out=F1[:], in0=F2[:], in1=m["rkb"][:], op=ALU.mult), reads=[BF2, Bw], writes=[BF1])
            P.op("dve", lambda e: e.tensor_tensor(out=F1[:], in0=F1[:], in1=r_, op=ALU.mult), reads=[BF1, Br], writes=[BF1])
            P.op("dve", lambda e: e.tensor_reduce(out=sm[:, 1, :], in_=hv(F1), axis=AX.X, op=ALU.add), reads=[BF1], writes=[Bsm])
            yield
            P.op("dve", lambda e: e.tensor_tensor(out=m["kt"][:], in0=F2[:], in1=m["Ginv"][:], op=ALU.mult),
                 reads=[BF2, m["BGinv"]], writes=[m["Bkt"]])
            P.op("pool", lambda e: e.tensor_tensor(out=m["rt"][:], in0=r_, in1=m["G"][:], op=ALU.mult),
                 reads=[Br, m["BG"]], writes=[m["Brt"]])
            P.op("pool", lambda e: e.tensor_tensor(out=F1[:], in0=m["kk"][:], in1=m["a"][:], op=ALU.mult),
                 reads=[m["Bkk"], m["Ba"]], writes=[BF1])
            P.op("dve", lambda e: e.tensor_tensor(out=m["bt"][:], in0=F1[:], in1=m["Ginv"][:], op=ALU.mult),
                 reads=[BF1, m["BGinv"]], writes=[m["Bbt"]])
            P.op("dve", lambda e: e.scalar_tensor_tensor(out=m["at"][:], in0=m["kk"][:], scalar=-1.0, in1=m["Gprev"][:],
                                                         op0=ALU.mult, op1=ALU.mult), reads=[m["Bkk"], m["BGprev"]], writes=[m["Bat"]])
            yield
            for src, Bsrc, dst, Bdst, two in ((m["at"], m["Bat"], m["arT"], m["BarT"], 0), (m["rt"], m["Brt"], m["arT"], m["BarT"], 1),
                                              (m["bt"], m["Bbt"], m["bT"], m["BbT"], None), (m["kt"], m["Bkt"], m["kT"], m["BkT"], None)):
                b = nb8(k)
                ptb = k.ps[b][0:64, :].bitcast(BF16)
                for h in range(16):
                    P.op("pe", lambda e, h=h, src=src, ptb=ptb: e.transpose(out=ptb[:, h * 64:(h + 1) * 64], in_=src[:, h * 64:(h + 1) * 64],
                                                                             identity=k.ident[0:64, 0:64]),
                         reads=[Bsrc, k.Bconst], writes=[k.Bps[b]])
                pv = ptb.rearrange("p (h t) -> p h t", h=16)
                if two is None:
                    P.op("act", lambda e, pv=pv, dst=dst: e.activation(out=hv(dst), in_=pv, func=AF.Copy), reads=[k.Bps[b]], writes=[Bdst])
                else:
                    P.op("act", lambda e, pv=pv, dst=dst, two=two: e.activation(out=dst[:, :, two, :], in_=pv, func=AF.Copy),
                         reads=[k.Bps[b]], writes=[Bdst])
            yield
            for (lt, Blt, M, BM) in ((m["bT"], m["BbT"], m["M1"], m["BM1"]), (m["kT"], m["BkT"], m["M2"], m["BM2"])):
                for g in range(4):
                    b = nb8(k)
                    for hh in range(4):
                        h = g * 4 + hh
                        P.op("pe", lambda e, b=b, h=h, hh=hh, lt=lt: e.matmul(
                            k.ps[b][0:64, hh * 128:(hh + 1) * 128], lhsT=lt[:, h * 64:(h + 1) * 64],
                            rhs=m["arT"][:, h].rearrange("p a t -> p (a t)"), start=True, stop=True),
                            reads=[Blt, m["BarT"]], writes=[k.Bps[b]])
                    P.op("dve", lambda e, b=b, g=g, M=M: e.tensor_tensor(
                        out=M[:, g * 4:(g + 1) * 4].rearrange("p h a t -> p h (a t)"),
                        in0=k.ps[b][0:64, :].rearrange("p (h x) -> p h x", h=4),
                        in1=k.m1[:].unsqueeze(1).to_broadcast([64, 4, 128]), op=ALU.mult),
                        reads=[k.Bps[b], k.Bconst], writes=[BM])
                    if M is m["M1"]:
                        P.op("dve", lambda e, b=b, g=g: e.tensor_tensor(
                            out=hv(m["QA"])[:, g * 4:(g + 1) * 4],
                            in0=k.ps[b][0:64, :].rearrange("p (h x) -> p h x", h=4)[:, :, 0:64],
                            in1=k.m1[:, 0:64].unsqueeze(1).to_broadcast([64, 4, 64]), op=ALU.mult),
                            reads=[k.Bps[b], k.Bconst], writes=[m["BQA"]])
            for g in range(2):
                b = nb8(k)
                for hh in range(8):
                    h = g * 8 + hh
                    P.op("pe", lambda e, b=b, h=h, hh=hh: e.matmul(k.ps[b][0:64, hh * 64:(hh + 1) * 64], lhsT=m["arT"][:, h, 0, :],
                                                                   rhs=m["bT"][:, h * 64:(h + 1) * 64], start=True, stop=True),
                         reads=[m["BarT"], m["BbT"]], writes=[k.Bps[b]])
                P.op("dve", lambda e, b=b, g=g: e.tensor_tensor(
                    out=hv(m["PA"])[:, g * 8:(g + 1) * 8], in0=k.ps[b][0:64, :].rearrange("p (h x) -> p h x", h=8),
                    in1=k.sl64[:].unsqueeze(1).to_broadcast([64, 8, 64]), op=ALU.mult),
                    reads=[k.Bps[b], k.Bconst], writes=[m["BPA"]])
            yield
            TTf = hv(m["TTf"])
            P.op("dve", lambda e: e.tensor_tensor(out=TTf, in0=hv(m["QA"]),
                                                  in1=k.ident[0:64, 0:64].unsqueeze(1).to_broadcast([64, 16, 64]), op=ALU.add),
                 reads=[m["BQA"], k.Bconst], writes=[m["BTTf"]])
            yield
            Qc, BQc = hv(m["QA"]), m["BQA"]
            Pc, BPc = hv(m["PA"]), m["BPA"]
            for lv in range(5):
                Qn, BQn = (hv(m["QB"]), m["BQB"]) if lv % 2 == 0 else (hv(m["QA"]), m["BQA"])
                Pn, BPn = (hv(m["PB"]), m["BPB"]) if lv % 2 == 0 else (hv(m["PA"]), m["BPA"])
                jobs = [(Qc, BQc, Pc, BPc, Pn, BPn)]
                if lv < 4:
                    jobs.append((Pc, BPc, Qc, BQc, Qn, BQn))
                for (L, BL, R, BR, O, BO) in jobs:
                    for g in range(2):
                        b = nb8(k)
                        for hh in range(8):
                            h = g * 8 + hh
                            P.op("pe", lambda e, b=b, h=h, hh=hh, L=L, R=R: e.matmul(k.ps[b][0:64, hh * 64:(hh + 1) * 64], lhsT=L[:, h, :],
                                                                                   rhs=R[:, h, :], start=True, stop=True),
                                 reads=[BL, BR], writes=[k.Bps[b]])
                        P.op("act", lambda e, b=b, g=g, O=O: e.activation(out=O[:, g * 8:(g + 1) * 8],
                                                                          in_=k.ps[b][0:64, :].rearrange("p (h x) -> p h x", h=8),
                                                                          func=AF.Copy), reads=[k.Bps[b]], writes=[BO])
                for g in range(2):
                    b = nb8(k)
                    for hh in range(8):
                        h = g * 8 + hh
                        P.op("pe", lambda e, b=b, h=h, hh=hh, Pn=Pn: e.matmul(k.ps[b][0:64, hh * 64:(hh + 1) * 64], lhsT=Pn[:, h, :],
                                                                             rhs=TTf[:, h, :], start=True, stop=True),
                             reads=[BPn, m["BTTf"]], writes=[k.Bps[b]])
                    P.op("dve", lambda e, b=b, g=g: e.tensor_tensor(out=TTf[:, g * 8:(g + 1) * 8],
                                                                    in0=k.ps[b][0:64, :].rearrange("p (h x) -> p h x", h=8),
                                                                    in1=TTf[:, g * 8:(g + 1) * 8], op=ALU.add),
                         reads=[k.Bps[b], m["BTTf"]], writes=[m["BTTf"]])
                Qc, BQc, Pc, BPc = Qn, BQn, Pn, BPn
                yield
            P.op("act", lambda e: e.activation(out=m["TT"][:], in_=m["TTf"][:], func=AF.Copy), reads=[m["BTTf"]], writes=[m["BTT"]])
            TT = hv(m["TT"])

            vh = lambda h: m["rkv"][:, c, 2, h * 64:(h + 1) * 64]
            yield
            headmm(evac_to(m["akv"], m["Bakv"]), [(lambda h: m["M2"][:, h, 0, :], vh, [m["BM2"], Bv])])
            headmm(evac_to(m["TAT"], m["BTAT"], "dve"),
                   [(lambda h: m["at"][:, h * 64:(h + 1) * 64], lambda h: TT[:, h, :], [m["Bat"], m["BTT"]])])
            yield

        def chunk_dep(m, c):
            F0, F1, F2 = m["F0"], m["F1"], m["F2"]
            BF0, BF1, BF2 = m["BF0"], m["BF1"], m["BF2"]
            sm, Bsm = m["sm"], m["Bsm"]
            cs_ = slice(c * 64, (c + 1) * 64)
            Br, Bk, Bv = m["Brkv"][c]
            vv = m["rkv"][:, c, 2, :].rearrange("p (h j) -> p h j", h=16)
            TT = hv(m["TT"])
            vh = lambda h: m["rkv"][:, c, 2, h * 64:(h + 1) * 64]
            headmm(evac_to(m["Usb"], m["BUsb"]),
                   [(lambda h: TT[:, h, :], lambda h: hv(m["akv"])[:, h, :], [m["BTT"], m["Bakv"]]),
                    (lambda h: hv(m["TAT"])[:, h, :], lambda h: hv(m["Hbf"])[:, h, :], [m["BTAT"], m["BHbf"]])])
            headmm(evac_to(F1, BF1),
                   [(lambda h: m["arT"][:, h, 1, :], lambda h: hv(m["Hbf"])[:, h, :], [m["BarT"], m["BHbf"]]),
                    (lambda h: m["M1"][:, h, 1, :], lambda h: hv(m["Usb"])[:, h, :], [m["BM1"], m["BUsb"]]),
                    (lambda h: m["M2"][:, h, 1, :], vh, [m["BM2"], Bv])])

            def evac_H(b, g):
                gs = slice(g * 8, (g + 1) * 8)
                P.op("dve", lambda e: e.tensor_tensor(out=hv(H)[:, gs], in0=k.ps[b][0:64, :].rearrange("p (h x) -> p h x", h=8),
                                                      in1=hv(H)[:, gs], op=ALU.add), reads=[k.Bps[b], BH], writes=[BH])
                P.op("dve", lambda e: e.tensor_tensor(out=hv(H)[:, gs], in0=hv(H)[:, gs],
                                                      in1=m["GL"][:, gs].unsqueeze(2).to_broadcast([64, 8, 64]), op=ALU.mult),
                     reads=[BH, m["BGL"]], writes=[BH])
            headmm(evac_H,
                   [(lambda h: m["bt"][:, h * 64:(h + 1) * 64], lambda h: hv(m["Usb"])[:, h, :], [m["Bbt"], m["BUsb"]]),
                    (lambda h: m["kt"][:, h * 64:(h + 1) * 64], vh, [m["Bkt"], Bv])])
            P.op("act", lambda e: e.activation(out=m["Hbf"][:], in_=H[:], func=AF.Copy), reads=[BH], writes=[m["BHbf"]])
            P.op("dve", lambda e: e.tensor_reduce(out=sm[:, 2, :], in_=hv(F1), axis=AX.X, op=ALU.add), reads=[BF1], writes=[Bsm])
            P.op("pool", lambda e: e.tensor_tensor(out=F2[:], in0=F1[:], in1=F1[:], op=ALU.mult), reads=[BF1], writes=[BF2])
            P.op("dve", lambda e: e.tensor_reduce(out=sm[:, 3, :], in_=hv(F2), axis=AX.X, op=ALU.add), reads=[BF2], writes=[Bsm])
            P.op("dve", lambda e: e.tensor_scalar(out=sm[:, 2, :], in0=sm[:, 2, :], scalar1=1.0 / 64.0, scalar2=None, op0=ALU.mult),
                 reads=[Bsm], writes=[Bsm])
            P.op("dve", lambda e: e.tensor_tensor(out=sm[:, 4, :], in0=sm[:, 2, :], in1=sm[:, 2, :], op=ALU.mult), reads=[Bsm], writes=[Bsm])
            P.op("dve", lambda e: e.scalar_tensor_tensor(out=sm[:, 3, :], in0=sm[:, 3, :], scalar=1.0 / 64.0, in1=sm[:, 4, :],
                                                         op0=ALU.mult, op1=ALU.subtract), reads=[Bsm], writes=[Bsm])
            P.op("pool", lambda e: e.tensor_scalar(out=sm[:, 3, :], in0=sm[:, 3, :], scalar1=64e-5, scalar2=None, op0=ALU.add),
                 reads=[Bsm], writes=[Bsm])
            P.op("pool", lambda e: e.tensor_tensor(out=sm[:, 3, :], in0=sm[:, 3, :], in1=k.mhalf[0:64, 0:1].to_broadcast([64, 16]),
                                                   op=ALU.pow), reads=[Bsm, k.Bconst], writes=[Bsm])
            P.op("dve", lambda e: e.tensor_tensor(out=hv(F1), in0=hv(F1), in1=bc(sm[:, 2, :]), op=ALU.subtract), reads=[BF1, Bsm], writes=[BF1])
            P.op("dve", lambda e: e.tensor_tensor(out=hv(F1), in0=hv(F1), in1=bc(sm[:, 3, :]), op=ALU.mult), reads=[BF1, Bsm], writes=[BF1])
            P.op("pool", lambda e: e.tensor_tensor(out=F1[:], in0=F1[:], in1=m["lgb"][:], op=ALU.mult), reads=[BF1, Bw], writes=[BF1])
            P.op("pool", lambda e: e.tensor_tensor(out=F1[:], in0=F1[:], in1=m["lbb"][:], op=ALU.add), reads=[BF1, Bw], writes=[BF1])
            P.op("dve", lambda e: e.tensor_tensor(out=hv(F2), in0=vv, in1=bc(sm[:, 1, :]), op=ALU.mult), reads=[Bv, Bsm], writes=[BF2])
            P.op("dve", lambda e: e.tensor_tensor(out=F1[:], in0=F1[:], in1=F2[:], op=ALU.add), reads=[BF1, BF2], writes=[BF1])
            P.op("dve", lambda e: e.tensor_tensor(out=m["zb"][:], in0=F1[:], in1=m["gsb"][:], op=ALU.mult),
                 reads=[BF1, m["Bgsb"]], writes=[m["Bzb"]])
            b = nb8(k)
            ptb = k.ps[b][:].bitcast(BF16)
            for kk in range(8):
                P.op("pe", lambda e, kk=kk, ptb=ptb: e.transpose(out=ptb[:, kk * 64:(kk + 1) * 64], in_=m["zb"][:, kk * 128:(kk + 1) * 128],
                                                                 identity=k.ident[0:64, 0:64]),
                     reads=[m["Bzb"], k.Bconst], writes=[k.Bps[b]])
            P.op("act", lambda e, ptb=ptb: e.activation(out=hT[:, :, cs_], in_=ptb[:, 0:512].rearrange("p (a t) -> p a t", a=8), func=AF.Copy),
                 reads=[k.Bps[b]], writes=[m["BhT"]])

        gens = [chunk_indep(cxs[0], 0), chunk_indep(cxs[1], 1)]
        while gens:
            for g_ in list(gens):
                try:
                    next(g_)
                except StopIteration:
                    gens.remove(g_)
        chunk_dep(cxs[0], 0)
        chunk_dep(cxs[1], 1)
        bo = [nb8(k), nb8(k)]
        for u in range(4):
            sl = ring_load(k, m, 12 + u)
            for c2 in range(2):
                kk = 2 * u + c2
                for half in range(2):
                    P.op("pe", lambda e, sl=sl, c2=c2, kk=kk, half=half: e.matmul(
                        k.ps[bo[half]][:], lhsT=hT[:, kk, :], rhs=m["ring"][:, sl, c2 * 1024 + half * 512:c2 * 1024 + (half + 1) * 512],
                        start=(kk == 0), stop=(kk == 7)), reads=[m["BhT"], m["Bring"][sl]], writes=[k.Bps[bo[half]]])
        postnorm_residual(k, xs_, bo, m["gpost"], m["Bgpost"], 1.0, sc)
        P.dma("sp", lambda e: e.dma_start(out=k.scrX[gt], in_=k.xres[:, xs_, :]), reads=[k.Bx[xs_]], writes=[k.Bscr[gt]],
              dkey=f"x{xs_}")


def rwkv_phase(k, layer, j):
    P = k.P
    if not hasattr(k, "scrX"):
        k.scrX = k.nc.dram_tensor("scrX", [NT, 128, D], F32, kind="Internal").ap()
        k.Bscr = [Buf(f"scr{i}") for i in range(NT)]
    P.barrier()
    for i in range(0, NT, 2):
        P.dma("sp", lambda e: e.dma_start(out=k.scrX[i:i + 2].rearrange("t p d -> p t d"), in_=k.xres[:, i:i + 2, :]),
              reads=[k.Bx[i], k.Bx[i + 1]], writes=[k.Bscr[i], k.Bscr[i + 1]], dkey=f"x{i // 2}")
    P.barrier()
    m = rwkv_setup(k, layer)
    P.dma("sp", lambda e: e.dma_start(out=m["gpost"][:], in_=k.ng_d[layer * 6 + 3, :].partition_broadcast(128)),
          writes=[m["Bgpost"]], dkey="gpost")
    rwkv_seq(k, m, layer, 0)
    rwkv_seq(k, m, layer, 1)
    P.barrier()
    for i in range(0, NT, 2):
        P.dma("sp", lambda e: e.dma_start(out=k.xres[:, i:i + 2, :], in_=k.scrX[i:i + 2].rearrange("t p d -> p t d")),
              reads=[k.Bscr[i], k.Bscr[i + 1]], writes=[k.Bx[i], k.Bx[i + 1]], dkey=f"x{i // 2}")
    P.barrier()


def _units_lin(w_in, n_f, tm0, n_tm, w_out):
    wf = w_in[:, :n_f * 128].reshape(8, 128, n_f, 128).transpose(2, 1, 0, 3)
    wf = wf.reshape(n_f // 2, 2, 128, 1024).transpose(0, 2, 1, 3).reshape(n_f // 2, 128, 2048)
    wt = w_in[:, tm0:tm0 + n_tm * 512].reshape(2, 4, 128, n_tm, 512).transpose(3, 0, 2, 1, 4)
    wt = wt.reshape(n_tm * 2, 128, 2048)
    nko = w_out.shape[0] // 128
    wo = w_out.reshape(nko // 2, 2, 128, 1024).transpose(0, 2, 1, 3).reshape(nko // 2, 128, 2048)
    return np.ascontiguousarray(np.concatenate([wf, wt, wo], axis=0), dtype=np.float32)


def _prep_inputs(inputs, x_override=None):
    xin = inputs["x"] if x_override is None else x_override
    x = np.ascontiguousarray(xin, dtype=np.float32).reshape(NCORES, NT, 128, D)
    wgu = inputs["ffn_w_gu"].reshape(8, 8, 128, 2, NJ, 128)
    wgu = np.ascontiguousarray(wgu.transpose(0, 4, 2, 3, 1, 5)).reshape(8 * NJ, 128, 2, 1024)
    wd = np.ascontiguousarray(inputs["ffn_w_down"]).reshape(8 * NJ, 128, 1024)
    ng = np.ascontiguousarray(inputs["norm_g"]).reshape(24, D)
    ngT = np.ascontiguousarray(ng.reshape(24, 8, 128).transpose(2, 0, 1)).reshape(128, 24 * 8)
    shared = {"wgu": wgu, "wd": wd, "ng": ng, "ngT": ngT}
    for j, layer in ((0, 0), (1, 3)):
        w_in = inputs["ml_w_in"][j]
        shared[f"wm{layer}"] = _units_lin(w_in, 8, 1024, 4, inputs["ml_w_out"][j])
        shared[f"wif{layer}"] = np.ascontiguousarray(w_in[:, 3072:3080].reshape(8, 128, 8).transpose(1, 0, 2)).reshape(128, 64)
        shared[f"bif{layer}"] = np.ascontiguousarray(inputs["ml_b_if"][j]).reshape(1, 8)
        shared[f"convT{layer}"] = np.ascontiguousarray(
            inputs["ml_conv_w"][j].reshape(4, 8, 128).transpose(2, 1, 0)).reshape(128, 32)
        shared[f"mng{layer}"] = np.ascontiguousarray(inputs["ml_norm_g"][j]).reshape(1, 1024)
    shared["wm2"] = _units_lin(inputs["rt_w_in"][0], 16, 2048, 8, inputs["rt_w_out"][0])
    wtm = lambda w: np.ascontiguousarray(w.reshape(2, 4, 128, 2, 512).transpose(3, 0, 2, 1, 4)).reshape(4, 128, 2048)
    wo = inputs["rw_w_out"][0].reshape(4, 2, 128, 1024).transpose(0, 2, 1, 3).reshape(4, 128, 2048)
    shared["rw_units"] = np.ascontiguousarray(np.concatenate(
        [wtm(inputs["rw_w_rkv"][0, 0]), wtm(inputs["rw_w_rkv"][0, 1]), wtm(inputs["rw_w_rkv"][0, 2]), wo], axis=0), dtype=np.float32)
    for nm in ("w1", "a1", "g1"):
        w = inputs["rw_" + nm][0]
        shared["rw_" + nm] = np.ascontiguousarray(w.reshape(8, 128, w.shape[1]).transpose(1, 0, 2))
    for nm in ("w2", "a2", "g2"):
        shared["rw_" + nm] = np.ascontiguousarray(inputs["rw_" + nm][0])
    shared["rw_w0r"] = np.ascontiguousarray(inputs["rw_w0"][0]).reshape(1, 1024)
    shared["rw_a0r"] = np.ascontiguousarray(inputs["rw_a0"][0]).reshape(1, 1024)
    shared["rw_muT"] = np.ascontiguousarray(inputs["rw_mu"][0].reshape(6, 8, 128).transpose(2, 0, 1)).reshape(128, 48)
    for nm, src in (("kkb", "rw_k_k"), ("kab", "rw_k_a"), ("lgb", "rw_ln_g"), ("lbb", "rw_ln_b"), ("rkb", "rw_r_k")):
        shared["rw_" + nm] = np.ascontiguousarray(inputs[src][0]).reshape(1, 1024)
    pos = np.ascontiguousarray(inputs["positions"]).astype(np.int32).reshape(NCORES, 1, 2 * SEQ)
    return [dict(shared, x=x[c], pos=pos[c]) for c in range(NCORES)]


def run_partial(inputs, subs=tuple(range(12)), trace=False, cores=NCORES, x_override=None):
    nc, names = build_program(tuple(subs))
    in_maps = _prep_inputs(inputs, x_override)
    in_maps = [{n: m[n] for n in names if n in m} for m in in_maps[:cores]]
    res = run_bass_kernel_spmd(nc, in_maps, core_ids=list(range(cores)), trace=trace)
    out = np.stack([np.asarray(r["out"]) for r in res.results], axis=0)
    return out.reshape(2 * cores, SEQ, D).astype(np.float32), res


def kernel(**inputs):
    out, _ = run_partial(inputs)
    return out
```

```python
import numpy as np
import concourse.bass as bass
import concourse.mybir as mybir
from concourse.bass_utils import run_bass_kernel_spmd

F32 = mybir.dt.float32
BF16 = mybir.dt.bfloat16
I32 = mybir.dt.int32
ALU = mybir.AluOpType
AF = mybir.ActivationFunctionType
AX = mybir.AxisListType

NCORES = 8
D = 1024
DFF = 2816
NJ = DFF // 128
TPC = 4096
NT = TPC // 128
SEQ = 2048
EPS = 1e-6

ENGS = ["pe", "dve", "act", "pool", "sp"]
CHUNK = 8000
SAME_ENG_SYNC = True


class Buf:
    __slots__ = ("name", "lw", "rd", "excl")

    def __init__(self, name, excl=False):
        self.name = name
        self.lw = None
        self.rd = {}
        self.excl = excl


def _freeze(fn):
    import types
    if fn is None or fn.__closure__ is None:
        return fn
    cells = []
    for c in fn.__closure__:
        try:
            cells.append(types.CellType(c.cell_contents))
        except ValueError:
            cells.append(c)
    return types.FunctionType(fn.__code__, fn.__globals__, fn.__name__, fn.__defaults__, tuple(cells))


class Prog:
    def __init__(self, nc):
        self.nc = nc
        self.ops = {e: [] for e in ENGS}
        self.cnt = {e: 0 for e in ENGS}
        self.seen = {e: {} for e in ENGS}
        self.dcnt = {}

    def _deps(self, eng, reads, writes):
        need = {}

        def add(k, v):
            if need.get(k, 0) < v:
                need[k] = v

        for b in reads:
            if b.lw is not None:
                add(*b.lw)
            if b.excl:
                for k, v in b.rd.items():
                    if k != ("e", eng):
                        add(k, v)
        for b in writes:
            if b.lw is not None:
                add(*b.lw)
            for k, v in b.rd.items():
                add(k, v)
        s = self.seen[eng]
        waits = []
        for k, v in need.items():
            if k == ("e", eng) and (eng == "pe" or not SAME_ENG_SYNC):
                continue
            if s.get(k, 0) >= v:
                continue
            s[k] = v
            waits.append((k, v))
        return waits

    def _mark(self, tok, reads, writes):
        k, v = tok
        for b in reads:
            if b.rd.get(k, 0) < v:
                b.rd[k] = v
        for b in writes:
            b.lw = tok
            b.rd = {}

    def op(self, eng, fn, reads=(), writes=()):
        waits = self._deps(eng, reads, writes)
        self.cnt[eng] += 1
        tok = (("e", eng), self.cnt[eng])
        self.ops[eng].append((_freeze(fn), waits, tok))
        self._mark(tok, reads, writes)

    def dma(self, eng, fn, reads=(), writes=(), dkey=None):
        waits = self._deps(eng, reads, writes)
        n = self.dcnt.get(dkey, 0) + 16
        self.dcnt[dkey] = n
        tok = (("d", dkey), n)
        self.ops[eng].append((_freeze(fn), waits, tok))
        self._mark(tok, reads, writes)

    def barrier(self):
        for e in ENGS:
            s = self.seen[e]
            waits = []
            for o in ENGS:
                if o == e or self.cnt[o] == 0:
                    continue
                k = ("e", o)
                if s.get(k, 0) < self.cnt[o]:
                    s[k] = self.cnt[o]
                    waits.append((k, self.cnt[o]))
            for dk, n in self.dcnt.items():
                k = ("d", dk)
                if s.get(k, 0) < n:
                    s[k] = n
                    waits.append((k, n))
            if waits:
                self.ops[e].append((None, waits, None))

    def run(self, final_bufs=()):
        nc = self.nc
        from contextlib import ExitStack
        with ExitStack() as st:
            sems = {}
            for e in ENGS:
                nchunks = max(1, (self.cnt[e] + CHUNK - 1) // CHUNK)
                for c in range(nchunks):
                    sems[(("e", e), c)] = st.enter_context(nc.semaphore(f"s_{e}_{c}"))
            for dk in self.dcnt:
                sems[(("d", dk), 0)] = st.enter_context(nc.semaphore(f"d_{dk}"))
            block = st.enter_context(nc.Block())

            def semval(k, v):
                if k[0] == "e":
                    c = (v - 1) // CHUNK
                    return sems[(k, c)], v - c * CHUNK
                return sems[(k, 0)], v

            def emit(ename, eng):
                for fn, waits, tok in self.ops[ename]:
                    for (k, v) in waits:
                        s, vv = semval(k, v)
                        eng.wait_ge(s, vv)
                    if fn is None:
                        continue
                    ins = fn(eng)
                    k, v = tok
                    if k[0] == "e":
                        s, vv = semval(k, v)
                        ins.then_inc(s, 1)
                    else:
                        ins.then_inc(sems[(k, 0)], 16)
                if ename == "sp":
                    need = {}
                    for b in final_bufs:
                        toks = list(b.rd.items())
                        if b.lw is not None:
                            toks.append(b.lw)
                        for k, v in toks:
                            need[k] = max(need.get(k, 0), v)
                    for k, v in need.items():
                        s, vv = semval(k, v)
                        eng.wait_ge(s, vv)

            @block.tensor
            def _(e):
                emit("pe", e)

            @block.vector
            def _(e):
                emit("dve", e)

            @block.scalar
            def _(e):
                emit("act", e)

            @block.gpsimd
            def _(e):
                emit("pool", e)

            @block.sync
            def _(e):
                emit("sp", e)


class Arena:
    def __init__(self, nc, base, limit):
        self.nc, self.base, self.limit = nc, base, limit
        self.off = base
        self.gen = 0

    def reset(self):
        self.off = self.base
        self.gen += 1

    def alloc(self, name, shape, dtype):
        nbytes = int(np.prod(shape[1:])) * (4 if dtype in (F32, I32) else 2)
        nbytes = (nbytes + 63) // 64 * 64
        assert self.off + nbytes <= self.limit, (name, self.off, nbytes, self.limit)
        t = self.nc.alloc_sbuf_tensor_at(f"{name}_g{self.gen}", list(shape), dtype, offset=self.off)
        self.off += nbytes
        return t


class K:
    pass


def build_program(subs=tuple(range(12))):
    nc = bass.Bass("TRN2", target_bir_lowering=False)
    P = Prog(nc)
    k = K()
    k.nc, k.P = nc, P
    k.dram_names = []

    def dram(name, shape, dtype=F32, kind="ExternalInput"):
        k.dram_names.append(name)
        return nc.dram_tensor(name, list(shape), dtype, kind=kind).ap()

    k.dram = dram
    k.x_d = dram("x", [NT, 128, D])
    k.out_d = dram("out", [NT, 128, D], kind="ExternalOutput")
    k.ng_d = dram("ng", [24, D])
    k.ngT_d = dram("ngT", [128, 24 * 8])
    if any(s % 3 != 1 for s in subs):
        k.wgu_d = dram("wgu", [8 * NJ, 128, 2, 1024])
        k.wd_d = dram("wd", [8 * NJ, 128, 1024])

    RES_BYTES = NT * D * 4
    SB0 = 16640
    k.xres = nc.alloc_sbuf_tensor_at("xres", [128, NT, D], F32, offset=SB0 + 4096)
    k.Bx = [Buf(f"x{i}") for i in range(NT)]
    pers = Arena(nc, SB0, SB0 + 4096)
    k.big_arena = Arena(nc, SB0 + 4096 + 2 * D * 4, 229376)
    k.ident = pers.alloc("ident", [128, 128], BF16)
    k.ngT = pers.alloc("ngT", [128, 24 * 8], F32)
    k.ones = pers.alloc("ones", [128, 128], BF16)
    k.mhalf = pers.alloc("mhalf", [128, 1], F32)
    k.tri = pers.alloc("tri", [128, 128], BF16)
    k.trif = pers.alloc("trif", [128, 128], F32)
    k.onesf = pers.alloc("onesf", [128, 128], F32)
    k.sl64 = pers.alloc("sl64", [64, 64], BF16)
    k.m1 = pers.alloc("m1", [64, 128], BF16)
    k.Bconst = Buf("const")
    k.arena = Arena(nc, SB0 + RES_BYTES + 4096, 229376)
    k.ps = [nc.alloc_psum_tensor(f"ps{i}", [128, 512], F32) for i in range(8)]
    k.Bps = [Buf(f"ps{i}", excl=True) for i in range(8)]
    k.rot = 0

    P.op("pool", lambda e: e.memset(k.ones[:], 1.0), writes=[k.Bconst])
    P.op("pool", lambda e: e.memset(k.onesf[:], 1.0), writes=[k.Bconst])
    P.op("pool", lambda e: e.affine_select(out=k.ident[:], in_=k.ones[:], pattern=[[-1, 128]],
                                           compare_op=ALU.is_equal, fill=0.0, base=0, channel_multiplier=1),
         reads=[k.Bconst], writes=[k.Bconst])
    for tt in (k.tri, k.trif):
        P.op("pool", lambda e, tt=tt: e.affine_select(out=tt[:], in_=(k.ones if tt is k.tri else k.onesf)[:],
                                                      pattern=[[1, 128]], compare_op=ALU.is_ge, fill=0.0, base=0,
                                                      channel_multiplier=-1),
             reads=[k.Bconst], writes=[k.Bconst])
    P.op("pool", lambda e: e.memset(k.mhalf[:], -0.5), writes=[k.Bconst])
    P.op("pool", lambda e: e.affine_select(out=k.sl64[:], in_=k.ones[0:64, 0:64], pattern=[[-1, 64]],
                                           compare_op=ALU.is_gt, fill=0.0, base=0, channel_multiplier=1),
         reads=[k.Bconst], writes=[k.Bconst])
    P.op("pool", lambda e: e.affine_select(out=k.m1[:, 0:64], in_=k.ones[0:64, 0:64], pattern=[[1, 64]],
                                           compare_op=ALU.is_gt, fill=0.0, base=0, channel_multiplier=-1),
         reads=[k.Bconst], writes=[k.Bconst])
    P.op("pool", lambda e: e.affine_select(out=k.m1[:, 64:128], in_=k.ones[0:64, 0:64], pattern=[[1, 64]],
                                           compare_op=ALU.is_ge, fill=0.0, base=0, channel_multiplier=-1),
         reads=[k.Bconst], writes=[k.Bconst])
    P.dma("sp", lambda e: e.dma_start(out=k.ngT[:], in_=k.ngT_d[:, :]), writes=[k.Bconst], dkey="ngT")

    for i in range(0, NT, 2):
        P.dma("sp", lambda e, i=i: e.dma_start(out=k.xres[:, i:i + 2, :],
                                                in_=k.x_d[i:i + 2].rearrange("t p d -> p t d")),
              writes=[k.Bx[i], k.Bx[i + 1]], dkey=f"x{i // 2}")

    for sub in subs:
        layer, s3 = sub // 3, sub % 3
        if s3 == 0:
            ffn_phase(k, layer, 0)
        elif s3 == 2:
            ffn_phase(k, layer, 1)
        elif layer % 3 == 0:
            lin_mixer_phase(k, layer, 0, layer // 3)
        elif layer % 3 == 2:
            lin_mixer_phase(k, layer, 2, layer // 3)
        else:
            rwkv_phase(k, layer, layer // 3)

    for i in range(0, 16 if RW_DEBUG else NT, 2):
        P.dma("sp", lambda e, i=i: e.dma_start(out=k.out_d[i:i + 2].rearrange("t p d -> p t d"),
                                                in_=k.xres[:, i:i + 2, :]),
              reads=[k.Bx[i], k.Bx[i + 1]], dkey=f"x{i // 2}")
    P.run(final_bufs=k.Bx)
    return nc, k.dram_names


def rstd_from_ssq(k, ssq_ap, rstd_ap, Bs, Br, n_part=128):
    P = k.P
    P.op("pool", lambda e: e.tensor_scalar(out=rstd_ap, in0=ssq_ap, scalar1=1.0 / D, scalar2=EPS,
                                           op0=ALU.mult, op1=ALU.add), reads=[Bs], writes=[Br])
    P.op("pool", lambda e: e.tensor_tensor(out=rstd_ap, in0=rstd_ap, in1=k.mhalf[0:n_part, :], op=ALU.pow),
         reads=[Br, k.Bconst], writes=[Br])


def prenorm_tile(k, ti, gidx, xnT, BxnT, col, sc):
    P = k.P
    par = ti % 2
    junk, Bjunk = sc["junk"], sc["Bjunk"]
    xs, Bxs = sc["xs"][par], sc["Bxs"][par]
    ssq, Bssq = sc["ssq"][par], sc["Bssq"][par]
    rstd, Brstd = sc["rstd"][par], sc["Brstd"][par]
    psb = sc["ps_tr"][par]
    P.op("act", lambda e: e.activation(out=junk[:], in_=k.xres[:, ti, :], func=AF.Square, accum_out=ssq[:]),
         reads=[k.Bx[ti]], writes=[Bjunk, Bssq])
    rstd_from_ssq(k, ssq[:], rstd[:], Bssq, Brstd)
    P.op("act", lambda e: e.activation(out=xs[:], in_=k.xres[:, ti, :], func=AF.Copy, scale=rstd[:]),
         reads=[k.Bx[ti], Brstd], writes=[Bxs])
    pst = k.ps[psb][:].bitcast(BF16)
    for kk in range(8):
        P.op("pe", lambda e, kk=kk: e.transpose(out=pst[:, kk * 128:(kk + 1) * 128],
                                                in_=xs[:, kk * 128:(kk + 1) * 128], identity=k.ident[:]),
             reads=[Bxs, k.Bconst], writes=[k.Bps[psb]])
    gT = k.ngT[:, gidx * 8:(gidx + 1) * 8].unsqueeze(2).to_broadcast([128, 8, 128])
    P.op("dve", lambda e: e.tensor_tensor(out=xnT[:, :, col:col + 128],
                                          in0=pst.rearrange("p (a t) -> p a t", a=8), in1=gT, op=ALU.mult),
         reads=[k.Bps[psb], k.Bconst], writes=[BxnT])


def postnorm_residual(k, ti, halves, gpost, Bgpost, coef, sc):
    P = k.P
    par = ti % 2
    junk, Bjunk = sc["junk"], sc["Bjunk"]
    ss2, Bss2 = sc["ss2"][par], sc["Bss2"][par]
    rs2, Brs2 = sc["rs2"][par], sc["Brs2"][par]
    tmp, Btmp = sc["tmp"][par], sc["Btmp"][par]
    for h, pb in enumerate(halves):
        P.op("act", lambda e, h=h, pb=pb: e.activation(out=junk[:, 0:512], in_=k.ps[pb][:], func=AF.Square,
                                                       accum_out=ss2[:, h:h + 1]),
             reads=[k.Bps[pb]], writes=[Bjunk, Bss2])
    P.op("dve", lambda e: e.tensor_tensor(out=ss2[:, 2:3], in0=ss2[:, 0:1], in1=ss2[:, 1:2], op=ALU.add),
         reads=[Bss2], writes=[Bss2])
    rstd_from_ssq(k, ss2[:, 2:3], rs2[:], Bss2, Brs2)
    for h, pb in enumerate(halves):
        P.op("dve", lambda e, h=h, pb=pb: e.scalar_tensor_tensor(
            out=tmp[:, h * 512:(h + 1) * 512], in0=k.ps[pb][:], scalar=rs2[:], in1=gpost[:, h * 512:(h + 1) * 512],
            op0=ALU.mult, op1=ALU.mult), reads=[k.Bps[pb], Brs2, Bgpost], writes=[Btmp])
    P.op("dve", lambda e: e.scalar_tensor_tensor(out=k.xres[:, ti, :], in0=tmp[:], scalar=float(coef),
                                                 in1=k.xres[:, ti, :], op0=ALU.mult, op1=ALU.add),
         reads=[Btmp, k.Bx[ti]], writes=[k.Bx[ti]])


def common_scratch(k, A, ntmp=2):
    sc = {}
    sc["junk"] = A.alloc("junk", [128, 1024], BF16)
    sc["Bjunk"] = Buf("junk")
    for nm, shape, dtype in [("xs", [128, 1024], BF16), ("ssq", [128, 1], F32), ("rstd", [128, 1], F32),
                             ("ss2", [128, 4], F32), ("rs2", [128, 1], F32), ("tmp", [128, 1024], F32)]:
        n = ntmp if nm in ("tmp", "xs") else 2
        ts = [A.alloc(f"{nm}{i}", shape, dtype) for i in range(n)]
        bs = [Buf(f"{nm}{i}") for i in range(n)]
        sc[nm] = [ts[i % n] for i in range(2)]
        sc["B" + nm] = [bs[i % n] for i in range(2)]
    return sc


NWGU, NWD = 5, 4
CASTW = True


def ffn_setup(k):
    if getattr(k, "ffn", None) is not None and k.ffn["gen"] == k.arena.gen:
        return k.ffn
    k.P.barrier()
    A = k.arena
    A.reset()
    f = {"gen": A.gen}
    f["sc"] = common_scratch(k, A, ntmp=1)
    f["sc"]["ps_tr"] = [4, 5]
    f["xnT"] = A.alloc("xnT", [128, 8, 512], BF16)
    f["BxnT"] = Buf("xnT")
    f["actT"] = A.alloc("actT", [128, NJ, 512], BF16)
    f["BactT"] = [Buf(f"actT{j}") for j in range(NJ)]
    f["wgu"] = A.alloc("wgu", [128, NWGU, 2, 1024], BF16)
    f["Bwgu"] = [Buf(f"wgu{i}") for i in range(NWGU)]
    f["wd"] = A.alloc("wd", [128, NWD, 1024], BF16)
    f["Bwd"] = [Buf(f"wd{i}") for i in range(NWD)]
    f["gpost"] = A.alloc("gpost", [128, 1024], F32)
    f["Bgpost"] = Buf("gpost")
    f["sg"] = [A.alloc(f"sg{i}", [128, 512], F32) for i in range(2)]
    f["Bsg"] = [Buf(f"sg{i}") for i in range(2)]
    f["wq"] = 0
    f["dq"] = 0
    k.ffn = f
    return f


def ffn_phase(k, layer, which):
    P = k.P
    f = ffn_setup(k)
    sc = f["sc"]
    fidx = layer * 2 + which
    g_pre = layer * 6 + (0 if which == 0 else 4)
    g_post = g_pre + 1
    xnT, BxnT, actT, BactT = f["xnT"], f["BxnT"], f["actT"], f["BactT"]
    P.dma("sp", lambda e: e.dma_start(out=f["gpost"][:], in_=k.ng_d[g_post, :].partition_broadcast(128)),
          writes=[f["Bgpost"]], dkey="gpost")
    NST = NT // 4
    if CASTW and not hasattr(k, "wgu_b"):
        k.wgu_b = k.nc.dram_tensor("wgu_b", [8 * NJ, 128, 2, 1024], BF16, kind="Internal").ap()
        k.wd_b = k.nc.dram_tensor("wd_b", [8 * NJ, 128, 1024], BF16, kind="Internal").ap()
        k.Bcast = [Buf(f"cast{i}") for i in range(8)]
        k.cast_done = set()
    use_bf = CASTW and fidx in k.cast_done
    nxt = fidx + 1
    casts = []
    if CASTW and which == 0:
        mw = mixer_units(k, layer)
        if not mw["cast"]:
            for u in range(mw["n"]):
                casts.append((mw["dst"][u], mw["src"][u], mw["B"], f"cwm{layer}"))
            mw["cast"] = True
    if CASTW and nxt < 8 and nxt not in k.cast_done:
        for j in range(NJ):
            casts.append((k.wgu_b[nxt * NJ + j], k.wgu_d[nxt * NJ + j], k.Bcast[nxt], f"cw{nxt}"))
            casts.append((k.wd_b[nxt * NJ + j], k.wd_d[nxt * NJ + j], k.Bcast[nxt], f"cw{nxt}"))
        k.cast_done.add(nxt)

    def emit_casts(n):
        for _ in range(n):
            if casts:
                dst, src, Bc, key = casts.pop(0)
                P.dma("pool", lambda e, dst=dst, src=src: e.dma_start(out=dst, in_=src), writes=[Bc], dkey=key)

    def prenorm_st(st):
        for t in range(4):
            prenorm_tile(k, st * 4 + t, g_pre, xnT, BxnT, t * 128, sc)

    prenorm_st(0)
    for st in range(NST):
        for j in range(NJ):
            slot = f["wq"] % NWGU
            f["wq"] += 1
            if use_bf:
                P.dma("sp", lambda e, j=j, slot=slot: e.dma_start(out=f["wgu"][:, slot], in_=k.wgu_b[fidx * NJ + j]),
                      reads=[k.Bcast[fidx]], writes=[f["Bwgu"][slot]], dkey=f"wgub{slot}")
            else:
                P.dma("pool", lambda e, j=j, slot=slot: e.dma_start(out=f["wgu"][:, slot], in_=k.wgu_d[fidx * NJ + j]),
                      writes=[f["Bwgu"][slot]], dkey=f"wgu{slot}")
            if j % 3 == 0:
                emit_casts(1)
            pg, pu = (0, 1) if j % 2 == 0 else (2, 3)
            for half, pb in ((0, pg), (1, pu)):
                for kk in range(8):
                    P.op("pe", lambda e, pb=pb, slot=slot, half=half, kk=kk: e.matmul(
                        k.ps[pb][:], lhsT=f["wgu"][:, slot, half, kk * 128:(kk + 1) * 128], rhs=xnT[:, kk, :],
                        start=(kk == 0), stop=(kk == 7)),
                        reads=[f["Bwgu"][slot], BxnT], writes=[k.Bps[pb]])
            sg, Bsg = f["sg"][j % 2], f["Bsg"][j % 2]
            P.op("act", lambda e, pg=pg, sg=sg: e.activation(out=sg[:], in_=k.ps[pg][:], func=AF.Silu),
                 reads=[k.Bps[pg]], writes=[Bsg])
            P.op("dve", lambda e, pu=pu, sg=sg, j=j: e.tensor_tensor(out=actT[:, j, :], in0=sg[:], in1=k.ps[pu][:],
                                                                     op=ALU.mult),
                 reads=[Bsg, k.Bps[pu]], writes=[BactT[j]])
        if st + 1 < NST:
            prenorm_st(st + 1)
        for j in range(NJ):
            slot = f["dq"] % NWD
            f["dq"] += 1
            if use_bf:
                P.dma("sp", lambda e, j=j, slot=slot: e.dma_start(out=f["wd"][:, slot, :], in_=k.wd_b[fidx * NJ + j]),
                      reads=[k.Bcast[fidx]], writes=[f["Bwd"][slot]], dkey=f"wdb{slot}")
            else:
                P.dma("pool", lambda e, j=j, slot=slot: e.dma_start(out=f["wd"][:, slot, :], in_=k.wd_d[fidx * NJ + j]),
                      writes=[f["Bwd"][slot]], dkey=f"wd{slot}")
            if j % 8 == 0:
                emit_casts(1)
            for tt in range(4):
                for half in range(2):
                    pb = tt * 2 + half
                    P.op("pe", lambda e, pb=pb, slot=slot, half=half, tt=tt, j=j: e.matmul(
                        k.ps[pb][:], lhsT=actT[:, j, tt * 128:(tt + 1) * 128],
                        rhs=f["wd"][:, slot, half * 512:(half + 1) * 512], start=(j == 0), stop=(j == NJ - 1)),
                        reads=[BactT[j], f["Bwd"][slot]], writes=[k.Bps[pb]])
        for tt in range(4):
            postnorm_residual(k, st * 4 + tt, [tt * 2, tt * 2 + 1], f["gpost"], f["Bgpost"], 0.5, sc)


import math
LN_S = {0: math.log(128 ** -0.5), 2: math.log(256 ** -0.5)}


def mixer_units(k, layer):
    if not hasattr(k, "mixw"):
        k.mixw = {}
    if layer not in k.mixw:
        kind = layer % 3
        if kind == 1:
            n = 16
            src = k.dram("rw_units", [n, 128, 2048])
        else:
            n = (8 if kind == 0 else 16) // 2 + 2 * (4 if kind == 0 else 8) + ((4 * (256 if kind == 0 else 512)) // 128) // 2
            src = k.dram(f"wm{layer}", [n, 128, 2048])
        dst = k.nc.dram_tensor(f"wm{layer}_b", [n, 128, 2048], BF16, kind="Internal").ap() if CASTW else None
        k.mixw[layer] = {"src": src, "dst": dst, "n": n, "B": Buf(f"mcast{layer}"), "cast": False}
    return k.mixw[layer]


def lin_setup(k, kind, layer):
    P = k.P
    P.barrier()
    A = k.arena
    A.reset()
    k.ffn = None
    m = {"kind": kind}
    H = 4
    NK = 1 if kind == 0 else 2
    DV = 256 if kind == 0 else 512
    NF = 8 if kind == 0 else 16
    HD = H * DV
    m.update(H=H, NK=NK, DV=DV, NF=NF, HD=HD, NKO=HD // 128, NTM=4 if kind == 0 else 8)
    m["sc"] = common_scratch(k, A, ntmp=1)
    m["sc"]["ps_tr"] = [6, 7]
    al = lambda nm, shape, dtype=BF16: (A.alloc(nm, shape, dtype), Buf(nm))
    m["xnT"], m["BxnT"] = al("xnT", [128, 8, 256])
    m["qk"], m["Bqk"] = al("qk", [128, NF, 256])
    m["vo"], _ = al("vo", [128, 2, HD])
    m["Bvo"] = [Buf("vo0"), Buf("vo1")]
    m["hbuf"], _ = al("hbuf", [128, 2, HD])
    m["Bhbuf"] = [Buf("hb0"), Buf("hb1")]
    m["hT"], m["BhT"] = al("hT", [128, HD // 128, 256])
    NR = 5 if kind == 0 else 2
    m["NR"] = NR
    m["ring"], _ = al("ring", [128, NR, 2048])
    m["Bring"] = [Buf(f"ring{i}") for i in range(NR)]
    m["rq"] = 0
    m["gpost"], m["Bgpost"] = al("gpost", [128, 1024], F32)
    m["Cbf"], m["BCbf"] = al("Cbf", [128, H * NK, DV])
    m["ktm"], m["Bktm"] = al("ktm", [128, H, NK * 128])
    m["PT"], m["BPT"] = al("PT", [128, H, 128])
    m["tmpC"], m["BtmpC"] = m["sc"]["tmp"][0], m["sc"]["Btmp"][0]
    m["e"], m["Be"] = al("e", [128, 2, 4], F32)
    m["base"], m["Bbase"] = al("base", [128, 2, 4], F32)
    m["cdec"], m["Bcdec"] = al("cdec", [128, 2, 4], F32)
    m["sm"], m["Bsm"] = al("sm", [128, 8, 4], F32)
    m["mw"] = mixer_units(k, layer)
    m["wd"] = m["mw"]["src"]
    if kind == 0:
        m["C"], m["BC"] = al("C", [128, H, DV], F32)
        m["nst"], m["Bnst"] = al("nst", [128, 4], F32)
        m["nbf"], m["Bnbf"] = al("nbf", [128, 4])
        m["pre"], m["Bpre"] = al("pre", [128, 8, 259], F32)
        m["acc"], m["Bacc"] = al("acc", [128, 256], F32)
        m["convT"], m["Bw"] = al("convT", [128, 32], F32)
        m["wif"], _ = al("wif", [128, 64])
        m["bifb"], _ = al("bifb", [128, 8], F32)
        m["mng"], _ = al("mng", [128, 1024], F32)
        m["gat"], m["Bgat"] = al("gat", [128, 2, 8], F32)
        m["gt2"], m["Bgt2"] = al("gt2", [128, 2, 8], F32)
        wif_d = k.dram(f"wif{layer}", [128, 64])
        bif_d = k.dram(f"bif{layer}", [1, 8])
        conv_d = k.dram(f"convT{layer}", [128, 32])
        mng_d = k.dram(f"mng{layer}", [1, 1024])
        Bw = m["Bw"]
        P.dma("pool", lambda e: e.dma_start(out=m["wif"][:], in_=wif_d[:, :]), writes=[Bw], dkey="mw0")
        P.dma("sp", lambda e: e.dma_start(out=m["bifb"][:], in_=bif_d[0, :].partition_broadcast(128)), writes=[Bw], dkey="mw1")
        P.dma("sp", lambda e: e.dma_start(out=m["convT"][:], in_=conv_d[:, :]), writes=[Bw], dkey="mw2")
        P.dma("sp", lambda e: e.dma_start(out=m["mng"][:], in_=mng_d[0, :].partition_broadcast(128)), writes=[Bw], dkey="mw3")
    else:
        if not hasattr(k, "pos_d"):
            k.pos_d = k.dram("pos", [1, 2 * SEQ], I32)
        m["posb"], m["Btrig"] = al("posb", [128, 256], F32)
        for nm in ("cosT", "sinT", "ta", "tb"):
            m[nm], _ = al(nm, [128, 256], F32)
        m["ang"] = m["posb"]
        m["ni"], _ = al("ni", [128, 256], I32)
        m["invf"], m["Binvf"] = al("invf", [128, 1], F32)
        m["ii"], _ = al("ii", [128, 1], I32)
        Bi = m["Binvf"]
        P.op("pool", lambda e: e.iota(m["ii"][:], pattern=[[0, 1]], base=0, channel_multiplier=1), writes=[Bi])
        P.op("dve", lambda e: e.tensor_copy(out=m["invf"][:], in_=m["ii"][:]), reads=[Bi], writes=[Bi])
        P.op("act", lambda e: e.activation(out=m["invf"][:], in_=m["invf"][:], func=AF.Exp,
                                           scale=-math.log(10000.0) / 127.0), reads=[Bi], writes=[Bi])
        Be = m["Be"]
        P.op("pool", lambda e: e.iota(m["ii"][:], pattern=[[0, 1]], base=1, channel_multiplier=1), writes=[Bi])
        P.op("dve", lambda e: e.tensor_copy(out=m["sm"][:, 0, 0:1], in_=m["ii"][:]), reads=[Bi], writes=[m["Bsm"]])
        for h in range(4):
            lg = math.log(1.0 - 2.0 ** (-5.0 - h))
            for tt in range(2):
                P.op("dve", lambda e, h=h, tt=tt, lg=lg: e.tensor_scalar(
                    out=m["e"][:, tt, h:h + 1], in0=m["sm"][:, 0, 0:1], scalar1=-lg, scalar2=LN_S[2],
                    op0=ALU.mult, op1=ALU.add), reads=[m["Bsm"]], writes=[Be])
                P.op("dve", lambda e, h=h, tt=tt, lg=lg: e.tensor_scalar(
                    out=m["base"][:, tt, h:h + 1], in0=m["sm"][:, 0, 0:1], scalar1=lg, scalar2=None,
                    op0=ALU.mult), reads=[m["Bsm"]], writes=[m["Bbase"]])
                P.op("pool", lambda e, h=h, tt=tt, lg=lg: e.memset(m["cdec"][:, tt, h:h + 1], math.exp(128.0 * lg)),
                     writes=[m["Bcdec"]])
        P.op("act", lambda e: e.activation(out=m["e"][:], in_=m["e"][:], func=AF.Exp), reads=[Be], writes=[Be])
        P.op("act", lambda e: e.activation(out=m["base"][:], in_=m["base"][:], func=AF.Exp),
             reads=[m["Bbase"]], writes=[m["Bbase"]])
    return m


def nbank(k):
    b = 4 + (k.rot % 4)
    k.rot += 1
    return b


def ring_load(k, m, unit):
    slot = m["rq"] % m["NR"]
    m["rq"] += 1
    mw = m.get("mw")
    if mw is not None and mw["cast"]:
        k.P.dma("sp", lambda e: e.dma_start(out=m["ring"][:, slot, :], in_=mw["dst"][unit]),
                reads=[mw["B"]], writes=[m["Bring"][slot]], dkey=f"mrb{slot}")
    else:
        k.P.dma("pool", lambda e: e.dma_start(out=m["ring"][:, slot, :], in_=m["wd"][unit]),
                writes=[m["Bring"][slot]], dkey=f"mr{slot}")
    return slot


def tm_proj(k, m, blocks, dst_off0):
    P = k.P
    for i, nb in enumerate(blocks):
        sa = ring_load(k, m, m["NF"] // 2 + 2 * nb)
        sb = ring_load(k, m, m["NF"] // 2 + 2 * nb + 1)
        for tt in range(2):
            b = nbank(k)
            for kk in range(8):
                sl = sa if kk < 4 else sb
                P.op("pe", lambda e, b=b, sl=sl, kk=kk, tt=tt: e.matmul(
                    k.ps[b][:], lhsT=m["xnT"][:, kk, tt * 128:(tt + 1) * 128],
                    rhs=m["ring"][:, sl, (kk % 4) * 512:(kk % 4 + 1) * 512], start=(kk == 0), stop=(kk == 7)),
                    reads=[m["BxnT"], m["Bring"][sl]], writes=[k.Bps[b]])
            off = dst_off0 + i * 512
            if (i + tt) % 2 == 0:
                P.op("act", lambda e, b=b, tt=tt, off=off: e.activation(out=m["vo"][:, tt, off:off + 512], in_=k.ps[b][:],
                                                                        func=AF.Copy),
                     reads=[k.Bps[b]], writes=[m["Bvo"][tt]])
            else:
                P.op("dve", lambda e, b=b, tt=tt, off=off: e.tensor_copy(out=m["vo"][:, tt, off:off + 512], in_=k.ps[b][:]),
                     reads=[k.Bps[b]], writes=[m["Bvo"][tt]])


def lin_mixer_phase(k, layer, kind, j):
    P = k.P
    m = lin_setup(k, kind, layer)
    sc = m["sc"]
    H, NK, DV, NF, HD, NKO, NTM = m["H"], m["NK"], m["DV"], m["NF"], m["HD"], m["NKO"], m["NTM"]
    g_pre, g_post = layer * 6 + 2, layer * 6 + 3
    xnT, qk, vo, hbuf, hT = m["xnT"], m["qk"], m["vo"], m["hbuf"], m["hT"]
    P.dma("sp", lambda e: e.dma_start(out=m["gpost"][:], in_=k.ng_d[g_post, :].partition_broadcast(128)),
          writes=[m["Bgpost"]], dkey="gpost")
    qidx = (lambda h, kc: h) if kind == 0 else (lambda h, kc: 2 * h + kc)
    kidx = (lambda h, kc: 4 + h) if kind == 0 else (lambda h, kc: 8 + 2 * h + kc)
    sm, Bsm = m["sm"], m["Bsm"]
    for st in range(NT // 2):
        first = (st % 8 == 0)
        for tt in range(2):
            prenorm_tile(k, st * 2 + tt, g_pre, xnT, m["BxnT"], tt * 128, sc)
        if first:
            P.op("pool", lambda e: e.memset(m["Cbf"][:], 0.0), writes=[m["BCbf"]])
            if kind == 0:
                P.op("pool", lambda e: e.memset(m["C"][:], 0.0), writes=[m["BC"]])
                P.op("pool", lambda e: e.memset(m["nst"][:], 0.0), writes=[m["Bnst"]])
                P.op("pool", lambda e: e.memset(m["nbf"][:], 0.0), writes=[m["Bnbf"]])
        if kind == 0:
            if first:
                P.op("pool", lambda e: e.memset(m["pre"][:, :, 0:3], 0.0), writes=[m["Bpre"]])
            else:
                P.op("pool", lambda e: e.tensor_copy(out=m["pre"][:, :, 0:3], in_=m["pre"][:, :, 256:259]),
                     reads=[m["Bpre"]], writes=[m["Bpre"]])
        else:
            Bt = m["Btrig"]
            P.dma("pool", lambda e, st=st: e.dma_start(out=m["posb"][:],
                                                        in_=k.pos_d[0, st * 256:(st + 1) * 256].partition_broadcast(128)),
                  writes=[Bt], dkey="posb")
            P.op("dve", lambda e: e.tensor_scalar(out=m["ang"][:], in0=m["posb"][:], scalar1=m["invf"][:], scalar2=None,
                                                  op0=ALU.mult), reads=[Bt, m["Binvf"]], writes=[Bt])
            for dst, shift in ((m["sinT"], 0.5), (m["cosT"], 0.75)):
                TWO_PI = 2.0 * math.pi
                P.op("dve", lambda e, shift=shift: e.tensor_scalar(out=m["ta"][:], in0=m["ang"][:], scalar1=1.0 / TWO_PI,
                                                                    scalar2=shift, op0=ALU.mult, op1=ALU.add),
                     reads=[Bt], writes=[Bt])
                P.op("dve", lambda e: e.tensor_copy(out=m["ni"][:], in_=m["ta"][:]), reads=[Bt], writes=[Bt])
                P.op("dve", lambda e: e.tensor_copy(out=m["tb"][:], in_=m["ni"][:]), reads=[Bt], writes=[Bt])
                P.op("dve", lambda e: e.tensor_tensor(out=m["ta"][:], in0=m["ta"][:], in1=m["tb"][:], op=ALU.subtract),
                     reads=[Bt], writes=[Bt])
                P.op("dve", lambda e: e.tensor_scalar(out=m["ta"][:], in0=m["ta"][:], scalar1=-0.5, scalar2=TWO_PI,
                                                      op0=ALU.add, op1=ALU.mult), reads=[Bt], writes=[Bt])
                P.op("dve", lambda e: e.tensor_scalar(out=m["tb"][:], in0=m["ta"][:], scalar1=math.pi, scalar2=-TWO_PI,
                                                      op0=ALU.is_gt, op1=ALU.mult), reads=[Bt], writes=[Bt])
                P.op("dve", lambda e: e.tensor_tensor(out=m["ta"][:], in0=m["ta"][:], in1=m["tb"][:], op=ALU.add),
                     reads=[Bt], writes=[Bt])
                P.op("dve", lambda e: e.tensor_scalar(out=m["tb"][:], in0=m["ta"][:], scalar1=-math.pi, scalar2=TWO_PI,
                                                      op0=ALU.is_lt, op1=ALU.mult), reads=[Bt], writes=[Bt])
                P.op("dve", lambda e: e.tensor_tensor(out=m["ta"][:], in0=m["ta"][:], in1=m["tb"][:], op=ALU.add),
                     reads=[Bt], writes=[Bt])
                P.op("dve", lambda e: e.tensor_scalar(out=m["ta"][:], in0=m["ta"][:], scalar1=-3.1415925, scalar2=3.1415925,
                                                      op0=ALU.max, op1=ALU.min), reads=[Bt], writes=[Bt])
                P.op("act", lambda e, dst=dst: e.activation(out=dst[:], in_=m["ta"][:], func=AF.Sin), reads=[Bt], writes=[Bt])
        for u in range(NF // 2):
            sl = ring_load(k, m, u)
            banks = []
            for c2 in range(2):
                b = nbank(k)
                banks.append(b)
                for kk in range(8):
                    P.op("pe", lambda e, b=b, sl=sl, c2=c2, kk=kk: e.matmul(
                        k.ps[b][:, 0:256], lhsT=m["ring"][:, sl, c2 * 1024 + kk * 128:c2 * 1024 + (kk + 1) * 128],
                        rhs=xnT[:, kk, :], start=(kk == 0), stop=(kk == 7)),
                        reads=[m["Bring"][sl], m["BxnT"]], writes=[k.Bps[b]])
                if kind == 0:
                    c = 2 * u + c2
                    P.op("act", lambda e, b=b, c=c: e.activation(out=m["pre"][:, c, 3:259], in_=k.ps[b][:, 0:256], func=AF.Copy),
                         reads=[k.Bps[b]], writes=[m["Bpre"]])
            if kind == 2:
                Bt = m["Btrig"]
                ba, bb = banks
                for o, (f1, f2, op) in enumerate(((m["cosT"], m["sinT"], ALU.subtract), (m["sinT"], m["cosT"], ALU.add))):
                    P.op("dve", lambda e, f1=f1: e.tensor_tensor(out=m["ta"][:], in0=k.ps[ba][:, 0:256], in1=f1[:], op=ALU.mult),
                         reads=[k.Bps[ba], Bt], writes=[Bt])
                    P.op("dve", lambda e, f2=f2: e.tensor_tensor(out=m["tb"][:], in0=k.ps[bb][:, 0:256], in1=f2[:], op=ALU.mult),
                         reads=[k.Bps[bb], Bt], writes=[Bt])
                    P.op("dve", lambda e, u=u, o=o, op=op: e.tensor_tensor(out=qk[:, 2 * u + o, :], in0=m["ta"][:], in1=m["tb"][:], op=op),
                         reads=[Bt], writes=[m["Bqk"]])
        if kind == 0:
            for c in range(8):
                cw = m["convT"]
                P.op("dve", lambda e, c=c: e.tensor_scalar(out=m["acc"][:], in0=m["pre"][:, c, 3:259],
                                                           scalar1=cw[:, c * 4 + 3:c * 4 + 4], scalar2=None, op0=ALU.mult),
                     reads=[m["Bpre"], m["Bw"]], writes=[m["Bacc"]])
                for tap in (2, 1, 0):
                    P.op("dve", lambda e, c=c, tap=tap: e.scalar_tensor_tensor(
                        out=m["acc"][:], in0=m["pre"][:, c, tap:tap + 256], scalar=cw[:, c * 4 + tap:c * 4 + tap + 1],
                        in1=m["acc"][:], op0=ALU.mult, op1=ALU.add), reads=[m["Bpre"], m["Bw"], m["Bacc"]], writes=[m["Bacc"]])
                P.op("act", lambda e, c=c: e.activation(out=qk[:, c, :], in_=m["acc"][:], func=AF.Silu),
                     reads=[m["Bacc"]], writes=[m["Bqk"]])
        tm_proj(k, m, list(range(NTM // 2)), 0)
        if kind == 0:
            gat, gt2, Bgat, Bgt2 = m["gat"], m["gt2"], m["Bgat"], m["Bgt2"]
            bg = nbank(k)
            for tt in range(2):
                for kk in range(8):
                    P.op("pe", lambda e, tt=tt, kk=kk: e.matmul(k.ps[bg][:, tt * 8:(tt + 1) * 8],
                                                                 lhsT=xnT[:, kk, tt * 128:(tt + 1) * 128],
                                                                 rhs=m["wif"][:, kk * 8:(kk + 1) * 8], start=(kk == 0), stop=(kk == 7)),
                         reads=[m["BxnT"], m["Bw"]], writes=[k.Bps[bg]])
                P.op("dve", lambda e, tt=tt: e.tensor_tensor(out=gat[:, tt, :], in0=k.ps[bg][:, tt * 8:(tt + 1) * 8],
                                                             in1=m["bifb"][:], op=ALU.add),
                     reads=[k.Bps[bg], m["Bw"]], writes=[Bgat])
            P.op("act", lambda e: e.activation(out=gat[:], in_=gat[:], func=AF.Tanh, scale=1.0 / 15.0), reads=[Bgat], writes=[Bgat])
            P.op("dve", lambda e: e.tensor_scalar(out=gat[:], in0=gat[:], scalar1=15.0, scalar2=None, op0=ALU.mult),
                 reads=[Bgat], writes=[Bgat])
            P.op("act", lambda e: e.activation(out=gt2[:, :, 4:8], in_=gat[:, :, 4:8], func=AF.Exp, scale=-1.0),
                 reads=[Bgat], writes=[Bgt2])
            P.op("dve", lambda e: e.tensor_scalar(out=gt2[:, :, 4:8], in0=gt2[:, :, 4:8], scalar1=1.0, scalar2=None, op0=ALU.add),
                 reads=[Bgt2], writes=[Bgt2])
            P.op("act", lambda e: e.activation(out=gt2[:, :, 4:8], in_=gt2[:, :, 4:8], func=AF.Ln), reads=[Bgt2], writes=[Bgt2])
            bc = nbank(k)
            for tt in range(2):
                P.op("pe", lambda e, tt=tt: e.matmul(k.ps[bc][:, tt * 4:(tt + 1) * 4], lhsT=k.trif[:], rhs=gt2[:, tt, 4:8],
                                                     start=True, stop=True), reads=[Bgt2, k.Bconst], writes=[k.Bps[bc]])
                P.op("pe", lambda e, tt=tt: e.matmul(k.ps[bc][:, 8 + tt * 4:8 + (tt + 1) * 4], lhsT=k.onesf[:], rhs=gt2[:, tt, 4:8],
                                                     start=True, stop=True), reads=[Bgt2, k.Bconst], writes=[k.Bps[bc]])
            cs = k.ps[bc][:, 0:8].rearrange("p (t h) -> p t h", t=2)
            tot = k.ps[bc][:, 8:16].rearrange("p (t h) -> p t h", t=2)
            P.op("dve", lambda e: e.scalar_tensor_tensor(out=m["e"][:], in0=cs, scalar=LN_S[0], in1=gat[:, :, 0:4],
                                                         op0=ALU.add, op1=ALU.add), reads=[k.Bps[bc], Bgat], writes=[m["Be"]])
            P.op("act", lambda e: e.activation(out=m["e"][:], in_=m["e"][:], func=AF.Exp), reads=[m["Be"]], writes=[m["Be"]])
            P.op("act", lambda e: e.activation(out=m["base"][:], in_=cs, func=AF.Exp), reads=[k.Bps[bc]], writes=[m["Bbase"]])
            P.op("act", lambda e: e.activation(out=m["cdec"][:], in_=tot, func=AF.Exp, scale=-1.0),
                 reads=[k.Bps[bc]], writes=[m["Bcdec"]])
        for tt in range(2):
            tok = slice(tt * 128, (tt + 1) * 128)
            bt = nbank(k)
            ptb = k.ps[bt][:].bitcast(BF16)
            for h in range(H):
                for kc in range(NK):
                    o = (h * NK + kc) * 128
                    P.op("pe", lambda e, h=h, kc=kc, o=o: e.transpose(out=ptb[:, o:o + 128], in_=qk[:, kidx(h, kc), tok],
                                                                       identity=k.ident[:]),
                         reads=[m["Bqk"], k.Bconst], writes=[k.Bps[bt]])
            for h in range(H):
                P.op("dve", lambda e, h=h: e.tensor_scalar(out=m["ktm"][:, h, :], in0=ptb[:, h * NK * 128:(h + 1) * NK * 128],
                                                           scalar1=m["e"][:, tt, h:h + 1], scalar2=None, op0=ALU.mult),
                     reads=[k.Bps[bt], m["Be"]], writes=[m["Bktm"]])
            bp = nbank(k)
            for h in range(H):
                for kc in range(NK):
                    P.op("pe", lambda e, h=h, kc=kc: e.matmul(k.ps[bp][:, h * 128:(h + 1) * 128], lhsT=qk[:, kidx(h, kc), tok],
                                                              rhs=qk[:, qidx(h, kc), tok], start=(kc == 0), stop=(kc == NK - 1)),
                         reads=[m["Bqk"]], writes=[k.Bps[bp]])
            for h in range(H):
                P.op("dve", lambda e, h=h: e.scalar_tensor_tensor(out=m["PT"][:, h, :], in0=k.ps[bp][:, h * 128:(h + 1) * 128],
                                                                  scalar=m["e"][:, tt, h:h + 1], in1=k.tri[:],
                                                                  op0=ALU.mult, op1=ALU.mult),
                     reads=[k.Bps[bp], m["Be"], k.Bconst], writes=[m["BPT"]])
            if kind == 0:
                bd = nbank(k)
                for h in range(H):
                    P.op("pe", lambda e, h=h: e.matmul(k.ps[bd][:, h:h + 1], lhsT=m["PT"][:, h, :], rhs=k.ones[:, 0:1],
                                                       start=True, stop=False), reads=[m["BPT"], k.Bconst], writes=[k.Bps[bd]])
                    P.op("pe", lambda e, h=h: e.matmul(k.ps[bd][:, h:h + 1], lhsT=qk[:, qidx(h, 0), tok], rhs=m["nbf"][:, h:h + 1],
                                                       start=False, stop=True), reads=[m["Bqk"], m["Bnbf"]], writes=[k.Bps[bd]])
                P.op("dve", lambda e: e.tensor_copy(out=sm[:, 0, :], in_=k.ps[bd][:, 0:4]), reads=[k.Bps[bd]], writes=[Bsm])
                P.op("dve", lambda e: e.tensor_scalar(out=sm[:, 6, :], in0=sm[:, 0, :], scalar1=-1.0, scalar2=None,
                                                      op0=ALU.mult), reads=[Bsm], writes=[Bsm])
                P.op("dve", lambda e: e.tensor_tensor(out=sm[:, 0, :], in0=sm[:, 0, :], in1=sm[:, 6, :], op=ALU.max),
                     reads=[Bsm], writes=[Bsm])
                P.op("dve", lambda e: e.tensor_tensor(out=sm[:, 0, :], in0=sm[:, 0, :], in1=m["base"][:, tt, :], op=ALU.max),
                     reads=[Bsm, m["Bbase"]], writes=[Bsm])
                P.op("dve", lambda e: e.reciprocal(out=sm[:, 1, :], in_=sm[:, 0, :]), reads=[Bsm], writes=[Bsm])
                basev = sm[:, 1, :]
            else:
                P.op("dve", lambda e: e.tensor_copy(out=sm[:, 1, :], in_=m["base"][:, tt, :]), reads=[m["Bbase"]], writes=[Bsm])
                basev = sm[:, 1, :]
            hpb = 512 // DV
            abank = {}
            for h in range(H):
                if h % hpb == 0:
                    ba = nbank(k)
                abank[h] = (ba, (h % hpb) * DV)
                ba, off = abank[h]
                P.op("pe", lambda e, h=h, ba=ba, off=off: e.matmul(k.ps[ba][:, off:off + DV], lhsT=m["PT"][:, h, :],
                                                                   rhs=vo[:, tt, h * DV:(h + 1) * DV], start=True, stop=False),
                     reads=[m["BPT"], m["Bvo"][tt]], writes=[k.Bps[ba]])
                for kc in range(NK):
                    P.op("pe", lambda e, h=h, kc=kc, ba=ba, off=off: e.matmul(
                        k.ps[ba][:, off:off + DV], lhsT=qk[:, qidx(h, kc), tok], rhs=m["Cbf"][:, h * NK + kc, :],
                        start=False, stop=(kc == NK - 1)), reads=[m["Bqk"], m["BCbf"]], writes=[k.Bps[ba]])
                P.op("act", lambda e, h=h, ba=ba, off=off: e.activation(out=sc["junk"][:, 0:DV], in_=k.ps[ba][:, off:off + DV],
                                                                        func=AF.Square, accum_out=sm[:, 2, h:h + 1]),
                     reads=[k.Bps[ba]], writes=[sc["Bjunk"], Bsm])
                if h % hpb == hpb - 1 or h == H - 1:
                    pass
            P.op("dve", lambda e: e.tensor_tensor(out=sm[:, 3, :], in0=basev, in1=basev, op=ALU.mult), reads=[Bsm], writes=[Bsm])
            P.op("dve", lambda e: e.tensor_tensor(out=sm[:, 3, :], in0=sm[:, 3, :], in1=sm[:, 2, :], op=ALU.mult), reads=[Bsm], writes=[Bsm])
            P.op("pool", lambda e: e.tensor_scalar(out=sm[:, 3, :], in0=sm[:, 3, :], scalar1=1.0 / DV, scalar2=EPS,
                                                   op0=ALU.mult, op1=ALU.add), reads=[Bsm], writes=[Bsm])
            P.op("pool", lambda e: e.tensor_tensor(out=sm[:, 3, :], in0=sm[:, 3, :], in1=k.mhalf[:, 0:1].to_broadcast([128, 4]),
                                                   op=ALU.pow), reads=[Bsm, k.Bconst], writes=[Bsm])
            P.op("dve", lambda e: e.tensor_tensor(out=sm[:, 4, :], in0=sm[:, 3, :], in1=basev, op=ALU.mult), reads=[Bsm], writes=[Bsm])
            for h in range(H):
                ba, off = abank[h]
                if kind == 0:
                    P.op("dve", lambda e, h=h, ba=ba, off=off: e.scalar_tensor_tensor(
                        out=hbuf[:, tt, h * DV:(h + 1) * DV], in0=k.ps[ba][:, off:off + DV], scalar=sm[:, 4, h:h + 1],
                        in1=m["mng"][:, h * DV:(h + 1) * DV], op0=ALU.mult, op1=ALU.mult),
                        reads=[k.Bps[ba], Bsm, m["Bw"]], writes=[m["Bhbuf"][tt]])
                else:
                    P.op("dve", lambda e, h=h, ba=ba, off=off: e.tensor_scalar(
                        out=hbuf[:, tt, h * DV:(h + 1) * DV], in0=k.ps[ba][:, off:off + DV], scalar1=sm[:, 4, h:h + 1],
                        scalar2=None, op0=ALU.mult), reads=[k.Bps[ba], Bsm], writes=[m["Bhbuf"][tt]])
            for h in range(H):
                cd = m["cdec"][:, tt, h:h + 1]
                for kc in range(NK):
                    bs = nbank(k)
                    P.op("pe", lambda e, h=h, kc=kc, bs=bs: e.matmul(k.ps[bs][:, 0:DV], lhsT=m["ktm"][:, h, kc * 128:(kc + 1) * 128],
                                                                     rhs=vo[:, tt, h * DV:(h + 1) * DV], start=True, stop=True),
                         reads=[m["Bktm"], m["Bvo"][tt]], writes=[k.Bps[bs]])
                    P.op("dve", lambda e, bs=bs, cd=cd: e.tensor_scalar(out=m["tmpC"][:, 0:DV], in0=k.ps[bs][:, 0:DV], scalar1=cd,
                                                                        scalar2=None, op0=ALU.mult),
                         reads=[k.Bps[bs], m["Bcdec"]], writes=[m["BtmpC"]])
                    if kind == 0:
                        P.op("dve", lambda e, h=h, cd=cd: e.scalar_tensor_tensor(out=m["C"][:, h, :], in0=m["C"][:, h, :], scalar=cd,
                                                                                 in1=m["tmpC"][:, 0:DV], op0=ALU.mult, op1=ALU.add),
                             reads=[m["BC"], m["Bcdec"], m["BtmpC"]], writes=[m["BC"]])
                        P.op("act", lambda e, h=h: e.activation(out=m["Cbf"][:, h, :], in_=m["C"][:, h, :], func=AF.Copy),
                             reads=[m["BC"]], writes=[m["BCbf"]])
                    else:
                        ci = h * NK + kc
                        P.op("dve", lambda e, ci=ci, cd=cd: e.scalar_tensor_tensor(out=m["Cbf"][:, ci, :], in0=m["Cbf"][:, ci, :], scalar=cd,
                                                                                   in1=m["tmpC"][:, 0:DV], op0=ALU.mult, op1=ALU.add),
                             reads=[m["BCbf"], m["Bcdec"], m["BtmpC"]], writes=[m["BCbf"]])
            if kind == 0:
                bn = nbank(k)
                for h in range(H):
                    P.op("pe", lambda e, h=h: e.matmul(k.ps[bn][:, h:h + 1], lhsT=m["ktm"][:, h, :], rhs=k.ones[:, 0:1],
                                                       start=True, stop=True), reads=[m["Bktm"], k.Bconst], writes=[k.Bps[bn]])
                P.op("dve", lambda e: e.tensor_tensor(out=sm[:, 5, :], in0=k.ps[bn][:, 0:4], in1=m["nst"][:], op=ALU.add),
                     reads=[k.Bps[bn], m["Bnst"]], writes=[Bsm])
                P.op("dve", lambda e: e.tensor_tensor(out=m["nst"][:], in0=sm[:, 5, :], in1=m["cdec"][:, tt, :], op=ALU.mult),
                     reads=[Bsm, m["Bcdec"]], writes=[m["Bnst"]])
                P.op("dve", lambda e: e.tensor_copy(out=m["nbf"][:], in_=m["nst"][:]), reads=[m["Bnst"]], writes=[m["Bnbf"]])
        tm_proj(k, m, list(range(NTM // 2, NTM)), 0)
        for tt in range(2):
            P.op("act", lambda e, tt=tt: e.activation(out=vo[:, tt, :], in_=vo[:, tt, :],
                                                      func=(AF.Sigmoid if kind == 0 else AF.Silu)),
                 reads=[m["Bvo"][tt]], writes=[m["Bvo"][tt]])
            P.op("dve", lambda e, tt=tt: e.tensor_tensor(out=hbuf[:, tt, :], in0=hbuf[:, tt, :], in1=vo[:, tt, :], op=ALU.mult),
                 reads=[m["Bhbuf"][tt], m["Bvo"][tt]], writes=[m["Bhbuf"][tt]])
            for g in range(NKO // 8):
                bt = nbank(k)
                ptb = k.ps[bt][:].bitcast(BF16)
                for i in range(8):
                    kk = g * 8 + i
                    P.op("pe", lambda e, tt=tt, kk=kk, i=i, ptb=ptb: e.transpose(out=ptb[:, i * 128:(i + 1) * 128],
                                                                                 in_=hbuf[:, tt, kk * 128:(kk + 1) * 128],
                                                                                 identity=k.ident[:]),
                         reads=[m["Bhbuf"][tt], k.Bconst], writes=[k.Bps[bt]])
                P.op("act", lambda e, tt=tt, g=g, ptb=ptb: e.activation(out=hT[:, g * 8:(g + 1) * 8, tt * 128:(tt + 1) * 128],
                                                                        in_=ptb.rearrange("p (a t) -> p a t", a=8), func=AF.Copy),
                     reads=[k.Bps[bt]], writes=[m["BhT"]])
        for u in range(NKO // 2):
            sl = ring_load(k, m, NF // 2 + 2 * NTM + u)
            for c2 in range(2):
                kk = 2 * u + c2
                for tt in range(2):
                    for half in range(2):
                        pb = tt * 2 + half
                        P.op("pe", lambda e, sl=sl, c2=c2, kk=kk, tt=tt, half=half, pb=pb: e.matmul(
                            k.ps[pb][:], lhsT=hT[:, kk, tt * 128:(tt + 1) * 128],
                            rhs=m["ring"][:, sl, c2 * 1024 + half * 512:c2 * 1024 + (half + 1) * 512],
                            start=(kk == 0), stop=(kk == NKO - 1)), reads=[m["BhT"], m["Bring"][sl]], writes=[k.Bps[pb]])
        for tt in range(2):
            postnorm_residual(k, st * 2 + tt, [tt * 2, tt * 2 + 1], m["gpost"], m["Bgpost"], 1.0, sc)


C0 = math.exp(-0.5)
RW_DEBUG = False
RW_STOP = 0


class _Stop(Exception):
    pass


def nb8(k):
    b = k.rot % 8
    k.rot += 1
    return b


def rwkv_setup(k, layer):
    P = k.P
    A = k.big_arena
    A.reset()
    k.ffn = None
    m = {}
    m["sc"] = common_scratch(k, A, ntmp=1)
    m["sc"]["ps_tr"] = [6, 7]
    al = lambda nm, shape, dtype=BF16: (A.alloc(nm, shape, dtype), Buf(nm))
    m["xnTh"], m["BxnT"] = al("xnTh", [128, 8, 129])
    m["xxT"], m["Bxx"] = al("xxT", [128, 8, 128])
    m["mixT"], _ = al("mixT", [128, 2, 8, 128])
    m["Bmix"] = [Buf(f"mix{i}") for i in range(2)]
    m["hT"], m["BhT"] = al("hT", [128, 8, 128])
    m["ring"], _ = al("ring", [128, 2, 2048])
    m["Bring"] = [Buf(f"ring{i}") for i in range(2)]
    m["NR"] = 2
    m["rq"] = 0
    m["gpost"], m["Bgpost"] = al("gpost", [128, 1024], F32)
    m["Bw"] = Buf("rw_w")
    for nm, shape, dtype in [("w1", [128, 8, 64], BF16), ("a1", [128, 8, 64], BF16), ("g1", [128, 8, 128], BF16),
                             ("w2", [64, 1024], BF16), ("a2", [64, 1024], BF16), ("g2", [128, 1024], BF16),
                             ("w0r", [64, 1024], F32), ("a0r", [64, 1024], F32), ("muT", [128, 48], F32),
                             ("kkb", [64, 1024], BF16), ("kab", [64, 1024], BF16), ("lgb", [64, 1024], BF16),
                             ("lbb", [64, 1024], BF16), ("rkb", [64, 1024], BF16)]:
        m[nm], _ = al(nm, shape, dtype)
    d = lambda nm, shape: k.dram(f"rw_{nm}", shape)
    m["mw"] = mixer_units(k, layer)
    m["wd"] = m["mw"]["src"]
    Bw = m["Bw"]
    for i, (nm, shape) in enumerate([("w1", [128, 8, 64]), ("a1", [128, 8, 64]), ("g1", [128, 8, 128]),
                                     ("w2", [64, 1024]), ("a2", [64, 1024]), ("g2", [128, 1024])]):
        src = d(nm, shape)
        P.dma("pool", lambda e, nm=nm, src=src: e.dma_start(out=m[nm][:], in_=src), writes=[Bw], dkey=f"rww{i}")
    for i, nm in enumerate(["w0r", "a0r"]):
        src = d(nm, [1, 1024])
        P.dma("sp", lambda e, nm=nm, src=src: e.dma_start(out=m[nm][:], in_=src[0, :].partition_broadcast(64)),
              writes=[Bw], dkey=f"rwr{i}")
    src = d("muT", [128, 48])
    P.dma("sp", lambda e, src=src: e.dma_start(out=m["muT"][:], in_=src), writes=[Bw], dkey="rwmu")
    for i, nm in enumerate(["kkb", "kab", "lgb", "lbb", "rkb"]):
        src = d(nm, [1, 1024])
        P.dma("pool", lambda e, nm=nm, src=src: e.dma_start(out=m[nm][:], in_=src[0, :].partition_broadcast(64)),
              writes=[Bw], dkey=f"rwb{i}")
    m["hidw"], m["Bhid"] = al("hidw", [64, 128])
    m["hida"], _ = al("hida", [64, 128])
    m["hidg"], _ = al("hidg", [128, 128])
    m["rkv"], _ = al("rkv", [64, 2, 3, 1024])
    m["Brkv"] = [[Buf(f"rkv{c}{i}") for i in range(3)] for c in range(2)]
    m["H"], m["BH"] = al("H", [64, 1024], F32)
    m["Hbf"], m["BHbf"] = al("Hbf", [64, 1024])

    def mk_cx(i):
        cx = dict(m)
        alc = lambda nm, shape, dtype=BF16: (A.alloc(f"{nm}c{i}", shape, dtype), Buf(f"{nm}c{i}"))
        for nm in ("F0", "F1", "F2", "PB", "TTf"):
            cx[nm], cx["B" + nm] = alc(nm, [64, 1024], F32)
        for nm in ("a", "gsb", "G", "Ginv", "Gprev", "kk", "bt", "at", "kt", "rt", "TT", "bT", "kT"):
            cx[nm], cx["B" + nm] = alc(nm, [64, 1024])
        for nm, src in (("akv", "G"), ("TAT", "Ginv"), ("Usb", "Gprev"), ("zb", "kk"), ("QA", "F0"), ("QB", "F1"), ("PA", "F2")):
            cx[nm], cx["B" + nm] = cx[src], cx["B" + src]
        for nm in ("M1", "M2", "arT"):
            cx[nm], cx["B" + nm] = alc(nm, [64, 16, 2, 64])
        cx["GL"], cx["BGL"] = alc("GL", [64, 16], F32)
        cx["sm"], cx["Bsm"] = alc("sm", [64, 8, 16], F32)
        return cx

    m["cx"] = [mk_cx(0), mk_cx(1)]
    m["eps24"], _ = al("eps24", [64, 1], F32)
    return m


def rwkv_seq(k, m, layer, seq):
    P = k.P
    sc = m["sc"]
    g_pre, g_post = layer * 6 + 2, layer * 6 + 3
    xnTh, xxT, mixT, hT = m["xnTh"], m["xxT"], m["mixT"], m["hT"]
    Bw = m["Bw"]
    H, BH = m["H"], m["BH"]
    cxs = m["cx"]
    hv = lambda t: t[:].rearrange("p (h j) -> p h j", h=16)
    bc = lambda ap: ap.unsqueeze(2).to_broadcast([64, 16, 64])
    P.op("pool", lambda e: e.memset(H[:], 0.0), writes=[BH])
    P.op("pool", lambda e: e.memset(m["Hbf"][:], 0.0), writes=[m["BHbf"]])

    def headmm(dst_evac, groups):
        for g in range(2):
            b = nb8(k)
            for hh in range(8):
                h = g * 8 + hh
                for gi, (lf, rf, rd) in enumerate(groups):
                    Lh, Rh, last = lf(h), rf(h), (gi == len(groups) - 1)
                    P.op("pe", lambda e, b=b, hh=hh, Lh=Lh, Rh=Rh, gi=gi, last=last: e.matmul(
                        k.ps[b][0:64, hh * 64:(hh + 1) * 64], lhsT=Lh, rhs=Rh, start=(gi == 0), stop=last),
                        reads=rd, writes=[k.Bps[b]])
            dst_evac(b, g)

    def evac_to(dst, Bdst, eng="act"):
        def f(b, g):
            if eng == "act":
                P.op("act", lambda e: e.activation(out=hv(dst)[:, g * 8:(g + 1) * 8],
                                                   in_=k.ps[b][0:64, :].rearrange("p (h x) -> p h x", h=8), func=AF.Copy),
                     reads=[k.Bps[b]], writes=[Bdst])
            else:
                P.op("dve", lambda e: e.tensor_copy(out=hv(dst)[:, g * 8:(g + 1) * 8],
                                                    in_=k.ps[b][0:64, :].rearrange("p (h x) -> p h x", h=8)),
                     reads=[k.Bps[b]], writes=[Bdst])
        return f

    for ti in range(16):
        xs_ = ti % 2
        gt = seq * 16 + ti
        P.dma("sp", lambda e: e.dma_start(out=k.xres[:, xs_, :], in_=k.scrX[gt]), reads=[k.Bscr[gt]], writes=[k.Bx[xs_]],
              dkey=f"x{xs_}")
        if ti == 0:
            P.op("pool", lambda e: e.memset(xnTh[:, :, 0:1], 0.0), writes=[m["BxnT"]])
        else:
            P.op("pool", lambda e: e.tensor_copy(out=xnTh[:, :, 0:1], in_=xnTh[:, :, 128:129]),
                 reads=[m["BxnT"]], writes=[m["BxnT"]])
        prenorm_tile(k, xs_, g_pre, xnTh, m["BxnT"], 1, sc)
        P.op("dve", lambda e: e.tensor_tensor(out=xxT[:], in0=xnTh[:, :, 0:128], in1=xnTh[:, :, 1:129], op=ALU.subtract),
             reads=[m["BxnT"]], writes=[m["Bxx"]])
        def make_mix(i):
            eng = "dve" if i % 2 == 0 else "pool"
            mu_b = m["muT"][:, i * 8:(i + 1) * 8].unsqueeze(2).to_broadcast([128, 8, 128])
            P.op(eng, lambda e, i=i, mu_b=mu_b: e.tensor_tensor(out=mixT[:, i % 2], in0=xxT[:], in1=mu_b, op=ALU.mult),
                 reads=[m["Bxx"], Bw], writes=[m["Bmix"][i % 2]])
            P.op(eng, lambda e, i=i: e.tensor_tensor(out=mixT[:, i % 2], in0=mixT[:, i % 2], in1=xnTh[:, :, 1:129], op=ALU.add),
                 reads=[m["Bmix"][i % 2], m["BxnT"]], writes=[m["Bmix"][i % 2]])
        for mi in range(3):
            make_mix(mi)
            for nb in range(2):
                sa = ring_load(k, m, mi * 4 + 2 * nb)
                sb = ring_load(k, m, mi * 4 + 2 * nb + 1)
                for c in range(2):
                    b = nb8(k)
                    for kk in range(8):
                        sl = sa if kk < 4 else sb
                        P.op("pe", lambda e, b=b, sl=sl, kk=kk, c=c, mi=mi: e.matmul(
                            k.ps[b][0:64, :], lhsT=mixT[:, mi % 2, kk, c * 64:(c + 1) * 64],
                            rhs=m["ring"][:, sl, (kk % 4) * 512:(kk % 4 + 1) * 512], start=(kk == 0), stop=(kk == 7)),
                            reads=[m["Bmix"][mi % 2], m["Bring"][sl]], writes=[k.Bps[b]])
                    P.op("act", lambda e, b=b, c=c, mi=mi, nb=nb: e.activation(
                        out=m["rkv"][:, c, mi, nb * 512:(nb + 1) * 512], in_=k.ps[b][0:64, :], func=AF.Copy),
                        reads=[k.Bps[b]], writes=[m["Brkv"][c][mi]])
        for (hid, w, mi, fn, np_) in ((m["hidw"], m["w1"], 3, AF.Tanh, 64), (m["hida"], m["a1"], 4, AF.Copy, 64),
                                      (m["hidg"], m["g1"], 5, AF.Sigmoid, 128)):
            make_mix(mi)
            b = nb8(k)
            for kk in range(8):
                P.op("pe", lambda e, b=b, w=w, mi=mi, kk=kk, np_=np_: e.matmul(
                    k.ps[b][0:np_, 0:128], lhsT=w[:, kk, :], rhs=mixT[:, mi % 2, kk, :], start=(kk == 0), stop=(kk == 7)),
                    reads=[Bw, m["Bmix"][mi % 2]], writes=[k.Bps[b]])
            P.op("act", lambda e, b=b, hid=hid, fn=fn, np_=np_: e.activation(out=hid[:], in_=k.ps[b][0:np_, 0:128], func=fn),
                 reads=[k.Bps[b]], writes=[m["Bhid"]])
        def chunk_indep(m, c):
            F0, F1, F2 = m["F0"], m["F1"], m["F2"]
            BF0, BF1, BF2 = m["BF0"], m["BF1"], m["BF2"]
            sm, Bsm = m["sm"], m["Bsm"]
            cs_ = slice(c * 64, (c + 1) * 64)
            r_, k_, v_ = m["rkv"][:, c, 0, :], m["rkv"][:, c, 1, :], m["rkv"][:, c, 2, :]
            Br, Bk, Bv = m["Brkv"][c]
            rv = m["rkv"][:, c, 0, :].rearrange("p (h j) -> p h j", h=16)
            vv = m["rkv"][:, c, 2, :].rearrange("p (h j) -> p h j", h=16)
            yield
            bw = [nb8(k), nb8(k)]
            for half in range(2):
                hs = slice(half * 512, (half + 1) * 512)
                P.op("pe", lambda e, half=half, hs=hs: e.matmul(k.ps[bw[half]][0:64, :], lhsT=m["hidw"][:, cs_], rhs=m["w2"][:, hs],
                                                                start=True, stop=True), reads=[m["Bhid"], Bw], writes=[k.Bps[bw[half]]])
                P.op("dve", lambda e, half=half, hs=hs: e.tensor_tensor(out=F0[:, hs], in0=k.ps[bw[half]][0:64, :], in1=m["w0r"][:, hs],
                                                                        op=ALU.add), reads=[k.Bps[bw[half]], Bw], writes=[BF0])
                P.op("act", lambda e, half=half, hs=hs: e.activation(out=F0[:, hs], in_=F0[:, hs], func=AF.Sigmoid),
                     reads=[BF0], writes=[BF0])
            ba = [nb8(k), nb8(k)]
            for half in range(2):
                hs = slice(half * 512, (half + 1) * 512)
                P.op("pe", lambda e, half=half, hs=hs: e.matmul(k.ps[ba[half]][0:64, :], lhsT=m["hida"][:, cs_], rhs=m["a2"][:, hs],
                                                                start=True, stop=True), reads=[m["Bhid"], Bw], writes=[k.Bps[ba[half]]])
                P.op("dve", lambda e, half=half, hs=hs: e.tensor_tensor(out=F1[:, hs], in0=k.ps[ba[half]][0:64, :], in1=m["a0r"][:, hs],
                                                                        op=ALU.add), reads=[k.Bps[ba[half]], Bw], writes=[BF1])
                P.op("act", lambda e, half=half, hs=hs: e.activation(out=m["a"][:, hs], in_=F1[:, hs], func=AF.Sigmoid),
                     reads=[BF1], writes=[m["Ba"]])
            for half in range(2):
                hs = slice(half * 512, (half + 1) * 512)
                b = nb8(k)
                P.op("pe", lambda e, b=b, hs=hs: e.matmul(k.ps[b][0:64, :], lhsT=m["hidg"][:, cs_], rhs=m["g2"][:, hs],
                                                          start=True, stop=True), reads=[m["Bhid"], Bw], writes=[k.Bps[b]])
                P.op("act", lambda e, b=b, hs=hs: e.activation(out=m["gsb"][:, hs], in_=k.ps[b][0:64, :], func=AF.Copy),
                     reads=[k.Bps[b]], writes=[m["Bgsb"]])
            yield
            bcs = [nb8(k), nb8(k)]
            for half in range(2):
                hs = slice(half * 512, (half + 1) * 512)
                b = bcs[half]
                P.op("pe", lambda e, b=b, hs=hs: e.matmul(k.ps[b][0:64, :], lhsT=k.trif[0:64, 0:64], rhs=F0[:, hs],
                                                          start=True, stop=True), reads=[BF0, k.Bconst], writes=[k.Bps[b]])
                P.op("act", lambda e, b=b, hs=hs: e.activation(out=m["G"][:, hs], in_=k.ps[b][0:64, :], func=AF.Exp, scale=-C0),
                     reads=[k.Bps[b]], writes=[m["BG"]])
                P.op("act", lambda e, b=b, hs=hs: e.activation(out=m["Ginv"][:, hs], in_=k.ps[b][0:64, :], func=AF.Exp, scale=C0),
                     reads=[k.Bps[b]], writes=[m["BGinv"]])
                P.op("dve", lambda e, b=b, hs=hs: e.tensor_tensor(out=F1[:, hs], in0=k.ps[b][0:64, :], in1=F0[:, hs], op=ALU.subtract),
                     reads=[k.Bps[b], BF0], writes=[BF1])
                P.op("act", lambda e, hs=hs: e.activation(out=m["Gprev"][:, hs], in_=F1[:, hs], func=AF.Exp, scale=-C0),
                     reads=[BF1], writes=[m["BGprev"]])
            bgl = nb8(k)
            for h in range(16):
                P.op("pe", lambda e, h=h: e.matmul(k.ps[bgl][0:64, 2 * h:2 * h + 2], lhsT=F0[:, h * 64:(h + 1) * 64], rhs=k.onesf[0:64, 0:2],
                                                   start=True, stop=True), reads=[BF0, k.Bconst], writes=[k.Bps[bgl]])
            P.op("act", lambda e: e.activation(out=m["GL"][:], in_=k.ps[bgl][0:64, 0:32].rearrange("p (h two) -> p h two", two=2)[:, :, 0],
                                               func=AF.Exp, scale=-C0),
                 reads=[k.Bps[bgl]], writes=[m["BGL"]])
            yield
            P.op("dve", lambda e: e.tensor_tensor(out=F1[:], in0=k_, in1=m["kkb"][:], op=ALU.mult), reads=[Bk, Bw], writes=[BF1])
            P.op("pool", lambda e: e.tensor_tensor(out=F2[:], in0=F1[:], in1=F1[:], op=ALU.mult), reads=[BF1], writes=[BF2])
            P.op("dve", lambda e: e.tensor_reduce(out=sm[:, 0, :], in_=hv(F2), axis=AX.X, op=ALU.add), reads=[BF2], writes=[Bsm])
            P.op("pool", lambda e: e.tensor_scalar(out=sm[:, 0, :], in0=sm[:, 0, :], scalar1=1e-24, scalar2=None, op0=ALU.max),
                 reads=[Bsm], writes=[Bsm])
            P.op("pool", lambda e: e.tensor_tensor(out=sm[:, 0, :], in0=sm[:, 0, :], in1=k.mhalf[0:64, 0:1].to_broadcast([64, 16]),
                                                   op=ALU.pow), reads=[Bsm, k.Bconst], writes=[Bsm])
            P.op("dve", lambda e: e.tensor_tensor(out=hv(m["kk"]), in0=hv(F1), in1=bc(sm[:, 0, :]), op=ALU.mult),
                 reads=[BF1, Bsm], writes=[m["Bkk"]])
            yield
            P.op("dve", lambda e: e.scalar_tensor_tensor(out=F1[:], in0=m["a"][:], scalar=-1.0, in1=m["kab"][:], op0=ALU.add, op1=ALU.mult),
                 reads=[m["Ba"], Bw], writes=[BF1])
            P.op("dve", lambda e: e.scalar_tensor_tensor(out=F2[:], in0=F1[:], scalar=1.0, in1=k_, op0=ALU.add, op1=ALU.mult),
                 reads=[BF1, Bk], writes=[BF2])
            yield
            P.op("pool", lambda e: e.tensor_tensor(out=F1[:], in0=F2[:], in1=m["rkb"][:], op=ALU.mult), reads=[BF2, Bw], writes=[BF1])
            P.op("dve", lambda e: e.tensor_tensor(out=F1[:], in0=F1[:], in1=r_, op=ALU.mult), reads=[BF1, Br], writes=[BF1])
            P.op("dve", lambda e: e.tensor_reduce(out=sm[:, 1, :], in_=hv(F1), axis=AX.X, op=ALU.add), reads=[BF1], writes=[Bsm])
            yield
            P.op("dve", lambda e: e.tensor_tensor(out=m["kt"][:], in0=F2[:], in1=m["Ginv"][:], op=ALU.mult),
                 reads=[BF2, m["BGinv"]], writes=[m["Bkt"]])
            P.op("pool", lambda e: e.tensor_tensor(out=m["rt"][:], in0=r_, in1=m["G"][:], op=ALU.mult),
                 reads=[Br, m["BG"]], writes=[m["Brt"]])
            P.op("pool", lambda e: e.tensor_tensor(out=F1[:], in0=m["kk"][:], in1=m["a"][:], op=ALU.mult),
                 reads=[m["Bkk"], m["Ba"]], writes=[BF1])
            P.op("dve", lambda e: e.tensor_tensor(out=m["bt"][:], in0=F1[:], in1=m["Ginv"][:], op=ALU.mult),
                 reads=[BF1, m["BGinv"]], writes=[m["Bbt"]])
            P.op("dve", lambda e: e.scalar_tensor_tensor(out=m["at"][:], in0=m["kk"][:], scalar=-1.0, in1=m["Gprev"][:],
                                                         op0=ALU.mult, op1=ALU.mult), reads=[m["Bkk"], m["BGprev"]], writes=[m["Bat"]])
            yield
            for src, Bsrc, dst, Bdst, two in ((m["at"], m["Bat"], m["arT"], m["BarT"], 0), (m["rt"], m["Brt"], m["arT"], m["BarT"], 1),
                                              (m["bt"], m["Bbt"], m["bT"], m["BbT"], None), (m["kt"], m["Bkt"], m["kT"], m["BkT"], None)):
                b = nb8(k)
                ptb = k.ps[b][0:64, :].bitcast(BF16)
                for h in range(16):
                    P.op("pe", lambda e, h=h, src=src, ptb=ptb: e.transpose(out=ptb[:, h * 64:(h + 1) * 64], in_=src[:, h * 64:(h + 1) * 64],
                                                                             identity=k.ident[0:64, 0:64]),
                         reads=[Bsrc, k.Bconst], writes=[k.Bps[b]])
                pv = ptb.rearrange("p (h t) -> p h t", h=16)
                if two is None:
                    P.op("act", lambda e, pv=pv, dst=dst: e.activation(out=hv(dst), in_=pv, func=AF.Copy), reads=[k.Bps[b]], writes=[Bdst])
                else:
                    P.op("act", lambda e, pv=pv, dst=dst, two=two: e.activation(out=dst[:, :, two, :], in_=pv, func=AF.Copy),
                         reads=[k.Bps[b]], writes=[Bdst])
            yield
            for (lt, Blt, M, BM) in ((m["bT"], m["BbT"], m["M1"], m["BM1"]), (m["kT"], m["BkT"], m["M2"], m["BM2"])):
                for g in range(4):
                    b = nb8(k)
                    for hh in range(4):
                        h = g * 4 + hh
                        P.op("pe", lambda e, b=b, h=h, hh=hh, lt=lt: e.matmul(
                            k.ps[b][0:64, hh * 128:(hh + 1) * 128], lhsT=lt[:, h * 64:(h + 1) * 64],
                            rhs=m["arT"][:, h].rearrange("p a t -> p (a t)"), start=True, stop=True),
                            reads=[Blt, m["BarT"]], writes=[k.Bps[b]])
                    P.op("dve", lambda e, b=b, g=g, M=M: e.tensor_tensor(
                        out=M[:, g * 4:(g + 1) * 4].rearrange("p h a t -> p h (a t)"),
                        in0=k.ps[b][0:64, :].rearrange("p (h x) -> p h x", h=4),
                        in1=k.m1[:].unsqueeze(1).to_broadcast([64, 4, 128]), op=ALU.mult),
                        reads=[k.Bps[b], k.Bconst], writes=[BM])
                    if M is m["M1"]:
                        P.op("dve", lambda e, b=b, g=g: e.tensor_tensor(
                            out=hv(m["QA"])[:, g * 4:(g + 1) * 4],
                            in0=k.ps[b][0:64, :].rearrange("p (h x) -> p h x", h=4)[:, :, 0:64],
                            in1=k.m1[:, 0:64].unsqueeze(1).to_broadcast([64, 4, 64]), op=ALU.mult),
                            reads=[k.Bps[b], k.Bconst], writes=[m["BQA"]])
            for g in range(2):
                b = nb8(k)
                for hh in range(8):
                    h = g * 8 + hh
                    P.op("pe", lambda e, b=b, h=h, hh=hh: e.matmul(k.ps[b][0:64, hh * 64:(hh + 1) * 64], lhsT=m["arT"][:, h, 0, :],
                                                                   rhs=m["bT"][:, h * 64:(h + 1) * 64], start=True, stop=True),
                         reads=[m["BarT"], m["BbT"]], writes=[k.Bps[b]])
                P.op("dve", lambda e, b=b, g=g: e.tensor_tensor(
                    out=hv(m["PA"])[:, g * 8:(g + 1) * 8], in0=k.ps[b][0:64, :].rearrange("p (h x) -> p h x", h=8),
                    in1=k.sl64[:].unsqueeze(1).to_broadcast([64, 8, 64]), op=ALU.mult),
                    reads=[k.Bps[b], k.Bconst], writes=[m["BPA"]])
            yield
            TTf = hv(m["TTf"])
            P.op("dve", lambda e: e.tensor_tensor(out=TTf, in0=hv(m["QA"]),
                                                  in1=k.ident[0:64, 0:64].unsqueeze(1).to_broadcast([64, 16, 64]), op=ALU.add),
                 reads=[m["BQA"], k.Bconst], writes=[m["BTTf"]])
            yield
            Qc, BQc = hv(m["QA"]), m["BQA"]
            Pc, BPc = hv(m["PA"]), m["BPA"]
            for lv in range(5):
                Qn, BQn = (hv(m["QB"]), m["BQB"]) if lv % 2 == 0 else (hv(m["QA"]), m["BQA"])
                Pn, BPn = (hv(m["PB"]), m["BPB"]) if lv % 2 == 0 else (hv(m["PA"]), m["BPA"])
                jobs = [(Qc, BQc, Pc, BPc, Pn, BPn)]
                if lv < 4:
                    jobs.append((Pc, BPc, Qc, BQc, Qn, BQn))
                for (L, BL, R, BR, O, BO) in jobs:
                    for g in range(2):
                        b = nb8(k)
                        for hh in range(8):
                            h = g * 8 + hh
                            P.op("pe", lambda e, b=b, h=h, hh=hh, L=L, R=R: e.matmul(k.ps[b][0:64, hh * 64:(hh + 1) * 64], lhsT=L[:, h, :],
                                                                                   rhs=R[:, h, :], start=True, stop=True),
                                 reads=[BL, BR], writes=[k.Bps[b]])
                        P.op("act", lambda e, b=b, g=g, O=O: e.activation(out=O[:, g * 8:(g + 1) * 8],
                                                                          in_=k.ps[b][0:64, :].rearrange("p (h x) -> p h x", h=8),
                                                                          func=AF.Copy), reads=[k.Bps[b]], writes=[BO])
                for g in range(2):
                    b = nb8(k)
                    for hh in range(8):
                        h = g * 8 + hh
                        P.op("pe", lambda e, b=b, h=h, hh=hh, Pn=Pn: e.matmul(k.ps[b][0:64, hh * 64:(hh + 1) * 64], lhsT=Pn[:, h, :],
                                                                             rhs=TTf[:, h, :], start=True, stop=True),
                             reads=[BPn, m["BTTf"]], writes=[k.Bps[b]])
                    P.op("dve", lambda e, b=b, g=g: e.tensor_tensor(out=TTf[:, g * 8:(g + 1) * 8],
                                                                    in0=k.ps[b][0:64, :].rearrange("p (h x) -> p h x", h=8),
                                                                    in1=TTf[:, g * 8:(g + 1) * 8], op=ALU.add),
                         reads=[k.Bps[b], m["BTTf"]], writes=[m["BTTf"]])
                Qc, BQc, Pc, BPc = Qn, BQn, Pn, BPn
                yield
            P.op("act", lambda e: e.activation(out=m["TT"][:], in_=m["TTf"][:], func=AF.Copy), reads=[m["BTTf"]], writes=[m["BTT"]])
            TT = hv(m["TT"])

            vh = lambda h: m["rkv"][:, c, 2, h * 64:(h + 1) * 64]
            yield
            headmm(evac_to(m["akv"], m["Bakv"]), [(lambda h: m["M2"][:, h, 0, :], vh, [m["BM2"], Bv])])
            headmm(evac_to(m["TAT"], m["BTAT"], "dve"),
                   [(lambda h: m["at"][:, h * 64:(h + 1) * 64], lambda h: TT[:, h, :], [m["Bat"], m["BTT"]])])
            yield

        def chunk_dep(m, c):
            F0, F1, F2 = m["F0"], m["F1"], m["F2"]
            BF0, BF1, BF2 = m["BF0"], m["BF1"], m["BF2"]
            sm, Bsm = m["sm"], m["Bsm"]
            cs_ = slice(c * 64, (c + 1) * 64)
            Br, Bk, Bv = m["Brkv"][c]
            vv = m["rkv"][:, c, 2, :].rearrange("p (h j) -> p h j", h=16)
            TT = hv(m["TT"])
            vh = lambda h: m["rkv"][:, c, 2, h * 64:(h + 1) * 64]
            headmm(evac_to(m["Usb"], m["BUsb"]),
                   [(lambda h: TT[:, h, :], lambda h: hv(m["akv"])[:, h, :], [m["BTT"], m["Bakv"]]),
                    (lambda h: hv(m["TAT"])[:, h, :], lambda h: hv(m["Hbf"])[:, h, :], [m["BTAT"], m["BHbf"]])])
            headmm(evac_to(F1, BF1),
                   [(lambda h: m["arT"][:, h, 1, :], lambda h: hv(m["Hbf"])[:, h, :], [m["BarT"], m["BHbf"]]),
                    (lambda h: m["M1"][:, h, 1, :], lambda h: hv(m["Usb"])[:, h, :], [m["BM1"], m["BUsb"]]),
                    (lambda h: m["M2"][:, h, 1, :], vh, [m["BM2"], Bv])])

            def evac_H(b, g):
                gs = slice(g * 8, (g + 1) * 8)
                P.op("dve", lambda e: e.tensor_tensor(out=hv(H)[:, gs], in0=k.ps[b][0:64, :].rearrange("p (h x) -> p h x", h=8),
                                                      in1=hv(H)[:, gs], op=ALU.add), reads=[k.Bps[b], BH], writes=[BH])
                P.op("dve", lambda e: e.tensor_tensor(out=hv(H)[:, gs], in0=hv(H)[:, gs],
                                                      in1=m["GL"][:, gs].unsqueeze(2).to_broadcast([64, 8, 64]), op=ALU.mult),
                     reads=[BH, m["BGL"]], writes=[BH])
            headmm(evac_H,
                   [(lambda h: m["bt"][:, h * 64:(h + 1) * 64], lambda h: hv(m["Usb"])[:, h, :], [m["Bbt"], m["BUsb"]]),
                    (lambda h: m["kt"][:, h * 64:(h + 1) * 64], vh, [m["Bkt"], Bv])])
            P.op("act", lambda e: e.activation(out=m["Hbf"][:], in_=H[:], func=AF.Copy), reads=[BH], writes=[m["BHbf"]])
            P.op("dve", lambda e: e.tensor_reduce(out=sm[:, 2, :], in_=hv(F1), axis=AX.X, op=ALU.add), reads=[BF1], writes=[Bsm])
            P.op("pool", lambda e: e.tensor_tensor(out=F2[:], in0=F1[:], in1=F1[:], op=ALU.mult), reads=[BF1], writes=[BF2])
            P.op("dve", lambda e: e.tensor_reduce(out=sm[:, 3, :], in_=hv(F2), axis=AX.X, op=ALU.add), reads=[BF2], writes=[Bsm])
            P.op("dve", lambda e: e.tensor_scalar(out=sm[:, 2, :], in0=sm[:, 2, :], scalar1=1.0 / 64.0, scalar2=None, op0=ALU.mult),
                 reads=[Bsm], writes=[Bsm])
            P.op("dve", lambda e: e.tensor_tensor(out=sm[:, 4, :], in0=sm[:, 2, :], in1=sm[:, 2, :], op=ALU.mult), reads=[Bsm], writes=[Bsm])
            P.op("dve", lambda e: e.scalar_tensor_tensor(out=sm[:, 3, :], in0=sm[:, 3, :], scalar=1.0 / 64.0, in1=sm[:, 4, :],
                                                         op0=ALU.mult, op1=ALU.subtract), reads=[Bsm], writes=[Bsm])
            P.op("pool", lambda e: e.tensor_scalar(out=sm[:, 3, :], in0=sm[:, 3, :], scalar1=64e-5, scalar2=None, op0=ALU.add),
                 reads=[Bsm], writes=[Bsm])
            P.op("pool", lambda e: e.tensor_tensor(out=sm[:, 3, :], in0=sm[:, 3, :], in1=k.mhalf[0:64, 0:1].to_broadcast([64, 16]),
                                                   op=ALU.pow), reads=[Bsm, k.Bconst], writes=[Bsm])
            P.op("dve", lambda e: e.tensor_tensor(out=hv(F1), in0=hv(F1), in1=bc(sm[:, 2, :]), op=ALU.subtract), reads=[BF1, Bsm], writes=[BF1])
            P.op("dve", lambda e: e.tensor_tensor(out=hv(F1), in0=hv(F1), in1=bc(sm[:, 3, :]), op=ALU.mult), reads=[BF1, Bsm], writes=[BF1])
            P.op("pool", lambda e: e.tensor_tensor(out=F1[:], in0=F1[:], in1=m["lgb"][:], op=ALU.mult), reads=[BF1, Bw], writes=[BF1])
            P.op("pool", lambda e: e.tensor_tensor(out=F1[:], in0=F1[:], in1=m["lbb"][:], op=ALU.add), reads=[BF1, Bw], writes=[BF1])
            P.op("dve", lambda e: e.tensor_tensor(out=hv(F2), in0=vv, in1=bc(sm[:, 1, :]), op=ALU.mult), reads=[Bv, Bsm], writes=[BF2])
            P.op("dve", lambda e: e.tensor_tensor(out=F1[:], in0=F1[:], in1=F2[:], op=ALU.add), reads=[BF1, BF2], writes=[BF1])
            P.op("dve", lambda e: e.tensor_tensor(out=m["zb"][:], in0=F1[:], in1=m["gsb"][:], op=ALU.mult),
                 reads=[BF1, m["Bgsb"]], writes=[m["Bzb"]])
            b = nb8(k)
            ptb = k.ps[b][:].bitcast(BF16)
            for kk in range(8):
                P.op("pe", lambda e, kk=kk, ptb=ptb: e.transpose(out=ptb[:, kk * 64:(kk + 1) * 64], in_=m["zb"][:, kk * 128:(kk + 1) * 128],
                                                                 identity=k.ident[0:64, 0:64]),
                     reads=[m["Bzb"], k.Bconst], writes=[k.Bps[b]])
            P.op("act", lambda e, ptb=ptb: e.activation(out=hT[:, :, cs_], in_=ptb[:, 0:512].rearrange("p (a t) -> p a t", a=8), func=AF.Copy),
                 reads=[k.Bps[b]], writes=[m["BhT"]])

        gens = [chunk_indep(cxs[0], 0), chunk_indep(cxs[1], 1)]
        while gens:
            for g_ in list(gens):
                try:
                    next(g_)
                except StopIteration:
                    gens.remove(g_)
        chunk_dep(cxs[0], 0)
        chunk_dep(cxs[1], 1)
        bo = [nb8(k), nb8(k)]
        for u in range(4):
            sl = ring_load(k, m, 12 + u)
            for c2 in range(2):
                kk = 2 * u + c2
                for half in range(2):
                    P.op("pe", lambda e, sl=sl, c2=c2, kk=kk, half=half: e.matmul(
                        k.ps[bo[half]][:], lhsT=hT[:, kk, :], rhs=m["ring"][:, sl, c2 * 1024 + half * 512:c2 * 1024 + (half + 1) * 512],
                        start=(kk == 0), stop=(kk == 7)), reads=[m["BhT"], m["Bring"][sl]], writes=[k.Bps[bo[half]]])
        postnorm_residual(k, xs_, bo, m["gpost"], m["Bgpost"], 1.0, sc)
        P.dma("sp", lambda e: e.dma_start(out=k.scrX[gt], in_=k.xres[:, xs_, :]), reads=[k.Bx[xs_]], writes=[k.Bscr[gt]],
              dkey=f"x{xs_}")


def rwkv_phase(k, layer, j):
    P = k.P
    if not hasattr(k, "scrX"):
        k.scrX = k.nc.dram_tensor("scrX", [NT, 128, D], F32, kind="Internal").ap()
        k.Bscr = [Buf(f"scr{i}") for i in range(NT)]
    P.barrier()
    for i in range(0, NT, 2):
        P.dma("sp", lambda e: e.dma_start(out=k.scrX[i:i + 2].rearrange("t p d -> p t d"), in_=k.xres[:, i:i + 2, :]),
              reads=[k.Bx[i], k.Bx[i + 1]], writes=[k.Bscr[i], k.Bscr[i + 1]], dkey=f"x{i // 2}")
    P.barrier()
    m = rwkv_setup(k, layer)
    P.dma("sp", lambda e: e.dma_start(out=m["gpost"][:], in_=k.ng_d[layer * 6 + 3, :].partition_broadcast(128)),
          writes=[m["Bgpost"]], dkey="gpost")
    rwkv_seq(k, m, layer, 0)
    rwkv_seq(k, m, layer, 1)
    P.barrier()
    for i in range(0, NT, 2):
        P.dma("sp", lambda e: e.dma_start(out=k.xres[:, i:i + 2, :], in_=k.scrX[i:i + 2].rearrange("t p d -> p t d")),
              reads=[k.Bscr[i], k.Bscr[i + 1]], writes=[k.Bx[i], k.Bx[i + 1]], dkey=f"x{i // 2}")
    P.barrier()


def _units_lin(w_in, n_f, tm0, n_tm, w_out):
    wf = w_in[:, :n_f * 128].reshape(8, 128, n_f, 128).transpose(2, 1, 0, 3)
    wf = wf.reshape(n_f // 2, 2, 128, 1024).transpose(0, 2, 1, 3).reshape(n_f // 2, 128, 2048)
    wt = w_in[:, tm0:tm0 + n_tm * 512].reshape(2, 4, 128, n_tm, 512).transpose(3, 0, 2, 1, 4)
    wt = wt.reshape(n_tm * 2, 128, 2048)
    nko = w_out.shape[0] // 128
    wo = w_out.reshape(nko // 2, 2, 128, 1024).transpose(0, 2, 1, 3).reshape(nko // 2, 128, 2048)
    return np.ascontiguousarray(np.concatenate([wf, wt, wo], axis=0), dtype=np.float32)


def _prep_inputs(inputs, x_override=None):
    xin = inputs["x"] if x_override is None else x_override
    x = np.ascontiguousarray(xin, dtype=np.float32).reshape(NCORES, NT, 128, D)
    wgu = inputs["ffn_w_gu"].reshape(8, 8, 128, 2, NJ, 128)
    wgu = np.ascontiguousarray(wgu.transpose(0, 4, 2, 3, 1, 5)).reshape(8 * NJ, 128, 2, 1024)
    wd = np.ascontiguousarray(inputs["ffn_w_down"]).reshape(8 * NJ, 128, 1024)
    ng = np.ascontiguousarray(inputs["norm_g"]).reshape(24, D)
    ngT = np.ascontiguousarray(ng.reshape(24, 8, 128).transpose(2, 0, 1)).reshape(128, 24 * 8)
    shared = {"wgu": wgu, "wd": wd, "ng": ng, "ngT": ngT}
    for j, layer in ((0, 0), (1, 3)):
        w_in = inputs["ml_w_in"][j]
        shared[f"wm{layer}"] = _units_lin(w_in, 8, 1024, 4, inputs["ml_w_out"][j])
        shared[f"wif{layer}"] = np.ascontiguousarray(w_in[:, 3072:3080].reshape(8, 128, 8).transpose(1, 0, 2)).reshape(128, 64)
        shared[f"bif{layer}"] = np.ascontiguousarray(inputs["ml_b_if"][j]).reshape(1, 8)
        shared[f"convT{layer}"] = np.ascontiguousarray(
            inputs["ml_conv_w"][j].reshape(4, 8, 128).transpose(2, 1, 0)).reshape(128, 32)
        shared[f"mng{layer}"] = np.ascontiguousarray(inputs["ml_norm_g"][j]).reshape(1, 1024)
    shared["wm2"] = _units_lin(inputs["rt_w_in"][0], 16, 2048, 8, inputs["rt_w_out"][0])
    wtm = lambda w: np.ascontiguousarray(w.reshape(2, 4, 128, 2, 512).transpose(3, 0, 2, 1, 4)).reshape(4, 128, 2048)
    wo = inputs["rw_w_out"][0].reshape(4, 2, 128, 1024).transpose(0, 2, 1, 3).reshape(4, 128, 2048)
    shared["rw_units"] = np.ascontiguousarray(np.concatenate(
        [wtm(inputs["rw_w_rkv"][0, 0]), wtm(inputs["rw_w_rkv"][0, 1]), wtm(inputs["rw_w_rkv"][0, 2]), wo], axis=0), dtype=np.float32)
    for nm in ("w1", "a1", "g1"):
        w = inputs["rw_" + nm][0]
        shared["rw_" + nm] = np.ascontiguousarray(w.reshape(8, 128, w.shape[1]).transpose(1, 0, 2))
    for nm in ("w2", "a2", "g2"):
        shared["rw_" + nm] = np.ascontiguousarray(inputs["rw_" + nm][0])
    shared["rw_w0r"] = np.ascontiguousarray(inputs["rw_w0"][0]).reshape(1, 1024)
    shared["rw_a0r"] = np.ascontiguousarray(inputs["rw_a0"][0]).reshape(1, 1024)
    shared["rw_muT"] = np.ascontiguousarray(inputs["rw_mu"][0].reshape(6, 8, 128).transpose(2, 0, 1)).reshape(128, 48)
    for nm, src in (("kkb", "rw_k_k"), ("kab", "rw_k_a"), ("lgb", "rw_ln_g"), ("lbb", "rw_ln_b"), ("rkb", "rw_r_k")):
        shared["rw_" + nm] = np.ascontiguousarray(inputs[src][0]).reshape(1, 1024)
    pos = np.ascontiguousarray(inputs["positions"]).astype(np.int32).reshape(NCORES, 1, 2 * SEQ)
    return [dict(shared, x=x[c], pos=pos[c]) for c in range(NCORES)]


def run_partial(inputs, subs=tuple(range(12)), trace=False, cores=NCORES, x_override=None):
    nc, names = build_program(tuple(subs))
    in_maps = _prep_inputs(inputs, x_override)
    in_maps = [{n: m[n] for n in names if n in m} for m in in_maps[:cores]]
    res = run_bass_kernel_spmd(nc, in_maps, core_ids=list(range(cores)), trace=trace)
    out = np.stack([np.asarray(r["out"]) for r in res.results], axis=0)
    return out.reshape(2 * cores, SEQ, D).astype(np.float32), res


def kernel(**inputs):
    out, _ = run_partial(inputs)
    return out
```
